# Optimizing a Trainium2 kernel written in Bass

```python
import jax, jax.numpy as jnp
from jax import lax
import numpy as np

D_MODEL = 1024
BATCH = 4
SEQ = 4096
DEPTH = 1

D_MIX = D_MODEL
D_ATTN = D_MIX // 2
D_REC = D_MIX - D_ATTN
HEAD_DIM = 64
N_HEADS = D_ATTN // HEAD_DIM
DILATED_PATTERNS = ((128, 1), (512, 4), (2048, 16))
REC_BLOCKS = 8
REC_BLOCK_W = D_REC // REC_BLOCKS
CONV_WIDTH = 4
RG_C = 8.0
N_EXPERTS = 32
TOP_K = 4
D_EXPERT = D_MODEL
SWIGLU_LIMIT = 7.0
SWIGLU_ALPHA = 1.702
EXPERT_BLOCK = 128
NORM_EPS = 1e-6
NEG_INF = -1e30
IN_COLS = 3 * D_ATTN + 2 * D_REC

kernel_name = "hymba_rglru_dilated_attn_moe_block"


def _rmsnorm(x, g):
    xf = x.astype(jnp.float32)
    y = xf * lax.rsqrt(jnp.mean(xf * xf, axis=-1, keepdims=True) + NORM_EPS)
    return (y * g.astype(jnp.float32)).astype(x.dtype)


def _modulate(h, shift, scale):
    return h * (1.0 + scale[:, None, :]) + shift[:, None, :]


def _dilated_branch(q, k, v, window, dilation):
    bsz, s, nh, e = q.shape
    span = window // dilation
    sub_len = s // dilation
    nb = -(-sub_len // span)
    lp = nb * span

    def to_sub(t):
        t = t.reshape(bsz, sub_len, dilation, nh, e).transpose(0, 2, 3, 1, 4)
        t = jnp.pad(t, ((0, 0), (0, 0), (0, 0), (0, lp - sub_len), (0, 0)))
        return t.reshape(bsz, dilation, nh, nb, span, e)

    def with_prev(t):
        prev = jnp.pad(t[:, :, :, :-1], ((0, 0), (0, 0), (0, 0), (1, 0), (0, 0), (0, 0)))
        return jnp.concatenate([prev, t], axis=4)

    qb = to_sub(q)
    kc = with_prev(to_sub(k))
    vc = with_prev(to_sub(v))
    scores = jnp.einsum('bdhnqe,bdhnke->bdhnqk', qb, kc) * (e ** -0.5)
    blk = jnp.arange(nb)[:, None, None] * span
    qpos = blk + jnp.arange(span)[None, :, None]
    kpos = blk - span + jnp.arange(2 * span)[None, None, :]
    rel = qpos - kpos
    mask = (rel >= 0) & (rel <= span) & (kpos >= 0)
    scores = jnp.where(mask, scores, NEG_INF)
    lse = jax.nn.logsumexp(scores, axis=-1)
    p = jnp.exp(scores - lse[..., None])
    o = jnp.einsum('bdhnqk,bdhnke->bdhnqe', p, vc)
    o = o.reshape(bsz, dilation, nh, lp, e)[:, :, :, :sub_len]
    o = o.transpose(0, 3, 1, 2, 4).reshape(bsz, s, nh, e)
    lse = lse.reshape(bsz, dilation, nh, lp)[:, :, :, :sub_len]
    lse = lse.transpose(0, 3, 1, 2).reshape(bsz, s, nh)
    return o, lse


def _dilated_attention(q, k, v):
    dt = q.dtype
    qf, kf, vf = q.astype(jnp.float32), k.astype(jnp.float32), v.astype(jnp.float32)
    outs, lses = [], []
    for window, dilation in DILATED_PATTERNS:
        o, lse = _dilated_branch(qf, kf, vf, window, dilation)
        outs.append(o)
        lses.append(lse)
    w = jax.nn.softmax(jnp.stack(lses, axis=0), axis=0)
    o = jnp.sum(w[..., None] * jnp.stack(outs, axis=0), axis=0)
    return o.astype(dt)


def _rglru_branch(xr, gr, conv_w, conv_b, w_a, b_a, w_x, b_x, lam):
    bsz, s, ch = xr.shape
    xc = lax.conv_general_dilated(
        xr, conv_w[:, None, :].astype(xr.dtype), window_strides=(1,),
        padding=[(CONV_WIDTH - 1, 0)], dimension_numbers=('NWC', 'WIO', 'NWC'),
        feature_group_count=ch) + conv_b
    xf = xc.astype(jnp.float32)
    xh = xf.reshape(bsz, s, REC_BLOCKS, REC_BLOCK_W)
    r = jax.nn.sigmoid(jnp.einsum('bsnh,nhk->bsnk', xh, w_a.astype(jnp.float32)).reshape(bsz, s, ch) + b_a)
    i = jax.nn.sigmoid(jnp.einsum('bsnh,nhk->bsnk', xh, w_x.astype(jnp.float32)).reshape(bsz, s, ch) + b_x)
    log_a = -RG_C * r * jax.nn.softplus(-lam.astype(jnp.float32))
    a = jnp.exp(log_a)
    mult = jnp.sqrt(-jnp.expm1(2.0 * log_a))
    u = mult * (i * xf)

    def combine(left, right):
        a1, b1 = left
        a2, b2 = right
        return a1 * a2, a2 * b1 + b2

    _, h = lax.associative_scan(combine, (a, u), axis=1)
    out = h * jax.nn.gelu(gr.astype(jnp.float32), approximate=True)
    return out.astype(xr.dtype)


def _moe(h, w_router, b_router, w_gate_up, b_gate_up, w_down, b_down):
    bsz, s, d = h.shape
    t = bsz * s
    ht = h.reshape(t, d)
    logits = (ht @ w_router + b_router).astype(jnp.float32)
    top_val, top_idx = lax.top_k(logits, TOP_K)
    gates = jax.nn.softmax(top_val, axis=-1)
    n_assign = t * TOP_K
    e_flat = top_idx.reshape(n_assign)
    tok_flat = jnp.arange(n_assign, dtype=jnp.int32) // TOP_K
    g_flat = gates.reshape(n_assign)
    order = jnp.argsort(e_flat)
    e_sorted = e_flat[order]
    tok_sorted = tok_flat[order]
    g_sorted = g_flat[order]
    counts = jnp.bincount(e_flat, length=N_EXPERTS)
    padded = ((counts + EXPERT_BLOCK - 1) // EXPERT_BLOCK) * EXPERT_BLOCK
    start = jnp.cumsum(counts) - counts
    pend = jnp.cumsum(padded)
    pstart = pend - padded
    rank = jnp.arange(n_assign, dtype=jnp.int32) - start[e_sorted]
    dest = pstart[e_sorted] + rank
    buf_len = (-(-n_assign // EXPERT_BLOCK) + N_EXPERTS) * EXPERT_BLOCK
    n_blocks = buf_len // EXPERT_BLOCK
    buf_tok = jnp.full((buf_len,), t, dtype=jnp.int32).at[dest].set(tok_sorted)
    ht_pad = jnp.concatenate([ht, jnp.zeros((1, d), ht.dtype)], axis=0)
    x_buf = ht_pad[buf_tok].reshape(n_blocks, EXPERT_BLOCK, d)
    blk_start = jnp.arange(n_blocks, dtype=pend.dtype) * EXPERT_BLOCK
    blk_exp = jnp.minimum(jnp.searchsorted(pend, blk_start, side='right'), N_EXPERTS - 1)

    def expert_block(args):
        xb, e = args
        gu = xb @ w_gate_up[e] + b_gate_up[e]
        gate, up = gu[:, :D_EXPERT], gu[:, D_EXPERT:]
        gate = jnp.minimum(gate, SWIGLU_LIMIT)
        up = jnp.clip(up, -SWIGLU_LIMIT, SWIGLU_LIMIT)
        glu = gate * jax.nn.sigmoid(gate * SWIGLU_ALPHA)
        return ((up + 1.0) * glu) @ w_down[e] + b_down[e]

    y_buf = lax.map(expert_block, (x_buf, blk_exp)).reshape(buf_len, d)
    y = jnp.zeros((t, d), jnp.float32).at[tok_sorted].add(
        y_buf[dest].astype(jnp.float32) * g_sorted[:, None])
    return y.reshape(bsz, s, d).astype(h.dtype)


def setup_inputs(seed: int = 0) -> dict:
    key = jax.random.key(seed)
    ks = jax.random.split(key, 24)
    nrm = jax.random.normal
    D = D_MODEL
    a0 = jax.random.uniform(ks[12], (DEPTH, D_REC), minval=0.9, maxval=0.999)
    sig = a0 ** (1.0 / RG_C)
    lam = jnp.log(sig) - jnp.log1p(-sig)
    return {
        'x': nrm(ks[0], (BATCH, SEQ, D), jnp.float32),
        'c': nrm(ks[1], (BATCH, D), jnp.float32),
        'w_ada': nrm(ks[2], (DEPTH, D, 6 * D)) * (0.5 * D ** -0.5),
        'b_ada': 0.01 * nrm(ks[3], (DEPTH, 6 * D)),
        'g_mix': 1.0 + 0.05 * nrm(ks[4], (DEPTH, D)),
        'w_in': nrm(ks[5], (DEPTH, D, IN_COLS)) * D ** -0.5,
        'conv_w': nrm(ks[6], (DEPTH, CONV_WIDTH, D_REC)) * CONV_WIDTH ** -0.5,
        'conv_b': 0.01 * nrm(ks[7], (DEPTH, D_REC)),
        'w_rg_a': nrm(ks[8], (DEPTH, REC_BLOCKS, REC_BLOCK_W, REC_BLOCK_W)) * REC_BLOCK_W ** -0.5,
        'b_rg_a': 0.01 * nrm(ks[9], (DEPTH, D_REC)),
        'w_rg_x': nrm(ks[10], (DEPTH, REC_BLOCKS, REC_BLOCK_W, REC_BLOCK_W)) * REC_BLOCK_W ** -0.5,
        'b_rg_x': 0.01 * nrm(ks[11], (DEPTH, D_REC)),
        'lam': lam,
        'w_out': nrm(ks[13], (DEPTH, D_MIX, D)) * D_MIX ** -0.5,
        'g_ffn': 1.0 + 0.05 * nrm(ks[14], (DEPTH, D)),
        'w_router': nrm(ks[15], (DEPTH, D, N_EXPERTS)) * D ** -0.5,
        'b_router': 0.01 * nrm(ks[16], (DEPTH, N_EXPERTS)),
        'w_gate_up': nrm(ks[17], (DEPTH, N_EXPERTS, D, 2 * D_EXPERT)) * D ** -0.5,
        'b_gate_up': 0.01 * nrm(ks[18], (DEPTH, N_EXPERTS, 2 * D_EXPERT)),
        'w_down': nrm(ks[19], (DEPTH, N_EXPERTS, D_EXPERT, D)) * D_EXPERT ** -0.5,
        'b_down': 0.01 * nrm(ks[20], (DEPTH, N_EXPERTS, D)),
        'g_final': 1.0 + 0.05 * nrm(ks[21], (D,)),
    }


def reference(x, c, w_ada, b_ada, g_mix, w_in, conv_w, conv_b, w_rg_a, b_rg_a, w_rg_x, b_rg_x,
              lam, w_out, g_ffn, w_router, b_router, w_gate_up, b_gate_up, w_down, b_down, g_final):
    bsz, s, _ = x.shape
    for l in range(DEPTH):
        mod = c @ w_ada[l] + b_ada[l]
        shift_m, scale_m, gate_m, shift_f, scale_f, gate_f = jnp.split(mod, 6, axis=-1)
        h = _modulate(_rmsnorm(x, g_mix[l]), shift_m, scale_m)
        z = h @ w_in[l]
        q, k, v, xr, gr = jnp.split(
            z, [D_ATTN, 2 * D_ATTN, 3 * D_ATTN, 3 * D_ATTN + D_REC], axis=-1)
        hd = (bsz, s, N_HEADS, HEAD_DIM)
        attn = _dilated_attention(q.reshape(hd), k.reshape(hd), v.reshape(hd)).reshape(bsz, s, D_ATTN)
        rec = _rglru_branch(xr, gr, conv_w[l], conv_b[l], w_rg_a[l], b_rg_a[l],
                            w_rg_x[l], b_rg_x[l], lam[l])
        y = jnp.concatenate([attn, rec], axis=-1) @ w_out[l]
        x = x + gate_m[:, None, :] * y
        h = _modulate(_rmsnorm(x, g_ffn[l]), shift_f, scale_f)
        x = x + gate_f[:, None, :] * _moe(h, w_router[l], b_router[l], w_gate_up[l],
                                          b_gate_up[l], w_down[l], b_down[l])
    return _rmsnorm(x, g_final)
```

```python
import numpy as np
from contextlib import ExitStack
import concourse.bass as bass
import concourse.mybir as mybir
from concourse.bass_utils import run_bass_kernel_spmd

F32 = mybir.dt.float32
BF16 = mybir.dt.bfloat16
AF = mybir.ActivationFunctionType
ALU = mybir.AluOpType

D = 1024
TOWN = 2048
TALL = 4096
NE = 32
EPS = 1e-6
GLU7 = float(np.float32(7.0) / (np.float32(1.0) + np.exp(np.float32(-1.702 * 7.0))))
SEM_LIMIT = 30000


class Res:
    __slots__ = ("w", "r")

    def __init__(self):
        self.w = None
        self.r = {}


def RL(n):
    return [Res() for _ in range(n)]


class Sched:
    ENGS = ("tensor", "vector", "scalar", "gpsimd", "sync")

    def __init__(self, nc, stack, n_dma_sems=8):
        self.nc = nc
        self.stack = stack
        self.ops = {e: [] for e in self.ENGS}
        self.known = {e: {} for e in self.ENGS}
        self.dom_sem = {}
        self.dom_max = {}
        self.ndom = 0
        self.cur_dom = {}
        self.cur_cnt = {}
        self.guard = None
        self.guard_snap = {}
        self.regs = {}
        for e in self.ENGS:
            self.cur_dom[e] = self._new_dom(e)
            self.cur_cnt[e] = 0
        self.dma_pool = {}
        for q in ("sync", "gpsimd"):
            self.dma_pool[q] = {"doms": [self._new_dom("dma_%s%d" % (q, i)) for i in range(n_dma_sems)],
                                "cnt": [0] * n_dma_sems, "rr": 0}

    def _new_dom(self, name):
        d = self.ndom
        self.ndom += 1
        self.dom_sem[d] = self.stack.enter_context(self.nc.semaphore("s_%s_%d" % (name, d)))
        self.dom_max[d] = 0
        return d

    def _collect(self, eng, reads, writes):
        need = {}
        for R in reads:
            if R.w is not None:
                d, v = R.w
                if need.get(d, 0) < v:
                    need[d] = v
        for R in writes:
            if R.w is not None:
                d, v = R.w
                if need.get(d, 0) < v:
                    need[d] = v
            for d, v in R.r.items():
                if need.get(d, 0) < v:
                    need[d] = v
        kn = self.known[eng]
        waits = []
        for d, v in need.items():
            if kn.get(d, 0) < v:
                kn[d] = v
                waits.append((d, v))
        return waits

    def _tick(self, eng):
        self.cur_cnt[eng] += 1
        d = self.cur_dom[eng]
        v = self.cur_cnt[eng]
        self.dom_max[d] = v
        return d, v

    def _mark(self, d, v, reads, writes):
        for R in reads:
            R.r[d] = v
        for R in writes:
            R.w = (d, v)
            R.r = {}

    def op(self, eng, fn, reads=(), writes=()):
        waits = self._collect(eng, reads, writes)
        d, v = self._tick(eng)
        self.ops[eng].append((waits, fn, self.dom_sem[d], 1, self.guard))
        self._mark(d, v, reads, writes)

    def group(self, eng, fns, reads=(), writes=()):
        waits = self._collect(eng, reads, writes)
        d, v = self._tick(eng)
        n = len(fns)
        for i, fn in enumerate(fns):
            self.ops[eng].append((waits if i == 0 else [], fn,
                                  self.dom_sem[d] if i == n - 1 else None, 1, self.guard))
        self._mark(d, v, reads, writes)

    def dma_fn(self, eng, fn, reads=(), writes=()):
        pool = self.dma_pool[eng]
        i = pool["rr"]
        pool["rr"] = (i + 1) % len(pool["doms"])
        d = pool["doms"][i]
        waits = self._collect(eng, reads, writes)
        prev = pool["cnt"][i]
        kn = self.known[eng]
        if prev > 0 and kn.get(d, 0) < prev:
            kn[d] = prev
            waits.append((d, prev))
        pool["cnt"][i] += 16
        v = pool["cnt"][i]
        self.dom_max[d] = v
        self.ops[eng].append((waits, fn, self.dom_sem[d], 16, self.guard))
        self._mark(d, v, reads, writes)

    def dma(self, out, in_, reads=(), writes=(), eng="sync"):
        def fn(e, out=out, in_=in_):
            return e.dma_start(out=out, in_=in_)
        self.dma_fn(eng, fn, reads, writes)

    def load_count(self, ap, reads):
        for eng in self.ENGS:
            waits = self._collect(eng, reads, ())
            sched = self

            def fn(e, eng=eng, ap=ap):
                return e.reg_load(sched.regs[eng], ap)
            self.ops[eng].append((waits, fn, None, 0, None))
        for R in reads:
            for eng in self.ENGS:
                pass

    def begin_guard(self, gid, thr):
        self.guard = (gid, thr, None, 0)
        self.guard_snap[gid] = dict(self.dom_max)

    def begin_inner(self, gid, thr):
        g = self.guard
        self.guard = (g[0], g[1], gid, thr)
        self.guard_snap[gid] = dict(self.dom_max)

    def end_inner(self):
        g = self.guard
        self.guard = (g[0], g[1], None, 0)

    def end_guard(self):
        self.guard = None

    def barrier(self):
        assert self.guard is None
        for eng in self.ENGS:
            kn = self.known[eng]
            waits = []
            for d, v in self.dom_max.items():
                if v > 0 and kn.get(d, 0) < v:
                    kn[d] = v
                    waits.append((d, v))
            if waits:
                self.ops[eng].append((waits, None, None, 0, None))

    def emit(self, block):
        sched = self

        def emit_one(e, item):
            waits, fn, sem, inc, _ = item
            for (d_, v) in waits:
                e.wait_ge(sched.dom_sem[d_], v)
            if fn is None:
                return
            ins = fn(e)
            if sem is not None:
                ins.then_inc(sem, inc)

        def skip_path(e, grp, snap):
            incs = []
            for waits, fn, sem, inc, _ in grp:
                for (d_, v) in waits:
                    v = min(v, snap.get(d_, 0))
                    if v > 0:
                        e.wait_ge(sched.dom_sem[d_], v)
                if sem is not None:
                    for k_ in range(len(incs)):
                        if incs[k_][0] is sem:
                            incs[k_][1] += inc
                            break
                    else:
                        incs.append([sem, inc])
            e.drain()
            for sem, tot in incs:
                e.sem_inc(sem, tot)

        def make(engname):
            def body(e):
                ops = sched.ops[engname]
                sched.regs[engname] = e.alloc_register("cnt_" + engname)
                reg = sched.regs[engname]
                i = 0
                n = len(ops)
                while i < n:
                    g = ops[i][4]
                    if g is None:
                        emit_one(e, ops[i])
                        i += 1
                        continue
                    j = i
                    while j < n and ops[j][4] is not None and ops[j][4][0] == g[0]:
                        j += 1
                    grp = ops[i:j]
                    with e.If_lt(reg, g[1]):
                        skip_path(e, grp, sched.guard_snap[g[0]])
                    with e.Else():
                        a = 0
                        m = len(grp)
                        while a < m:
                            gi = grp[a][4]
                            if gi[2] is None:
                                emit_one(e, grp[a])
                                a += 1
                                continue
                            b = a
                            while b < m and grp[b][4][2] == gi[2]:
                                b += 1
                            sub = grp[a:b]
                            with e.If_lt(reg, gi[3]):
                                skip_path(e, sub, sched.guard_snap[gi[2]])
                            with e.Else():
                                for it in sub:
                                    emit_one(e, it)
                            a = b
                    i = j
            return body
        for engname in self.ENGS:
            if self.ops[engname]:
                getattr(block, engname)(make(engname))


def mk(method, **kw):
    return lambda e: getattr(e, method)(**kw)


def build(debug=False, n_experts=NE, stage=3):
    nc = bass.Bass("TRN2", target_bir_lowering=False)

    def din(name, shape):
        return nc.dram_tensor(name, shape, F32, kind="ExternalInput").ap()

    x_all = din("x_all", [TALL, D])
    c_rep = din("c_rep", [128, 8, 128])
    flag_d = din("flag", [128, 1])
    w_ada = din("w_ada", [D, 6 * D])
    b_ada = din("b_ada", [1, 6 * D])
    g_mix = din("g_mix", [1, D])
    g_ffn = din("g_ffn", [1, D])
    g_final = din("g_final", [1, D])
    w_in = din("w_in", [D, 2560])
    w_out = din("w_out", [D, D])
    recp = din("recp", [128, 4, 8])
    wa_bd = din("wa_bd", [128, 4, 128])
    wx_bd = din("wx_bd", [128, 4, 128])
    w_router = din("w_router", [D, NE])
    b_router = din("b_router", [1, NE])
    w_gu = din("w_gate_up", [n_experts, D, 2 * D])
    bgu_t = din("bgu_t", [128, NE, 16])
    b_gu_d = din("b_gu", [NE, 2 * D])
    w_dn = din("w_down", [n_experts, D, D])
    b_dn = din("b_down", [NE, D])
    out_d = nc.dram_tensor("out", [TOWN, D], F32, kind="ExternalOutput").ap()
    dbg = {}
    if debug:
        def dout(name, shape):
            dbg[name] = nc.dram_tensor(name, shape, F32, kind="ExternalOutput").ap()
        dout("d_mod", [128, 6 * D])
        dout("d_hT", [128, 8 * 512])
        dout("d_mixT", [128, 8 * TOWN])
        dout("d_x1", [TOWN, D])
        dout("d_gates", [128, 16 * NE])

    with ExitStack() as st:
        S = Sched(nc, st)

        def sbt(stack, name, shape, dt):
            return stack.enter_context(nc.sbuf_tensor(name, shape, dt))

        arena = sbt(st, "arena", [128, 16384], F32)
        hT = arena[:].bitcast(BF16).rearrange("p (k t) -> p k t", k=8)
        x1 = arena[:].rearrange("p (t f) -> p t f", t=16)
        r_hT = RL(32)
        r_x1 = RL(16)
        bufA = sbt(st, "bufA", [128, 8, TOWN], BF16)
        r_bufA = [RL(16) for _ in range(8)]
        mv3 = sbt(st, "mv3", [128, D], F32); r_mv3 = Res()
        mv4 = sbt(st, "mv4", [128, D], F32); r_mv4 = Res()
        mv5 = sbt(st, "mv5", [128, D], F32); r_mv5 = Res()
        xb = [sbt(st, "xb%d" % i, [128, D], F32) for i in range(2)]; r_xb = RL(2)
        tmpf = sbt(st, "tmpf", [128, D], F32); r_tmpf = Res()
        hb = [sbt(st, "hb%d" % i, [128, D], BF16) for i in range(2)]; r_hb = RL(2)
        junk = sbt(st, "junk", [128, D], BF16)
        ss = sbt(st, "ss", [128, 64], F32); r_ss = Res()
        ms = sbt(st, "ms", [128, 64], F32); r_ms = Res()
        rstd = sbt(st, "rstd", [128, 64], F32); r_rstd = Res()
        ident = sbt(st, "ident", [128, 128], BF16); r_ident = Res()
        identf = sbt(st, "identf", [128, 128], F32); r_identf = Res()
        neghalf = sbt(st, "neghalf", [128, 1], F32); r_nh = Res()
        flag = sbt(st, "flag_sb", [128, 1], F32); r_flag = Res()
        flagb = sbt(st, "flagb", [128, 1], F32); r_flagb = Res()
        negmask = sbt(st, "negmask", [128, 512], BF16); r_negmask = Res()
        maskB = sbt(st, "maskB", [128, 512], BF16); r_maskB = Res()
        ones_a = sbt(st, "ones_a", [128, 128], BF16); r_ones = Res()
        ones_b = sbt(st, "ones_b", [128, 128], BF16)

        banks = [st.enter_context(nc.psum_tensor("bank%d" % i, [128, 512], F32)) for i in range(8)]
        r_bank = RL(8)
        bank_rr = [0]

        def pb():
            i = bank_rr[0]
            bank_rr[0] = (i + 1) % 8
            return banks[i], r_bank[i]

        cp_rr = [0]
        evac_dve_only = [False]

        def evac(out, in_, reads, writes):
            cp_rr[0] ^= 1
            if cp_rr[0] and not evac_dve_only[0]:
                S.op("scalar", mk("activation", out=out, in_=in_, func=AF.Copy), reads=reads, writes=writes)
            else:
                S.op("vector", mk("tensor_copy", out=out, in_=in_), reads=reads, writes=writes)

        S.dma(flag[:], flag_d, writes=[r_flag])
        S.op("gpsimd", mk("memset", ap=neghalf[:], constant=-0.5), writes=[r_nh])
        S.op("gpsimd", mk("memset", ap=ident[:], constant=1.0), writes=[r_ident])
        S.op("gpsimd", mk("affine_select", out=ident[:], in_=ident[:], pattern=[[-1, 128]],
                          compare_op=ALU.is_equal, fill=0.0, base=0, channel_multiplier=1),
             reads=[r_ident], writes=[r_ident])
        S.op("gpsimd", mk("memset", ap=identf[:], constant=1.0), writes=[r_identf])
        S.op("gpsimd", mk("affine_select", out=identf[:], in_=identf[:], pattern=[[-1, 128]],
                          compare_op=ALU.is_equal, fill=0.0, base=0, channel_multiplier=1),
             reads=[r_identf], writes=[r_identf])
        S.op("gpsimd", mk("memset", ap=negmask[:], constant=0.0), writes=[r_negmask])
        for blk in range(4):
            sl = negmask[:, blk * 128:(blk + 1) * 128]
            if blk % 2 == 0:
                S.op("gpsimd", mk("affine_select", out=sl, in_=sl, pattern=[[-1, 128]], compare_op=ALU.is_ge,
                                  fill=-30000.0, base=0, channel_multiplier=1), reads=[r_negmask], writes=[r_negmask])
            else:
                S.op("gpsimd", mk("affine_select", out=sl, in_=sl, pattern=[[1, 128]], compare_op=ALU.is_ge,
                                  fill=-30000.0, base=0, channel_multiplier=-1), reads=[r_negmask], writes=[r_negmask])
        S.op("vector", mk("tensor_scalar", out=flagb[:], in0=flag[:], scalar1=-1.0, scalar2=30000.0,
                          op0=ALU.add, op1=ALU.mult), reads=[r_flag], writes=[r_flagb])
        S.op("vector", mk("tensor_copy", out=maskB[:], in_=negmask[:]), reads=[r_negmask], writes=[r_maskB])
        for blk in (0, 2):
            sl = maskB[:, blk * 128:(blk + 1) * 128]
            S.op("vector", mk("tensor_scalar", out=sl, in0=sl, scalar1=flagb[:, 0:1], scalar2=None, op0=ALU.add),
                 reads=[r_maskB, r_flagb], writes=[r_maskB])
        S.op("gpsimd", mk("memset", ap=ones_a[:], constant=0.0), writes=[r_ones])
        S.op("gpsimd", mk("memset", ap=ones_a[:, 0:64], constant=1.0), writes=[r_ones])
        S.op("gpsimd", mk("memset", ap=ones_b[:], constant=0.0), writes=[r_ones])
        S.op("gpsimd", mk("memset", ap=ones_b[:, 64:128], constant=1.0), writes=[r_ones])

        def wview(w2d, c0, n):
            return w2d[:, c0:c0 + n].rearrange("(kc p) n -> p kc n", p=128)

        def rms_tile(src, r_src, col, A_vec, r_A, B_vec, r_B, hbt, r_hbt):
            S.op("scalar", mk("activation", out=junk[:], in_=src, func=AF.Square, accum_out=ss[:, col:col + 1]),
                 reads=[r_src], writes=[r_ss])
            S.op("vector", mk("tensor_scalar", out=ms[:, col:col + 1], in0=ss[:, col:col + 1], scalar1=1.0 / D,
                              scalar2=EPS, op0=ALU.mult, op1=ALU.add), reads=[r_ss], writes=[r_ms])
            S.op("gpsimd", mk("tensor_tensor", out=rstd[:, col:col + 1], in0=ms[:, col:col + 1], in1=neghalf[:],
                              op=ALU.pow), reads=[r_ms, r_nh], writes=[r_rstd])
            if B_vec is None:
                S.op("vector", mk("scalar_tensor_tensor", out=hbt, in0=src, scalar=rstd[:, col:col + 1], in1=A_vec,
                                  op0=ALU.mult, op1=ALU.mult), reads=[r_src, r_rstd, r_A], writes=[r_hbt])
                return
            S.op("vector", mk("scalar_tensor_tensor", out=tmpf[:], in0=src, scalar=rstd[:, col:col + 1], in1=A_vec,
                              op0=ALU.mult, op1=ALU.mult), reads=[r_src, r_rstd, r_A], writes=[r_tmpf])
            S.op("gpsimd", mk("tensor_tensor", out=hbt, in0=tmpf[:], in1=B_vec, op=ALU.add),
                 reads=[r_tmpf, r_B], writes=[r_hbt])

        def transpose_tile(hbt, r_hbt, dstT, r_dst, k0=0, k1=8):
            bk, r_bk = pb()
            pv = bk[:].bitcast(BF16).rearrange("p (k t) -> p k t", k=8)
            S.group("tensor", [mk("transpose", out=pv[:, kc, :], in_=hbt[:, kc * 128:(kc + 1) * 128], identity=ident[:])
                               for kc in range(k0, k1)], reads=[r_hbt, r_ident], writes=[r_bk])
            evac(dstT, pv[:, k0:k1, :], [r_bk], r_dst)

        with ExitStack() as st_mix:
            mv2 = sbt(st_mix, "mv2", [128, D], F32); r_mv2 = Res()
            with ExitStack() as st_a:
                mv0 = sbt(st_a, "mv0", [128, D], F32); r_mv0 = Res()
                mv1 = sbt(st_a, "mv1", [128, D], F32); r_mv1 = Res()
                mvs = [mv0, mv1, mv2, mv3, mv4, mv5]
                r_mvs = [r_mv0, r_mv1, r_mv2, r_mv3, r_mv4, r_mv5]
                with ExitStack() as st0:
                    bada = sbt(st0, "bada", [128, 6 * D], F32); r_bada = Res()
                    gmix_bc = sbt(st0, "gmix_bc", [128, D], F32); r_gmix = Res()
                    gffn_bc = sbt(st0, "gffn_bc", [128, D], F32); r_gffn = Res()
                    wab = [sbt(st0, "wab%d" % i, [128, 8, 512], BF16) for i in range(2)]; r_wab = RL(2)
                    c_bf = sbt(st0, "c_bf", [128, 8, 128], BF16); r_cbf = Res()
                    S.dma(c_bf[:], c_rep, writes=[r_cbf], eng="gpsimd")
                    S.dma(bada[:], b_ada.partition_broadcast(128), writes=[r_bada])
                    S.dma(gmix_bc[:], g_mix.partition_broadcast(128), writes=[r_gmix])
                    S.dma(gffn_bc[:], g_ffn.partition_broadcast(128), writes=[r_gffn])
                    for cc in range(12):
                        wb_, r_wb_ = wab[cc % 2], r_wab[cc % 2]
                        S.dma(wb_[:], wview(w_ada, cc * 512, 512), writes=[r_wb_], eng="gpsimd")
                        bk, r_bk = pb()
                        S.group("tensor", [mk("matmul", out=bk[:], lhsT=c_bf[:, kc, :], rhs=wb_[:, kc, :],
                                              start=(kc == 0), stop=(kc == 7)) for kc in range(8)],
                                reads=[r_cbf, r_wb_], writes=[r_bk])
                        dst = mvs[cc // 2][:, (cc % 2) * 512:(cc % 2 + 1) * 512]
                        S.op("vector", mk("tensor_tensor", out=dst, in0=bk[:], in1=bada[:, cc * 512:(cc + 1) * 512],
                                          op=ALU.add), reads=[r_bk, r_bada], writes=[r_mvs[cc // 2]])
                    if debug:
                        for i in range(6):
                            S.dma(dbg["d_mod"][:, i * D:(i + 1) * D], mvs[i][:], reads=[r_mvs[i]])
                    S.op("vector", mk("scalar_tensor_tensor", out=mv1[:], in0=mv1[:], scalar=1.0, in1=gmix_bc[:],
                                      op0=ALU.add, op1=ALU.mult), reads=[r_gmix], writes=[r_mv1])
                    S.op("vector", mk("scalar_tensor_tensor", out=mv4[:], in0=mv4[:], scalar=1.0, in1=gffn_bc[:],
                                      op0=ALU.add, op1=ALU.mult), reads=[r_gffn], writes=[r_mv4])
                    S.barrier()
                for t in range(32):
                    xs, r_xs = xb[t % 2], r_xb[t % 2]
                    S.dma(xs[:], x_all[t * 128:(t + 1) * 128, :], writes=[r_xs])
                    rms_tile(xs[:], r_xs, t, mv1[:], r_mv1, mv0[:], r_mv0, hb[t % 2][:], r_hb[t % 2])
                    transpose_tile(hb[t % 2], r_hb[t % 2], hT[:, :, t * 128:(t + 1) * 128], [r_hT[t]])
                if debug:
                    S.barrier()
                    with ExitStack() as st_d:
                        dtmp = sbt(st_d, "dtmp", [128, 8 * 512], F32); r_dtmp = Res()
                        S.op("vector", mk("tensor_copy", out=dtmp[:].rearrange("p (k t) -> p k t", k=8),
                                          in_=hT[:, :, 1792:2304]), reads=r_hT, writes=[r_dtmp])
                        S.dma(dbg["d_hT"], dtmp[:], reads=[r_dtmp])
                        S.barrier()
                S.barrier()
            mixT = bufA
            with ExitStack() as st_r:
                NB = 1024
                xr = sbt(st_r, "xr", [128, NB + 3], F32); r_xr = Res()
                xc = sbt(st_r, "xc", [128, NB], F32); r_xc = Res()
                xcb = sbt(st_r, "xcb", [128, NB], BF16); r_xcb = Res()
                rg = sbt(st_r, "rg", [128, NB], F32); r_rg = Res()
                ig = sbt(st_r, "ig", [128, NB], F32); r_ig = Res()
                ag = sbt(st_r, "ag", [128, NB], F32); r_ag = Res()
                t1 = sbt(st_r, "t1", [128, NB], F32); r_t1 = Res()
                hs = sbt(st_r, "hs", [128, NB], F32); r_hs = Res()
                gg = sbt(st_r, "gg", [128, NB], F32); r_gg = Res()
                wxr = [sbt(st_r, "wxr%d" % i, [128, 8, 128], BF16) for i in range(2)]; r_wxr = RL(2)
                wgr = [sbt(st_r, "wgr%d" % i, [128, 8, 128], BF16) for i in range(2)]; r_wgr = RL(2)
                wbd = [sbt(st_r, "wbd%d" % i, [128, 4, 128], BF16) for i in range(2)]; r_wbd = RL(2)
                rp = sbt(st_r, "rp", [128, 4, 8], F32); r_rp = Res()
                clam = sbt(st_r, "clam", [128, 4], F32); r_clam = Res()
                state = sbt(st_r, "state", [128, 4], F32); r_state = Res()
                S.dma(rp[:], recp, writes=[r_rp])
                S.dma(wbd[0][:], wa_bd, writes=[r_wbd[0]], eng="gpsimd")
                S.dma(wbd[1][:], wx_bd, writes=[r_wbd[1]], eng="gpsimd")
                S.op("scalar", mk("activation", out=clam[:], in_=rp[:, :, 7], func=AF.Exp, scale=-1.0),
                     reads=[r_rp], writes=[r_clam])
                S.op("scalar", mk("activation", out=clam[:], in_=clam[:], func=AF.Ln, bias=1.0, scale=1.0),
                     reads=[r_clam], writes=[r_clam])
                S.op("vector", mk("tensor_scalar", out=clam[:], in0=clam[:], scalar1=-8.0, scalar2=None, op0=ALU.mult),
                     reads=[r_clam], writes=[r_clam])
                S.op("vector", mk("memset", ap=state[:], constant=0.0), writes=[r_state])
                for cch in range(4):
                    sl = cch % 2
                    S.dma(wxr[sl][:], wview(w_in, 1536 + cch * 128, 128), writes=[r_wxr[sl]], eng="gpsimd")
                    S.dma(wgr[sl][:], wview(w_in, 2048 + cch * 128, 128), writes=[r_wgr[sl]], eng="gpsimd")
                    S.op("vector", mk("memset", ap=xr[:, 0:3], constant=0.0), writes=[r_xr])
                    for seg in range(4):
                        t0 = seg * NB
                        rh = r_hT[seg * 8:(seg + 1) * 8]
                        if seg == 2:
                            S.op("vector", mk("tensor_scalar", out=xr[:, 0:3], in0=xr[:, 0:3], scalar1=flag[:, 0:1],
                                              scalar2=None, op0=ALU.mult), reads=[r_flag], writes=[r_xr])
                            S.op("vector", mk("tensor_scalar", out=state[:, cch:cch + 1], in0=state[:, cch:cch + 1],
                                              scalar1=flag[:, 0:1], scalar2=None, op0=ALU.mult),
                                 reads=[r_flag], writes=[r_state])
                        for h2 in range(2):
                            bk, r_bk = pb()
                            S.group("tensor", [mk("matmul", out=bk[:], lhsT=wxr[sl][:, kc, :],
                                                  rhs=hT[:, kc, t0 + h2 * 512:t0 + (h2 + 1) * 512],
                                                  start=(kc == 0), stop=(kc == 7)) for kc in range(8)],
                                    reads=[r_wxr[sl]] + rh, writes=[r_bk])
                            S.op("scalar", mk("activation", out=xr[:, 3 + h2 * 512:3 + (h2 + 1) * 512], in_=bk[:],
                                              func=AF.Copy), reads=[r_bk], writes=[r_xr])
                        S.op("vector", mk("tensor_scalar", out=xc[:], in0=xr[:, 3:NB + 3], scalar1=rp[:, cch, 3:4],
                                          scalar2=rp[:, cch, 4:5], op0=ALU.mult, op1=ALU.add),
                             reads=[r_xr, r_rp], writes=[r_xc])
                        for j in range(3):
                            S.op("vector", mk("scalar_tensor_tensor", out=xc[:], in0=xr[:, j:j + NB],
                                              scalar=rp[:, cch, j:j + 1], in1=xc[:], op0=ALU.mult, op1=ALU.add),
                                 reads=[r_xr, r_rp], writes=[r_xc])
                        S.op("vector", mk("tensor_copy", out=xr[:, 0:3], in_=xr[:, NB:NB + 3]), writes=[r_xr])
                        S.op("scalar", mk("activation", out=xcb[:], in_=xc[:], func=AF.Copy), reads=[r_xc], writes=[r_xcb])
                        for which, (dst, r_dst, bcol) in enumerate(((rg, r_rg, 5), (ig, r_ig, 6))):
                            for h2 in range(2):
                                bk, r_bk = pb()
                                S.group("tensor", [mk("matmul", out=bk[:], lhsT=wbd[which][:, cch, :],
                                                      rhs=xcb[:, h2 * 512:(h2 + 1) * 512], start=True, stop=True)],
                                        reads=[r_wbd[which], r_xcb], writes=[r_bk])
                                S.op("scalar", mk("activation", out=dst[:, h2 * 512:(h2 + 1) * 512], in_=bk[:],
                                                  func=AF.Sigmoid, bias=rp[:, cch, bcol:bcol + 1], scale=1.0),
                                     reads=[r_bk, r_rp], writes=[r_dst])
                        S.op("scalar", mk("activation", out=ag[:], in_=rg[:], func=AF.Exp, scale=clam[:, cch:cch + 1]),
                             reads=[r_rg, r_clam], writes=[r_ag])
                        S.op("vector", mk("tensor_tensor", out=t1[:], in0=ag[:], in1=ag[:], op=ALU.mult),
                             reads=[r_ag], writes=[r_t1])
                        S.op("vector", mk("tensor_scalar", out=t1[:], in0=t1[:], scalar1=-1.0, scalar2=1.0,
                                          op0=ALU.mult, op1=ALU.add), reads=[r_t1], writes=[r_t1])
                        S.op("vector", mk("tensor_scalar", out=t1[:], in0=t1[:], scalar1=1e-30, scalar2=None,
                                          op0=ALU.max), reads=[r_t1], writes=[r_t1])
                        S.op("scalar", mk("activation", out=t1[:], in_=t1[:], func=AF.Sqrt), reads=[r_t1], writes=[r_t1])
                        S.op("gpsimd", mk("tensor_tensor", out=ig[:], in0=ig[:], in1=xc[:], op=ALU.mult),
                             reads=[r_xc], writes=[r_ig])
                        S.op("gpsimd", mk("tensor_tensor", out=ig[:], in0=ig[:], in1=t1[:], op=ALU.mult),
                             reads=[r_t1], writes=[r_ig])
                        S.op("vector", mk("tensor_tensor_scan", out=hs[:], data0=ag[:], data1=ig[:],
                                          initial=state[:, cch:cch + 1], op0=ALU.mult, op1=ALU.add),
                             reads=[r_ag, r_ig, r_state], writes=[r_hs])
                        S.op("vector", mk("tensor_copy", out=state[:, cch:cch + 1], in_=hs[:, NB - 1:NB]),
                             reads=[r_hs], writes=[r_state])
                        if seg >= 2:
                            o0 = (seg - 2) * NB
                            for h2 in range(2):
                                bk, r_bk = pb()
                                S.group("tensor", [mk("matmul", out=bk[:], lhsT=wgr[sl][:, kc, :],
                                                      rhs=hT[:, kc, t0 + h2 * 512:t0 + (h2 + 1) * 512],
                                                      start=(kc == 0), stop=(kc == 7)) for kc in range(8)],
                                        reads=[r_wgr[sl]] + rh, writes=[r_bk])
                                S.op("scalar", mk("activation", out=gg[:, h2 * 512:(h2 + 1) * 512], in_=bk[:],
                                                  func=AF.Gelu_apprx_tanh), reads=[r_bk], writes=[r_gg])
                            S.op("gpsimd", mk("tensor_tensor", out=mixT[:, 4 + cch, o0:o0 + NB], in0=hs[:], in1=gg[:],
                                              op=ALU.mult), reads=[r_hs, r_gg],
                                 writes=r_bufA[4 + cch][(seg - 2) * 8:(seg - 1) * 8])
                S.barrier()
            with ExitStack() as st_at:
                wqkv = sbt(st_at, "wqkv", [128, 3, 8, 128], BF16); r_wqkv = Res()
                qTa = sbt(st_at, "qTa", [128, TOWN], BF16); r_qTa = Res()
                qTb = sbt(st_at, "qTb", [128, TOWN], BF16); r_qTb = Res()
                kT = sbt(st_at, "kT", [128, TALL], BF16); r_kT = Res()
                Vta = sbt(st_at, "Vta", [128, 32, 128], BF16); r_Vt = RL(8)
                Vtb = sbt(st_at, "Vtb", [128, 32, 128], BF16)
                accN = sbt(st_at, "accN", [128, TOWN], F32); r_accN = Res()
                accD = sbt(st_at, "accD", [128, TOWN], F32); r_accD = Res()
                PT = [sbt(st_at, "PT%d" % i, [128, 512], BF16) for i in range(2)]; r_PT = RL(2)
                pt_rr = 0
                S.op("gpsimd", mk("memset", ap=Vta[:], constant=0.0), writes=r_Vt)
                S.op("gpsimd", mk("memset", ap=Vtb[:], constant=0.0), writes=r_Vt)
                S.op("gpsimd", mk("memset", ap=qTa[:], constant=0.0), writes=[r_qTa])
                S.op("gpsimd", mk("memset", ap=qTb[:], constant=0.0), writes=[r_qTb])
                for fc in range(4):
                    for i3 in range(3):
                        S.dma(wqkv[:, i3, :, :], wview(w_in, i3 * 512 + fc * 128, 128), writes=[r_wqkv], eng="gpsimd")
                    for tb in range(4):
                        bk, r_bk = pb()
                        c0 = TOWN + tb * 512
                        S.group("tensor", [mk("matmul", out=bk[:], lhsT=wqkv[:, 0, kc, :], rhs=hT[:, kc, c0:c0 + 512],
                                              start=(kc == 0), stop=(kc == 7)) for kc in range(8)],
                                reads=[r_wqkv] + r_hT[16 + tb * 4:16 + (tb + 1) * 4], writes=[r_bk])
                        S.op("scalar", mk("activation", out=qTa[0:64, tb * 512:(tb + 1) * 512], in_=bk[0:64, :],
                                          func=AF.Copy), reads=[r_bk], writes=[r_qTa])
                        S.op("vector", mk("tensor_copy", out=qTb[64:128, tb * 512:(tb + 1) * 512], in_=bk[64:128, :]),
                             reads=[r_bk], writes=[r_qTb])
                    for tb in range(8):
                        bk, r_bk = pb()
                        c0 = tb * 512
                        S.group("tensor", [mk("matmul", out=bk[:], lhsT=wqkv[:, 1, kc, :], rhs=hT[:, kc, c0:c0 + 512],
                                              start=(kc == 0), stop=(kc == 7)) for kc in range(8)],
                                reads=[r_wqkv] + r_hT[tb * 4:(tb + 1) * 4], writes=[r_bk])
                        evac(kT[:, c0:c0 + 512], bk[:], [r_bk], [r_kT])
                    for pat, d in enumerate((1, 4, 16)):
                        L = TALL // d
                        nj = L // 128
                        for g4 in range(8):
                            bk, r_bk = pb()
                            fns = []
                            for u in range(4):
                                ti = g4 * 4 + u
                                r_, j_ = divmod(ti, nj)
                                s0 = r_ + d * 128 * j_
                                for kc in range(8):
                                    fns.append(mk("matmul", out=bk[:, u * 128:(u + 1) * 128],
                                                  lhsT=hT[:, kc, s0:s0 + d * 127 + 1:d], rhs=wqkv[:, 2, kc, :],
                                                  start=(kc == 0), stop=(kc == 7)))
                            S.group("tensor", fns, reads=[r_wqkv] + r_hT, writes=[r_bk])
                            bv = bk[:].rearrange("p (u c) -> p u c", u=4)
                            S.op("scalar", mk("activation", out=Vta[:, g4 * 4:(g4 + 1) * 4, 0:64], in_=bv[:, :, 0:64],
                                              func=AF.Copy), reads=[r_bk], writes=[r_Vt[g4]])
                            S.op("vector", mk("tensor_copy", out=Vtb[:, g4 * 4:(g4 + 1) * 4, 64:128], in_=bv[:, :, 64:128]),
                                 reads=[r_bk], writes=[r_Vt[g4]])
                        for su in range(4):
                            bkN, r_bkN = pb()
                            bkD, r_bkD = pb()
                            for u in range(4):
                                if d == 1:
                                    r_, jq = 0, 16 + 4 * su + u
                                elif d == 4:
                                    r_, jq = u, 4 + su
                                else:
                                    r_, jq = 4 * su + u, 1
                                q0 = r_ + d * 128 * jq - TOWN
                                kp0 = r_ + d * 128 * (jq - 1)
                                kc0 = r_ + d * 128 * jq
                                tip = r_ * nj + jq - 1
                                tic = r_ * nj + jq
                                boundary = (jq == nj // 2)
                                span = d * 127 + 1
                                bkS, r_bkS = pb()
                                msk = maskB if boundary else negmask
                                fns = [mk("matmul", out=bkS[:], lhsT=ident[:], rhs=msk[:], start=True, stop=False)]
                                for bi, (qq, k0) in enumerate(((qTa, kp0), (qTa, kc0), (qTb, kp0), (qTb, kc0))):
                                    fns.append(mk("matmul", out=bkS[:, bi * 128:(bi + 1) * 128],
                                                  lhsT=kT[:, k0:k0 + span:d], rhs=qq[:, q0:q0 + span:d],
                                                  start=False, stop=(bi == 3)))
                                S.group("tensor", fns, reads=[r_ident, r_negmask, r_maskB, r_kT, r_qTa, r_qTb],
                                        writes=[r_bkS])
                                pt, r_pt = PT[pt_rr], r_PT[pt_rr]
                                pt_rr ^= 1
                                S.op("scalar", mk("activation", out=pt[:], in_=bkS[:], func=AF.Exp, scale=0.125),
                                     reads=[r_bkS], writes=[r_pt])
                                oc = slice(u * 128, (u + 1) * 128)
                                fnsN = [
                                    mk("matmul", out=bkN[:, oc], lhsT=Vta[:, tip, :], rhs=pt[:, 0:128], start=True, stop=False),
                                    mk("matmul", out=bkN[:, oc], lhsT=Vta[:, tic, :], rhs=pt[:, 128:256], start=False, stop=False),
                                    mk("matmul", out=bkN[:, oc], lhsT=Vtb[:, tip, :], rhs=pt[:, 256:384], start=False, stop=False),
                                    mk("matmul", out=bkN[:, oc], lhsT=Vtb[:, tic, :], rhs=pt[:, 384:512], start=False, stop=True),
                                ]
                                S.group("tensor", fnsN, reads=[r_pt, r_Vt[tip // 4], r_Vt[tic // 4]], writes=[r_bkN])
                                fnsD = [
                                    mk("matmul", out=bkD[:, oc], lhsT=ones_a[:], rhs=pt[:, 0:128], start=True, stop=False),
                                    mk("matmul", out=bkD[:, oc], lhsT=ones_a[:], rhs=pt[:, 128:256], start=False, stop=False),
                                    mk("matmul", out=bkD[:, oc], lhsT=ones_b[:], rhs=pt[:, 256:384], start=False, stop=False),
                                    mk("matmul", out=bkD[:, oc], lhsT=ones_b[:], rhs=pt[:, 384:512], start=False, stop=True),
                                ]
                                S.group("tensor", fnsD, reads=[r_pt, r_ones], writes=[r_bkD])
                            for acc, r_acc, bkX, r_bkX, eng in ((accN, r_accN, bkN, r_bkN, "vector"),
                                                               (accD, r_accD, bkD, r_bkD, "gpsimd")):
                                src = bkX[:].rearrange("p (u i) -> p u i", u=4)
                                if d == 1:
                                    dst = acc[:, su * 512:(su + 1) * 512].rearrange("p (u i) -> p u i", u=4)
                                elif d == 4:
                                    dst = acc[:, su * 512:(su + 1) * 512].rearrange("p (i r) -> p r i", r=4)
                                else:
                                    dst = acc[:].rearrange("p (i r) -> p r i", r=16)[:, 4 * su:4 * su + 4, :]
                                if d == 1:
                                    S.op("vector" if eng == "vector" else "scalar",
                                         mk("tensor_copy", out=dst, in_=src) if eng == "vector" else
                                         mk("activation", out=dst, in_=src, func=AF.Copy),
                                         reads=[r_bkX], writes=[r_acc])
                                else:
                                    S.op("vector", mk("tensor_tensor", out=dst, in0=dst, in1=src, op=ALU.add),
                                         reads=[r_bkX], writes=[r_acc])
                    S.op("vector", mk("reciprocal", out=accD[:], in_=accD[:]), reads=[r_accD], writes=[r_accD])
                    S.op("vector", mk("tensor_tensor", out=mixT[:, fc, :], in0=accN[:], in1=accD[:], op=ALU.mult),
                         reads=[r_accN, r_accD], writes=r_bufA[fc])
                S.barrier()
            if debug:
                with ExitStack() as st_d:
                    dtmp = sbt(st_d, "dtmp2", [128, 8 * 512], F32); r_dtmp = Res()
                    for q4 in range(4):
                        S.op("vector", mk("tensor_copy", out=dtmp[:].rearrange("p (k t) -> p k t", k=8),
                                          in_=mixT[:, :, q4 * 512:(q4 + 1) * 512]), reads=[], writes=[r_dtmp])
                        S.dma(dbg["d_mixT"].rearrange("p (k t) -> p k t", k=8)[:, :, q4 * 512:(q4 + 1) * 512],
                              dtmp[:].rearrange("p (k t) -> p k t", k=8), reads=[r_dtmp])
                    S.barrier()
            with ExitStack() as st_o:
                wout = sbt(st_o, "wout", [128, 8, D], BF16); r_wout = Res()
                S.dma(wout[:, 0:4, :], w_out[0:512, :].rearrange("(kc p) n -> p kc n", p=128), writes=[r_wout], eng="gpsimd")
                S.dma(wout[:, 4:8, :], w_out[512:1024, :].rearrange("(kc p) n -> p kc n", p=128), writes=[r_wout], eng="gpsimd")
                for t in range(16):
                    xs, r_xs = xb[t % 2], r_xb[t % 2]
                    S.dma(xs[:], x_all[TOWN + t * 128:TOWN + (t + 1) * 128, :], writes=[r_xs])
                    for cc in range(2):
                        bk, r_bk = pb()
                        cs = slice(cc * 512, (cc + 1) * 512)
                        S.group("tensor", [mk("matmul", out=bk[:], lhsT=mixT[:, kc, t * 128:(t + 1) * 128],
                                              rhs=wout[:, kc, cs], start=(kc == 0), stop=(kc == 7)) for kc in range(8)],
                                reads=[r_wout] + [r_bufA[kc][t] for kc in range(8)], writes=[r_bk])
                        S.op("vector", mk("tensor_tensor", out=tmpf[:, cs], in0=bk[:], in1=mv2[:, cs], op=ALU.mult),
                             reads=[r_bk, r_mv2], writes=[r_tmpf])
                        S.op("gpsimd", mk("tensor_tensor", out=x1[:, t, cs], in0=tmpf[:, cs], in1=xs[:, cs], op=ALU.add),
                             reads=[r_tmpf, r_xs], writes=[r_x1[t]])
                S.barrier()
        if debug:
            for t in range(16):
                S.dma(dbg["d_x1"][t * 128:(t + 1) * 128, :], x1[:, t, :], reads=[r_x1[t]])
        I32 = mybir.dt.int32
        CAP = TOWN
        x_buf = nc.dram_tensor("x_buf", [NE * CAP, D], BF16, kind="Internal").ap()
        y_buf = nc.dram_tensor("y_buf", [NE * CAP, D], F32, kind="Internal").ap()
        with ExitStack() as st_f:
            wgus = [bufA, sbt(st_f, "wgu1", [128, 8, 2 * D], BF16)]; r_wgus = RL(2)
            wgus[0] = bufA[:].rearrange("p k t -> p (k t)").rearrange("p (k t) -> p k t", k=8)
            wd = sbt(st_f, "wd", [128, 8, D], BF16); r_wd = Res()
            gk = sbt(st_f, "gk", [128, 16, 4], F32); r_gk = RL(16)
            desti = sbt(st_f, "desti", [128, 16, 4], I32); r_desti = RL(16)
            cnti = sbt(st_f, "cnti", [128, NE], I32); r_cnti = Res()

            bgrow = [sbt(st_f, "bgrow%d" % i, [1, 2 * D], BF16) for i in range(2)]; r_bgrow = RL(2)
            ones_r = sbt(st_f, "ones_r", [1, 128], BF16); r_ones_r = Res()
            S.op("gpsimd", mk("memset", ap=ones_r[:], constant=1.0), writes=[r_ones_r])

            def load_wgu(e):
                w_, r_w = wgus[e % 2], r_wgus[e % 2]
                for q4 in range(4):
                    S.dma(w_[:, :, q4 * 512:(q4 + 1) * 512], wview(w_gu[e], q4 * 512, 512), writes=[r_w], eng="gpsimd")
                S.dma(bgrow[e % 2][:], b_gu_d[e:e + 1, :], writes=[r_bgrow[e % 2]], eng="gpsimd")
                S.op("gpsimd", mk("tensor_scalar", out=bgrow[e % 2][0:1, D:2 * D], in0=bgrow[e % 2][0:1, D:2 * D], scalar1=1.0,
                                  scalar2=None, op0=ALU.add), reads=[], writes=[r_bgrow[e % 2]])

            def load_wd(e):
                S.dma(wd[:, 0:4, :], w_dn[e][0:512, :].rearrange("(kc p) n -> p kc n", p=128), writes=[r_wd], eng="gpsimd")
                S.dma(wd[:, 4:8, :], w_dn[e][512:1024, :].rearrange("(kc p) n -> p kc n", p=128), writes=[r_wd], eng="gpsimd")

            load_wgu(0)
            load_wd(0)
            with ExitStack() as st_r2:
                wr = sbt(st_r2, "wr", [128, 8, NE], BF16); r_wr = Res()
                brt = sbt(st_r2, "brt", [128, NE], F32); r_brt = Res()
                lg = sbt(st_r2, "lg", [128, NE], F32); r_lg = Res()
                ex = sbt(st_r2, "ex", [128, NE], F32); r_ex = Res()
                gt = sbt(st_r2, "gt", [128, NE], F32); r_gt = Res()
                posb = sbt(st_r2, "posb", [128, NE], F32); r_posb = Res()
                scr4 = sbt(st_r2, "scr4", [128, 4, NE], F32); r_scr = Res()
                ebase = sbt(st_r2, "ebase", [128, NE], F32); r_ebase = Res()
                m8 = sbt(st_r2, "m8", [128, 8], F32); r_m8 = Res()
                e4 = sbt(st_r2, "e4", [128, 4], F32); r_e4 = Res()
                destf = sbt(st_r2, "destf", [128, 4], F32); r_destf = Res()
                nmx = sbt(st_r2, "nmx", [128, 1], F32); r_nmx = Res()
                sm = sbt(st_r2, "sm", [128, 1], F32); r_sm = Res()
                gT = sbt(st_r2, "gT", [32, 128], F32); r_gT = Res()
                bdn = sbt(st_r2, "bdn", [32, D], F32); r_bdn = Res()
                maskall = sbt(st_r2, "maskall", [128, 16, NE], BF16); r_mask = RL(16)
                ltri = sbt(st_r2, "ltri", [128, 128], BF16); r_ltri = Res()
                ones_f = sbt(st_r2, "ones_full", [128, 128], BF16); r_onesf = Res()
                h2Tt = sbt(st_r2, "h2Tt", [128, 8, 128], BF16); r_h2Tt = Res()
                S.dma(wr[:], w_router.rearrange("(kc p) n -> p kc n", p=128), writes=[r_wr], eng="gpsimd")
                S.dma(brt[:], b_router.partition_broadcast(128), writes=[r_brt])
                S.dma(bdn[:], b_dn, writes=[r_bdn])
                S.op("gpsimd", mk("iota", out=ebase[:], pattern=[[CAP, NE]], base=0, channel_multiplier=0,
                                  allow_small_or_imprecise_dtypes=True), writes=[r_ebase])
                S.op("gpsimd", mk("memset", ap=ones_f[:], constant=1.0), writes=[r_onesf])
                S.op("gpsimd", mk("memset", ap=ltri[:], constant=1.0), writes=[r_ltri])
                S.op("gpsimd", mk("affine_select", out=ltri[:], in_=ltri[:], pattern=[[1, 128]], compare_op=ALU.is_gt,
                                  fill=0.0, base=0, channel_multiplier=-1), reads=[r_ltri], writes=[r_ltri])
                for t in range(16):
                    hbt, r_hbt = hb[t % 2], r_hb[t % 2]
                    rms_tile(x1[:, t, :], r_x1[t], 32 + t, mv4[:], r_mv4, mv3[:], r_mv3, hbt[:], r_hbt)
                    transpose_tile(hbt, r_hbt, h2Tt[:], [r_h2Tt])
                    bk, r_bk = pb()
                    S.group("tensor", [mk("matmul", out=bk[:, 0:NE], lhsT=h2Tt[:, kc, :], rhs=wr[:, kc, :],
                                          start=(kc == 0), stop=(kc == 7)) for kc in range(8)],
                            reads=[r_wr, r_h2Tt], writes=[r_bk])
                    S.op("vector", mk("tensor_tensor", out=lg[:], in0=bk[:, 0:NE], in1=brt[:], op=ALU.add),
                         reads=[r_bk, r_brt], writes=[r_lg])
                    S.op("vector", mk("max", out=m8[:], in_=lg[:]), reads=[r_lg], writes=[r_m8])
                    S.op("vector", mk("tensor_scalar", out=nmx[:], in0=m8[:, 0:1], scalar1=-1.0, scalar2=None, op0=ALU.mult),
                         reads=[r_m8], writes=[r_nmx])
                    S.op("scalar", mk("activation", out=ex[:], in_=lg[:], func=AF.Exp, bias=nmx[:, 0:1], scale=1.0),
                         reads=[r_lg, r_nmx], writes=[r_ex])
                    S.op("scalar", mk("activation", out=e4[:], in_=m8[:, 0:4], func=AF.Exp, bias=nmx[:, 0:1], scale=1.0),
                         reads=[r_m8, r_nmx], writes=[r_e4])
                    S.op("vector", mk("tensor_scalar", out=maskall[:, t, :], in0=lg[:], scalar1=m8[:, 3:4], scalar2=None,
                                      op0=ALU.is_ge), reads=[r_lg, r_m8], writes=[r_mask[t]])
                    S.op("vector", mk("tensor_tensor", out=ex[:], in0=ex[:], in1=maskall[:, t, :], op=ALU.mult),
                         reads=[r_mask[t]], writes=[r_ex])
                    S.op("vector", mk("tensor_reduce", out=sm[:], in_=ex[:], axis=mybir.AxisListType.X, op=ALU.add),
                         reads=[r_ex], writes=[r_sm])
                    S.op("vector", mk("reciprocal", out=sm[:], in_=sm[:]), reads=[r_sm], writes=[r_sm])
                    S.op("vector", mk("tensor_scalar", out=gt[:], in0=ex[:], scalar1=sm[:, 0:1], scalar2=None,
                                      op0=ALU.mult), reads=[r_ex, r_sm], writes=[r_gt])
                    S.op("vector", mk("tensor_scalar", out=gk[:, t, :], in0=e4[:], scalar1=sm[:, 0:1], scalar2=None,
                                      op0=ALU.mult), reads=[r_e4, r_sm], writes=[r_gk[t]])
                    bkp, r_bkp = pb()
                    fns = [mk("matmul", out=bkp[:, 0:NE], lhsT=ones_f[:], rhs=maskall[:, tp, :], start=(tp == 0), stop=False)
                           for tp in range(t)]
                    fns.append(mk("matmul", out=bkp[:, 0:NE], lhsT=ltri[:], rhs=maskall[:, t, :], start=(t == 0), stop=True))
                    S.group("tensor", fns, reads=[r_onesf, r_ltri] + r_mask[:t + 1], writes=[r_bkp])
                    S.op("vector", mk("tensor_tensor", out=posb[:], in0=bkp[:, 0:NE], in1=ebase[:], op=ALU.add),
                         reads=[r_bkp, r_ebase], writes=[r_posb])
                    for k in range(4):
                        S.op("vector", mk("scalar_tensor_tensor", out=scr4[:, k, :], in0=lg[:], scalar=m8[:, k:k + 1], in1=posb[:],
                                          op0=ALU.is_equal, op1=ALU.mult),
                             reads=[r_lg, r_m8, r_posb], writes=[r_scr])
                    S.op("vector", mk("tensor_reduce", out=destf[:], in_=scr4[:], axis=mybir.AxisListType.X, op=ALU.add),
                         reads=[r_scr], writes=[r_destf])
                    S.op("vector", mk("tensor_scalar", out=destf[:], in0=destf[:], scalar1=0.0, scalar2=float(NE * CAP - 1),
                                      op0=ALU.max, op1=ALU.min), reads=[r_destf], writes=[r_destf])
                    S.op("vector", mk("tensor_copy", out=desti[:, t, :], in_=destf[:]), reads=[r_destf], writes=[r_desti[t]])
                    for k in range(4):
                        def sc(e, t=t, k=k, hbt=hbt):
                            return e.indirect_dma_start(out=x_buf[:, :],
                                                        out_offset=bass.IndirectOffsetOnAxis(ap=desti[:, t, k:k + 1], axis=0),
                                                        in_=hbt[:, :], in_offset=None)
                        S.dma_fn("gpsimd", sc, reads=[r_desti[t], r_hbt], writes=[])
                    bk2, r_bk2 = pb()
                    S.group("tensor", [mk("transpose", out=bk2[0:NE, 0:128], in_=gt[:], identity=identf[:])],
                            reads=[r_gt, r_identf], writes=[r_bk2])
                    S.op("vector", mk("tensor_copy", out=gT[:], in_=bk2[0:NE, 0:128]), reads=[r_bk2], writes=[r_gT])
                    for cc in range(2):
                        cs = slice(cc * 512, (cc + 1) * 512)
                        bk3, r_bk3 = pb()
                        S.group("tensor", [mk("matmul", out=bk3[:], lhsT=gT[:], rhs=bdn[:, cs], start=True, stop=True)],
                                reads=[r_gT, r_bdn], writes=[r_bk3])
                        S.op("vector", mk("tensor_tensor", out=tmpf[:, cs], in0=bk3[:], in1=mv5[:, cs], op=ALU.mult),
                             reads=[r_bk3, r_mv5], writes=[r_tmpf])
                        S.op("gpsimd", mk("tensor_tensor", out=x1[:, t, cs], in0=tmpf[:, cs], in1=x1[:, t, cs], op=ALU.add),
                             reads=[r_tmpf], writes=[r_x1[t]])
                bkc, r_bkc = pb()
                S.group("tensor", [mk("matmul", out=bkc[:, 0:NE], lhsT=ones_f[:], rhs=maskall[:, tp, :], start=(tp == 0),
                                      stop=(tp == 15)) for tp in range(16)], reads=[r_onesf] + r_mask, writes=[r_bkc])
                S.op("vector", mk("tensor_copy", out=cnti[:], in_=bkc[:, 0:NE]), reads=[r_bkc], writes=[r_cnti])
                if debug:
                    S.dma(dbg["d_gates"][:, 0:64], gk[:].rearrange("p t e -> p (t e)"), reads=r_gk)
                S.barrier()
            S.dma(mv3[:], g_final.partition_broadcast(128), writes=[r_mv3])
            with ExitStack() as st_m:
                xblk = [hb[0], hb[1]]; r_xblk = RL(2)
                xT1 = sbt(st_m, "xT1", [128, 8, 128], BF16)
                xTs = [junk[:].rearrange("p (k t) -> p k t", k=8), xT1[:]]; r_xT = RL(2)
                actT = [sbt(st_m, "actT%d" % i, [128, 8, 128], BF16) for i in range(2)]; r_act = [RL(2), RL(2)]
                yblk = [xb[0], xb[1]]; r_yblk = RL(2)
                sgx = sbt(st_m, "sgx", [128, D], F32)
                ucx = sbt(st_m, "ucx", [128, D], F32)
                gc2 = [[tmpf[:, 0:512], tmpf[:, 512:1024]], [sgx[:, 0:512], sgx[:, 512:1024]]]; r_gc2 = [RL(2), RL(2)]
                actk = [sbt(st_m, "actk%d" % i, [128, D], BF16) for i in range(2)]; r_actk = [RL(2), RL(2)]
                ucs = [[mv4[:, 0:512], mv4[:, 512:1024]], [ucx[:, 0:512], ucx[:, 512:1024]]]; r_uc = [RL(2), RL(2)]
                evac_dve_only[0] = True
                blk_rr = 0
                for e in range(n_experts if stage >= 2 else 0):
                    w_, r_w = wgus[e % 2], r_wgus[e % 2]
                    if e + 1 < n_experts:
                        load_wgu(e + 1)
                    S.load_count(cnti[0:1, e:e + 1], [r_cnti])
                    for jp in range(0, 16, 2):
                        S.begin_guard((e, jp), 128 * jp + 1)
                        pair = (jp, jp + 1)

                        NESTED = False

                        def inner(j, ph):
                            if j != jp and NESTED:
                                S.begin_inner((e, j, ph), 128 * j + 1)

                        def inner_end(j):
                            if j != jp and NESTED:
                                S.end_inner()
                        for j in pair:
                            s_ = j % 2
                            row0 = e * CAP + 128 * j
                            inner(j, 1)
                            S.dma(xblk[s_][:], x_buf[row0:row0 + 128, :], writes=[r_xblk[s_]])
                            transpose_tile(xblk[s_], r_xblk[s_], xTs[s_], [r_xT[s_]])
                            inner_end(j)
                        for j in pair:
                            s_ = j % 2
                            xT = xTs[s_]
                            inner(j, 2)
                            for half in range(2):
                                gub = []
                                for c0 in (half * 512, D + half * 512):
                                    bkX, r_bkX = pb()
                                    fns = [mk("matmul", out=bkX[:], lhsT=ones_r[0:1, :], rhs=bgrow[e % 2][0:1, c0:c0 + 512],
                                              start=True, stop=False)]
                                    for kc in range(8):
                                        fns.append(mk("matmul", out=bkX[:], lhsT=xT[:, kc, :], rhs=w_[:, kc, c0:c0 + 512],
                                                      start=False, stop=(kc == 7)))
                                    S.group("tensor", fns, reads=[r_w, r_xT[s_], r_ones_r, r_bgrow[e % 2]], writes=[r_bkX])
                                    gub.append((bkX, r_bkX))
                                (bkG, r_bkG), (bkU, r_bkU) = gub
                                uc, r_uc_ = ucs[s_][half], r_uc[s_][half]
                                gc, r_gc_ = gc2[s_][half], r_gc2[s_][half]
                                S.op("scalar", mk("activation", out=gc, in_=bkG[:], func=AF.Gelu_apprx_sigmoid),
                                     reads=[r_bkG], writes=[r_gc_])
                                S.op("vector", mk("tensor_scalar", out=uc, in0=bkU[:], scalar1=8.0, scalar2=-6.0,
                                                  op0=ALU.min, op1=ALU.max), reads=[r_bkU], writes=[r_uc_])
                                S.op("vector", mk("scalar_tensor_tensor", out=actk[s_][:, half * 512:(half + 1) * 512], in0=gc,
                                                  scalar=GLU7, in1=uc, op0=ALU.min, op1=ALU.mult),
                                     reads=[r_uc_, r_gc_], writes=[r_actk[s_][half]])
                            inner_end(j)
                        for j in pair:
                            s_ = j % 2
                            row0 = e * CAP + 128 * j
                            inner(j, 3)
                            for half in range(2):
                                transpose_tile(actk[s_], r_actk[s_][half], actT[s_][:, 4 * half:4 * half + 4, :],
                                               [r_act[s_][half]], k0=4 * half, k1=4 * half + 4)
                            for cc in range(2):
                                cs = slice(cc * 512, (cc + 1) * 512)
                                bk, r_bk = pb()
                                S.group("tensor", [mk("matmul", out=bk[:], lhsT=actT[s_][:, kc, :], rhs=wd[:, kc, cs],
                                                      start=(kc == 0), stop=(kc == 7)) for kc in range(8)],
                                        reads=[r_wd] + r_act[s_], writes=[r_bk])
                                S.op("vector", mk("tensor_tensor", out=yblk[s_][:, cs], in0=bk[:], in1=mv5[:, cs], op=ALU.mult),
                                     reads=[r_bk, r_mv5], writes=[r_yblk[s_]])
                            S.dma(y_buf[row0:row0 + 128, :], yblk[s_][:], reads=[r_yblk[s_]], eng="gpsimd")
                            inner_end(j)
                        S.end_guard()
                    if e + 1 < n_experts:
                        load_wd(e + 1)
                evac_dve_only[0] = False
                S.barrier()
            ykb = [mv4, tmpf]; r_ykb = RL(2)
            gi = 0
            for t in range(16):
                for k in range(4 if stage >= 3 else 0):
                    yk, r_yk = ykb[gi % 2], r_ykb[gi % 2]
                    gi += 1

                    def ga(e, t=t, k=k, yk=yk):
                        return e.indirect_dma_start(out=yk[:, :], out_offset=None, in_=y_buf[:, :],
                                                    in_offset=bass.IndirectOffsetOnAxis(ap=desti[:, t, k:k + 1], axis=0))
                    S.dma_fn("gpsimd", ga, reads=[r_desti[t]], writes=[r_yk])
                    S.op("vector", mk("scalar_tensor_tensor", out=x1[:, t, :], in0=yk[:], scalar=gk[:, t, k:k + 1],
                                      in1=x1[:, t, :], op0=ALU.mult, op1=ALU.add),
                         reads=[r_yk, r_gk[t]], writes=[r_x1[t]])
                ob = xb[t % 2]; r_ob = r_xb[t % 2]
                rms_tile(x1[:, t, :], r_x1[t], 48 + t, mv3[:], r_mv3, None, None, ob[:], r_ob)
                S.dma(out_d[t * 128:(t + 1) * 128, :], ob[:], reads=[r_ob])
            S.barrier()
        with nc.Block() as block:
            S.emit(block)
    return nc


_NC_CACHE = {}


def make_in_maps(inputs, cores, n_experts=NE):
    f = lambda a: np.ascontiguousarray(np.asarray(a, dtype=np.float32))
    x = f(inputs["x"]); c = f(inputs["c"])
    w_ada = f(inputs["w_ada"])[0]; b_ada = f(inputs["b_ada"])[0][None, :]
    g_mix = f(inputs["g_mix"])[0][None, :]; g_ffn = f(inputs["g_ffn"])[0][None, :]
    g_final = f(inputs["g_final"])[None, :]
    w_in = f(inputs["w_in"])[0]; w_out = f(inputs["w_out"])[0]
    conv_w = f(inputs["conv_w"])[0]; conv_b = f(inputs["conv_b"])[0]
    b_a = f(inputs["b_rg_a"])[0]; b_x = f(inputs["b_rg_x"])[0]; lam = f(inputs["lam"])[0]
    recp = np.zeros((128, 4, 8), np.float32)
    cols = [conv_w[0], conv_w[1], conv_w[2], conv_w[3], conv_b, b_a, b_x, lam]
    for j, v in enumerate(cols):
        recp[:, :, j] = v.reshape(4, 128).T
    def blockdiag(w):
        w = f(w)[0]
        o = np.zeros((128, 4, 128), np.float32)
        for blk in range(8):
            cch, hh = divmod(blk, 2)
            o[hh * 64:(hh + 1) * 64, cch, hh * 64:(hh + 1) * 64] = w[blk]
        return o
    wa_bd = blockdiag(inputs["w_rg_a"]); wx_bd = blockdiag(inputs["w_rg_x"])
    w_router = f(inputs["w_router"])[0]; b_router = f(inputs["b_router"])[0][None, :]
    w_gu = f(inputs["w_gate_up"])[0][:n_experts]; b_gu = f(inputs["b_gate_up"])[0]
    bgu_t = np.ascontiguousarray(b_gu.reshape(NE, 16, 128).transpose(2, 0, 1))
    w_dn = f(inputs["w_down"])[0][:n_experts]; b_dn = f(inputs["b_down"])[0]
    maps = []
    for core in cores:
        b, half = divmod(core, 2)
        x_all = np.zeros((TALL, D), np.float32)
        if half == 0:
            x_all[TOWN:] = x[b, :TOWN]
        else:
            x_all[:] = x[b]
        c_rep = np.ascontiguousarray(np.broadcast_to(c[b].reshape(8, 128).T[:, :, None], (128, 8, 128)))
        flag = np.full((128, 1), float(half), np.float32)
        maps.append({
            "x_all": x_all, "c_rep": c_rep, "flag": flag, "w_ada": w_ada, "b_ada": b_ada, "g_mix": g_mix,
            "g_ffn": g_ffn, "g_final": g_final, "w_in": w_in, "w_out": w_out, "recp": recp, "wa_bd": wa_bd,
            "wx_bd": wx_bd, "w_router": w_router, "b_router": b_router, "w_gate_up": w_gu, "bgu_t": bgu_t, "b_gu": b_gu,
            "w_down": w_dn, "b_down": b_dn,
        })
    return maps


def kernel(**inputs):
    if "nc" not in _NC_CACHE:
        _NC_CACHE["nc"] = build()
    nc = _NC_CACHE["nc"]
    cores = list(range(8))
    in_maps = make_in_maps(inputs, cores)
    res = run_bass_kernel_spmd(nc, in_maps, core_ids=cores)
    out = np.zeros((4, 4096, D), np.float32)
    for core in cores:
        b, half = divmod(core, 2)
        out[b, half * TOWN:(half + 1) * TOWN] = res.results[core]["out"]
    return out
```

```python
import numpy as np
from contextlib import ExitStack
import concourse.bass as bass
import concourse.mybir as mybir
from concourse.bass_utils import run_bass_kernel_spmd

F32 = mybir.dt.float32
BF16 = mybir.dt.bfloat16
AF = mybir.ActivationFunctionType
ALU = mybir.AluOpType

D = 1024
TOWN = 2048
TALL = 4096
NE = 32
EPS = 1e-6
GLU7 = float(np.float32(7.0) / (np.float32(1.0) + np.exp(np.float32(-1.702 * 7.0))))
SEM_LIMIT = 30000


class Res:
    __slots__ = ("w", "r")

    def __init__(self):
        self.w = None
        self.r = {}


def RL(n):
    return [Res() for _ in range(n)]


class Sched:
    ENGS = ("tensor", "vector", "scalar", "gpsimd", "sync")

    def __init__(self, nc, stack, n_dma_sems=8):
        self.nc = nc
        self.stack = stack
        self.ops = {e: [] for e in self.ENGS}
        self.known = {e: {} for e in self.ENGS}
        self.dom_sem = {}
        self.dom_max = {}
        self.ndom = 0
        self.cur_dom = {}
        self.cur_cnt = {}
        self.guard = None
        self.guard_snap = {}
        self.regs = {}
        for e in self.ENGS:
            self.cur_dom[e] = self._new_dom(e)
            self.cur_cnt[e] = 0
        self.dma_pool = {}
        for q in ("sync", "gpsimd"):
            self.dma_pool[q] = {"doms": [self._new_dom("dma_%s%d" % (q, i)) for i in range(n_dma_sems)],
                                "cnt": [0] * n_dma_sems, "rr": 0}

    def _new_dom(self, name):
        d = self.ndom
        self.ndom += 1
        self.dom_sem[d] = self.stack.enter_context(self.nc.semaphore("s_%s_%d" % (name, d)))
        self.dom_max[d] = 0
        return d

    def _collect(self, eng, reads, writes):
        need = {}
        for R in reads:
            if R.w is not None:
                d, v = R.w
                if need.get(d, 0) < v:
                    need[d] = v
        for R in writes:
            if R.w is not None:
                d, v = R.w
                if need.get(d, 0) < v:
                    need[d] = v
            for d, v in R.r.items():
                if need.get(d, 0) < v:
                    need[d] = v
        kn = self.known[eng]
        waits = []
        for d, v in need.items():
            if eng == "tensor" and d == self.cur_dom["tensor"]:
                continue
            if kn.get(d, 0) < v:
                kn[d] = v
                waits.append((d, v))
        return waits

    def _tick(self, eng):
        self.cur_cnt[eng] += 1
        d = self.cur_dom[eng]
        v = self.cur_cnt[eng]
        self.dom_max[d] = v
        return d, v

    def _mark(self, d, v, reads, writes):
        for R in reads:
            R.r[d] = v
        for R in writes:
            R.w = (d, v)
            R.r = {}

    def op(self, eng, fn, reads=(), writes=()):
        waits = self._collect(eng, reads, writes)
        d, v = self._tick(eng)
        self.ops[eng].append((waits, fn, self.dom_sem[d], 1, self.guard))
        self._mark(d, v, reads, writes)

    def group(self, eng, fns, reads=(), writes=()):
        waits = self._collect(eng, reads, writes)
        d, v = self._tick(eng)
        n = len(fns)
        for i, fn in enumerate(fns):
            self.ops[eng].append((waits if i == 0 else [], fn,
                                  self.dom_sem[d] if i == n - 1 else None, 1, self.guard))
        self._mark(d, v, reads, writes)

    def dma_fn(self, eng, fn, reads=(), writes=()):
        pool = self.dma_pool[eng]
        i = pool["rr"]
        pool["rr"] = (i + 1) % len(pool["doms"])
        d = pool["doms"][i]
        waits = self._collect(eng, reads, writes)
        prev = pool["cnt"][i]
        kn = self.known[eng]
        if prev > 0 and kn.get(d, 0) < prev:
            kn[d] = prev
            waits.append((d, prev))
        pool["cnt"][i] += 16
        v = pool["cnt"][i]
        self.dom_max[d] = v
        self.ops[eng].append((waits, fn, self.dom_sem[d], 16, self.guard))
        self._mark(d, v, reads, writes)

    def dma(self, out, in_, reads=(), writes=(), eng="sync"):
        def fn(e, out=out, in_=in_):
            return e.dma_start(out=out, in_=in_)
        self.dma_fn(eng, fn, reads, writes)

    def load_count(self, ap, reads):
        for eng in self.ENGS:
            waits = self._collect(eng, reads, ())
            sched = self

            def fn(e, eng=eng, ap=ap):
                return e.reg_load(sched.regs[eng], ap)
            self.ops[eng].append((waits, fn, None, 0, None))
        for R in reads:
            for eng in self.ENGS:
                pass

    def begin_guard(self, gid, thr):
        self.guard = (gid, thr, None, 0)
        self.guard_snap[gid] = dict(self.dom_max)

    def begin_inner(self, gid, thr):
        g = self.guard
        self.guard = (g[0], g[1], gid, thr)
        self.guard_snap[gid] = dict(self.dom_max)

    def end_inner(self):
        g = self.guard
        self.guard = (g[0], g[1], None, 0)

    def end_guard(self):
        self.guard = None

    def barrier(self):
        assert self.guard is None
        for eng in self.ENGS:
            kn = self.known[eng]
            waits = []
            for d, v in self.dom_max.items():
                if v > 0 and kn.get(d, 0) < v:
                    kn[d] = v
                    waits.append((d, v))
            if waits:
                self.ops[eng].append((waits, None, None, 0, None))

    def emit(self, block):
        sched = self

        def emit_one(e, item):
            waits, fn, sem, inc, _ = item
            for (d_, v) in waits:
                e.wait_ge(sched.dom_sem[d_], v)
            if fn is None:
                return
            ins = fn(e)
            if sem is not None:
                ins.then_inc(sem, inc)

        def skip_path(e, grp, snap):
            incs = []
            need = {}
            for waits, fn, sem, inc, _ in grp:
                for (d_, v) in waits:
                    v = min(v, snap.get(d_, 0))
                    if v > need.get(d_, 0):
                        need[d_] = v
            for d_, v in need.items():
                e.wait_ge(sched.dom_sem[d_], v)
            for waits, fn, sem, inc, _ in grp:
                if sem is not None:
                    for k_ in range(len(incs)):
                        if incs[k_][0] is sem:
                            incs[k_][1] += inc
                            break
                    else:
                        incs.append([sem, inc])
            e.drain()
            for sem, tot in incs:
                e.sem_inc(sem, tot)

        def make(engname):
            def body(e):
                ops = sched.ops[engname]
                sched.regs[engname] = e.alloc_register("cnt_" + engname)
                reg = sched.regs[engname]
                i = 0
                n = len(ops)
                while i < n:
                    g = ops[i][4]
                    if g is None:
                        emit_one(e, ops[i])
                        i += 1
                        continue
                    j = i
                    while j < n and ops[j][4] is not None and ops[j][4][0] == g[0]:
                        j += 1
                    grp = ops[i:j]
                    with e.If_lt(reg, g[1]):
                        skip_path(e, grp, sched.guard_snap[g[0]])
                    with e.Else():
                        a = 0
                        m = len(grp)
                        while a < m:
                            gi = grp[a][4]
                            if gi[2] is None:
                                emit_one(e, grp[a])
                                a += 1
                                continue
                            b = a
                            while b < m and grp[b][4][2] == gi[2]:
                                b += 1
                            sub = grp[a:b]
                            with e.If_lt(reg, gi[3]):
                                skip_path(e, sub, sched.guard_snap[gi[2]])
                            with e.Else():
                                for it in sub:
                                    emit_one(e, it)
                            a = b
                    i = j
            return body
        for engname in self.ENGS:
            if self.ops[engname]:
                getattr(block, engname)(make(engname))


def mk(method, **kw):
    return lambda e: getattr(e, method)(**kw)


def build(debug=False, n_experts=NE, stage=3):
    nc = bass.Bass("TRN2", target_bir_lowering=False)

    def din(name, shape):
        return nc.dram_tensor(name, shape, F32, kind="ExternalInput").ap()

    x_all = din("x_all", [TALL, D])
    c_rep = din("c_rep", [128, 8, 128])
    flag_d = din("flag", [128, 1])
    w_ada = din("w_ada", [D, 6 * D])
    b_ada = din("b_ada", [1, 6 * D])
    g_mix = din("g_mix", [1, D])
    g_ffn = din("g_ffn", [1, D])
    g_final = din("g_final", [1, D])
    w_in = din("w_in", [D, 2560])
    w_out = din("w_out", [D, D])
    recp = din("recp", [128, 4, 8])
    wa_bd = din("wa_bd", [128, 4, 128])
    wx_bd = din("wx_bd", [128, 4, 128])
    w_router = din("w_router", [D, NE])
    b_router = din("b_router", [1, NE])
    w_gu = din("w_gate_up", [n_experts, D, 2 * D])
    bgu_t = din("bgu_t", [128, NE, 16])
    b_gu_d = din("b_gu", [NE, 2 * D])
    w_dn = din("w_down", [n_experts, D, D])
    b_dn = din("b_down", [NE, D])
    out_d = nc.dram_tensor("out", [TOWN, D], F32, kind="ExternalOutput").ap()
    dbg = {}
    if debug:
        def dout(name, shape):
            dbg[name] = nc.dram_tensor(name, shape, F32, kind="ExternalOutput").ap()
        dout("d_mod", [128, 6 * D])
        dout("d_hT", [128, 8 * 512])
        dout("d_mixT", [128, 8 * TOWN])
        dout("d_x1", [TOWN, D])
        dout("d_gates", [128, 16 * NE])

    with ExitStack() as st:
        S = Sched(nc, st)

        def sbt(stack, name, shape, dt):
            return stack.enter_context(nc.sbuf_tensor(name, shape, dt))

        arena = sbt(st, "arena", [128, 16384], F32)
        hT = arena[:].bitcast(BF16).rearrange("p (k t) -> p k t", k=8)
        x1 = arena[:].rearrange("p (t f) -> p t f", t=16)
        r_hT = RL(32)
        r_x1 = RL(16)
        bufA = sbt(st, "bufA", [128, 8, TOWN], BF16)
        r_bufA = [RL(16) for _ in range(8)]
        mv3 = sbt(st, "mv3", [128, D], F32); r_mv3 = Res()
        mv4 = sbt(st, "mv4", [128, D], F32); r_mv4 = Res()
        mv5 = sbt(st, "mv5", [128, D], F32); r_mv5 = Res()
        xb = [sbt(st, "xb%d" % i, [128, D], F32) for i in range(2)]; r_xb = RL(2)
        tmpf = sbt(st, "tmpf", [128, D], F32); r_tmpf = Res()
        hb = [sbt(st, "hb%d" % i, [128, D], BF16) for i in range(2)]; r_hb = RL(2)
        junk = sbt(st, "junk", [128, D], BF16)
        ss = sbt(st, "ss", [128, 64], F32); r_ss = Res()
        ms = sbt(st, "ms", [128, 64], F32); r_ms = Res()
        rstd = sbt(st, "rstd", [128, 64], F32); r_rstd = Res()
        ident = sbt(st, "ident", [128, 128], BF16); r_ident = Res()
        identf = sbt(st, "identf", [128, 128], F32); r_identf = Res()
        neghalf = sbt(st, "neghalf", [128, 1], F32); r_nh = Res()
        flag = sbt(st, "flag_sb", [128, 1], F32); r_flag = Res()
        flagb = sbt(st, "flagb", [128, 1], F32); r_flagb = Res()
        negmask = sbt(st, "negmask", [128, 512], BF16); r_negmask = Res()
        maskB = sbt(st, "maskB", [128, 512], BF16); r_maskB = Res()
        ones_a = sbt(st, "ones_a", [128, 128], BF16); r_ones = Res()
        ones_b = sbt(st, "ones_b", [128, 128], BF16)

        banks = [st.enter_context(nc.psum_tensor("bank%d" % i, [128, 512], F32)) for i in range(8)]
        r_bank = RL(8)
        bank_rr = [0]

        def pb():
            i = bank_rr[0]
            bank_rr[0] = (i + 1) % 8
            return banks[i], r_bank[i]

        cp_rr = [0]
        evac_dve_only = [False]

        def evac(out, in_, reads, writes):
            cp_rr[0] ^= 1
            if cp_rr[0] and not evac_dve_only[0]:
                S.op("scalar", mk("activation", out=out, in_=in_, func=AF.Copy), reads=reads, writes=writes)
            else:
                S.op("vector", mk("tensor_copy", out=out, in_=in_), reads=reads, writes=writes)

        S.dma(flag[:], flag_d, writes=[r_flag])
        S.op("gpsimd", mk("memset", ap=neghalf[:], constant=-0.5), writes=[r_nh])
        S.op("gpsimd", mk("memset", ap=ident[:], constant=1.0), writes=[r_ident])
        S.op("gpsimd", mk("affine_select", out=ident[:], in_=ident[:], pattern=[[-1, 128]],
                          compare_op=ALU.is_equal, fill=0.0, base=0, channel_multiplier=1),
             reads=[r_ident], writes=[r_ident])
        S.op("gpsimd", mk("memset", ap=identf[:], constant=1.0), writes=[r_identf])
        S.op("gpsimd", mk("affine_select", out=identf[:], in_=identf[:], pattern=[[-1, 128]],
                          compare_op=ALU.is_equal, fill=0.0, base=0, channel_multiplier=1),
             reads=[r_identf], writes=[r_identf])
        S.op("gpsimd", mk("memset", ap=negmask[:], constant=0.0), writes=[r_negmask])
        for blk in range(4):
            sl = negmask[:, blk * 128:(blk + 1) * 128]
            if blk % 2 == 0:
                S.op("gpsimd", mk("affine_select", out=sl, in_=sl, pattern=[[-1, 128]], compare_op=ALU.is_ge,
                                  fill=-30000.0, base=0, channel_multiplier=1), reads=[r_negmask], writes=[r_negmask])
            else:
                S.op("gpsimd", mk("affine_select", out=sl, in_=sl, pattern=[[1, 128]], compare_op=ALU.is_ge,
                                  fill=-30000.0, base=0, channel_multiplier=-1), reads=[r_negmask], writes=[r_negmask])
        S.op("vector", mk("tensor_scalar", out=flagb[:], in0=flag[:], scalar1=-1.0, scalar2=30000.0,
                          op0=ALU.add, op1=ALU.mult), reads=[r_flag], writes=[r_flagb])
        S.op("vector", mk("tensor_copy", out=maskB[:], in_=negmask[:]), reads=[r_negmask], writes=[r_maskB])
        for blk in (0, 2):
            sl = maskB[:, blk * 128:(blk + 1) * 128]
            S.op("vector", mk("tensor_scalar", out=sl, in0=sl, scalar1=flagb[:, 0:1], scalar2=None, op0=ALU.add),
                 reads=[r_maskB, r_flagb], writes=[r_maskB])
        S.op("gpsimd", mk("memset", ap=ones_a[:], constant=0.0), writes=[r_ones])
        S.op("gpsimd", mk("memset", ap=ones_a[:, 0:64], constant=1.0), writes=[r_ones])
        S.op("gpsimd", mk("memset", ap=ones_b[:], constant=0.0), writes=[r_ones])
        S.op("gpsimd", mk("memset", ap=ones_b[:, 64:128], constant=1.0), writes=[r_ones])

        def wview(w2d, c0, n):
            return w2d[:, c0:c0 + n].rearrange("(kc p) n -> p kc n", p=128)

        def rms_tile(src, r_src, col, A_vec, r_A, B_vec, r_B, hbt, r_hbt):
            S.op("scalar", mk("activation", out=junk[:], in_=src, func=AF.Square, accum_out=ss[:, col:col + 1]),
                 reads=[r_src], writes=[r_ss])
            S.op("vector", mk("tensor_scalar", out=ms[:, col:col + 1], in0=ss[:, col:col + 1], scalar1=1.0 / D,
                              scalar2=EPS, op0=ALU.mult, op1=ALU.add), reads=[r_ss], writes=[r_ms])
            S.op("gpsimd", mk("tensor_tensor", out=rstd[:, col:col + 1], in0=ms[:, col:col + 1], in1=neghalf[:],
                              op=ALU.pow), reads=[r_ms, r_nh], writes=[r_rstd])
            if B_vec is None:
                S.op("vector", mk("scalar_tensor_tensor", out=hbt, in0=src, scalar=rstd[:, col:col + 1], in1=A_vec,
                                  op0=ALU.mult, op1=ALU.mult), reads=[r_src, r_rstd, r_A], writes=[r_hbt])
                return
            S.op("vector", mk("scalar_tensor_tensor", out=tmpf[:], in0=src, scalar=rstd[:, col:col + 1], in1=A_vec,
                              op0=ALU.mult, op1=ALU.mult), reads=[r_src, r_rstd, r_A], writes=[r_tmpf])
            S.op("gpsimd", mk("tensor_tensor", out=hbt, in0=tmpf[:], in1=B_vec, op=ALU.add),
                 reads=[r_tmpf, r_B], writes=[r_hbt])

        def transpose_tile(hbt, r_hbt, dstT, r_dst, k0=0, k1=8):
            bk, r_bk = pb()
            pv = bk[:].bitcast(BF16).rearrange("p (k t) -> p k t", k=8)
            S.group("tensor", [mk("transpose", out=pv[:, kc, :], in_=hbt[:, kc * 128:(kc + 1) * 128], identity=ident[:])
                               for kc in range(k0, k1)], reads=[r_hbt, r_ident], writes=[r_bk])
            evac(dstT, pv[:, k0:k1, :], [r_bk], r_dst)

        with ExitStack() as st_mix:
            mv2 = sbt(st_mix, "mv2", [128, D], F32); r_mv2 = Res()
            with ExitStack() as st_a:
                mv0 = sbt(st_a, "mv0", [128, D], F32); r_mv0 = Res()
                mv1 = sbt(st_a, "mv1", [128, D], F32); r_mv1 = Res()
                mvs = [mv0, mv1, mv2, mv3, mv4, mv5]
                r_mvs = [r_mv0, r_mv1, r_mv2, r_mv3, r_mv4, r_mv5]
                with ExitStack() as st0:
                    bada = sbt(st0, "bada", [128, 6 * D], F32); r_bada = Res()
                    gmix_bc = sbt(st0, "gmix_bc", [128, D], F32); r_gmix = Res()
                    gffn_bc = sbt(st0, "gffn_bc", [128, D], F32); r_gffn = Res()
                    wab = [sbt(st0, "wab%d" % i, [128, 8, 512], BF16) for i in range(2)]; r_wab = RL(2)
                    c_bf = sbt(st0, "c_bf", [128, 8, 128], BF16); r_cbf = Res()
                    S.dma(c_bf[:], c_rep, writes=[r_cbf], eng="gpsimd")
                    S.dma(bada[:], b_ada.partition_broadcast(128), writes=[r_bada])
                    S.dma(gmix_bc[:], g_mix.partition_broadcast(128), writes=[r_gmix])
                    S.dma(gffn_bc[:], g_ffn.partition_broadcast(128), writes=[r_gffn])
                    for cc in range(12):
                        wb_, r_wb_ = wab[cc % 2], r_wab[cc % 2]
                        S.dma(wb_[:], wview(w_ada, cc * 512, 512), writes=[r_wb_], eng="gpsimd")
                        bk, r_bk = pb()
                        S.group("tensor", [mk("matmul", out=bk[:], lhsT=c_bf[:, kc, :], rhs=wb_[:, kc, :],
                                              start=(kc == 0), stop=(kc == 7)) for kc in range(8)],
                                reads=[r_cbf, r_wb_], writes=[r_bk])
                        dst = mvs[cc // 2][:, (cc % 2) * 512:(cc % 2 + 1) * 512]
                        S.op("vector", mk("tensor_tensor", out=dst, in0=bk[:], in1=bada[:, cc * 512:(cc + 1) * 512],
                                          op=ALU.add), reads=[r_bk, r_bada], writes=[r_mvs[cc // 2]])
                    if debug:
                        for i in range(6):
                            S.dma(dbg["d_mod"][:, i * D:(i + 1) * D], mvs[i][:], reads=[r_mvs[i]])
                    S.op("vector", mk("scalar_tensor_tensor", out=mv1[:], in0=mv1[:], scalar=1.0, in1=gmix_bc[:],
                                      op0=ALU.add, op1=ALU.mult), reads=[r_gmix], writes=[r_mv1])
                    S.op("vector", mk("scalar_tensor_tensor", out=mv4[:], in0=mv4[:], scalar=1.0, in1=gffn_bc[:],
                                      op0=ALU.add, op1=ALU.mult), reads=[r_gffn], writes=[r_mv4])
                    S.barrier()
                for t in range(32):
                    xs, r_xs = xb[t % 2], r_xb[t % 2]
                    S.dma(xs[:], x_all[t * 128:(t + 1) * 128, :], writes=[r_xs])
                    rms_tile(xs[:], r_xs, t, mv1[:], r_mv1, mv0[:], r_mv0, hb[t % 2][:], r_hb[t % 2])
                    transpose_tile(hb[t % 2], r_hb[t % 2], hT[:, :, t * 128:(t + 1) * 128], [r_hT[t]])
                if debug:
                    S.barrier()
                    with ExitStack() as st_d:
                        dtmp = sbt(st_d, "dtmp", [128, 8 * 512], F32); r_dtmp = Res()
                        S.op("vector", mk("tensor_copy", out=dtmp[:].rearrange("p (k t) -> p k t", k=8),
                                          in_=hT[:, :, 1792:2304]), reads=r_hT, writes=[r_dtmp])
                        S.dma(dbg["d_hT"], dtmp[:], reads=[r_dtmp])
                        S.barrier()
                S.barrier()
            mixT = bufA
            with ExitStack() as st_r:
                NB = 1024
                xr = sbt(st_r, "xr", [128, NB + 3], F32); r_xr = Res()
                xc = sbt(st_r, "xc", [128, NB], F32); r_xc = Res()
                xcb = sbt(st_r, "xcb", [128, NB], BF16); r_xcb = Res()
                rg = sbt(st_r, "rg", [128, NB], F32); r_rg = Res()
                ig = sbt(st_r, "ig", [128, NB], F32); r_ig = Res()
                ag = sbt(st_r, "ag", [128, NB], F32); r_ag = Res()
                t1 = sbt(st_r, "t1", [128, NB], F32); r_t1 = Res()
                hs = sbt(st_r, "hs", [128, NB], F32); r_hs = Res()
                gg = sbt(st_r, "gg", [128, NB], F32); r_gg = Res()
                wxr = [sbt(st_r, "wxr%d" % i, [128, 8, 128], BF16) for i in range(2)]; r_wxr = RL(2)
                wgr = [sbt(st_r, "wgr%d" % i, [128, 8, 128], BF16) for i in range(2)]; r_wgr = RL(2)
                wbd = [sbt(st_r, "wbd%d" % i, [128, 4, 128], BF16) for i in range(2)]; r_wbd = RL(2)
                rp = sbt(st_r, "rp", [128, 4, 8], F32); r_rp = Res()
                clam = sbt(st_r, "clam", [128, 4], F32); r_clam = Res()
                state = sbt(st_r, "state", [128, 4], F32); r_state = Res()
                S.dma(rp[:], recp, writes=[r_rp])
                S.dma(wbd[0][:], wa_bd, writes=[r_wbd[0]], eng="gpsimd")
                S.dma(wbd[1][:], wx_bd, writes=[r_wbd[1]], eng="gpsimd")
                S.op("scalar", mk("activation", out=clam[:], in_=rp[:, :, 7], func=AF.Exp, scale=-1.0),
                     reads=[r_rp], writes=[r_clam])
                S.op("scalar", mk("activation", out=clam[:], in_=clam[:], func=AF.Ln, bias=1.0, scale=1.0),
                     reads=[r_clam], writes=[r_clam])
                S.op("vector", mk("tensor_scalar", out=clam[:], in0=clam[:], scalar1=-8.0, scalar2=None, op0=ALU.mult),
                     reads=[r_clam], writes=[r_clam])
                S.op("vector", mk("memset", ap=state[:], constant=0.0), writes=[r_state])
                for cch in range(4):
                    sl = cch % 2
                    S.dma(wxr[sl][:], wview(w_in, 1536 + cch * 128, 128), writes=[r_wxr[sl]], eng="gpsimd")
                    S.dma(wgr[sl][:], wview(w_in, 2048 + cch * 128, 128), writes=[r_wgr[sl]], eng="gpsimd")
                    S.op("vector", mk("memset", ap=xr[:, 0:3], constant=0.0), writes=[r_xr])
                    for seg in range(4):
                        t0 = seg * NB
                        rh = r_hT[seg * 8:(seg + 1) * 8]
                        if seg == 2:
                            S.op("vector", mk("tensor_scalar", out=xr[:, 0:3], in0=xr[:, 0:3], scalar1=flag[:, 0:1],
                                              scalar2=None, op0=ALU.mult), reads=[r_flag], writes=[r_xr])
                            S.op("vector", mk("tensor_scalar", out=state[:, cch:cch + 1], in0=state[:, cch:cch + 1],
                                              scalar1=flag[:, 0:1], scalar2=None, op0=ALU.mult),
                                 reads=[r_flag], writes=[r_state])
                        for h2 in range(2):
                            bk, r_bk = pb()
                            S.group("tensor", [mk("matmul", out=bk[:], lhsT=wxr[sl][:, kc, :],
                                                  rhs=hT[:, kc, t0 + h2 * 512:t0 + (h2 + 1) * 512],
                                                  start=(kc == 0), stop=(kc == 7)) for kc in range(8)],
                                    reads=[r_wxr[sl]] + rh, writes=[r_bk])
                            S.op("scalar", mk("activation", out=xr[:, 3 + h2 * 512:3 + (h2 + 1) * 512], in_=bk[:],
                                              func=AF.Copy), reads=[r_bk], writes=[r_xr])
                        S.op("vector", mk("tensor_scalar", out=xc[:], in0=xr[:, 3:NB + 3], scalar1=rp[:, cch, 3:4],
                                          scalar2=rp[:, cch, 4:5], op0=ALU.mult, op1=ALU.add),
                             reads=[r_xr, r_rp], writes=[r_xc])
                        for j in range(3):
                            S.op("vector", mk("scalar_tensor_tensor", out=xc[:], in0=xr[:, j:j + NB],
                                              scalar=rp[:, cch, j:j + 1], in1=xc[:], op0=ALU.mult, op1=ALU.add),
                                 reads=[r_xr, r_rp], writes=[r_xc])
                        S.op("vector", mk("tensor_copy", out=xr[:, 0:3], in_=xr[:, NB:NB + 3]), writes=[r_xr])
                        S.op("scalar", mk("activation", out=xcb[:], in_=xc[:], func=AF.Copy), reads=[r_xc], writes=[r_xcb])
                        for which, (dst, r_dst, bcol) in enumerate(((rg, r_rg, 5), (ig, r_ig, 6))):
                            for h2 in range(2):
                                bk, r_bk = pb()
                                S.group("tensor", [mk("matmul", out=bk[:], lhsT=wbd[which][:, cch, :],
                                                      rhs=xcb[:, h2 * 512:(h2 + 1) * 512], start=True, stop=True)],
                                        reads=[r_wbd[which], r_xcb], writes=[r_bk])
                                S.op("scalar", mk("activation", out=dst[:, h2 * 512:(h2 + 1) * 512], in_=bk[:],
                                                  func=AF.Sigmoid, bias=rp[:, cch, bcol:bcol + 1], scale=1.0),
                                     reads=[r_bk, r_rp], writes=[r_dst])
                        S.op("scalar", mk("activation", out=ag[:], in_=rg[:], func=AF.Exp, scale=clam[:, cch:cch + 1]),
                             reads=[r_rg, r_clam], writes=[r_ag])
                        S.op("vector", mk("tensor_tensor", out=t1[:], in0=ag[:], in1=ag[:], op=ALU.mult),
                             reads=[r_ag], writes=[r_t1])
                        S.op("vector", mk("tensor_scalar", out=t1[:], in0=t1[:], scalar1=-1.0, scalar2=1.0,
                                          op0=ALU.mult, op1=ALU.add), reads=[r_t1], writes=[r_t1])
                        S.op("vector", mk("tensor_scalar", out=t1[:], in0=t1[:], scalar1=1e-30, scalar2=None,
                                          op0=ALU.max), reads=[r_t1], writes=[r_t1])
                        S.op("scalar", mk("activation", out=t1[:], in_=t1[:], func=AF.Sqrt), reads=[r_t1], writes=[r_t1])
                        S.op("gpsimd", mk("tensor_tensor", out=ig[:], in0=ig[:], in1=xc[:], op=ALU.mult),
                             reads=[r_xc], writes=[r_ig])
                        S.op("gpsimd", mk("tensor_tensor", out=ig[:], in0=ig[:], in1=t1[:], op=ALU.mult),
                             reads=[r_t1], writes=[r_ig])
                        S.op("vector", mk("tensor_tensor_scan", out=hs[:], data0=ag[:], data1=ig[:],
                                          initial=state[:, cch:cch + 1], op0=ALU.mult, op1=ALU.add),
                             reads=[r_ag, r_ig, r_state], writes=[r_hs])
                        S.op("vector", mk("tensor_copy", out=state[:, cch:cch + 1], in_=hs[:, NB - 1:NB]),
                             reads=[r_hs], writes=[r_state])
                        if seg >= 2:
                            o0 = (seg - 2) * NB
                            for h2 in range(2):
                                bk, r_bk = pb()
                                S.group("tensor", [mk("matmul", out=bk[:], lhsT=wgr[sl][:, kc, :],
                                                      rhs=hT[:, kc, t0 + h2 * 512:t0 + (h2 + 1) * 512],
                                                      start=(kc == 0), stop=(kc == 7)) for kc in range(8)],
                                        reads=[r_wgr[sl]] + rh, writes=[r_bk])
                                S.op("scalar", mk("activation", out=gg[:, h2 * 512:(h2 + 1) * 512], in_=bk[:],
                                                  func=AF.Gelu_apprx_tanh), reads=[r_bk], writes=[r_gg])
                            S.op("gpsimd", mk("tensor_tensor", out=mixT[:, 4 + cch, o0:o0 + NB], in0=hs[:], in1=gg[:],
                                              op=ALU.mult), reads=[r_hs, r_gg],
                                 writes=r_bufA[4 + cch][(seg - 2) * 8:(seg - 1) * 8])
                S.barrier()
            with ExitStack() as st_at:
                wqkv = sbt(st_at, "wqkv", [128, 3, 8, 128], BF16); r_wqkv = Res()
                qTa = sbt(st_at, "qTa", [128, TOWN], BF16); r_qTa = Res()
                qTb = sbt(st_at, "qTb", [128, TOWN], BF16); r_qTb = Res()
                kT = sbt(st_at, "kT", [128, TALL], BF16); r_kT = Res()
                Vta = sbt(st_at, "Vta", [128, 32, 128], BF16); r_Vt = RL(8)
                Vtb = sbt(st_at, "Vtb", [128, 32, 128], BF16)
                accN = sbt(st_at, "accN", [128, TOWN], F32); r_accN = Res()
                accD = sbt(st_at, "accD", [128, TOWN], F32); r_accD = Res()
                PT = [sbt(st_at, "PT%d" % i, [128, 512], BF16) for i in range(2)]; r_PT = RL(2)
                pt_rr = 0
                S.op("gpsimd", mk("memset", ap=Vta[:], constant=0.0), writes=r_Vt)
                S.op("gpsimd", mk("memset", ap=Vtb[:], constant=0.0), writes=r_Vt)
                S.op("gpsimd", mk("memset", ap=qTa[:], constant=0.0), writes=[r_qTa])
                S.op("gpsimd", mk("memset", ap=qTb[:], constant=0.0), writes=[r_qTb])
                for fc in range(4):
                    for i3 in range(3):
                        S.dma(wqkv[:, i3, :, :], wview(w_in, i3 * 512 + fc * 128, 128), writes=[r_wqkv], eng="gpsimd")
                    for tb in range(4):
                        bk, r_bk = pb()
                        c0 = TOWN + tb * 512
                        S.group("tensor", [mk("matmul", out=bk[:], lhsT=wqkv[:, 0, kc, :], rhs=hT[:, kc, c0:c0 + 512],
                                              start=(kc == 0), stop=(kc == 7)) for kc in range(8)],
                                reads=[r_wqkv] + r_hT[16 + tb * 4:16 + (tb + 1) * 4], writes=[r_bk])
                        S.op("scalar", mk("activation", out=qTa[0:64, tb * 512:(tb + 1) * 512], in_=bk[0:64, :],
                                          func=AF.Copy), reads=[r_bk], writes=[r_qTa])
                        S.op("vector", mk("tensor_copy", out=qTb[64:128, tb * 512:(tb + 1) * 512], in_=bk[64:128, :]),
                             reads=[r_bk], writes=[r_qTb])
                    for tb in range(8):
                        bk, r_bk = pb()
                        c0 = tb * 512
                        S.group("tensor", [mk("matmul", out=bk[:], lhsT=wqkv[:, 1, kc, :], rhs=hT[:, kc, c0:c0 + 512],
                                              start=(kc == 0), stop=(kc == 7)) for kc in range(8)],
                                reads=[r_wqkv] + r_hT[tb * 4:(tb + 1) * 4], writes=[r_bk])
                        evac(kT[:, c0:c0 + 512], bk[:], [r_bk], [r_kT])
                    for pat, d in enumerate((1, 4, 16)):
                        L = TALL // d
                        nj = L // 128
                        for g4 in range(8):
                            bk, r_bk = pb()
                            fns = []
                            for u in range(4):
                                ti = g4 * 4 + u
                                r_, j_ = divmod(ti, nj)
                                s0 = r_ + d * 128 * j_
                                for kc in range(8):
                                    fns.append(mk("matmul", out=bk[:, u * 128:(u + 1) * 128],
                                                  lhsT=hT[:, kc, s0:s0 + d * 127 + 1:d], rhs=wqkv[:, 2, kc, :],
                                                  start=(kc == 0), stop=(kc == 7)))
                            S.group("tensor", fns, reads=[r_wqkv] + r_hT, writes=[r_bk])
                            bv = bk[:].rearrange("p (u c) -> p u c", u=4)
                            S.op("scalar", mk("activation", out=Vta[:, g4 * 4:(g4 + 1) * 4, 0:64], in_=bv[:, :, 0:64],
                                              func=AF.Copy), reads=[r_bk], writes=[r_Vt[g4]])
                            S.op("vector", mk("tensor_copy", out=Vtb[:, g4 * 4:(g4 + 1) * 4, 64:128], in_=bv[:, :, 64:128]),
                                 reads=[r_bk], writes=[r_Vt[g4]])
                        for su in range(4):
                            bkN, r_bkN = pb()
                            bkD, r_bkD = pb()
                            for u in range(4):
                                if d == 1:
                                    r_, jq = 0, 16 + 4 * su + u
                                elif d == 4:
                                    r_, jq = u, 4 + su
                                else:
                                    r_, jq = 4 * su + u, 1
                                q0 = r_ + d * 128 * jq - TOWN
                                kp0 = r_ + d * 128 * (jq - 1)
                                kc0 = r_ + d * 128 * jq
                                tip = r_ * nj + jq - 1
                                tic = r_ * nj + jq
                                boundary = (jq == nj // 2)
                                span = d * 127 + 1
                                bkS, r_bkS = pb()
                                msk = maskB if boundary else negmask
                                fns = [mk("matmul", out=bkS[:], lhsT=ident[:], rhs=msk[:], start=True, stop=False)]
                                for bi, (qq, k0) in enumerate(((qTa, kp0), (qTa, kc0), (qTb, kp0), (qTb, kc0))):
                                    fns.append(mk("matmul", out=bkS[:, bi * 128:(bi + 1) * 128],
                                                  lhsT=kT[:, k0:k0 + span:d], rhs=qq[:, q0:q0 + span:d],
                                                  start=False, stop=(bi == 3)))
                                S.group("tensor", fns, reads=[r_ident, r_negmask, r_maskB, r_kT, r_qTa, r_qTb],
                                        writes=[r_bkS])
                                pt, r_pt = PT[pt_rr], r_PT[pt_rr]
                                pt_rr ^= 1
                                S.op("scalar", mk("activation", out=pt[:], in_=bkS[:], func=AF.Exp, scale=0.125),
                                     reads=[r_bkS], writes=[r_pt])
                                oc = slice(u * 128, (u + 1) * 128)
                                fnsN = [
                                    mk("matmul", out=bkN[:, oc], lhsT=Vta[:, tip, :], rhs=pt[:, 0:128], start=True, stop=False),
                                    mk("matmul", out=bkN[:, oc], lhsT=Vta[:, tic, :], rhs=pt[:, 128:256], start=False, stop=False),
                                    mk("matmul", out=bkN[:, oc], lhsT=Vtb[:, tip, :], rhs=pt[:, 256:384], start=False, stop=False),
                                    mk("matmul", out=bkN[:, oc], lhsT=Vtb[:, tic, :], rhs=pt[:, 384:512], start=False, stop=True),
                                ]
                                S.group("tensor", fnsN, reads=[r_pt, r_Vt[tip // 4], r_Vt[tic // 4]], writes=[r_bkN])
                                fnsD = [
                                    mk("matmul", out=bkD[:, oc], lhsT=ones_a[:], rhs=pt[:, 0:128], start=True, stop=False),
                                    mk("matmul", out=bkD[:, oc], lhsT=ones_a[:], rhs=pt[:, 128:256], start=False, stop=False),
                                    mk("matmul", out=bkD[:, oc], lhsT=ones_b[:], rhs=pt[:, 256:384], start=False, stop=False),
                                    mk("matmul", out=bkD[:, oc], lhsT=ones_b[:], rhs=pt[:, 384:512], start=False, stop=True),
                                ]
                                S.group("tensor", fnsD, reads=[r_pt, r_ones], writes=[r_bkD])
                            for acc, r_acc, bkX, r_bkX, eng in ((accN, r_accN, bkN, r_bkN, "vector"),
                                                               (accD, r_accD, bkD, r_bkD, "gpsimd")):
                                src = bkX[:].rearrange("p (u i) -> p u i", u=4)
                                if d == 1:
                                    dst = acc[:, su * 512:(su + 1) * 512].rearrange("p (u i) -> p u i", u=4)
                                elif d == 4:
                                    dst = acc[:, su * 512:(su + 1) * 512].rearrange("p (i r) -> p r i", r=4)
                                else:
                                    dst = acc[:].rearrange("p (i r) -> p r i", r=16)[:, 4 * su:4 * su + 4, :]
                                if d == 1:
                                    S.op("vector" if eng == "vector" else "scalar",
                                         mk("tensor_copy", out=dst, in_=src) if eng == "vector" else
                                         mk("activation", out=dst, in_=src, func=AF.Copy),
                                         reads=[r_bkX], writes=[r_acc])
                                else:
                                    S.op("vector", mk("tensor_tensor", out=dst, in0=dst, in1=src, op=ALU.add),
                                         reads=[r_bkX], writes=[r_acc])
                    S.op("vector", mk("reciprocal", out=accD[:], in_=accD[:]), reads=[r_accD], writes=[r_accD])
                    S.op("vector", mk("tensor_tensor", out=mixT[:, fc, :], in0=accN[:], in1=accD[:], op=ALU.mult),
                         reads=[r_accN, r_accD], writes=r_bufA[fc])
                S.barrier()
            if debug:
                with ExitStack() as st_d:
                    dtmp = sbt(st_d, "dtmp2", [128, 8 * 512], F32); r_dtmp = Res()
                    for q4 in range(4):
                        S.op("vector", mk("tensor_copy", out=dtmp[:].rearrange("p (k t) -> p k t", k=8),
                                          in_=mixT[:, :, q4 * 512:(q4 + 1) * 512]), reads=[], writes=[r_dtmp])
                        S.dma(dbg["d_mixT"].rearrange("p (k t) -> p k t", k=8)[:, :, q4 * 512:(q4 + 1) * 512],
                              dtmp[:].rearrange("p (k t) -> p k t", k=8), reads=[r_dtmp])
                    S.barrier()
            with ExitStack() as st_o:
                wout = sbt(st_o, "wout", [128, 8, D], BF16); r_wout = Res()
                S.dma(wout[:, 0:4, :], w_out[0:512, :].rearrange("(kc p) n -> p kc n", p=128), writes=[r_wout], eng="gpsimd")
                S.dma(wout[:, 4:8, :], w_out[512:1024, :].rearrange("(kc p) n -> p kc n", p=128), writes=[r_wout], eng="gpsimd")
                for t in range(16):
                    xs, r_xs = xb[t % 2], r_xb[t % 2]
                    S.dma(xs[:], x_all[TOWN + t * 128:TOWN + (t + 1) * 128, :], writes=[r_xs])
                    for cc in range(2):
                        bk, r_bk = pb()
                        cs = slice(cc * 512, (cc + 1) * 512)
                        S.group("tensor", [mk("matmul", out=bk[:], lhsT=mixT[:, kc, t * 128:(t + 1) * 128],
                                              rhs=wout[:, kc, cs], start=(kc == 0), stop=(kc == 7)) for kc in range(8)],
                                reads=[r_wout] + [r_bufA[kc][t] for kc in range(8)], writes=[r_bk])
                        S.op("vector", mk("tensor_tensor", out=tmpf[:, cs], in0=bk[:], in1=mv2[:, cs], op=ALU.mult),
                             reads=[r_bk, r_mv2], writes=[r_tmpf])
                        S.op("gpsimd", mk("tensor_tensor", out=x1[:, t, cs], in0=tmpf[:, cs], in1=xs[:, cs], op=ALU.add),
                             reads=[r_tmpf, r_xs], writes=[r_x1[t]])
                S.barrier()
        if debug:
            for t in range(16):
                S.dma(dbg["d_x1"][t * 128:(t + 1) * 128, :], x1[:, t, :], reads=[r_x1[t]])
        I32 = mybir.dt.int32
        CAP = TOWN
        x_buf = nc.dram_tensor("x_buf", [NE * CAP, D], BF16, kind="Internal").ap()
        y_buf = nc.dram_tensor("y_buf", [NE * CAP, D], F32, kind="Internal").ap()
        with ExitStack() as st_f:
            wgus = [bufA, sbt(st_f, "wgu1", [128, 8, 2 * D], BF16)]; r_wgus = RL(2)
            wgus[0] = bufA[:].rearrange("p k t -> p (k t)").rearrange("p (k t) -> p k t", k=8)
            wd = sbt(st_f, "wd", [128, 8, D], BF16); r_wd = Res()
            gk = sbt(st_f, "gk", [128, 16, 4], F32); r_gk = RL(16)
            desti = sbt(st_f, "desti", [128, 16, 4], I32); r_desti = RL(16)
            cnti = sbt(st_f, "cnti", [128, NE], I32); r_cnti = Res()

            bgrow = [sbt(st_f, "bgrow%d" % i, [2, 2 * D], BF16) for i in range(2)]; r_bgrow = RL(2)
            ones_r = sbt(st_f, "ones_r", [2, 128], BF16); r_ones_r = Res()
            S.op("gpsimd", mk("memset", ap=ones_r[:], constant=1.0), writes=[r_ones_r])
            for i in range(2):
                S.op("gpsimd", mk("memset", ap=bgrow[i][:, 0:D], constant=0.0), writes=[r_bgrow[i]])
                S.op("gpsimd", mk("memset", ap=bgrow[i][:, D:2 * D], constant=1.0), writes=[r_bgrow[i]])

            def load_wgu(e):
                w_, r_w = wgus[e % 2], r_wgus[e % 2]
                for q4 in range(4):
                    S.dma(w_[:, :, q4 * 512:(q4 + 1) * 512], wview(w_gu[e], q4 * 512, 512), writes=[r_w], eng="gpsimd")
                S.dma(bgrow[e % 2][0:1, :], b_gu_d[e:e + 1, :], writes=[r_bgrow[e % 2]], eng="gpsimd")

            def load_wd(e):
                S.dma(wd[:, 0:4, :], w_dn[e][0:512, :].rearrange("(kc p) n -> p kc n", p=128), writes=[r_wd], eng="gpsimd")
                S.dma(wd[:, 4:8, :], w_dn[e][512:1024, :].rearrange("(kc p) n -> p kc n", p=128), writes=[r_wd], eng="gpsimd")

            load_wgu(0)
            load_wd(0)
            with ExitStack() as st_r2:
                wr = sbt(st_r2, "wr", [128, 8, NE], BF16); r_wr = Res()
                brt = sbt(st_r2, "brt", [128, NE], F32); r_brt = Res()
                lg = sbt(st_r2, "lg", [128, NE], F32); r_lg = Res()
                ex = sbt(st_r2, "ex", [128, NE], F32); r_ex = Res()
                gt = sbt(st_r2, "gt", [128, NE], F32); r_gt = Res()
                posb = sbt(st_r2, "posb", [128, NE], F32); r_posb = Res()
                scr4 = sbt(st_r2, "scr4", [128, 4, NE], F32); r_scr = Res()
                ebase = sbt(st_r2, "ebase", [128, NE], F32); r_ebase = Res()
                m8 = sbt(st_r2, "m8", [128, 8], F32); r_m8 = Res()
                e4 = sbt(st_r2, "e4", [128, 4], F32); r_e4 = Res()
                destf = sbt(st_r2, "destf", [128, 4], F32); r_destf = Res()
                nmx = sbt(st_r2, "nmx", [128, 1], F32); r_nmx = Res()
                sm = sbt(st_r2, "sm", [128, 1], F32); r_sm = Res()
                gT = sbt(st_r2, "gT", [32, 128], F32); r_gT = Res()
                bdn = sbt(st_r2, "bdn", [32, D], F32); r_bdn = Res()
                maskall = sbt(st_r2, "maskall", [128, 16, NE], BF16); r_mask = RL(16)
                ltri = sbt(st_r2, "ltri", [128, 128], BF16); r_ltri = Res()
                ones_f = sbt(st_r2, "ones_full", [128, 128], BF16); r_onesf = Res()
                h2Tt = sbt(st_r2, "h2Tt", [128, 8, 128], BF16); r_h2Tt = Res()
                S.dma(wr[:], w_router.rearrange("(kc p) n -> p kc n", p=128), writes=[r_wr], eng="gpsimd")
                S.dma(brt[:], b_router.partition_broadcast(128), writes=[r_brt])
                S.dma(bdn[:], b_dn, writes=[r_bdn])
                S.op("gpsimd", mk("iota", out=ebase[:], pattern=[[CAP, NE]], base=0, channel_multiplier=0,
                                  allow_small_or_imprecise_dtypes=True), writes=[r_ebase])
                S.op("gpsimd", mk("memset", ap=ones_f[:], constant=1.0), writes=[r_onesf])
                S.op("gpsimd", mk("memset", ap=ltri[:], constant=1.0), writes=[r_ltri])
                S.op("gpsimd", mk("affine_select", out=ltri[:], in_=ltri[:], pattern=[[1, 128]], compare_op=ALU.is_gt,
                                  fill=0.0, base=0, channel_multiplier=-1), reads=[r_ltri], writes=[r_ltri])
                for t in range(16):
                    hbt, r_hbt = hb[t % 2], r_hb[t % 2]
                    rms_tile(x1[:, t, :], r_x1[t], 32 + t, mv4[:], r_mv4, mv3[:], r_mv3, hbt[:], r_hbt)
                    transpose_tile(hbt, r_hbt, h2Tt[:], [r_h2Tt])
                    bk, r_bk = pb()
                    S.group("tensor", [mk("matmul", out=bk[:, 0:NE], lhsT=h2Tt[:, kc, :], rhs=wr[:, kc, :],
                                          start=(kc == 0), stop=(kc == 7)) for kc in range(8)],
                            reads=[r_wr, r_h2Tt], writes=[r_bk])
                    S.op("vector", mk("tensor_tensor", out=lg[:], in0=bk[:, 0:NE], in1=brt[:], op=ALU.add),
                         reads=[r_bk, r_brt], writes=[r_lg])
                    S.op("vector", mk("max", out=m8[:], in_=lg[:]), reads=[r_lg], writes=[r_m8])
                    S.op("vector", mk("tensor_scalar", out=nmx[:], in0=m8[:, 0:1], scalar1=-1.0, scalar2=None, op0=ALU.mult),
                         reads=[r_m8], writes=[r_nmx])
                    S.op("scalar", mk("activation", out=ex[:], in_=lg[:], func=AF.Exp, bias=nmx[:, 0:1], scale=1.0),
                         reads=[r_lg, r_nmx], writes=[r_ex])
                    S.op("scalar", mk("activation", out=e4[:], in_=m8[:, 0:4], func=AF.Exp, bias=nmx[:, 0:1], scale=1.0),
                         reads=[r_m8, r_nmx], writes=[r_e4])
                    S.op("vector", mk("tensor_scalar", out=maskall[:, t, :], in0=lg[:], scalar1=m8[:, 3:4], scalar2=None,
                                      op0=ALU.is_ge), reads=[r_lg, r_m8], writes=[r_mask[t]])
                    S.op("vector", mk("tensor_tensor", out=ex[:], in0=ex[:], in1=maskall[:, t, :], op=ALU.mult),
                         reads=[r_mask[t]], writes=[r_ex])
                    S.op("vector", mk("tensor_reduce", out=sm[:], in_=ex[:], axis=mybir.AxisListType.X, op=ALU.add),
                         reads=[r_ex], writes=[r_sm])
                    S.op("vector", mk("reciprocal", out=sm[:], in_=sm[:]), reads=[r_sm], writes=[r_sm])
                    S.op("vector", mk("tensor_scalar", out=gt[:], in0=ex[:], scalar1=sm[:, 0:1], scalar2=None,
                                      op0=ALU.mult), reads=[r_ex, r_sm], writes=[r_gt])
                    S.op("vector", mk("tensor_scalar", out=gk[:, t, :], in0=e4[:], scalar1=sm[:, 0:1], scalar2=None,
                                      op0=ALU.mult), reads=[r_e4, r_sm], writes=[r_gk[t]])
                    bkp, r_bkp = pb()
                    fns = [mk("matmul", out=bkp[:, 0:NE], lhsT=ones_f[:], rhs=maskall[:, tp, :], start=(tp == 0), stop=False)
                           for tp in range(t)]
                    fns.append(mk("matmul", out=bkp[:, 0:NE], lhsT=ltri[:], rhs=maskall[:, t, :], start=(t == 0), stop=True))
                    S.group("tensor", fns, reads=[r_onesf, r_ltri] + r_mask[:t + 1], writes=[r_bkp])
                    S.op("vector", mk("tensor_tensor", out=posb[:], in0=bkp[:, 0:NE], in1=ebase[:], op=ALU.add),
                         reads=[r_bkp, r_ebase], writes=[r_posb])
                    for k in range(4):
                        S.op("vector", mk("scalar_tensor_tensor", out=scr4[:, k, :], in0=lg[:], scalar=m8[:, k:k + 1], in1=posb[:],
                                          op0=ALU.is_equal, op1=ALU.mult),
                             reads=[r_lg, r_m8, r_posb], writes=[r_scr])
                    S.op("vector", mk("tensor_reduce", out=destf[:], in_=scr4[:], axis=mybir.AxisListType.X, op=ALU.add),
                         reads=[r_scr], writes=[r_destf])
                    S.op("vector", mk("tensor_scalar", out=destf[:], in0=destf[:], scalar1=0.0, scalar2=float(NE * CAP - 1),
                                      op0=ALU.max, op1=ALU.min), reads=[r_destf], writes=[r_destf])
                    S.op("vector", mk("tensor_copy", out=desti[:, t, :], in_=destf[:]), reads=[r_destf], writes=[r_desti[t]])
                    for k in range(4):
                        def sc(e, t=t, k=k, hbt=hbt):
                            return e.indirect_dma_start(out=x_buf[:, :],
                                                        out_offset=bass.IndirectOffsetOnAxis(ap=desti[:, t, k:k + 1], axis=0),
                                                        in_=hbt[:, :], in_offset=None)
                        S.dma_fn("gpsimd", sc, reads=[r_desti[t], r_hbt], writes=[])
                    bk2, r_bk2 = pb()
                    S.group("tensor", [mk("transpose", out=bk2[0:NE, 0:128], in_=gt[:], identity=identf[:])],
                            reads=[r_gt, r_identf], writes=[r_bk2])
                    S.op("vector", mk("tensor_copy", out=gT[:], in_=bk2[0:NE, 0:128]), reads=[r_bk2], writes=[r_gT])
                    for cc in range(2):
                        cs = slice(cc * 512, (cc + 1) * 512)
                        bk3, r_bk3 = pb()
                        S.group("tensor", [mk("matmul", out=bk3[:], lhsT=gT[:], rhs=bdn[:, cs], start=True, stop=True)],
                                reads=[r_gT, r_bdn], writes=[r_bk3])
                        S.op("vector", mk("tensor_tensor", out=tmpf[:, cs], in0=bk3[:], in1=mv5[:, cs], op=ALU.mult),
                             reads=[r_bk3, r_mv5], writes=[r_tmpf])
                        S.op("gpsimd", mk("tensor_tensor", out=x1[:, t, cs], in0=tmpf[:, cs], in1=x1[:, t, cs], op=ALU.add),
                             reads=[r_tmpf], writes=[r_x1[t]])
                bkc, r_bkc = pb()
                S.group("tensor", [mk("matmul", out=bkc[:, 0:NE], lhsT=ones_f[:], rhs=maskall[:, tp, :], start=(tp == 0),
                                      stop=(tp == 15)) for tp in range(16)], reads=[r_onesf] + r_mask, writes=[r_bkc])
                S.op("vector", mk("tensor_copy", out=cnti[:], in_=bkc[:, 0:NE]), reads=[r_bkc], writes=[r_cnti])
                if debug:
                    S.dma(dbg["d_gates"][:, 0:64], gk[:].rearrange("p t e -> p (t e)"), reads=r_gk)
                S.barrier()
            S.dma(mv3[:], g_final.partition_broadcast(128), writes=[r_mv3])
            with ExitStack() as st_m:
                xblk = [hb[0], hb[1]]; r_xblk = RL(2)
                xT1 = sbt(st_m, "xT1", [128, 8, 128], BF16)
                xTs = [junk[:].rearrange("p (k t) -> p k t", k=8), xT1[:]]; r_xT = RL(2)
                actT = [sbt(st_m, "actT%d" % i, [128, 8, 128], BF16) for i in range(2)]; r_act = [RL(2), RL(2)]
                yblk = [xb[0], xb[1]]; r_yblk = RL(2)
                sgx = sbt(st_m, "sgx", [128, D], F32)
                ucx = sbt(st_m, "ucx", [128, D], F32)
                gc2 = [[tmpf[:, 0:512], tmpf[:, 512:1024]], [sgx[:, 0:512], sgx[:, 512:1024]]]; r_gc2 = [RL(2), RL(2)]
                actk = [sbt(st_m, "actk%d" % i, [128, D], BF16) for i in range(2)]; r_actk = [RL(2), RL(2)]
                ucs = [[mv4[:, 0:512], mv4[:, 512:1024]], [ucx[:, 0:512], ucx[:, 512:1024]]]; r_uc = [RL(2), RL(2)]
                evac_dve_only[0] = True
                blk_rr = 0
                for e in range(n_experts if stage >= 2 else 0):
                    w_, r_w = wgus[e % 2], r_wgus[e % 2]
                    if e + 1 < n_experts:
                        load_wgu(e + 1)
                    S.load_count(cnti[0:1, e:e + 1], [r_cnti])
                    for jp in range(0, 16, 2):
                        S.begin_guard((e, jp), 128 * jp + 1)
                        pair = (jp, jp + 1)

                        NESTED = True

                        def inner(j, ph):
                            if j != jp and NESTED:
                                S.begin_inner((e, j, ph), 128 * j + 1)

                        def inner_end(j):
                            if j != jp and NESTED:
                                S.end_inner()
                        for j in pair:
                            s_ = j % 2
                            row0 = e * CAP + 128 * j
                            inner(j, 1)
                            S.dma(xblk[s_][:], x_buf[row0:row0 + 128, :], writes=[r_xblk[s_]])
                            transpose_tile(xblk[s_], r_xblk[s_], xTs[s_], [r_xT[s_]])
                            inner_end(j)
                        for j in pair:
                            s_ = j % 2
                            xT = xTs[s_]
                            inner(j, 2)
                            for half in range(2):
                                gub = []
                                for c0 in (half * 512, D + half * 512):
                                    bkX, r_bkX = pb()
                                    fns = [mk("matmul", out=bkX[:], lhsT=ones_r[0:2, :], rhs=bgrow[e % 2][0:2, c0:c0 + 512],
                                              start=True, stop=False)]
                                    for kc in range(8):
                                        fns.append(mk("matmul", out=bkX[:], lhsT=xT[:, kc, :], rhs=w_[:, kc, c0:c0 + 512],
                                                      start=False, stop=(kc == 7)))
                                    S.group("tensor", fns, reads=[r_w, r_xT[s_], r_ones_r, r_bgrow[e % 2]], writes=[r_bkX])
                                    gub.append((bkX, r_bkX))
                                (bkG, r_bkG), (bkU, r_bkU) = gub
                                uc, r_uc_ = ucs[s_][half], r_uc[s_][half]
                                gc, r_gc_ = gc2[s_][half], r_gc2[s_][half]
                                S.op("scalar", mk("activation", out=gc, in_=bkG[:], func=AF.Gelu_apprx_sigmoid),
                                     reads=[r_bkG], writes=[r_gc_])
                                S.op("vector", mk("tensor_scalar", out=uc, in0=bkU[:], scalar1=8.0, scalar2=-6.0,
                                                  op0=ALU.min, op1=ALU.max), reads=[r_bkU], writes=[r_uc_])
                                S.op("vector", mk("scalar_tensor_tensor", out=actk[s_][:, half * 512:(half + 1) * 512], in0=gc,
                                                  scalar=GLU7, in1=uc, op0=ALU.min, op1=ALU.mult),
                                     reads=[r_uc_, r_gc_], writes=[r_actk[s_][half]])
                            inner_end(j)
                        for j in pair:
                            s_ = j % 2
                            row0 = e * CAP + 128 * j
                            inner(j, 3)
                            for half in range(2):
                                transpose_tile(actk[s_], r_actk[s_][half], actT[s_][:, 4 * half:4 * half + 4, :],
                                               [r_act[s_][half]], k0=4 * half, k1=4 * half + 4)
                            for cc in range(2):
                                cs = slice(cc * 512, (cc + 1) * 512)
                                bk, r_bk = pb()
                                S.group("tensor", [mk("matmul", out=bk[:], lhsT=actT[s_][:, kc, :], rhs=wd[:, kc, cs],
                                                      start=(kc == 0), stop=(kc == 7)) for kc in range(8)],
                                        reads=[r_wd] + r_act[s_], writes=[r_bk])
                                S.op("vector", mk("tensor_tensor", out=yblk[s_][:, cs], in0=bk[:], in1=mv5[:, cs], op=ALU.mult),
                                     reads=[r_bk, r_mv5], writes=[r_yblk[s_]])
                            S.dma(y_buf[row0:row0 + 128, :], yblk[s_][:], reads=[r_yblk[s_]], eng="gpsimd")
                            inner_end(j)
                        S.end_guard()
                    if e + 1 < n_experts:
                        load_wd(e + 1)
                evac_dve_only[0] = False
                S.barrier()
            ykb = [mv4, tmpf]; r_ykb = RL(2)
            gi = 0
            for t in range(16):
                for k in range(4 if stage >= 3 else 0):
                    yk, r_yk = ykb[gi % 2], r_ykb[gi % 2]
                    gi += 1

                    def ga(e, t=t, k=k, yk=yk):
                        return e.indirect_dma_start(out=yk[:, :], out_offset=None, in_=y_buf[:, :],
                                                    in_offset=bass.IndirectOffsetOnAxis(ap=desti[:, t, k:k + 1], axis=0))
                    S.dma_fn("gpsimd", ga, reads=[r_desti[t]], writes=[r_yk])
                    S.op("vector", mk("scalar_tensor_tensor", out=x1[:, t, :], in0=yk[:], scalar=gk[:, t, k:k + 1],
                                      in1=x1[:, t, :], op0=ALU.mult, op1=ALU.add),
                         reads=[r_yk, r_gk[t]], writes=[r_x1[t]])
                ob = xb[t % 2]; r_ob = r_xb[t % 2]
                rms_tile(x1[:, t, :], r_x1[t], 48 + t, mv3[:], r_mv3, None, None, ob[:], r_ob)
                S.dma(out_d[t * 128:(t + 1) * 128, :], ob[:], reads=[r_ob])
            S.barrier()
        with nc.Block() as block:
            S.emit(block)
    return nc


_NC_CACHE = {}


def make_in_maps(inputs, cores, n_experts=NE):
    f = lambda a: np.ascontiguousarray(np.asarray(a, dtype=np.float32))
    x = f(inputs["x"]); c = f(inputs["c"])
    w_ada = f(inputs["w_ada"])[0]; b_ada = f(inputs["b_ada"])[0][None, :]
    g_mix = f(inputs["g_mix"])[0][None, :]; g_ffn = f(inputs["g_ffn"])[0][None, :]
    g_final = f(inputs["g_final"])[None, :]
    w_in = f(inputs["w_in"])[0]; w_out = f(inputs["w_out"])[0]
    conv_w = f(inputs["conv_w"])[0]; conv_b = f(inputs["conv_b"])[0]
    b_a = f(inputs["b_rg_a"])[0]; b_x = f(inputs["b_rg_x"])[0]; lam = f(inputs["lam"])[0]
    recp = np.zeros((128, 4, 8), np.float32)
    cols = [conv_w[0], conv_w[1], conv_w[2], conv_w[3], conv_b, b_a, b_x, lam]
    for j, v in enumerate(cols):
        recp[:, :, j] = v.reshape(4, 128).T
    def blockdiag(w):
        w = f(w)[0]
        o = np.zeros((128, 4, 128), np.float32)
        for blk in range(8):
            cch, hh = divmod(blk, 2)
            o[hh * 64:(hh + 1) * 64, cch, hh * 64:(hh + 1) * 64] = w[blk]
        return o
    wa_bd = blockdiag(inputs["w_rg_a"]); wx_bd = blockdiag(inputs["w_rg_x"])
    w_router = f(inputs["w_router"])[0]; b_router = f(inputs["b_router"])[0][None, :]
    w_gu = f(inputs["w_gate_up"])[0][:n_experts]; b_gu = f(inputs["b_gate_up"])[0]
    bgu_t = np.ascontiguousarray(b_gu.reshape(NE, 16, 128).transpose(2, 0, 1))
    w_dn = f(inputs["w_down"])[0][:n_experts]; b_dn = f(inputs["b_down"])[0]
    maps = []
    for core in cores:
        b, half = divmod(core, 2)
        x_all = np.zeros((TALL, D), np.float32)
        if half == 0:
            x_all[TOWN:] = x[b, :TOWN]
        else:
            x_all[:] = x[b]
        c_rep = np.ascontiguousarray(np.broadcast_to(c[b].reshape(8, 128).T[:, :, None], (128, 8, 128)))
        flag = np.full((128, 1), float(half), np.float32)
        maps.append({
            "x_all": x_all, "c_rep": c_rep, "flag": flag, "w_ada": w_ada, "b_ada": b_ada, "g_mix": g_mix,
            "g_ffn": g_ffn, "g_final": g_final, "w_in": w_in, "w_out": w_out, "recp": recp, "wa_bd": wa_bd,
            "wx_bd": wx_bd, "w_router": w_router, "b_router": b_router, "w_gate_up": w_gu, "bgu_t": bgu_t, "b_gu": b_gu,
            "w_down": w_dn, "b_down": b_dn,
        })
    return maps


def kernel(**inputs):
    if "nc" not in _NC_CACHE:
        _NC_CACHE["nc"] = build()
    nc = _NC_CACHE["nc"]
    cores = list(range(8))
    in_maps = make_in_maps(inputs, cores)
    res = run_bass_kernel_spmd(nc, in_maps, core_ids=cores)
    out = np.zeros((4, 4096, D), np.float32)
    for core in cores:
        b, half = divmod(core, 2)
        out[b, half * TOWN:(half + 1) * TOWN] = res.results[core]["out"]
    return out
```

```python
import numpy as np
from contextlib import ExitStack
import concourse.bass as bass
import concourse.mybir as mybir
from concourse.bass_utils import run_bass_kernel_spmd

F32 = mybir.dt.float32
BF16 = mybir.dt.bfloat16
AF = mybir.ActivationFunctionType
ALU = mybir.AluOpType

D = 1024
TOWN = 2048
TALL = 4096
NE = 32
EPS = 1e-6
GLU7 = float(np.float32(7.0) / (np.float32(1.0) + np.exp(np.float32(-1.702 * 7.0))))
SEM_LIMIT = 30000


class Res:
    __slots__ = ("w", "r")

    def __init__(self):
        self.w = None
        self.r = {}


def RL(n):
    return [Res() for _ in range(n)]


class Sched:
    ENGS = ("tensor", "vector", "scalar", "gpsimd", "sync")

    def __init__(self, nc, stack, n_dma_sems=8):
        self.nc = nc
        self.stack = stack
        self.ops = {e: [] for e in self.ENGS}
        self.known = {e: {} for e in self.ENGS}
        self.dom_sem = {}
        self.dom_max = {}
        self.ndom = 0
        self.cur_dom = {}
        self.cur_cnt = {}
        self.guard = None
        self.guard_snap = {}
        self.regs = {}
        for e in self.ENGS:
            self.cur_dom[e] = self._new_dom(e)
            self.cur_cnt[e] = 0
        self.dma_pool = {}
        for q in ("sync", "gpsimd"):
            self.dma_pool[q] = {"doms": [self._new_dom("dma_%s%d" % (q, i)) for i in range(n_dma_sems)],
                                "cnt": [0] * n_dma_sems, "rr": 0}

    def _new_dom(self, name):
        d = self.ndom
        self.ndom += 1
        self.dom_sem[d] = self.stack.enter_context(self.nc.semaphore("s_%s_%d" % (name, d)))
        self.dom_max[d] = 0
        return d

    def _collect(self, eng, reads, writes):
        need = {}
        for R in reads:
            if R.w is not None:
                d, v = R.w
                if need.get(d, 0) < v:
                    need[d] = v
        for R in writes:
            if R.w is not None:
                d, v = R.w
                if need.get(d, 0) < v:
                    need[d] = v
            for d, v in R.r.items():
                if need.get(d, 0) < v:
                    need[d] = v
        kn = self.known[eng]
        waits = []
        for d, v in need.items():
            if eng == "tensor" and d == self.cur_dom["tensor"]:
                continue
            if kn.get(d, 0) < v:
                kn[d] = v
                waits.append((d, v))
        return waits

    def _tick(self, eng):
        self.cur_cnt[eng] += 1
        d = self.cur_dom[eng]
        v = self.cur_cnt[eng]
        self.dom_max[d] = v
        return d, v

    def _mark(self, d, v, reads, writes):
        for R in reads:
            R.r[d] = v
        for R in writes:
            R.w = (d, v)
            R.r = {}

    def op(self, eng, fn, reads=(), writes=()):
        waits = self._collect(eng, reads, writes)
        d, v = self._tick(eng)
        self.ops[eng].append((waits, fn, self.dom_sem[d], 1, self.guard))
        self._mark(d, v, reads, writes)

    def group(self, eng, fns, reads=(), writes=()):
        waits = self._collect(eng, reads, writes)
        d, v = self._tick(eng)
        n = len(fns)
        for i, fn in enumerate(fns):
            self.ops[eng].append((waits if i == 0 else [], fn,
                                  self.dom_sem[d] if i == n - 1 else None, 1, self.guard))
        self._mark(d, v, reads, writes)

    def dma_fn(self, eng, fn, reads=(), writes=()):
        pool = self.dma_pool[eng]
        i = pool["rr"]
        pool["rr"] = (i + 1) % len(pool["doms"])
        d = pool["doms"][i]
        waits = self._collect(eng, reads, writes)
        prev = pool["cnt"][i]
        kn = self.known[eng]
        if prev > 0 and kn.get(d, 0) < prev:
            kn[d] = prev
            waits.append((d, prev))
        pool["cnt"][i] += 16
        v = pool["cnt"][i]
        self.dom_max[d] = v
        self.ops[eng].append((waits, fn, self.dom_sem[d], 16, self.guard))
        self._mark(d, v, reads, writes)

    def dma(self, out, in_, reads=(), writes=(), eng="sync"):
        def fn(e, out=out, in_=in_):
            return e.dma_start(out=out, in_=in_)
        self.dma_fn(eng, fn, reads, writes)

    def load_count(self, ap, reads):
        for eng in self.ENGS:
            waits = self._collect(eng, reads, ())
            sched = self

            def fn(e, eng=eng, ap=ap):
                return e.reg_load(sched.regs[eng], ap)
            self.ops[eng].append((waits, fn, None, 0, None))
        for R in reads:
            for eng in self.ENGS:
                pass

    def begin_guard(self, gid, thr):
        self.guard = (gid, thr, None, 0)
        self.guard_snap[gid] = dict(self.dom_max)

    def begin_inner(self, gid, thr):
        g = self.guard
        self.guard = (g[0], g[1], gid, thr)
        self.guard_snap[gid] = dict(self.dom_max)

    def end_inner(self):
        g = self.guard
        self.guard = (g[0], g[1], None, 0)

    def end_guard(self):
        self.guard = None

    def barrier(self):
        assert self.guard is None
        for eng in self.ENGS:
            kn = self.known[eng]
            waits = []
            for d, v in self.dom_max.items():
                if v > 0 and kn.get(d, 0) < v:
                    kn[d] = v
                    waits.append((d, v))
            if waits:
                self.ops[eng].append((waits, None, None, 0, None))

    def emit(self, block):
        sched = self

        def emit_one(e, item):
            waits, fn, sem, inc, _ = item
            for (d_, v) in waits:
                e.wait_ge(sched.dom_sem[d_], v)
            if fn is None:
                return
            ins = fn(e)
            if sem is not None:
                ins.then_inc(sem, inc)

        def skip_path(e, grp, snap):
            incs = []
            need = {}
            for waits, fn, sem, inc, _ in grp:
                for (d_, v) in waits:
                    v = min(v, snap.get(d_, 0))
                    if v > need.get(d_, 0):
                        need[d_] = v
            for d_, v in need.items():
                e.wait_ge(sched.dom_sem[d_], v)
            for waits, fn, sem, inc, _ in grp:
                if sem is not None:
                    for k_ in range(len(incs)):
                        if incs[k_][0] is sem:
                            incs[k_][1] += inc
                            break
                    else:
                        incs.append([sem, inc])
            e.drain()
            for sem, tot in incs:
                e.sem_inc(sem, tot)

        def make(engname):
            def body(e):
                ops = sched.ops[engname]
                sched.regs[engname] = e.alloc_register("cnt_" + engname)
                reg = sched.regs[engname]
                i = 0
                n = len(ops)
                while i < n:
                    g = ops[i][4]
                    if g is None:
                        emit_one(e, ops[i])
                        i += 1
                        continue
                    j = i
                    while j < n and ops[j][4] is not None and ops[j][4][0] == g[0]:
                        j += 1
                    grp = ops[i:j]
                    with e.If_lt(reg, g[1]):
                        skip_path(e, grp, sched.guard_snap[g[0]])
                    with e.Else():
                        a = 0
                        m = len(grp)
                        while a < m:
                            gi = grp[a][4]
                            if gi[2] is None:
                                emit_one(e, grp[a])
                                a += 1
                                continue
                            b = a
                            while b < m and grp[b][4][2] == gi[2]:
                                b += 1
                            sub = grp[a:b]
                            with e.If_lt(reg, gi[3]):
                                skip_path(e, sub, sched.guard_snap[gi[2]])
                            with e.Else():
                                for it in sub:
                                    emit_one(e, it)
                            a = b
                    i = j
            return body
        for engname in self.ENGS:
            if self.ops[engname]:
                getattr(block, engname)(make(engname))


def mk(method, **kw):
    return lambda e: getattr(e, method)(**kw)


def build(debug=False, n_experts=NE, stage=3):
    nc = bass.Bass("TRN2", target_bir_lowering=False)

    def din(name, shape):
        return nc.dram_tensor(name, shape, F32, kind="ExternalInput").ap()

    x_all = din("x_all", [TALL, D])
    c_rep = din("c_rep", [128, 8, 128])
    flag_d = din("flag", [128, 1])
    w_ada = din("w_ada", [D, 6 * D])
    b_ada = din("b_ada", [1, 6 * D])
    g_mix = din("g_mix", [1, D])
    g_ffn = din("g_ffn", [1, D])
    g_final = din("g_final", [1, D])
    w_in = din("w_in", [D, 2560])
    w_out = din("w_out", [D, D])
    recp = din("recp", [128, 4, 8])
    wa_bd = din("wa_bd", [128, 4, 128])
    wx_bd = din("wx_bd", [128, 4, 128])
    w_router = din("w_router", [D, NE])
    b_router = din("b_router", [1, NE])
    w_gu = din("w_gate_up", [n_experts, D, 2 * D])
    bgu_t = din("bgu_t", [128, NE, 16])
    b_gu_d = din("b_gu", [NE, 2 * D])
    w_dn = din("w_down", [n_experts, D, D])
    b_dn = din("b_down", [NE, D])
    out_d = nc.dram_tensor("out", [TOWN, D], F32, kind="ExternalOutput").ap()
    dbg = {}
    if debug:
        def dout(name, shape):
            dbg[name] = nc.dram_tensor(name, shape, F32, kind="ExternalOutput").ap()
        dout("d_mod", [128, 6 * D])
        dout("d_hT", [128, 8 * 512])
        dout("d_mixT", [128, 8 * TOWN])
        dout("d_x1", [TOWN, D])
        dout("d_gates", [128, 16 * NE])

    with ExitStack() as st:
        S = Sched(nc, st)

        def sbt(stack, name, shape, dt):
            return stack.enter_context(nc.sbuf_tensor(name, shape, dt))

        arena = sbt(st, "arena", [128, 16384], F32)
        hT = arena[:].bitcast(BF16).rearrange("p (k t) -> p k t", k=8)
        x1 = arena[:].rearrange("p (t f) -> p t f", t=16)
        r_hT = RL(32)
        r_x1 = RL(16)
        bufA = sbt(st, "bufA", [128, 8, TOWN], BF16)
        r_bufA = [RL(16) for _ in range(8)]
        mv3 = sbt(st, "mv3", [128, D], F32); r_mv3 = Res()
        mv4 = sbt(st, "mv4", [128, D], F32); r_mv4 = Res()
        mv5 = sbt(st, "mv5", [128, D], F32); r_mv5 = Res()
        xb = [sbt(st, "xb%d" % i, [128, D], F32) for i in range(2)]; r_xb = RL(2)
        tmpf = sbt(st, "tmpf", [128, D], F32); r_tmpf = Res()
        hb = [sbt(st, "hb%d" % i, [128, D], BF16) for i in range(2)]; r_hb = RL(2)
        junk = sbt(st, "junk", [128, D], BF16)
        ss = sbt(st, "ss", [128, 64], F32); r_ss = RL(64)
        ms = sbt(st, "ms", [128, 64], F32); r_ms = RL(64)
        rstd = sbt(st, "rstd", [128, 64], F32); r_rstd = RL(64)
        ident = sbt(st, "ident", [128, 128], BF16); r_ident = Res()
        identf = sbt(st, "identf", [128, 128], F32); r_identf = Res()
        neghalf = sbt(st, "neghalf", [128, 1], F32); r_nh = Res()
        flag = sbt(st, "flag_sb", [128, 1], F32); r_flag = Res()
        flagb = sbt(st, "flagb", [128, 1], F32); r_flagb = Res()
        negmask = sbt(st, "negmask", [128, 512], BF16); r_negmask = Res()
        maskB = sbt(st, "maskB", [128, 512], BF16); r_maskB = Res()
        ones_a = sbt(st, "ones_a", [128, 128], BF16); r_ones = Res()
        ones_b = sbt(st, "ones_b", [128, 128], BF16)

        banks = [st.enter_context(nc.psum_tensor("bank%d" % i, [128, 512], F32)) for i in range(8)]
        r_bank = RL(8)
        bank_rr = [0]

        def pb():
            i = bank_rr[0]
            bank_rr[0] = (i + 1) % 8
            return banks[i], r_bank[i]

        cp_rr = [0]
        evac_dve_only = [False]

        def evac(out, in_, reads, writes):
            cp_rr[0] ^= 1
            if cp_rr[0] and not evac_dve_only[0]:
                S.op("scalar", mk("activation", out=out, in_=in_, func=AF.Copy), reads=reads, writes=writes)
            else:
                S.op("vector", mk("tensor_copy", out=out, in_=in_), reads=reads, writes=writes)

        S.dma(flag[:], flag_d, writes=[r_flag])
        S.op("gpsimd", mk("memset", ap=neghalf[:], constant=-0.5), writes=[r_nh])
        S.op("gpsimd", mk("memset", ap=ident[:], constant=1.0), writes=[r_ident])
        S.op("gpsimd", mk("affine_select", out=ident[:], in_=ident[:], pattern=[[-1, 128]],
                          compare_op=ALU.is_equal, fill=0.0, base=0, channel_multiplier=1),
             reads=[r_ident], writes=[r_ident])
        S.op("gpsimd", mk("memset", ap=identf[:], constant=1.0), writes=[r_identf])
        S.op("gpsimd", mk("affine_select", out=identf[:], in_=identf[:], pattern=[[-1, 128]],
                          compare_op=ALU.is_equal, fill=0.0, base=0, channel_multiplier=1),
             reads=[r_identf], writes=[r_identf])
        S.op("gpsimd", mk("memset", ap=negmask[:], constant=0.0), writes=[r_negmask])
        for blk in range(4):
            sl = negmask[:, blk * 128:(blk + 1) * 128]
            if blk % 2 == 0:
                S.op("gpsimd", mk("affine_select", out=sl, in_=sl, pattern=[[-1, 128]], compare_op=ALU.is_ge,
                                  fill=-30000.0, base=0, channel_multiplier=1), reads=[r_negmask], writes=[r_negmask])
            else:
                S.op("gpsimd", mk("affine_select", out=sl, in_=sl, pattern=[[1, 128]], compare_op=ALU.is_ge,
                                  fill=-30000.0, base=0, channel_multiplier=-1), reads=[r_negmask], writes=[r_negmask])
        S.op("vector", mk("tensor_scalar", out=flagb[:], in0=flag[:], scalar1=-1.0, scalar2=30000.0,
                          op0=ALU.add, op1=ALU.mult), reads=[r_flag], writes=[r_flagb])
        S.op("vector", mk("tensor_copy", out=maskB[:], in_=negmask[:]), reads=[r_negmask], writes=[r_maskB])
        for blk in (0, 2):
            sl = maskB[:, blk * 128:(blk + 1) * 128]
            S.op("vector", mk("tensor_scalar", out=sl, in0=sl, scalar1=flagb[:, 0:1], scalar2=None, op0=ALU.add),
                 reads=[r_maskB, r_flagb], writes=[r_maskB])
        S.op("gpsimd", mk("memset", ap=ones_a[:], constant=0.0), writes=[r_ones])
        S.op("gpsimd", mk("memset", ap=ones_a[:, 0:64], constant=1.0), writes=[r_ones])
        S.op("gpsimd", mk("memset", ap=ones_b[:], constant=0.0), writes=[r_ones])
        S.op("gpsimd", mk("memset", ap=ones_b[:, 64:128], constant=1.0), writes=[r_ones])

        def wview(w2d, c0, n):
            return w2d[:, c0:c0 + n].rearrange("(kc p) n -> p kc n", p=128)

        def rms_tile(src, r_src, col, A_vec, r_A, B_vec, r_B, hbt, r_hbt):
            S.op("scalar", mk("activation", out=junk[:], in_=src, func=AF.Square, accum_out=ss[:, col:col + 1]),
                 reads=[r_src], writes=[r_ss[col]])
            S.op("vector", mk("tensor_scalar", out=ms[:, col:col + 1], in0=ss[:, col:col + 1], scalar1=1.0 / D,
                              scalar2=EPS, op0=ALU.mult, op1=ALU.add), reads=[r_ss[col]], writes=[r_ms[col]])
            S.op("gpsimd", mk("tensor_tensor", out=rstd[:, col:col + 1], in0=ms[:, col:col + 1], in1=neghalf[:],
                              op=ALU.pow), reads=[r_ms[col], r_nh], writes=[r_rstd[col]])
            if B_vec is None:
                S.op("vector", mk("scalar_tensor_tensor", out=hbt, in0=src, scalar=rstd[:, col:col + 1], in1=A_vec,
                                  op0=ALU.mult, op1=ALU.mult), reads=[r_src, r_rstd[col], r_A], writes=[r_hbt])
                return
            S.op("vector", mk("scalar_tensor_tensor", out=tmpf[:], in0=src, scalar=rstd[:, col:col + 1], in1=A_vec,
                              op0=ALU.mult, op1=ALU.mult), reads=[r_src, r_rstd[col], r_A], writes=[r_tmpf])
            S.op("vector", mk("tensor_tensor", out=hbt, in0=tmpf[:], in1=B_vec, op=ALU.add),
                 reads=[r_tmpf, r_B], writes=[r_hbt])

        def transpose_tile(hbt, r_hbt, dstT, r_dst, k0=0, k1=8):
            bk, r_bk = pb()
            pv = bk[:].bitcast(BF16).rearrange("p (k t) -> p k t", k=8)
            S.group("tensor", [mk("transpose", out=pv[:, kc, :], in_=hbt[:, kc * 128:(kc + 1) * 128], identity=ident[:])
                               for kc in range(k0, k1)], reads=[r_hbt, r_ident], writes=[r_bk])
            evac(dstT, pv[:, k0:k1, :], [r_bk], r_dst)

        with ExitStack() as st_mix:
            mv2 = sbt(st_mix, "mv2", [128, D], F32); r_mv2 = Res()
            with ExitStack() as st_a:
                mv0 = sbt(st_a, "mv0", [128, D], F32); r_mv0 = Res()
                mv1 = sbt(st_a, "mv1", [128, D], F32); r_mv1 = Res()
                mvs = [mv0, mv1, mv2, mv3, mv4, mv5]
                r_mvs = [r_mv0, r_mv1, r_mv2, r_mv3, r_mv4, r_mv5]
                with ExitStack() as st0:
                    bada = sbt(st0, "bada", [128, 6 * D], F32); r_bada = Res()
                    gmix_bc = sbt(st0, "gmix_bc", [128, D], F32); r_gmix = Res()
                    gffn_bc = sbt(st0, "gffn_bc", [128, D], F32); r_gffn = Res()
                    wab = [sbt(st0, "wab%d" % i, [128, 8, 512], BF16) for i in range(2)]; r_wab = RL(2)
                    c_bf = sbt(st0, "c_bf", [128, 8, 128], BF16); r_cbf = Res()
                    S.dma(c_bf[:], c_rep, writes=[r_cbf], eng="gpsimd")
                    S.dma(bada[:], b_ada.partition_broadcast(128), writes=[r_bada])
                    S.dma(gmix_bc[:], g_mix.partition_broadcast(128), writes=[r_gmix])
                    S.dma(gffn_bc[:], g_ffn.partition_broadcast(128), writes=[r_gffn])
                    for cc in range(12):
                        wb_, r_wb_ = wab[cc % 2], r_wab[cc % 2]
                        S.dma(wb_[:], wview(w_ada, cc * 512, 512), writes=[r_wb_], eng="gpsimd")
                        bk, r_bk = pb()
                        S.group("tensor", [mk("matmul", out=bk[:], lhsT=c_bf[:, kc, :], rhs=wb_[:, kc, :],
                                              start=(kc == 0), stop=(kc == 7)) for kc in range(8)],
                                reads=[r_cbf, r_wb_], writes=[r_bk])
                        dst = mvs[cc // 2][:, (cc % 2) * 512:(cc % 2 + 1) * 512]
                        S.op("vector", mk("tensor_tensor", out=dst, in0=bk[:], in1=bada[:, cc * 512:(cc + 1) * 512],
                                          op=ALU.add), reads=[r_bk, r_bada], writes=[r_mvs[cc // 2]])
                    if debug:
                        for i in range(6):
                            S.dma(dbg["d_mod"][:, i * D:(i + 1) * D], mvs[i][:], reads=[r_mvs[i]])
                    S.op("vector", mk("scalar_tensor_tensor", out=mv1[:], in0=mv1[:], scalar=1.0, in1=gmix_bc[:],
                                      op0=ALU.add, op1=ALU.mult), reads=[r_gmix], writes=[r_mv1])
                    S.op("vector", mk("scalar_tensor_tensor", out=mv4[:], in0=mv4[:], scalar=1.0, in1=gffn_bc[:],
                                      op0=ALU.add, op1=ALU.mult), reads=[r_gffn], writes=[r_mv4])
                    S.barrier()
                for t in range(32):
                    xs, r_xs = xb[t % 2], r_xb[t % 2]
                    S.dma(xs[:], x_all[t * 128:(t + 1) * 128, :], writes=[r_xs])
                    rms_tile(xs[:], r_xs, t, mv1[:], r_mv1, mv0[:], r_mv0, hb[t % 2][:], r_hb[t % 2])
                    transpose_tile(hb[t % 2], r_hb[t % 2], hT[:, :, t * 128:(t + 1) * 128], [r_hT[t]])
                if debug:
                    S.barrier()
                    with ExitStack() as st_d:
                        dtmp = sbt(st_d, "dtmp", [128, 8 * 512], F32); r_dtmp = Res()
                        S.op("vector", mk("tensor_copy", out=dtmp[:].rearrange("p (k t) -> p k t", k=8),
                                          in_=hT[:, :, 1792:2304]), reads=r_hT, writes=[r_dtmp])
                        S.dma(dbg["d_hT"], dtmp[:], reads=[r_dtmp])
                        S.barrier()
                S.barrier()
            mixT = bufA
            with ExitStack() as st_r:
                NB = 1024
                xr = sbt(st_r, "xr", [128, NB + 3], F32); r_xr = Res()
                xc = sbt(st_r, "xc", [128, NB], F32); r_xc = Res()
                xcb = sbt(st_r, "xcb", [128, NB], BF16); r_xcb = Res()
                rg = sbt(st_r, "rg", [128, NB], F32); r_rg = Res()
                ig = sbt(st_r, "ig", [128, NB], F32); r_ig = Res()
                ag = sbt(st_r, "ag", [128, NB], F32); r_ag = Res()
                t1 = sbt(st_r, "t1", [128, NB], F32); r_t1 = Res()
                hs = sbt(st_r, "hs", [128, NB], F32); r_hs = Res()
                gg = sbt(st_r, "gg", [128, NB], F32); r_gg = Res()
                wxr = [sbt(st_r, "wxr%d" % i, [128, 8, 128], BF16) for i in range(2)]; r_wxr = RL(2)
                wgr = [sbt(st_r, "wgr%d" % i, [128, 8, 128], BF16) for i in range(2)]; r_wgr = RL(2)
                wbd = [sbt(st_r, "wbd%d" % i, [128, 4, 128], BF16) for i in range(2)]; r_wbd = RL(2)
                rp = sbt(st_r, "rp", [128, 4, 8], F32); r_rp = Res()
                clam = sbt(st_r, "clam", [128, 4], F32); r_clam = Res()
                state = sbt(st_r, "state", [128, 4], F32); r_state = Res()
                S.dma(rp[:], recp, writes=[r_rp])
                S.dma(wbd[0][:], wa_bd, writes=[r_wbd[0]], eng="gpsimd")
                S.dma(wbd[1][:], wx_bd, writes=[r_wbd[1]], eng="gpsimd")
                S.op("scalar", mk("activation", out=clam[:], in_=rp[:, :, 7], func=AF.Exp, scale=-1.0),
                     reads=[r_rp], writes=[r_clam])
                S.op("scalar", mk("activation", out=clam[:], in_=clam[:], func=AF.Ln, bias=1.0, scale=1.0),
                     reads=[r_clam], writes=[r_clam])
                S.op("vector", mk("tensor_scalar", out=clam[:], in0=clam[:], scalar1=-8.0, scalar2=None, op0=ALU.mult),
                     reads=[r_clam], writes=[r_clam])
                S.op("vector", mk("memset", ap=state[:], constant=0.0), writes=[r_state])
                for cch in range(4):
                    sl = cch % 2
                    S.dma(wxr[sl][:], wview(w_in, 1536 + cch * 128, 128), writes=[r_wxr[sl]], eng="gpsimd")
                    S.dma(wgr[sl][:], wview(w_in, 2048 + cch * 128, 128), writes=[r_wgr[sl]], eng="gpsimd")
                    S.op("vector", mk("memset", ap=xr[:, 0:3], constant=0.0), writes=[r_xr])
                    for seg in range(4):
                        t0 = seg * NB
                        rh = r_hT[seg * 8:(seg + 1) * 8]
                        if seg == 2:
                            S.op("vector", mk("tensor_scalar", out=xr[:, 0:3], in0=xr[:, 0:3], scalar1=flag[:, 0:1],
                                              scalar2=None, op0=ALU.mult), reads=[r_flag], writes=[r_xr])
                            S.op("vector", mk("tensor_scalar", out=state[:, cch:cch + 1], in0=state[:, cch:cch + 1],
                                              scalar1=flag[:, 0:1], scalar2=None, op0=ALU.mult),
                                 reads=[r_flag], writes=[r_state])
                        for h2 in range(2):
                            bk, r_bk = pb()
                            S.group("tensor", [mk("matmul", out=bk[:], lhsT=wxr[sl][:, kc, :],
                                                  rhs=hT[:, kc, t0 + h2 * 512:t0 + (h2 + 1) * 512],
                                                  start=(kc == 0), stop=(kc == 7)) for kc in range(8)],
                                    reads=[r_wxr[sl]] + rh, writes=[r_bk])
                            S.op("scalar", mk("activation", out=xr[:, 3 + h2 * 512:3 + (h2 + 1) * 512], in_=bk[:],
                                              func=AF.Copy), reads=[r_bk], writes=[r_xr])
                        S.op("vector", mk("tensor_scalar", out=xc[:], in0=xr[:, 3:NB + 3], scalar1=rp[:, cch, 3:4],
                                          scalar2=rp[:, cch, 4:5], op0=ALU.mult, op1=ALU.add),
                             reads=[r_xr, r_rp], writes=[r_xc])
                        for j in range(3):
                            S.op("vector", mk("scalar_tensor_tensor", out=xc[:], in0=xr[:, j:j + NB],
                                              scalar=rp[:, cch, j:j + 1], in1=xc[:], op0=ALU.mult, op1=ALU.add),
                                 reads=[r_xr, r_rp], writes=[r_xc])
                        S.op("vector", mk("tensor_copy", out=xr[:, 0:3], in_=xr[:, NB:NB + 3]), writes=[r_xr])
                        S.op("scalar", mk("activation", out=xcb[:], in_=xc[:], func=AF.Copy), reads=[r_xc], writes=[r_xcb])
                        for which, (dst, r_dst, bcol) in enumerate(((rg, r_rg, 5), (ig, r_ig, 6))):
                            for h2 in range(2):
                                bk, r_bk = pb()
                                S.group("tensor", [mk("matmul", out=bk[:], lhsT=wbd[which][:, cch, :],
                                                      rhs=xcb[:, h2 * 512:(h2 + 1) * 512], start=True, stop=True)],
                                        reads=[r_wbd[which], r_xcb], writes=[r_bk])
                                S.op("scalar", mk("activation", out=dst[:, h2 * 512:(h2 + 1) * 512], in_=bk[:],
                                                  func=AF.Sigmoid, bias=rp[:, cch, bcol:bcol + 1], scale=1.0),
                                     reads=[r_bk, r_rp], writes=[r_dst])
                        S.op("scalar", mk("activation", out=ag[:], in_=rg[:], func=AF.Exp, scale=clam[:, cch:cch + 1]),
                             reads=[r_rg, r_clam], writes=[r_ag])
                        S.op("vector", mk("tensor_tensor", out=t1[:], in0=ag[:], in1=ag[:], op=ALU.mult),
                             reads=[r_ag], writes=[r_t1])
                        S.op("vector", mk("tensor_scalar", out=t1[:], in0=t1[:], scalar1=-1.0, scalar2=1.0,
                                          op0=ALU.mult, op1=ALU.add), reads=[r_t1], writes=[r_t1])
                        S.op("vector", mk("tensor_scalar", out=t1[:], in0=t1[:], scalar1=1e-30, scalar2=None,
                                          op0=ALU.max), reads=[r_t1], writes=[r_t1])
                        S.op("scalar", mk("activation", out=t1[:], in_=t1[:], func=AF.Sqrt), reads=[r_t1], writes=[r_t1])
                        S.op("gpsimd", mk("tensor_tensor", out=ig[:], in0=ig[:], in1=xc[:], op=ALU.mult),
                             reads=[r_xc], writes=[r_ig])
                        S.op("gpsimd", mk("tensor_tensor", out=ig[:], in0=ig[:], in1=t1[:], op=ALU.mult),
                             reads=[r_t1], writes=[r_ig])
                        S.op("vector", mk("tensor_tensor_scan", out=hs[:], data0=ag[:], data1=ig[:],
                                          initial=state[:, cch:cch + 1], op0=ALU.mult, op1=ALU.add),
                             reads=[r_ag, r_ig, r_state], writes=[r_hs])
                        S.op("vector", mk("tensor_copy", out=state[:, cch:cch + 1], in_=hs[:, NB - 1:NB]),
                             reads=[r_hs], writes=[r_state])
                        if seg >= 2:
                            o0 = (seg - 2) * NB
                            for h2 in range(2):
                                bk, r_bk = pb()
                                S.group("tensor", [mk("matmul", out=bk[:], lhsT=wgr[sl][:, kc, :],
                                                      rhs=hT[:, kc, t0 + h2 * 512:t0 + (h2 + 1) * 512],
                                                      start=(kc == 0), stop=(kc == 7)) for kc in range(8)],
                                        reads=[r_wgr[sl]] + rh, writes=[r_bk])
                                S.op("scalar", mk("activation", out=gg[:, h2 * 512:(h2 + 1) * 512], in_=bk[:],
                                                  func=AF.Gelu_apprx_tanh), reads=[r_bk], writes=[r_gg])
                            S.op("gpsimd", mk("tensor_tensor", out=mixT[:, 4 + cch, o0:o0 + NB], in0=hs[:], in1=gg[:],
                                              op=ALU.mult), reads=[r_hs, r_gg],
                                 writes=r_bufA[4 + cch][(seg - 2) * 8:(seg - 1) * 8])
                S.barrier()
            with ExitStack() as st_at:
                wqkv = sbt(st_at, "wqkv", [128, 3, 8, 128], BF16); r_wqkv = Res()
                qTa = sbt(st_at, "qTa", [128, TOWN], BF16); r_qTa = Res()
                qTb = sbt(st_at, "qTb", [128, TOWN], BF16); r_qTb = Res()
                kT = sbt(st_at, "kT", [128, TALL], BF16); r_kT = Res()
                Vta = sbt(st_at, "Vta", [128, 32, 128], BF16); r_Vt = RL(8)
                Vtb = sbt(st_at, "Vtb", [128, 32, 128], BF16)
                accN = sbt(st_at, "accN", [128, TOWN], F32); r_accN = Res()
                accD = sbt(st_at, "accD", [128, TOWN], F32); r_accD = Res()
                PT = [sbt(st_at, "PT%d" % i, [128, 512], BF16) for i in range(2)]; r_PT = RL(2)
                pt_rr = 0
                S.op("gpsimd", mk("memset", ap=Vta[:], constant=0.0), writes=r_Vt)
                S.op("gpsimd", mk("memset", ap=Vtb[:], constant=0.0), writes=r_Vt)
                S.op("gpsimd", mk("memset", ap=qTa[:], constant=0.0), writes=[r_qTa])
                S.op("gpsimd", mk("memset", ap=qTb[:], constant=0.0), writes=[r_qTb])
                for fc in range(4):
                    for i3 in range(3):
                        S.dma(wqkv[:, i3, :, :], wview(w_in, i3 * 512 + fc * 128, 128), writes=[r_wqkv], eng="gpsimd")
                    for tb in range(4):
                        bk, r_bk = pb()
                        c0 = TOWN + tb * 512
                        S.group("tensor", [mk("matmul", out=bk[:], lhsT=wqkv[:, 0, kc, :], rhs=hT[:, kc, c0:c0 + 512],
                                              start=(kc == 0), stop=(kc == 7)) for kc in range(8)],
                                reads=[r_wqkv] + r_hT[16 + tb * 4:16 + (tb + 1) * 4], writes=[r_bk])
                        S.op("scalar", mk("activation", out=qTa[0:64, tb * 512:(tb + 1) * 512], in_=bk[0:64, :],
                                          func=AF.Copy), reads=[r_bk], writes=[r_qTa])
                        S.op("vector", mk("tensor_copy", out=qTb[64:128, tb * 512:(tb + 1) * 512], in_=bk[64:128, :]),
                             reads=[r_bk], writes=[r_qTb])
                    for tb in range(8):
                        bk, r_bk = pb()
                        c0 = tb * 512
                        S.group("tensor", [mk("matmul", out=bk[:], lhsT=wqkv[:, 1, kc, :], rhs=hT[:, kc, c0:c0 + 512],
                                              start=(kc == 0), stop=(kc == 7)) for kc in range(8)],
                                reads=[r_wqkv] + r_hT[tb * 4:(tb + 1) * 4], writes=[r_bk])
                        evac(kT[:, c0:c0 + 512], bk[:], [r_bk], [r_kT])
                    for pat, d in enumerate((1, 4, 16)):
                        L = TALL // d
                        nj = L // 128
                        for g4 in range(8):
                            bk, r_bk = pb()
                            fns = []
                            for u in range(4):
                                ti = g4 * 4 + u
                                r_, j_ = divmod(ti, nj)
                                s0 = r_ + d * 128 * j_
                                for kc in range(8):
                                    fns.append(mk("matmul", out=bk[:, u * 128:(u + 1) * 128],
                                                  lhsT=hT[:, kc, s0:s0 + d * 127 + 1:d], rhs=wqkv[:, 2, kc, :],
                                                  start=(kc == 0), stop=(kc == 7)))
                            S.group("tensor", fns, reads=[r_wqkv] + r_hT, writes=[r_bk])
                            bv = bk[:].rearrange("p (u c) -> p u c", u=4)
                            S.op("scalar", mk("activation", out=Vta[:, g4 * 4:(g4 + 1) * 4, 0:64], in_=bv[:, :, 0:64],
                                              func=AF.Copy), reads=[r_bk], writes=[r_Vt[g4]])
                            S.op("vector", mk("tensor_copy", out=Vtb[:, g4 * 4:(g4 + 1) * 4, 64:128], in_=bv[:, :, 64:128]),
                                 reads=[r_bk], writes=[r_Vt[g4]])
                        for su in range(4):
                            bkN, r_bkN = pb()
                            bkD, r_bkD = pb()
                            for u in range(4):
                                if d == 1:
                                    r_, jq = 0, 16 + 4 * su + u
                                elif d == 4:
                                    r_, jq = u, 4 + su
                                else:
                                    r_, jq = 4 * su + u, 1
                                q0 = r_ + d * 128 * jq - TOWN
                                kp0 = r_ + d * 128 * (jq - 1)
                                kc0 = r_ + d * 128 * jq
                                tip = r_ * nj + jq - 1
                                tic = r_ * nj + jq
                                boundary = (jq == nj // 2)
                                span = d * 127 + 1
                                bkS, r_bkS = pb()
                                msk = maskB if boundary else negmask
                                fns = [mk("matmul", out=bkS[:], lhsT=ident[:], rhs=msk[:], start=True, stop=False)]
                                for bi, (qq, k0) in enumerate(((qTa, kp0), (qTa, kc0), (qTb, kp0), (qTb, kc0))):
                                    fns.append(mk("matmul", out=bkS[:, bi * 128:(bi + 1) * 128],
                                                  lhsT=kT[:, k0:k0 + span:d], rhs=qq[:, q0:q0 + span:d],
                                                  start=False, stop=(bi == 3)))
                                S.group("tensor", fns, reads=[r_ident, r_negmask, r_maskB, r_kT, r_qTa, r_qTb],
                                        writes=[r_bkS])
                                pt, r_pt = PT[pt_rr], r_PT[pt_rr]
                                pt_rr ^= 1
                                S.op("scalar", mk("activation", out=pt[:], in_=bkS[:], func=AF.Exp, scale=0.125),
                                     reads=[r_bkS], writes=[r_pt])
                                oc = slice(u * 128, (u + 1) * 128)
                                fnsN = [
                                    mk("matmul", out=bkN[:, oc], lhsT=Vta[:, tip, :], rhs=pt[:, 0:128], start=True, stop=False),
                                    mk("matmul", out=bkN[:, oc], lhsT=Vta[:, tic, :], rhs=pt[:, 128:256], start=False, stop=False),
                                    mk("matmul", out=bkN[:, oc], lhsT=Vtb[:, tip, :], rhs=pt[:, 256:384], start=False, stop=False),
                                    mk("matmul", out=bkN[:, oc], lhsT=Vtb[:, tic, :], rhs=pt[:, 384:512], start=False, stop=True),
                                ]
                                S.group("tensor", fnsN, reads=[r_pt, r_Vt[tip // 4], r_Vt[tic // 4]], writes=[r_bkN])
                                fnsD = [
                                    mk("matmul", out=bkD[:, oc], lhsT=ones_a[:], rhs=pt[:, 0:128], start=True, stop=False),
                                    mk("matmul", out=bkD[:, oc], lhsT=ones_a[:], rhs=pt[:, 128:256], start=False, stop=False),
                                    mk("matmul", out=bkD[:, oc], lhsT=ones_b[:], rhs=pt[:, 256:384], start=False, stop=False),
                                    mk("matmul", out=bkD[:, oc], lhsT=ones_b[:], rhs=pt[:, 384:512], start=False, stop=True),
                                ]
                                S.group("tensor", fnsD, reads=[r_pt, r_ones], writes=[r_bkD])
                            for acc, r_acc, bkX, r_bkX, eng in ((accN, r_accN, bkN, r_bkN, "vector"),
                                                               (accD, r_accD, bkD, r_bkD, "gpsimd")):
                                src = bkX[:].rearrange("p (u i) -> p u i", u=4)
                                if d == 1:
                                    dst = acc[:, su * 512:(su + 1) * 512].rearrange("p (u i) -> p u i", u=4)
                                elif d == 4:
                                    dst = acc[:, su * 512:(su + 1) * 512].rearrange("p (i r) -> p r i", r=4)
                                else:
                                    dst = acc[:].rearrange("p (i r) -> p r i", r=16)[:, 4 * su:4 * su + 4, :]
                                if d == 1:
                                    S.op("vector" if eng == "vector" else "scalar",
                                         mk("tensor_copy", out=dst, in_=src) if eng == "vector" else
                                         mk("activation", out=dst, in_=src, func=AF.Copy),
                                         reads=[r_bkX], writes=[r_acc])
                                else:
                                    S.op("vector", mk("tensor_tensor", out=dst, in0=dst, in1=src, op=ALU.add),
                                         reads=[r_bkX], writes=[r_acc])
                    S.op("vector", mk("reciprocal", out=accD[:], in_=accD[:]), reads=[r_accD], writes=[r_accD])
                    S.op("vector", mk("tensor_tensor", out=mixT[:, fc, :], in0=accN[:], in1=accD[:], op=ALU.mult),
                         reads=[r_accN, r_accD], writes=r_bufA[fc])
                S.barrier()
            if debug:
                with ExitStack() as st_d:
                    dtmp = sbt(st_d, "dtmp2", [128, 8 * 512], F32); r_dtmp = Res()
                    for q4 in range(4):
                        S.op("vector", mk("tensor_copy", out=dtmp[:].rearrange("p (k t) -> p k t", k=8),
                                          in_=mixT[:, :, q4 * 512:(q4 + 1) * 512]), reads=[], writes=[r_dtmp])
                        S.dma(dbg["d_mixT"].rearrange("p (k t) -> p k t", k=8)[:, :, q4 * 512:(q4 + 1) * 512],
                              dtmp[:].rearrange("p (k t) -> p k t", k=8), reads=[r_dtmp])
                    S.barrier()
            with ExitStack() as st_o:
                wout = sbt(st_o, "wout", [128, 8, D], BF16); r_wout = Res()
                S.dma(wout[:, 0:4, :], w_out[0:512, :].rearrange("(kc p) n -> p kc n", p=128), writes=[r_wout], eng="gpsimd")
                S.dma(wout[:, 4:8, :], w_out[512:1024, :].rearrange("(kc p) n -> p kc n", p=128), writes=[r_wout], eng="gpsimd")
                for t in range(16):
                    xs, r_xs = xb[t % 2], r_xb[t % 2]
                    S.dma(xs[:], x_all[TOWN + t * 128:TOWN + (t + 1) * 128, :], writes=[r_xs])
                    for cc in range(2):
                        bk, r_bk = pb()
                        cs = slice(cc * 512, (cc + 1) * 512)
                        S.group("tensor", [mk("matmul", out=bk[:], lhsT=mixT[:, kc, t * 128:(t + 1) * 128],
                                              rhs=wout[:, kc, cs], start=(kc == 0), stop=(kc == 7)) for kc in range(8)],
                                reads=[r_wout] + [r_bufA[kc][t] for kc in range(8)], writes=[r_bk])
                        S.op("vector", mk("tensor_tensor", out=tmpf[:, cs], in0=bk[:], in1=mv2[:, cs], op=ALU.mult),
                             reads=[r_bk, r_mv2], writes=[r_tmpf])
                        S.op("gpsimd", mk("tensor_tensor", out=x1[:, t, cs], in0=tmpf[:, cs], in1=xs[:, cs], op=ALU.add),
                             reads=[r_tmpf, r_xs], writes=[r_x1[t]])
                S.barrier()
        if debug:
            for t in range(16):
                S.dma(dbg["d_x1"][t * 128:(t + 1) * 128, :], x1[:, t, :], reads=[r_x1[t]])
        I32 = mybir.dt.int32
        CAP = TOWN
        x_buf = nc.dram_tensor("x_buf", [NE * CAP, D], BF16, kind="Internal").ap()
        y_buf = nc.dram_tensor("y_buf", [NE * CAP, D], F32, kind="Internal").ap()
        with ExitStack() as st_f:
            wgus = [bufA, sbt(st_f, "wgu1", [128, 8, 2 * D], BF16)]; r_wgus = RL(2)
            wgus[0] = bufA[:].rearrange("p k t -> p (k t)").rearrange("p (k t) -> p k t", k=8)
            wd = sbt(st_f, "wd", [128, 8, D], BF16); r_wd = Res()
            gk = sbt(st_f, "gk", [128, 16, 4], F32); r_gk = RL(16)
            desti = sbt(st_f, "desti", [128, 16, 4], I32); r_desti = RL(16)
            cnti = sbt(st_f, "cnti", [128, NE], I32); r_cnti = Res()

            bgrow = [sbt(st_f, "bgrow%d" % i, [2, 2 * D], BF16) for i in range(2)]; r_bgrow = RL(2)
            ones_r = sbt(st_f, "ones_r", [2, 128], BF16); r_ones_r = Res()
            S.op("gpsimd", mk("memset", ap=ones_r[:], constant=1.0), writes=[r_ones_r])
            for i in range(2):
                S.op("gpsimd", mk("memset", ap=bgrow[i][:, 0:D], constant=0.0), writes=[r_bgrow[i]])
                S.op("gpsimd", mk("memset", ap=bgrow[i][:, D:2 * D], constant=1.0), writes=[r_bgrow[i]])

            def load_wgu(e):
                w_, r_w = wgus[e % 2], r_wgus[e % 2]
                for q4 in range(4):
                    S.dma(w_[:, :, q4 * 512:(q4 + 1) * 512], wview(w_gu[e], q4 * 512, 512), writes=[r_w], eng="gpsimd")
                S.dma(bgrow[e % 2][0:1, :], b_gu_d[e:e + 1, :], writes=[r_bgrow[e % 2]], eng="gpsimd")

            def load_wd(e):
                S.dma(wd[:, 0:4, :], w_dn[e][0:512, :].rearrange("(kc p) n -> p kc n", p=128), writes=[r_wd], eng="gpsimd")
                S.dma(wd[:, 4:8, :], w_dn[e][512:1024, :].rearrange("(kc p) n -> p kc n", p=128), writes=[r_wd], eng="gpsimd")

            load_wgu(0)
            load_wd(0)
            with ExitStack() as st_r2:
                wr = sbt(st_r2, "wr", [128, 8, NE], BF16); r_wr = Res()
                brt = sbt(st_r2, "brt", [128, NE], F32); r_brt = Res()
                lg = sbt(st_r2, "lg", [128, NE], F32); r_lg = Res()
                ex = sbt(st_r2, "ex", [128, NE], F32); r_ex = Res()
                gt = sbt(st_r2, "gt", [128, NE], F32); r_gt = Res()
                posb = sbt(st_r2, "posb", [128, NE], F32); r_posb = Res()
                scr4 = sbt(st_r2, "scr4", [128, 4, NE], F32); r_scr = Res()
                ebase = sbt(st_r2, "ebase", [128, NE], F32); r_ebase = Res()
                m8 = sbt(st_r2, "m8", [128, 8], F32); r_m8 = Res()
                e4 = sbt(st_r2, "e4", [128, 4], F32); r_e4 = Res()
                destf = sbt(st_r2, "destf", [128, 4], F32); r_destf = Res()
                nmx = sbt(st_r2, "nmx", [128, 1], F32); r_nmx = Res()
                sm = sbt(st_r2, "sm", [128, 1], F32); r_sm = Res()
                gT = sbt(st_r2, "gT", [32, 128], F32); r_gT = Res()
                bdn = sbt(st_r2, "bdn", [32, D], F32); r_bdn = Res()
                maskall = sbt(st_r2, "maskall", [128, 16, NE], BF16); r_mask = RL(16)
                ltri = sbt(st_r2, "ltri", [128, 128], BF16); r_ltri = Res()
                ones_f = sbt(st_r2, "ones_full", [128, 128], BF16); r_onesf = Res()
                h2Tt = sbt(st_r2, "h2Tt", [128, 8, 128], BF16); r_h2Tt = Res()
                S.dma(wr[:], w_router.rearrange("(kc p) n -> p kc n", p=128), writes=[r_wr], eng="gpsimd")
                S.dma(brt[:], b_router.partition_broadcast(128), writes=[r_brt])
                S.dma(bdn[:], b_dn, writes=[r_bdn])
                S.op("gpsimd", mk("iota", out=ebase[:], pattern=[[CAP, NE]], base=0, channel_multiplier=0,
                                  allow_small_or_imprecise_dtypes=True), writes=[r_ebase])
                S.op("gpsimd", mk("memset", ap=ones_f[:], constant=1.0), writes=[r_onesf])
                S.op("gpsimd", mk("memset", ap=ltri[:], constant=1.0), writes=[r_ltri])
                S.op("gpsimd", mk("affine_select", out=ltri[:], in_=ltri[:], pattern=[[1, 128]], compare_op=ALU.is_gt,
                                  fill=0.0, base=0, channel_multiplier=-1), reads=[r_ltri], writes=[r_ltri])
                for t in range(16):
                    hbt, r_hbt = hb[t % 2], r_hb[t % 2]
                    rms_tile(x1[:, t, :], r_x1[t], 32 + t, mv4[:], r_mv4, mv3[:], r_mv3, hbt[:], r_hbt)
                    transpose_tile(hbt, r_hbt, h2Tt[:], [r_h2Tt])
                    bk, r_bk = pb()
                    S.group("tensor", [mk("matmul", out=bk[:, 0:NE], lhsT=h2Tt[:, kc, :], rhs=wr[:, kc, :],
                                          start=(kc == 0), stop=(kc == 7)) for kc in range(8)],
                            reads=[r_wr, r_h2Tt], writes=[r_bk])
                    S.op("vector", mk("tensor_tensor", out=lg[:], in0=bk[:, 0:NE], in1=brt[:], op=ALU.add),
                         reads=[r_bk, r_brt], writes=[r_lg])
                    S.op("vector", mk("max", out=m8[:], in_=lg[:]), reads=[r_lg], writes=[r_m8])
                    S.op("vector", mk("tensor_scalar", out=nmx[:], in0=m8[:, 0:1], scalar1=-1.0, scalar2=None, op0=ALU.mult),
                         reads=[r_m8], writes=[r_nmx])
                    S.op("scalar", mk("activation", out=ex[:], in_=lg[:], func=AF.Exp, bias=nmx[:, 0:1], scale=1.0),
                         reads=[r_lg, r_nmx], writes=[r_ex])
                    S.op("scalar", mk("activation", out=e4[:], in_=m8[:, 0:4], func=AF.Exp, bias=nmx[:, 0:1], scale=1.0),
                         reads=[r_m8, r_nmx], writes=[r_e4])
                    S.op("vector", mk("tensor_scalar", out=maskall[:, t, :], in0=lg[:], scalar1=m8[:, 3:4], scalar2=None,
                                      op0=ALU.is_ge), reads=[r_lg, r_m8], writes=[r_mask[t]])
                    S.op("vector", mk("tensor_tensor", out=ex[:], in0=ex[:], in1=maskall[:, t, :], op=ALU.mult),
                         reads=[r_mask[t]], writes=[r_ex])
                    S.op("vector", mk("tensor_reduce", out=sm[:], in_=ex[:], axis=mybir.AxisListType.X, op=ALU.add),
                         reads=[r_ex], writes=[r_sm])
                    S.op("vector", mk("reciprocal", out=sm[:], in_=sm[:]), reads=[r_sm], writes=[r_sm])
                    S.op("vector", mk("tensor_scalar", out=gt[:], in0=ex[:], scalar1=sm[:, 0:1], scalar2=None,
                                      op0=ALU.mult), reads=[r_ex, r_sm], writes=[r_gt])
                    S.op("vector", mk("tensor_scalar", out=gk[:, t, :], in0=e4[:], scalar1=sm[:, 0:1], scalar2=None,
                                      op0=ALU.mult), reads=[r_e4, r_sm], writes=[r_gk[t]])
                    bkp, r_bkp = pb()
                    fns = [mk("matmul", out=bkp[:, 0:NE], lhsT=ones_f[:], rhs=maskall[:, tp, :], start=(tp == 0), stop=False)
                           for tp in range(t)]
                    fns.append(mk("matmul", out=bkp[:, 0:NE], lhsT=ltri[:], rhs=maskall[:, t, :], start=(t == 0), stop=True))
                    S.group("tensor", fns, reads=[r_onesf, r_ltri] + r_mask[:t + 1], writes=[r_bkp])
                    S.op("vector", mk("tensor_tensor", out=posb[:], in0=bkp[:, 0:NE], in1=ebase[:], op=ALU.add),
                         reads=[r_bkp, r_ebase], writes=[r_posb])
                    for k in range(4):
                        S.op("vector", mk("scalar_tensor_tensor", out=scr4[:, k, :], in0=lg[:], scalar=m8[:, k:k + 1], in1=posb[:],
                                          op0=ALU.is_equal, op1=ALU.mult),
                             reads=[r_lg, r_m8, r_posb], writes=[r_scr])
                    S.op("vector", mk("tensor_reduce", out=destf[:], in_=scr4[:], axis=mybir.AxisListType.X, op=ALU.add),
                         reads=[r_scr], writes=[r_destf])
                    S.op("vector", mk("tensor_scalar", out=destf[:], in0=destf[:], scalar1=0.0, scalar2=float(NE * CAP - 1),
                                      op0=ALU.max, op1=ALU.min), reads=[r_destf], writes=[r_destf])
                    S.op("vector", mk("tensor_copy", out=desti[:, t, :], in_=destf[:]), reads=[r_destf], writes=[r_desti[t]])
                    for k in range(4):
                        def sc(e, t=t, k=k, hbt=hbt):
                            return e.indirect_dma_start(out=x_buf[:, :],
                                                        out_offset=bass.IndirectOffsetOnAxis(ap=desti[:, t, k:k + 1], axis=0),
                                                        in_=hbt[:, :], in_offset=None)
                        S.dma_fn("gpsimd", sc, reads=[r_desti[t], r_hbt], writes=[])
                    bk2, r_bk2 = pb()
                    S.group("tensor", [mk("transpose", out=bk2[0:NE, 0:128], in_=gt[:], identity=identf[:])],
                            reads=[r_gt, r_identf], writes=[r_bk2])
                    S.op("vector", mk("tensor_copy", out=gT[:], in_=bk2[0:NE, 0:128]), reads=[r_bk2], writes=[r_gT])
                    for cc in range(2):
                        cs = slice(cc * 512, (cc + 1) * 512)
                        bk3, r_bk3 = pb()
                        S.group("tensor", [mk("matmul", out=bk3[:], lhsT=gT[:], rhs=bdn[:, cs], start=True, stop=True)],
                                reads=[r_gT, r_bdn], writes=[r_bk3])
                        S.op("vector", mk("tensor_tensor", out=tmpf[:, cs], in0=bk3[:], in1=mv5[:, cs], op=ALU.mult),
                             reads=[r_bk3, r_mv5], writes=[r_tmpf])
                        S.op("gpsimd", mk("tensor_tensor", out=x1[:, t, cs], in0=tmpf[:, cs], in1=x1[:, t, cs], op=ALU.add),
                             reads=[r_tmpf], writes=[r_x1[t]])
                bkc, r_bkc = pb()
                S.group("tensor", [mk("matmul", out=bkc[:, 0:NE], lhsT=ones_f[:], rhs=maskall[:, tp, :], start=(tp == 0),
                                      stop=(tp == 15)) for tp in range(16)], reads=[r_onesf] + r_mask, writes=[r_bkc])
                S.op("vector", mk("tensor_copy", out=cnti[:], in_=bkc[:, 0:NE]), reads=[r_bkc], writes=[r_cnti])
                if debug:
                    S.dma(dbg["d_gates"][:, 0:64], gk[:].rearrange("p t e -> p (t e)"), reads=r_gk)
                S.barrier()
            S.dma(mv3[:], g_final.partition_broadcast(128), writes=[r_mv3])
            with ExitStack() as st_m:
                xblk = [hb[0], hb[1]]; r_xblk = RL(2)
                xT1 = sbt(st_m, "xT1", [128, 8, 128], BF16)
                xTs = [junk[:].rearrange("p (k t) -> p k t", k=8), xT1[:]]; r_xT = RL(2)
                actT = [sbt(st_m, "actT%d" % i, [128, 8, 128], BF16) for i in range(2)]; r_act = [RL(2), RL(2)]
                yblk = [xb[0], xb[1]]; r_yblk = RL(2)
                sgx = sbt(st_m, "sgx", [128, D], F32)
                ucx = sbt(st_m, "ucx", [128, D], F32)
                gc2 = [[tmpf[:, 0:512], tmpf[:, 512:1024]], [sgx[:, 0:512], sgx[:, 512:1024]]]; r_gc2 = [RL(2), RL(2)]
                actk = [sbt(st_m, "actk%d" % i, [128, D], BF16) for i in range(2)]; r_actk = [RL(2), RL(2)]
                ucs = [[mv4[:, 0:512], mv4[:, 512:1024]], [ucx[:, 0:512], ucx[:, 512:1024]]]; r_uc = [RL(2), RL(2)]
                evac_dve_only[0] = True
                blk_rr = 0
                for e in range(n_experts if stage >= 2 else 0):
                    w_, r_w = wgus[e % 2], r_wgus[e % 2]
                    if e + 1 < n_experts:
                        load_wgu(e + 1)
                    S.load_count(cnti[0:1, e:e + 1], [r_cnti])
                    for jp in range(0, 16, 2):
                        S.begin_guard((e, jp), 128 * jp + 1)
                        pair = (jp, jp + 1)

                        NESTED = True

                        def inner(j, ph):
                            if j != jp and NESTED:
                                S.begin_inner((e, j, ph), 128 * j + 1)

                        def inner_end(j):
                            if j != jp and NESTED:
                                S.end_inner()
                        for j in pair:
                            s_ = j % 2
                            row0 = e * CAP + 128 * j
                            inner(j, 1)
                            S.dma(xblk[s_][:], x_buf[row0:row0 + 128, :], writes=[r_xblk[s_]])
                            transpose_tile(xblk[s_], r_xblk[s_], xTs[s_], [r_xT[s_]])
                            inner_end(j)
                        for j in pair:
                            s_ = j % 2
                            xT = xTs[s_]
                            inner(j, 2)
                            for half in range(2):
                                gub = []
                                for c0 in (half * 512, D + half * 512):
                                    bkX, r_bkX = pb()
                                    fns = [mk("matmul", out=bkX[:], lhsT=ones_r[0:2, :], rhs=bgrow[e % 2][0:2, c0:c0 + 512],
                                              start=True, stop=False)]
                                    for kc in range(8):
                                        fns.append(mk("matmul", out=bkX[:], lhsT=xT[:, kc, :], rhs=w_[:, kc, c0:c0 + 512],
                                                      start=False, stop=(kc == 7)))
                                    S.group("tensor", fns, reads=[r_w, r_xT[s_], r_ones_r, r_bgrow[e % 2]], writes=[r_bkX])
                                    gub.append((bkX, r_bkX))
                                (bkG, r_bkG), (bkU, r_bkU) = gub
                                uc, r_uc_ = ucs[s_][half], r_uc[s_][half]
                                gc, r_gc_ = gc2[s_][half], r_gc2[s_][half]
                                S.op("scalar", mk("activation", out=gc, in_=bkG[:], func=AF.Gelu_apprx_sigmoid),
                                     reads=[r_bkG], writes=[r_gc_])
                                S.op("vector", mk("tensor_scalar", out=uc, in0=bkU[:], scalar1=8.0, scalar2=-6.0,
                                                  op0=ALU.min, op1=ALU.max), reads=[r_bkU], writes=[r_uc_])
                                S.op("vector", mk("scalar_tensor_tensor", out=actk[s_][:, half * 512:(half + 1) * 512], in0=gc,
                                                  scalar=GLU7, in1=uc, op0=ALU.min, op1=ALU.mult),
                                     reads=[r_uc_, r_gc_], writes=[r_actk[s_][half]])
                            inner_end(j)
                        for j in pair:
                            s_ = j % 2
                            row0 = e * CAP + 128 * j
                            inner(j, 3)
                            for half in range(2):
                                transpose_tile(actk[s_], r_actk[s_][half], actT[s_][:, 4 * half:4 * half + 4, :],
                                               [r_act[s_][half]], k0=4 * half, k1=4 * half + 4)
                            for cc in range(2):
                                cs = slice(cc * 512, (cc + 1) * 512)
                                bk, r_bk = pb()
                                S.group("tensor", [mk("matmul", out=bk[:], lhsT=actT[s_][:, kc, :], rhs=wd[:, kc, cs],
                                                      start=(kc == 0), stop=(kc == 7)) for kc in range(8)],
                                        reads=[r_wd] + r_act[s_], writes=[r_bk])
                                S.op("vector", mk("tensor_tensor", out=yblk[s_][:, cs], in0=bk[:], in1=mv5[:, cs], op=ALU.mult),
                                     reads=[r_bk, r_mv5], writes=[r_yblk[s_]])
                            S.dma(y_buf[row0:row0 + 128, :], yblk[s_][:], reads=[r_yblk[s_]], eng="gpsimd")
                            inner_end(j)
                        S.end_guard()
                    if e + 1 < n_experts:
                        load_wd(e + 1)
                evac_dve_only[0] = False
                S.barrier()
            st_c = st_f.enter_context(ExitStack())
            ykb = [mv4, tmpf] + [sbt(st_c, "ykb%d" % i, [128, D], F32) for i in range(4)]; r_ykb = RL(6)
            gi = 0
            for t in range(16):
                for k in range(4 if stage >= 3 else 0):
                    yk, r_yk = ykb[gi % 6], r_ykb[gi % 6]
                    gi += 1

                    def ga(e, t=t, k=k, yk=yk):
                        return e.indirect_dma_start(out=yk[:, :], out_offset=None, in_=y_buf[:, :],
                                                    in_offset=bass.IndirectOffsetOnAxis(ap=desti[:, t, k:k + 1], axis=0))
                    S.dma_fn("gpsimd", ga, reads=[r_desti[t]], writes=[r_yk])
                    S.op("vector", mk("scalar_tensor_tensor", out=x1[:, t, :], in0=yk[:], scalar=gk[:, t, k:k + 1],
                                      in1=x1[:, t, :], op0=ALU.mult, op1=ALU.add),
                         reads=[r_yk, r_gk[t]], writes=[r_x1[t]])
                ob = xb[t % 2]; r_ob = r_xb[t % 2]
                rms_tile(x1[:, t, :], r_x1[t], 48 + t, mv3[:], r_mv3, None, None, ob[:], r_ob)
                S.dma(out_d[t * 128:(t + 1) * 128, :], ob[:], reads=[r_ob])
            S.barrier()
        with nc.Block() as block:
            S.emit(block)
    return nc


_NC_CACHE = {}


def make_in_maps(inputs, cores, n_experts=NE):
    f = lambda a: np.ascontiguousarray(np.asarray(a, dtype=np.float32))
    x = f(inputs["x"]); c = f(inputs["c"])
    w_ada = f(inputs["w_ada"])[0]; b_ada = f(inputs["b_ada"])[0][None, :]
    g_mix = f(inputs["g_mix"])[0][None, :]; g_ffn = f(inputs["g_ffn"])[0][None, :]
    g_final = f(inputs["g_final"])[None, :]
    w_in = f(inputs["w_in"])[0]; w_out = f(inputs["w_out"])[0]
    conv_w = f(inputs["conv_w"])[0]; conv_b = f(inputs["conv_b"])[0]
    b_a = f(inputs["b_rg_a"])[0]; b_x = f(inputs["b_rg_x"])[0]; lam = f(inputs["lam"])[0]
    recp = np.zeros((128, 4, 8), np.float32)
    cols = [conv_w[0], conv_w[1], conv_w[2], conv_w[3], conv_b, b_a, b_x, lam]
    for j, v in enumerate(cols):
        recp[:, :, j] = v.reshape(4, 128).T
    def blockdiag(w):
        w = f(w)[0]
        o = np.zeros((128, 4, 128), np.float32)
        for blk in range(8):
            cch, hh = divmod(blk, 2)
            o[hh * 64:(hh + 1) * 64, cch, hh * 64:(hh + 1) * 64] = w[blk]
        return o
    wa_bd = blockdiag(inputs["w_rg_a"]); wx_bd = blockdiag(inputs["w_rg_x"])
    w_router = f(inputs["w_router"])[0]; b_router = f(inputs["b_router"])[0][None, :]
    w_gu = f(inputs["w_gate_up"])[0][:n_experts]; b_gu = f(inputs["b_gate_up"])[0]
    bgu_t = np.ascontiguousarray(b_gu.reshape(NE, 16, 128).transpose(2, 0, 1))
    w_dn = f(inputs["w_down"])[0][:n_experts]; b_dn = f(inputs["b_down"])[0]
    maps = []
    for core in cores:
        b, half = divmod(core, 2)
        x_all = np.zeros((TALL, D), np.float32)
        if half == 0:
            x_all[TOWN:] = x[b, :TOWN]
        else:
            x_all[:] = x[b]
        c_rep = np.ascontiguousarray(np.broadcast_to(c[b].reshape(8, 128).T[:, :, None], (128, 8, 128)))
        flag = np.full((128, 1), float(half), np.float32)
        maps.append({
            "x_all": x_all, "c_rep": c_rep, "flag": flag, "w_ada": w_ada, "b_ada": b_ada, "g_mix": g_mix,
            "g_ffn": g_ffn, "g_final": g_final, "w_in": w_in, "w_out": w_out, "recp": recp, "wa_bd": wa_bd,
            "wx_bd": wx_bd, "w_router": w_router, "b_router": b_router, "w_gate_up": w_gu, "bgu_t": bgu_t, "b_gu": b_gu,
            "w_down": w_dn, "b_down": b_dn,
        })
    return maps


def kernel(**inputs):
    if "nc" not in _NC_CACHE:
        _NC_CACHE["nc"] = build()
    nc = _NC_CACHE["nc"]
    cores = list(range(8))
    in_maps = make_in_maps(inputs, cores)
    res = run_bass_kernel_spmd(nc, in_maps, core_ids=cores)
    out = np.zeros((4, 4096, D), np.float32)
    for core in cores:
        b, half = divmod(core, 2)
        out[b, half * TOWN:(half + 1) * TOWN] = res.results[core]["out"]
    return out
```

```python
import numpy as np
from contextlib import ExitStack
import concourse.bass as bass
import concourse.mybir as mybir
from concourse.bass_utils import run_bass_kernel_spmd

F32 = mybir.dt.float32
BF16 = mybir.dt.bfloat16
AF = mybir.ActivationFunctionType
ALU = mybir.AluOpType

D = 1024
TOWN = 2048
TALL = 4096
NE = 32
EPS = 1e-6
GLU7 = float(np.float32(7.0) / (np.float32(1.0) + np.exp(np.float32(-1.702 * 7.0))))
SEM_LIMIT = 30000


class Res:
    __slots__ = ("w", "r")

    def __init__(self):
        self.w = None
        self.r = {}


def RL(n):
    return [Res() for _ in range(n)]


class Sched:
    ENGS = ("tensor", "vector", "scalar", "gpsimd", "sync")

    def __init__(self, nc, stack, n_dma_sems=8):
        self.nc = nc
        self.stack = stack
        self.ops = {e: [] for e in self.ENGS}
        self.known = {e: {} for e in self.ENGS}
        self.dom_sem = {}
        self.dom_max = {}
        self.ndom = 0
        self.cur_dom = {}
        self.cur_cnt = {}
        self.guard = None
        self.guard_snap = {}
        self.regs = {}
        for e in self.ENGS:
            self.cur_dom[e] = self._new_dom(e)
            self.cur_cnt[e] = 0
        self.dma_pool = {}
        for q in ("sync", "gpsimd"):
            self.dma_pool[q] = {"doms": [self._new_dom("dma_%s%d" % (q, i)) for i in range(n_dma_sems)],
                                "cnt": [0] * n_dma_sems, "rr": 0}

    def _new_dom(self, name):
        d = self.ndom
        self.ndom += 1
        self.dom_sem[d] = self.stack.enter_context(self.nc.semaphore("s_%s_%d" % (name, d)))
        self.dom_max[d] = 0
        return d

    def _collect(self, eng, reads, writes):
        need = {}
        for R in reads:
            if R.w is not None:
                d, v = R.w
                if need.get(d, 0) < v:
                    need[d] = v
        for R in writes:
            if R.w is not None:
                d, v = R.w
                if need.get(d, 0) < v:
                    need[d] = v
            for d, v in R.r.items():
                if need.get(d, 0) < v:
                    need[d] = v
        kn = self.known[eng]
        waits = []
        for d, v in need.items():
            if eng == "tensor" and d == self.cur_dom["tensor"]:
                continue
            if kn.get(d, 0) < v:
                kn[d] = v
                waits.append((d, v))
        return waits

    def _tick(self, eng):
        self.cur_cnt[eng] += 1
        d = self.cur_dom[eng]
        v = self.cur_cnt[eng]
        self.dom_max[d] = v
        return d, v

    def _mark(self, d, v, reads, writes):
        for R in reads:
            R.r[d] = v
        for R in writes:
            R.w = (d, v)
            R.r = {}

    def op(self, eng, fn, reads=(), writes=()):
        waits = self._collect(eng, reads, writes)
        d, v = self._tick(eng)
        self.ops[eng].append((waits, fn, self.dom_sem[d], 1, self.guard))
        self._mark(d, v, reads, writes)

    def group(self, eng, fns, reads=(), writes=()):
        waits = self._collect(eng, reads, writes)
        d, v = self._tick(eng)
        n = len(fns)
        for i, fn in enumerate(fns):
            self.ops[eng].append((waits if i == 0 else [], fn,
                                  self.dom_sem[d] if i == n - 1 else None, 1, self.guard))
        self._mark(d, v, reads, writes)

    def dma_fn(self, eng, fn, reads=(), writes=()):
        pool = self.dma_pool[eng]
        i = pool["rr"]
        pool["rr"] = (i + 1) % len(pool["doms"])
        d = pool["doms"][i]
        waits = self._collect(eng, reads, writes)
        prev = pool["cnt"][i]
        kn = self.known[eng]
        if prev > 0 and kn.get(d, 0) < prev:
            kn[d] = prev
            waits.append((d, prev))
        pool["cnt"][i] += 16
        v = pool["cnt"][i]
        self.dom_max[d] = v
        self.ops[eng].append((waits, fn, self.dom_sem[d], 16, self.guard))
        self._mark(d, v, reads, writes)

    def dma(self, out, in_, reads=(), writes=(), eng="sync"):
        def fn(e, out=out, in_=in_):
            return e.dma_start(out=out, in_=in_)
        self.dma_fn(eng, fn, reads, writes)

    def load_count(self, ap, reads):
        for eng in self.ENGS:
            waits = self._collect(eng, reads, ())
            sched = self

            def fn(e, eng=eng, ap=ap):
                return e.reg_load(sched.regs[eng], ap)
            self.ops[eng].append((waits, fn, None, 0, None))
        for R in reads:
            for eng in self.ENGS:
                pass

    def begin_guard(self, gid, thr):
        self.guard = (gid, thr, None, 0)
        self.guard_snap[gid] = dict(self.dom_max)

    def begin_inner(self, gid, thr):
        g = self.guard
        self.guard = (g[0], g[1], gid, thr)
        self.guard_snap[gid] = dict(self.dom_max)

    def end_inner(self):
        g = self.guard
        self.guard = (g[0], g[1], None, 0)

    def end_guard(self):
        self.guard = None

    def barrier(self):
        assert self.guard is None
        for eng in self.ENGS:
            kn = self.known[eng]
            waits = []
            for d, v in self.dom_max.items():
                if v > 0 and kn.get(d, 0) < v:
                    kn[d] = v
                    waits.append((d, v))
            if waits:
                self.ops[eng].append((waits, None, None, 0, None))

    def emit(self, block):
        sched = self

        def emit_one(e, item):
            waits, fn, sem, inc, _ = item
            for (d_, v) in waits:
                e.wait_ge(sched.dom_sem[d_], v)
            if fn is None:
                return
            ins = fn(e)
            if sem is not None:
                ins.then_inc(sem, inc)

        def skip_path(e, grp, snap):
            incs = []
            need = {}
            for waits, fn, sem, inc, _ in grp:
                for (d_, v) in waits:
                    v = min(v, snap.get(d_, 0))
                    if v > need.get(d_, 0):
                        need[d_] = v
            for d_, v in need.items():
                e.wait_ge(sched.dom_sem[d_], v)
            for waits, fn, sem, inc, _ in grp:
                if sem is not None:
                    for k_ in range(len(incs)):
                        if incs[k_][0] is sem:
                            incs[k_][1] += inc
                            break
                    else:
                        incs.append([sem, inc])
            e.drain()
            for sem, tot in incs:
                e.sem_inc(sem, tot)

        def make(engname):
            def body(e):
                ops = sched.ops[engname]
                sched.regs[engname] = e.alloc_register("cnt_" + engname)
                reg = sched.regs[engname]
                i = 0
                n = len(ops)
                while i < n:
                    g = ops[i][4]
                    if g is None:
                        emit_one(e, ops[i])
                        i += 1
                        continue
                    j = i
                    while j < n and ops[j][4] is not None and ops[j][4][0] == g[0]:
                        j += 1
                    grp = ops[i:j]
                    with e.If_lt(reg, g[1]):
                        skip_path(e, grp, sched.guard_snap[g[0]])
                    with e.Else():
                        a = 0
                        m = len(grp)
                        while a < m:
                            gi = grp[a][4]
                            if gi[2] is None:
                                emit_one(e, grp[a])
                                a += 1
                                continue
                            b = a
                            while b < m and grp[b][4][2] == gi[2]:
                                b += 1
                            sub = grp[a:b]
                            with e.If_lt(reg, gi[3]):
                                skip_path(e, sub, sched.guard_snap[gi[2]])
                            with e.Else():
                                for it in sub:
                                    emit_one(e, it)
                            a = b
                    i = j
            return body
        for engname in self.ENGS:
            if self.ops[engname]:
                getattr(block, engname)(make(engname))


def mk(method, **kw):
    return lambda e: getattr(e, method)(**kw)


def build(debug=False, n_experts=NE, stage=3):
    nc = bass.Bass("TRN2", target_bir_lowering=False)

    def din(name, shape):
        return nc.dram_tensor(name, shape, F32, kind="ExternalInput").ap()

    x_all = din("x_all", [TALL, D])
    c_rep = din("c_rep", [128, 8, 128])
    flag_d = din("flag", [128, 1])
    w_ada = din("w_ada", [D, 6 * D])
    b_ada = din("b_ada", [1, 6 * D])
    g_mix = din("g_mix", [1, D])
    g_ffn = din("g_ffn", [1, D])
    g_final = din("g_final", [1, D])
    w_in = din("w_in", [D, 2560])
    w_out = din("w_out", [D, D])
    recp = din("recp", [128, 4, 8])
    wa_bd = din("wa_bd", [128, 4, 128])
    wx_bd = din("wx_bd", [128, 4, 128])
    w_router = din("w_router", [D, NE])
    b_router = din("b_router", [1, NE])
    w_gu = din("w_gate_up", [n_experts, D, 2 * D])
    bgu_t = din("bgu_t", [128, NE, 16])
    b_gu_d = din("b_gu", [NE, 2 * D])
    w_dn = din("w_down", [n_experts, D, D])
    b_dn = din("b_down", [NE, D])
    out_d = nc.dram_tensor("out", [TOWN, D], F32, kind="ExternalOutput").ap()
    dbg = {}
    if debug:
        def dout(name, shape):
            dbg[name] = nc.dram_tensor(name, shape, F32, kind="ExternalOutput").ap()
        dout("d_mod", [128, 6 * D])
        dout("d_hT", [128, 8 * 512])
        dout("d_mixT", [128, 8 * TOWN])
        dout("d_x1", [TOWN, D])
        dout("d_gates", [128, 16 * NE])

    with ExitStack() as st:
        S = Sched(nc, st)

        def sbt(stack, name, shape, dt):
            return stack.enter_context(nc.sbuf_tensor(name, shape, dt))

        arena = sbt(st, "arena", [128, 16384], F32)
        hT = arena[:].bitcast(BF16).rearrange("p (k t) -> p k t", k=8)
        x1 = arena[:].rearrange("p (t f) -> p t f", t=16)
        r_hT = RL(32)
        r_x1 = RL(16)
        bufA = sbt(st, "bufA", [128, 8, TOWN], BF16)
        r_bufA = [RL(16) for _ in range(8)]
        mv3 = sbt(st, "mv3", [128, D], F32); r_mv3 = Res()
        mv4 = sbt(st, "mv4", [128, D], F32); r_mv4 = Res()
        mv5 = sbt(st, "mv5", [128, D], F32); r_mv5 = Res()
        xb = [sbt(st, "xb%d" % i, [128, D], F32) for i in range(2)]; r_xb = RL(2)
        tmpf = sbt(st, "tmpf", [128, D], F32); r_tmpf = Res()
        hb = [sbt(st, "hb%d" % i, [128, D], BF16) for i in range(2)]; r_hb = RL(2)
        junk = sbt(st, "junk", [128, D], BF16)
        ss = sbt(st, "ss", [128, 64], F32); r_ss = RL(64)
        ms = sbt(st, "ms", [128, 64], F32); r_ms = RL(64)
        rstd = sbt(st, "rstd", [128, 64], F32); r_rstd = RL(64)
        ident = sbt(st, "ident", [128, 128], BF16); r_ident = Res()
        identf = sbt(st, "identf", [128, 128], F32); r_identf = Res()
        neghalf = sbt(st, "neghalf", [128, 1], F32); r_nh = Res()
        flag = sbt(st, "flag_sb", [128, 1], F32); r_flag = Res()
        flagb = sbt(st, "flagb", [128, 1], F32); r_flagb = Res()
        negmask = sbt(st, "negmask", [128, 512], BF16); r_negmask = Res()
        maskB = sbt(st, "maskB", [128, 512], BF16); r_maskB = Res()
        ones_a = sbt(st, "ones_a", [128, 128], BF16); r_ones = Res()
        ones_b = sbt(st, "ones_b", [128, 128], BF16)

        banks = [st.enter_context(nc.psum_tensor("bank%d" % i, [128, 512], F32)) for i in range(8)]
        r_bank = RL(8)
        bank_rr = [0]

        def pb():
            i = bank_rr[0]
            bank_rr[0] = (i + 1) % 8
            return banks[i], r_bank[i]

        cp_rr = [0]
        evac_dve_only = [False]

        def evac(out, in_, reads, writes):
            cp_rr[0] ^= 1
            if cp_rr[0] and not evac_dve_only[0]:
                S.op("scalar", mk("activation", out=out, in_=in_, func=AF.Copy), reads=reads, writes=writes)
            else:
                S.op("vector", mk("tensor_copy", out=out, in_=in_), reads=reads, writes=writes)

        S.dma(flag[:], flag_d, writes=[r_flag])
        S.op("gpsimd", mk("memset", ap=neghalf[:], constant=-0.5), writes=[r_nh])
        S.op("gpsimd", mk("memset", ap=ident[:], constant=1.0), writes=[r_ident])
        S.op("gpsimd", mk("affine_select", out=ident[:], in_=ident[:], pattern=[[-1, 128]],
                          compare_op=ALU.is_equal, fill=0.0, base=0, channel_multiplier=1),
             reads=[r_ident], writes=[r_ident])
        S.op("gpsimd", mk("memset", ap=identf[:], constant=1.0), writes=[r_identf])
        S.op("gpsimd", mk("affine_select", out=identf[:], in_=identf[:], pattern=[[-1, 128]],
                          compare_op=ALU.is_equal, fill=0.0, base=0, channel_multiplier=1),
             reads=[r_identf], writes=[r_identf])
        S.op("gpsimd", mk("memset", ap=negmask[:], constant=0.0), writes=[r_negmask])
        for blk in range(4):
            sl = negmask[:, blk * 128:(blk + 1) * 128]
            if blk % 2 == 0:
                S.op("gpsimd", mk("affine_select", out=sl, in_=sl, pattern=[[-1, 128]], compare_op=ALU.is_ge,
                                  fill=-30000.0, base=0, channel_multiplier=1), reads=[r_negmask], writes=[r_negmask])
            else:
                S.op("gpsimd", mk("affine_select", out=sl, in_=sl, pattern=[[1, 128]], compare_op=ALU.is_ge,
                                  fill=-30000.0, base=0, channel_multiplier=-1), reads=[r_negmask], writes=[r_negmask])
        S.op("vector", mk("tensor_scalar", out=flagb[:], in0=flag[:], scalar1=-1.0, scalar2=30000.0,
                          op0=ALU.add, op1=ALU.mult), reads=[r_flag], writes=[r_flagb])
        S.op("vector", mk("tensor_copy", out=maskB[:], in_=negmask[:]), reads=[r_negmask], writes=[r_maskB])
        for blk in (0, 2):
            sl = maskB[:, blk * 128:(blk + 1) * 128]
            S.op("vector", mk("tensor_scalar", out=sl, in0=sl, scalar1=flagb[:, 0:1], scalar2=None, op0=ALU.add),
                 reads=[r_maskB, r_flagb], writes=[r_maskB])
        S.op("gpsimd", mk("memset", ap=ones_a[:], constant=0.0), writes=[r_ones])
        S.op("gpsimd", mk("memset", ap=ones_a[:, 0:64], constant=1.0), writes=[r_ones])
        S.op("gpsimd", mk("memset", ap=ones_b[:], constant=0.0), writes=[r_ones])
        S.op("gpsimd", mk("memset", ap=ones_b[:, 64:128], constant=1.0), writes=[r_ones])

        def wview(w2d, c0, n):
            return w2d[:, c0:c0 + n].rearrange("(kc p) n -> p kc n", p=128)

        def rms_tile(src, r_src, col, A_vec, r_A, B_vec, r_B, hbt, r_hbt):
            S.op("scalar", mk("activation", out=junk[:], in_=src, func=AF.Square, accum_out=ss[:, col:col + 1]),
                 reads=[r_src], writes=[r_ss[col]])
            S.op("vector", mk("tensor_scalar", out=ms[:, col:col + 1], in0=ss[:, col:col + 1], scalar1=1.0 / D,
                              scalar2=EPS, op0=ALU.mult, op1=ALU.add), reads=[r_ss[col]], writes=[r_ms[col]])
            S.op("gpsimd", mk("tensor_tensor", out=rstd[:, col:col + 1], in0=ms[:, col:col + 1], in1=neghalf[:],
                              op=ALU.pow), reads=[r_ms[col], r_nh], writes=[r_rstd[col]])
            if B_vec is None:
                S.op("vector", mk("scalar_tensor_tensor", out=hbt, in0=src, scalar=rstd[:, col:col + 1], in1=A_vec,
                                  op0=ALU.mult, op1=ALU.mult), reads=[r_src, r_rstd[col], r_A], writes=[r_hbt])
                return
            S.op("vector", mk("scalar_tensor_tensor", out=tmpf[:], in0=src, scalar=rstd[:, col:col + 1], in1=A_vec,
                              op0=ALU.mult, op1=ALU.mult), reads=[r_src, r_rstd[col], r_A], writes=[r_tmpf])
            S.op("vector", mk("tensor_tensor", out=hbt, in0=tmpf[:], in1=B_vec, op=ALU.add),
                 reads=[r_tmpf, r_B], writes=[r_hbt])

        def transpose_tile(hbt, r_hbt, dstT, r_dst, k0=0, k1=8):
            bk, r_bk = pb()
            pv = bk[:].bitcast(BF16).rearrange("p (k t) -> p k t", k=8)
            S.group("tensor", [mk("transpose", out=pv[:, kc, :], in_=hbt[:, kc * 128:(kc + 1) * 128], identity=ident[:])
                               for kc in range(k0, k1)], reads=[r_hbt, r_ident], writes=[r_bk])
            evac(dstT, pv[:, k0:k1, :], [r_bk], r_dst)

        with ExitStack() as st_mix:
            mv2 = sbt(st_mix, "mv2", [128, D], F32); r_mv2 = Res()
            with ExitStack() as st_a:
                mv0 = sbt(st_a, "mv0", [128, D], F32); r_mv0 = Res()
                mv1 = sbt(st_a, "mv1", [128, D], F32); r_mv1 = Res()
                mvs = [mv0, mv1, mv2, mv3, mv4, mv5]
                r_mvs = [r_mv0, r_mv1, r_mv2, r_mv3, r_mv4, r_mv5]
                with ExitStack() as st0:
                    bada = sbt(st0, "bada", [128, 6 * D], F32); r_bada = Res()
                    gmix_bc = sbt(st0, "gmix_bc", [128, D], F32); r_gmix = Res()
                    gffn_bc = sbt(st0, "gffn_bc", [128, D], F32); r_gffn = Res()
                    wab = [sbt(st0, "wab%d" % i, [128, 8, 512], BF16) for i in range(2)]; r_wab = RL(2)
                    c_bf = sbt(st0, "c_bf", [128, 8, 128], BF16); r_cbf = Res()
                    S.dma(c_bf[:], c_rep, writes=[r_cbf], eng="gpsimd")
                    S.dma(bada[:], b_ada.partition_broadcast(128), writes=[r_bada])
                    S.dma(gmix_bc[:], g_mix.partition_broadcast(128), writes=[r_gmix])
                    S.dma(gffn_bc[:], g_ffn.partition_broadcast(128), writes=[r_gffn])
                    for cc in range(12):
                        wb_, r_wb_ = wab[cc % 2], r_wab[cc % 2]
                        S.dma(wb_[:], wview(w_ada, cc * 512, 512), writes=[r_wb_], eng="gpsimd")
                        bk, r_bk = pb()
                        S.group("tensor", [mk("matmul", out=bk[:], lhsT=c_bf[:, kc, :], rhs=wb_[:, kc, :],
                                              start=(kc == 0), stop=(kc == 7)) for kc in range(8)],
                                reads=[r_cbf, r_wb_], writes=[r_bk])
                        dst = mvs[cc // 2][:, (cc % 2) * 512:(cc % 2 + 1) * 512]
                        S.op("vector", mk("tensor_tensor", out=dst, in0=bk[:], in1=bada[:, cc * 512:(cc + 1) * 512],
                                          op=ALU.add), reads=[r_bk, r_bada], writes=[r_mvs[cc // 2]])
                    if debug:
                        for i in range(6):
                            S.dma(dbg["d_mod"][:, i * D:(i + 1) * D], mvs[i][:], reads=[r_mvs[i]])
                    S.op("vector", mk("scalar_tensor_tensor", out=mv1[:], in0=mv1[:], scalar=1.0, in1=gmix_bc[:],
                                      op0=ALU.add, op1=ALU.mult), reads=[r_gmix], writes=[r_mv1])
                    S.op("vector", mk("scalar_tensor_tensor", out=mv4[:], in0=mv4[:], scalar=1.0, in1=gffn_bc[:],
                                      op0=ALU.add, op1=ALU.mult), reads=[r_gffn], writes=[r_mv4])
                    S.barrier()
                for t in range(32):
                    xs, r_xs = xb[t % 2], r_xb[t % 2]
                    S.dma(xs[:], x_all[t * 128:(t + 1) * 128, :], writes=[r_xs])
                    rms_tile(xs[:], r_xs, t, mv1[:], r_mv1, mv0[:], r_mv0, hb[t % 2][:], r_hb[t % 2])
                    transpose_tile(hb[t % 2], r_hb[t % 2], hT[:, :, t * 128:(t + 1) * 128], [r_hT[t]])
                if debug:
                    S.barrier()
                    with ExitStack() as st_d:
                        dtmp = sbt(st_d, "dtmp", [128, 8 * 512], F32); r_dtmp = Res()
                        S.op("vector", mk("tensor_copy", out=dtmp[:].rearrange("p (k t) -> p k t", k=8),
                                          in_=hT[:, :, 1792:2304]), reads=r_hT, writes=[r_dtmp])
                        S.dma(dbg["d_hT"], dtmp[:], reads=[r_dtmp])
                        S.barrier()
                S.barrier()
            mixT = bufA
            with ExitStack() as st_r:
                NB = 1024
                xr = sbt(st_r, "xr", [128, NB + 3], F32); r_xr = Res()
                xc = sbt(st_r, "xc", [128, NB], F32); r_xc = Res()
                xcb = sbt(st_r, "xcb", [128, NB], BF16); r_xcb = Res()
                rg = sbt(st_r, "rg", [128, NB], F32); r_rg = Res()
                ig = sbt(st_r, "ig", [128, NB], F32); r_ig = Res()
                ag = sbt(st_r, "ag", [128, NB], F32); r_ag = Res()
                t1 = sbt(st_r, "t1", [128, NB], F32); r_t1 = Res()
                hs = sbt(st_r, "hs", [128, NB], F32); r_hs = Res()
                gg = sbt(st_r, "gg", [128, NB], F32); r_gg = Res()
                wxr = [sbt(st_r, "wxr%d" % i, [128, 8, 128], BF16) for i in range(2)]; r_wxr = RL(2)
                wgr = [sbt(st_r, "wgr%d" % i, [128, 8, 128], BF16) for i in range(2)]; r_wgr = RL(2)
                wbd = [sbt(st_r, "wbd%d" % i, [128, 4, 128], BF16) for i in range(2)]; r_wbd = RL(2)
                rp = sbt(st_r, "rp", [128, 4, 8], F32); r_rp = Res()
                clam = sbt(st_r, "clam", [128, 4], F32); r_clam = Res()
                state = sbt(st_r, "state", [128, 4], F32); r_state = Res()
                S.dma(rp[:], recp, writes=[r_rp])
                S.dma(wbd[0][:], wa_bd, writes=[r_wbd[0]], eng="gpsimd")
                S.dma(wbd[1][:], wx_bd, writes=[r_wbd[1]], eng="gpsimd")
                S.op("scalar", mk("activation", out=clam[:], in_=rp[:, :, 7], func=AF.Exp, scale=-1.0),
                     reads=[r_rp], writes=[r_clam])
                S.op("scalar", mk("activation", out=clam[:], in_=clam[:], func=AF.Ln, bias=1.0, scale=1.0),
                     reads=[r_clam], writes=[r_clam])
                S.op("vector", mk("tensor_scalar", out=clam[:], in0=clam[:], scalar1=-8.0, scalar2=None, op0=ALU.mult),
                     reads=[r_clam], writes=[r_clam])
                S.op("vector", mk("memset", ap=state[:], constant=0.0), writes=[r_state])
                for cch in range(4):
                    sl = cch % 2
                    S.dma(wxr[sl][:], wview(w_in, 1536 + cch * 128, 128), writes=[r_wxr[sl]], eng="gpsimd")
                    S.dma(wgr[sl][:], wview(w_in, 2048 + cch * 128, 128), writes=[r_wgr[sl]], eng="gpsimd")
                    S.op("vector", mk("memset", ap=xr[:, 0:3], constant=0.0), writes=[r_xr])
                    for seg in range(4):
                        t0 = seg * NB
                        rh = r_hT[seg * 8:(seg + 1) * 8]
                        if seg == 2:
                            S.op("vector", mk("tensor_scalar", out=xr[:, 0:3], in0=xr[:, 0:3], scalar1=flag[:, 0:1],
                                              scalar2=None, op0=ALU.mult), reads=[r_flag], writes=[r_xr])
                            S.op("vector", mk("tensor_scalar", out=state[:, cch:cch + 1], in0=state[:, cch:cch + 1],
                                              scalar1=flag[:, 0:1], scalar2=None, op0=ALU.mult),
                                 reads=[r_flag], writes=[r_state])
                        for h2 in range(2):
                            bk, r_bk = pb()
                            S.group("tensor", [mk("matmul", out=bk[:], lhsT=wxr[sl][:, kc, :],
                                                  rhs=hT[:, kc, t0 + h2 * 512:t0 + (h2 + 1) * 512],
                                                  start=(kc == 0), stop=(kc == 7)) for kc in range(8)],
                                    reads=[r_wxr[sl]] + rh, writes=[r_bk])
                            S.op("scalar", mk("activation", out=xr[:, 3 + h2 * 512:3 + (h2 + 1) * 512], in_=bk[:],
                                              func=AF.Copy), reads=[r_bk], writes=[r_xr])
                        S.op("vector", mk("tensor_scalar", out=xc[:], in0=xr[:, 3:NB + 3], scalar1=rp[:, cch, 3:4],
                                          scalar2=rp[:, cch, 4:5], op0=ALU.mult, op1=ALU.add),
                             reads=[r_xr, r_rp], writes=[r_xc])
                        for j in range(3):
                            S.op("vector", mk("scalar_tensor_tensor", out=xc[:], in0=xr[:, j:j + NB],
                                              scalar=rp[:, cch, j:j + 1], in1=xc[:], op0=ALU.mult, op1=ALU.add),
                                 reads=[r_xr, r_rp], writes=[r_xc])
                        S.op("vector", mk("tensor_copy", out=xr[:, 0:3], in_=xr[:, NB:NB + 3]), writes=[r_xr])
                        S.op("scalar", mk("activation", out=xcb[:], in_=xc[:], func=AF.Copy), reads=[r_xc], writes=[r_xcb])
                        for which, (dst, r_dst, bcol) in enumerate(((rg, r_rg, 5), (ig, r_ig, 6))):
                            for h2 in range(2):
                                bk, r_bk = pb()
                                S.group("tensor", [mk("matmul", out=bk[:], lhsT=wbd[which][:, cch, :],
                                                      rhs=xcb[:, h2 * 512:(h2 + 1) * 512], start=True, stop=True)],
                                        reads=[r_wbd[which], r_xcb], writes=[r_bk])
                                S.op("scalar", mk("activation", out=dst[:, h2 * 512:(h2 + 1) * 512], in_=bk[:],
                                                  func=AF.Sigmoid, bias=rp[:, cch, bcol:bcol + 1], scale=1.0),
                                     reads=[r_bk, r_rp], writes=[r_dst])
                        S.op("scalar", mk("activation", out=ag[:], in_=rg[:], func=AF.Exp, scale=clam[:, cch:cch + 1]),
                             reads=[r_rg, r_clam], writes=[r_ag])
                        S.op("vector", mk("tensor_tensor", out=t1[:], in0=ag[:], in1=ag[:], op=ALU.mult),
                             reads=[r_ag], writes=[r_t1])
                        S.op("vector", mk("tensor_scalar", out=t1[:], in0=t1[:], scalar1=-1.0, scalar2=1.0,
                                          op0=ALU.mult, op1=ALU.add), reads=[r_t1], writes=[r_t1])
                        S.op("vector", mk("tensor_scalar", out=t1[:], in0=t1[:], scalar1=1e-30, scalar2=None,
                                          op0=ALU.max), reads=[r_t1], writes=[r_t1])
                        S.op("scalar", mk("activation", out=t1[:], in_=t1[:], func=AF.Sqrt), reads=[r_t1], writes=[r_t1])
                        S.op("gpsimd", mk("tensor_tensor", out=ig[:], in0=ig[:], in1=xc[:], op=ALU.mult),
                             reads=[r_xc], writes=[r_ig])
                        S.op("gpsimd", mk("tensor_tensor", out=ig[:], in0=ig[:], in1=t1[:], op=ALU.mult),
                             reads=[r_t1], writes=[r_ig])
                        S.op("vector", mk("tensor_tensor_scan", out=hs[:], data0=ag[:], data1=ig[:],
                                          initial=state[:, cch:cch + 1], op0=ALU.mult, op1=ALU.add),
                             reads=[r_ag, r_ig, r_state], writes=[r_hs])
                        S.op("vector", mk("tensor_copy", out=state[:, cch:cch + 1], in_=hs[:, NB - 1:NB]),
                             reads=[r_hs], writes=[r_state])
                        if seg >= 2:
                            o0 = (seg - 2) * NB
                            for h2 in range(2):
                                bk, r_bk = pb()
                                S.group("tensor", [mk("matmul", out=bk[:], lhsT=wgr[sl][:, kc, :],
                                                      rhs=hT[:, kc, t0 + h2 * 512:t0 + (h2 + 1) * 512],
                                                      start=(kc == 0), stop=(kc == 7)) for kc in range(8)],
                                        reads=[r_wgr[sl]] + rh, writes=[r_bk])
                                S.op("scalar", mk("activation", out=gg[:, h2 * 512:(h2 + 1) * 512], in_=bk[:],
                                                  func=AF.Gelu_apprx_tanh), reads=[r_bk], writes=[r_gg])
                            S.op("gpsimd", mk("tensor_tensor", out=mixT[:, 4 + cch, o0:o0 + NB], in0=hs[:], in1=gg[:],
                                              op=ALU.mult), reads=[r_hs, r_gg],
                                 writes=r_bufA[4 + cch][(seg - 2) * 8:(seg - 1) * 8])
                S.barrier()
            with ExitStack() as st_at:
                wqkv = sbt(st_at, "wqkv", [128, 3, 8, 128], BF16); r_wqkv = Res()
                qTa = sbt(st_at, "qTa", [128, TOWN], BF16); r_qTa = Res()
                qTb = sbt(st_at, "qTb", [128, TOWN], BF16); r_qTb = Res()
                kT = sbt(st_at, "kT", [128, TALL], BF16); r_kT = Res()
                vT = sbt(st_at, "vT", [128, TALL], BF16); r_vT = Res()
                Vta = sbt(st_at, "Vta", [128, 32, 128], BF16); r_Vt = RL(8)
                Vtb = sbt(st_at, "Vtb", [128, 32, 128], BF16)
                accN = sbt(st_at, "accN", [128, TOWN], F32); r_accN = Res()
                accD = sbt(st_at, "accD", [128, TOWN], F32); r_accD = Res()
                PT = [sbt(st_at, "PT%d" % i, [128, 512], BF16) for i in range(2)]; r_PT = RL(2)
                pt_rr = 0
                S.op("gpsimd", mk("memset", ap=Vta[:], constant=0.0), writes=r_Vt)
                S.op("gpsimd", mk("memset", ap=Vtb[:], constant=0.0), writes=r_Vt)
                S.op("gpsimd", mk("memset", ap=qTa[:], constant=0.0), writes=[r_qTa])
                S.op("gpsimd", mk("memset", ap=qTb[:], constant=0.0), writes=[r_qTb])
                for fc in range(4):
                    for i3 in range(3):
                        S.dma(wqkv[:, i3, :, :], wview(w_in, i3 * 512 + fc * 128, 128), writes=[r_wqkv], eng="gpsimd")
                    for tb in range(4):
                        bk, r_bk = pb()
                        c0 = TOWN + tb * 512
                        S.group("tensor", [mk("matmul", out=bk[:], lhsT=wqkv[:, 0, kc, :], rhs=hT[:, kc, c0:c0 + 512],
                                              start=(kc == 0), stop=(kc == 7)) for kc in range(8)],
                                reads=[r_wqkv] + r_hT[16 + tb * 4:16 + (tb + 1) * 4], writes=[r_bk])
                        S.op("scalar", mk("activation", out=qTa[0:64, tb * 512:(tb + 1) * 512], in_=bk[0:64, :],
                                          func=AF.Copy), reads=[r_bk], writes=[r_qTa])
                        S.op("vector", mk("tensor_copy", out=qTb[64:128, tb * 512:(tb + 1) * 512], in_=bk[64:128, :]),
                             reads=[r_bk], writes=[r_qTb])
                    for tb in range(8):
                        bk, r_bk = pb()
                        c0 = tb * 512
                        S.group("tensor", [mk("matmul", out=bk[:], lhsT=wqkv[:, 1, kc, :], rhs=hT[:, kc, c0:c0 + 512],
                                              start=(kc == 0), stop=(kc == 7)) for kc in range(8)],
                                reads=[r_wqkv] + r_hT[tb * 4:(tb + 1) * 4], writes=[r_bk])
                        evac(kT[:, c0:c0 + 512], bk[:], [r_bk], [r_kT])
                    for tb in range(8):
                        bk, r_bk = pb()
                        c0 = tb * 512
                        S.group("tensor", [mk("matmul", out=bk[:], lhsT=wqkv[:, 2, kc, :], rhs=hT[:, kc, c0:c0 + 512],
                                              start=(kc == 0), stop=(kc == 7)) for kc in range(8)],
                                reads=[r_wqkv] + r_hT[tb * 4:(tb + 1) * 4], writes=[r_bk])
                        evac(vT[:, c0:c0 + 512], bk[:], [r_bk], [r_vT])
                    for pat, d in enumerate((1, 4, 16)):
                        L = TALL // d
                        nj = L // 128
                        for g8 in range(4):
                            bk, r_bk = pb()
                            pv = bk[:].bitcast(BF16).rearrange("p (u c) -> p u c", u=8)
                            fns = []
                            for u in range(8):
                                ti = g8 * 8 + u
                                r_, j_ = divmod(ti, nj)
                                s0 = r_ + d * 128 * j_
                                fns.append(mk("transpose", out=pv[:, u, :], in_=vT[:, s0:s0 + d * 127 + 1:d], identity=ident[:]))
                            S.group("tensor", fns, reads=[r_vT, r_ident], writes=[r_bk])
                            S.op("scalar", mk("activation", out=Vta[:, g8 * 8:(g8 + 1) * 8, 0:64], in_=pv[:, :, 0:64],
                                              func=AF.Copy), reads=[r_bk], writes=[r_Vt[2 * g8], r_Vt[2 * g8 + 1]])
                            S.op("vector", mk("tensor_copy", out=Vtb[:, g8 * 8:(g8 + 1) * 8, 64:128], in_=pv[:, :, 64:128]),
                                 reads=[r_bk], writes=[r_Vt[2 * g8], r_Vt[2 * g8 + 1]])
                        for su in range(4):
                            bkN, r_bkN = pb()
                            bkD, r_bkD = pb()
                            for u in range(4):
                                if d == 1:
                                    r_, jq = 0, 16 + 4 * su + u
                                elif d == 4:
                                    r_, jq = u, 4 + su
                                else:
                                    r_, jq = 4 * su + u, 1
                                q0 = r_ + d * 128 * jq - TOWN
                                kp0 = r_ + d * 128 * (jq - 1)
                                kc0 = r_ + d * 128 * jq
                                tip = r_ * nj + jq - 1
                                tic = r_ * nj + jq
                                boundary = (jq == nj // 2)
                                span = d * 127 + 1
                                bkS, r_bkS = pb()
                                msk = maskB if boundary else negmask
                                fns = [mk("matmul", out=bkS[:], lhsT=ident[:], rhs=msk[:], start=True, stop=False)]
                                for bi, (qq, k0) in enumerate(((qTa, kp0), (qTa, kc0), (qTb, kp0), (qTb, kc0))):
                                    fns.append(mk("matmul", out=bkS[:, bi * 128:(bi + 1) * 128],
                                                  lhsT=kT[:, k0:k0 + span:d], rhs=qq[:, q0:q0 + span:d],
                                                  start=False, stop=(bi == 3)))
                                S.group("tensor", fns, reads=[r_ident, r_negmask, r_maskB, r_kT, r_qTa, r_qTb],
                                        writes=[r_bkS])
                                pt, r_pt = PT[pt_rr], r_PT[pt_rr]
                                pt_rr ^= 1
                                S.op("scalar", mk("activation", out=pt[:], in_=bkS[:], func=AF.Exp, scale=0.125),
                                     reads=[r_bkS], writes=[r_pt])
                                oc = slice(u * 128, (u + 1) * 128)
                                fnsN = [
                                    mk("matmul", out=bkN[:, oc], lhsT=Vta[:, tip, :], rhs=pt[:, 0:128], start=True, stop=False),
                                    mk("matmul", out=bkN[:, oc], lhsT=Vta[:, tic, :], rhs=pt[:, 128:256], start=False, stop=False),
                                    mk("matmul", out=bkN[:, oc], lhsT=Vtb[:, tip, :], rhs=pt[:, 256:384], start=False, stop=False),
                                    mk("matmul", out=bkN[:, oc], lhsT=Vtb[:, tic, :], rhs=pt[:, 384:512], start=False, stop=True),
                                ]
                                S.group("tensor", fnsN, reads=[r_pt, r_Vt[tip // 4], r_Vt[tic // 4]], writes=[r_bkN])
                                fnsD = [
                                    mk("matmul", out=bkD[:, oc], lhsT=ones_a[:], rhs=pt[:, 0:128], start=True, stop=False),
                                    mk("matmul", out=bkD[:, oc], lhsT=ones_a[:], rhs=pt[:, 128:256], start=False, stop=False),
                                    mk("matmul", out=bkD[:, oc], lhsT=ones_b[:], rhs=pt[:, 256:384], start=False, stop=False),
                                    mk("matmul", out=bkD[:, oc], lhsT=ones_b[:], rhs=pt[:, 384:512], start=False, stop=True),
                                ]
                                S.group("tensor", fnsD, reads=[r_pt, r_ones], writes=[r_bkD])
                            for acc, r_acc, bkX, r_bkX, eng in ((accN, r_accN, bkN, r_bkN, "vector"),
                                                               (accD, r_accD, bkD, r_bkD, "gpsimd")):
                                src = bkX[:].rearrange("p (u i) -> p u i", u=4)
                                if d == 1:
                                    dst = acc[:, su * 512:(su + 1) * 512].rearrange("p (u i) -> p u i", u=4)
                                elif d == 4:
                                    dst = acc[:, su * 512:(su + 1) * 512].rearrange("p (i r) -> p r i", r=4)
                                else:
                                    dst = acc[:].rearrange("p (i r) -> p r i", r=16)[:, 4 * su:4 * su + 4, :]
                                if d == 1:
                                    S.op("vector" if eng == "vector" else "scalar",
                                         mk("tensor_copy", out=dst, in_=src) if eng == "vector" else
                                         mk("activation", out=dst, in_=src, func=AF.Copy),
                                         reads=[r_bkX], writes=[r_acc])
                                else:
                                    S.op("vector", mk("tensor_tensor", out=dst, in0=dst, in1=src, op=ALU.add),
                                         reads=[r_bkX], writes=[r_acc])
                    S.op("vector", mk("reciprocal", out=accD[:], in_=accD[:]), reads=[r_accD], writes=[r_accD])
                    S.op("vector", mk("tensor_tensor", out=mixT[:, fc, :], in0=accN[:], in1=accD[:], op=ALU.mult),
                         reads=[r_accN, r_accD], writes=r_bufA[fc])
                S.barrier()
            if debug:
                with ExitStack() as st_d:
                    dtmp = sbt(st_d, "dtmp2", [128, 8 * 512], F32); r_dtmp = Res()
                    for q4 in range(4):
                        S.op("vector", mk("tensor_copy", out=dtmp[:].rearrange("p (k t) -> p k t", k=8),
                                          in_=mixT[:, :, q4 * 512:(q4 + 1) * 512]), reads=[], writes=[r_dtmp])
                        S.dma(dbg["d_mixT"].rearrange("p (k t) -> p k t", k=8)[:, :, q4 * 512:(q4 + 1) * 512],
                              dtmp[:].rearrange("p (k t) -> p k t", k=8), reads=[r_dtmp])
                    S.barrier()
            with ExitStack() as st_o:
                wout = sbt(st_o, "wout", [128, 8, D], BF16); r_wout = Res()
                S.dma(wout[:, 0:4, :], w_out[0:512, :].rearrange("(kc p) n -> p kc n", p=128), writes=[r_wout], eng="gpsimd")
                S.dma(wout[:, 4:8, :], w_out[512:1024, :].rearrange("(kc p) n -> p kc n", p=128), writes=[r_wout], eng="gpsimd")
                for t in range(16):
                    xs, r_xs = xb[t % 2], r_xb[t % 2]
                    S.dma(xs[:], x_all[TOWN + t * 128:TOWN + (t + 1) * 128, :], writes=[r_xs])
                    for cc in range(2):
                        bk, r_bk = pb()
                        cs = slice(cc * 512, (cc + 1) * 512)
                        S.group("tensor", [mk("matmul", out=bk[:], lhsT=mixT[:, kc, t * 128:(t + 1) * 128],
                                              rhs=wout[:, kc, cs], start=(kc == 0), stop=(kc == 7)) for kc in range(8)],
                                reads=[r_wout] + [r_bufA[kc][t] for kc in range(8)], writes=[r_bk])
                        S.op("vector", mk("tensor_tensor", out=tmpf[:, cs], in0=bk[:], in1=mv2[:, cs], op=ALU.mult),
                             reads=[r_bk, r_mv2], writes=[r_tmpf])
                        S.op("gpsimd", mk("tensor_tensor", out=x1[:, t, cs], in0=tmpf[:, cs], in1=xs[:, cs], op=ALU.add),
                             reads=[r_tmpf, r_xs], writes=[r_x1[t]])
                S.barrier()
        if debug:
            for t in range(16):
                S.dma(dbg["d_x1"][t * 128:(t + 1) * 128, :], x1[:, t, :], reads=[r_x1[t]])
        I32 = mybir.dt.int32
        CAP = TOWN
        x_buf = nc.dram_tensor("x_buf", [NE * CAP, D], BF16, kind="Internal").ap()
        y_buf = nc.dram_tensor("y_buf", [NE * CAP, D], F32, kind="Internal").ap()
        with ExitStack() as st_f:
            wgus = [bufA, sbt(st_f, "wgu1", [128, 8, 2 * D], BF16)]; r_wgus = RL(2)
            wgus[0] = bufA[:].rearrange("p k t -> p (k t)").rearrange("p (k t) -> p k t", k=8)
            wd = sbt(st_f, "wd", [128, 8, D], BF16); r_wd = Res()
            gk = sbt(st_f, "gk", [128, 16, 4], F32); r_gk = RL(16)
            desti = sbt(st_f, "desti", [128, 16, 4], I32); r_desti = RL(16)
            cnti = sbt(st_f, "cnti", [128, NE], I32); r_cnti = Res()

            bgrow = [sbt(st_f, "bgrow%d" % i, [2, 2 * D], BF16) for i in range(2)]; r_bgrow = RL(2)
            ones_r = sbt(st_f, "ones_r", [2, 128], BF16); r_ones_r = Res()
            S.op("gpsimd", mk("memset", ap=ones_r[:], constant=1.0), writes=[r_ones_r])
            for i in range(2):
                S.op("gpsimd", mk("memset", ap=bgrow[i][:, 0:D], constant=0.0), writes=[r_bgrow[i]])
                S.op("gpsimd", mk("memset", ap=bgrow[i][:, D:2 * D], constant=1.0), writes=[r_bgrow[i]])

            def load_wgu(e):
                w_, r_w = wgus[e % 2], r_wgus[e % 2]
                for q4 in range(4):
                    S.dma(w_[:, :, q4 * 512:(q4 + 1) * 512], wview(w_gu[e], q4 * 512, 512), writes=[r_w], eng="gpsimd")
                S.dma(bgrow[e % 2][0:1, :], b_gu_d[e:e + 1, :], writes=[r_bgrow[e % 2]], eng="gpsimd")

            def load_wd(e):
                S.dma(wd[:, 0:4, :], w_dn[e][0:512, :].rearrange("(kc p) n -> p kc n", p=128), writes=[r_wd], eng="gpsimd")
                S.dma(wd[:, 4:8, :], w_dn[e][512:1024, :].rearrange("(kc p) n -> p kc n", p=128), writes=[r_wd], eng="gpsimd")

            load_wgu(0)
            load_wd(0)
            with ExitStack() as st_r2:
                wr = sbt(st_r2, "wr", [128, 8, NE], BF16); r_wr = Res()
                brt = sbt(st_r2, "brt", [128, NE], F32); r_brt = Res()
                lg = sbt(st_r2, "lg", [128, NE], F32); r_lg = Res()
                ex = sbt(st_r2, "ex", [128, NE], F32); r_ex = Res()
                gt = sbt(st_r2, "gt", [128, NE], F32); r_gt = Res()
                posb = sbt(st_r2, "posb", [128, NE], F32); r_posb = Res()
                scr4 = sbt(st_r2, "scr4", [128, 4, NE], F32); r_scr = Res()
                ebase = sbt(st_r2, "ebase", [128, NE], F32); r_ebase = Res()
                m8 = sbt(st_r2, "m8", [128, 8], F32); r_m8 = Res()
                e4 = sbt(st_r2, "e4", [128, 4], F32); r_e4 = Res()
                destf = sbt(st_r2, "destf", [128, 4], F32); r_destf = Res()
                nmx = sbt(st_r2, "nmx", [128, 1], F32); r_nmx = Res()
                sm = sbt(st_r2, "sm", [128, 1], F32); r_sm = Res()
                gT = sbt(st_r2, "gT", [32, 128], F32); r_gT = Res()
                bdn = sbt(st_r2, "bdn", [32, D], F32); r_bdn = Res()
                maskall = sbt(st_r2, "maskall", [128, 16, NE], BF16); r_mask = RL(16)
                ltri = sbt(st_r2, "ltri", [128, 128], BF16); r_ltri = Res()
                ones_f = sbt(st_r2, "ones_full", [128, 128], BF16); r_onesf = Res()
                h2Tt = sbt(st_r2, "h2Tt", [128, 8, 128], BF16); r_h2Tt = Res()
                S.dma(wr[:], w_router.rearrange("(kc p) n -> p kc n", p=128), writes=[r_wr], eng="gpsimd")
                S.dma(brt[:], b_router.partition_broadcast(128), writes=[r_brt])
                S.dma(bdn[:], b_dn, writes=[r_bdn])
                S.op("gpsimd", mk("iota", out=ebase[:], pattern=[[CAP, NE]], base=0, channel_multiplier=0,
                                  allow_small_or_imprecise_dtypes=True), writes=[r_ebase])
                S.op("gpsimd", mk("memset", ap=ones_f[:], constant=1.0), writes=[r_onesf])
                S.op("gpsimd", mk("memset", ap=ltri[:], constant=1.0), writes=[r_ltri])
                S.op("gpsimd", mk("affine_select", out=ltri[:], in_=ltri[:], pattern=[[1, 128]], compare_op=ALU.is_gt,
                                  fill=0.0, base=0, channel_multiplier=-1), reads=[r_ltri], writes=[r_ltri])
                for t in range(16):
                    hbt, r_hbt = hb[t % 2], r_hb[t % 2]
                    rms_tile(x1[:, t, :], r_x1[t], 32 + t, mv4[:], r_mv4, mv3[:], r_mv3, hbt[:], r_hbt)
                    transpose_tile(hbt, r_hbt, h2Tt[:], [r_h2Tt])
                    bk, r_bk = pb()
                    S.group("tensor", [mk("matmul", out=bk[:, 0:NE], lhsT=h2Tt[:, kc, :], rhs=wr[:, kc, :],
                                          start=(kc == 0), stop=(kc == 7)) for kc in range(8)],
                            reads=[r_wr, r_h2Tt], writes=[r_bk])
                    S.op("vector", mk("tensor_tensor", out=lg[:], in0=bk[:, 0:NE], in1=brt[:], op=ALU.add),
                         reads=[r_bk, r_brt], writes=[r_lg])
                    S.op("vector", mk("max", out=m8[:], in_=lg[:]), reads=[r_lg], writes=[r_m8])
                    S.op("vector", mk("tensor_scalar", out=nmx[:], in0=m8[:, 0:1], scalar1=-1.0, scalar2=None, op0=ALU.mult),
                         reads=[r_m8], writes=[r_nmx])
                    S.op("scalar", mk("activation", out=ex[:], in_=lg[:], func=AF.Exp, bias=nmx[:, 0:1], scale=1.0),
                         reads=[r_lg, r_nmx], writes=[r_ex])
                    S.op("scalar", mk("activation", out=e4[:], in_=m8[:, 0:4], func=AF.Exp, bias=nmx[:, 0:1], scale=1.0),
                         reads=[r_m8, r_nmx], writes=[r_e4])
                    S.op("vector", mk("tensor_scalar", out=maskall[:, t, :], in0=lg[:], scalar1=m8[:, 3:4], scalar2=None,
                                      op0=ALU.is_ge), reads=[r_lg, r_m8], writes=[r_mask[t]])
                    S.op("vector", mk("tensor_tensor", out=ex[:], in0=ex[:], in1=maskall[:, t, :], op=ALU.mult),
                         reads=[r_mask[t]], writes=[r_ex])
                    S.op("vector", mk("tensor_reduce", out=sm[:], in_=ex[:], axis=mybir.AxisListType.X, op=ALU.add),
                         reads=[r_ex], writes=[r_sm])
                    S.op("vector", mk("reciprocal", out=sm[:], in_=sm[:]), reads=[r_sm], writes=[r_sm])
                    S.op("vector", mk("tensor_scalar", out=gt[:], in0=ex[:], scalar1=sm[:, 0:1], scalar2=None,
                                      op0=ALU.mult), reads=[r_ex, r_sm], writes=[r_gt])
                    S.op("vector", mk("tensor_scalar", out=gk[:, t, :], in0=e4[:], scalar1=sm[:, 0:1], scalar2=None,
                                      op0=ALU.mult), reads=[r_e4, r_sm], writes=[r_gk[t]])
                    bkp, r_bkp = pb()
                    fns = [mk("matmul", out=bkp[:, 0:NE], lhsT=ones_f[:], rhs=maskall[:, tp, :], start=(tp == 0), stop=False)
                           for tp in range(t)]
                    fns.append(mk("matmul", out=bkp[:, 0:NE], lhsT=ltri[:], rhs=maskall[:, t, :], start=(t == 0), stop=True))
                    S.group("tensor", fns, reads=[r_onesf, r_ltri] + r_mask[:t + 1], writes=[r_bkp])
                    S.op("vector", mk("tensor_tensor", out=posb[:], in0=bkp[:, 0:NE], in1=ebase[:], op=ALU.add),
                         reads=[r_bkp, r_ebase], writes=[r_posb])
                    for k in range(4):
                        S.op("vector", mk("scalar_tensor_tensor", out=scr4[:, k, :], in0=lg[:], scalar=m8[:, k:k + 1], in1=posb[:],
                                          op0=ALU.is_equal, op1=ALU.mult),
                             reads=[r_lg, r_m8, r_posb], writes=[r_scr])
                    S.op("vector", mk("tensor_reduce", out=destf[:], in_=scr4[:], axis=mybir.AxisListType.X, op=ALU.add),
                         reads=[r_scr], writes=[r_destf])
                    S.op("vector", mk("tensor_scalar", out=destf[:], in0=destf[:], scalar1=0.0, scalar2=float(NE * CAP - 1),
                                      op0=ALU.max, op1=ALU.min), reads=[r_destf], writes=[r_destf])
                    S.op("vector", mk("tensor_copy", out=desti[:, t, :], in_=destf[:]), reads=[r_destf], writes=[r_desti[t]])
                    for k in range(4):
                        def sc(e, t=t, k=k, hbt=hbt):
                            return e.indirect_dma_start(out=x_buf[:, :],
                                                        out_offset=bass.IndirectOffsetOnAxis(ap=desti[:, t, k:k + 1], axis=0),
                                                        in_=hbt[:, :], in_offset=None)
                        S.dma_fn("gpsimd", sc, reads=[r_desti[t], r_hbt], writes=[])
                    bk2, r_bk2 = pb()
                    S.group("tensor", [mk("transpose", out=bk2[0:NE, 0:128], in_=gt[:], identity=identf[:])],
                            reads=[r_gt, r_identf], writes=[r_bk2])
                    S.op("vector", mk("tensor_copy", out=gT[:], in_=bk2[0:NE, 0:128]), reads=[r_bk2], writes=[r_gT])
                    for cc in range(2):
                        cs = slice(cc * 512, (cc + 1) * 512)
                        bk3, r_bk3 = pb()
                        S.group("tensor", [mk("matmul", out=bk3[:], lhsT=gT[:], rhs=bdn[:, cs], start=True, stop=True)],
                                reads=[r_gT, r_bdn], writes=[r_bk3])
                        S.op("vector", mk("tensor_tensor", out=tmpf[:, cs], in0=bk3[:], in1=mv5[:, cs], op=ALU.mult),
                             reads=[r_bk3, r_mv5], writes=[r_tmpf])
                        S.op("gpsimd", mk("tensor_tensor", out=x1[:, t, cs], in0=tmpf[:, cs], in1=x1[:, t, cs], op=ALU.add),
                             reads=[r_tmpf], writes=[r_x1[t]])
                bkc, r_bkc = pb()
                S.group("tensor", [mk("matmul", out=bkc[:, 0:NE], lhsT=ones_f[:], rhs=maskall[:, tp, :], start=(tp == 0),
                                      stop=(tp == 15)) for tp in range(16)], reads=[r_onesf] + r_mask, writes=[r_bkc])
                S.op("vector", mk("tensor_copy", out=cnti[:], in_=bkc[:, 0:NE]), reads=[r_bkc], writes=[r_cnti])
                if debug:
                    S.dma(dbg["d_gates"][:, 0:64], gk[:].rearrange("p t e -> p (t e)"), reads=r_gk)
                S.barrier()
            S.dma(mv3[:], g_final.partition_broadcast(128), writes=[r_mv3])
            with ExitStack() as st_m:
                xblk = [hb[0], hb[1]]; r_xblk = RL(2)
                xT1 = sbt(st_m, "xT1", [128, 8, 128], BF16)
                xTs = [junk[:].rearrange("p (k t) -> p k t", k=8), xT1[:]]; r_xT = RL(2)
                actT = [sbt(st_m, "actT%d" % i, [128, 8, 128], BF16) for i in range(2)]; r_act = [RL(2), RL(2)]
                yblk = [xb[0], xb[1]]; r_yblk = RL(2)
                sgx = sbt(st_m, "sgx", [128, D], F32)
                ucx = sbt(st_m, "ucx", [128, D], F32)
                gc2 = [[tmpf[:, 0:512], tmpf[:, 512:1024]], [sgx[:, 0:512], sgx[:, 512:1024]]]; r_gc2 = [RL(2), RL(2)]
                actk = [sbt(st_m, "actk%d" % i, [128, D], BF16) for i in range(2)]; r_actk = [RL(2), RL(2)]
                ucs = [[mv4[:, 0:512], mv4[:, 512:1024]], [ucx[:, 0:512], ucx[:, 512:1024]]]; r_uc = [RL(2), RL(2)]
                evac_dve_only[0] = True
                blk_rr = 0
                for e in range(n_experts if stage >= 2 else 0):
                    w_, r_w = wgus[e % 2], r_wgus[e % 2]
                    if e + 1 < n_experts:
                        load_wgu(e + 1)
                    S.load_count(cnti[0:1, e:e + 1], [r_cnti])
                    for jp in range(0, 16, 2):
                        S.begin_guard((e, jp), 128 * jp + 1)
                        pair = (jp, jp + 1)

                        NESTED = True

                        def inner(j, ph):
                            if j != jp and NESTED:
                                S.begin_inner((e, j, ph), 128 * j + 1)

                        def inner_end(j):
                            if j != jp and NESTED:
                                S.end_inner()
                        for j in pair:
                            s_ = j % 2
                            row0 = e * CAP + 128 * j
                            inner(j, 1)
                            S.dma(xblk[s_][:], x_buf[row0:row0 + 128, :], writes=[r_xblk[s_]])
                            transpose_tile(xblk[s_], r_xblk[s_], xTs[s_], [r_xT[s_]])
                            inner_end(j)
                        for j in pair:
                            s_ = j % 2
                            xT = xTs[s_]
                            inner(j, 2)
                            for half in range(2):
                                gub = []
                                for c0 in (half * 512, D + half * 512):
                                    bkX, r_bkX = pb()
                                    fns = [mk("matmul", out=bkX[:], lhsT=ones_r[0:2, :], rhs=bgrow[e % 2][0:2, c0:c0 + 512],
                                              start=True, stop=False)]
                                    for kc in range(8):
                                        fns.append(mk("matmul", out=bkX[:], lhsT=xT[:, kc, :], rhs=w_[:, kc, c0:c0 + 512],
                                                      start=False, stop=(kc == 7)))
                                    S.group("tensor", fns, reads=[r_w, r_xT[s_], r_ones_r, r_bgrow[e % 2]], writes=[r_bkX])
                                    gub.append((bkX, r_bkX))
                                (bkG, r_bkG), (bkU, r_bkU) = gub
                                uc, r_uc_ = ucs[s_][half], r_uc[s_][half]
                                gc, r_gc_ = gc2[s_][half], r_gc2[s_][half]
                                S.op("scalar", mk("activation", out=gc, in_=bkG[:], func=AF.Gelu_apprx_sigmoid),
                                     reads=[r_bkG], writes=[r_gc_])
                                S.op("vector", mk("tensor_scalar", out=uc, in0=bkU[:], scalar1=8.0, scalar2=-6.0,
                                                  op0=ALU.min, op1=ALU.max), reads=[r_bkU], writes=[r_uc_])
                                S.op("vector", mk("scalar_tensor_tensor", out=actk[s_][:, half * 512:(half + 1) * 512], in0=gc,
                                                  scalar=GLU7, in1=uc, op0=ALU.min, op1=ALU.mult),
                                     reads=[r_uc_, r_gc_], writes=[r_actk[s_][half]])
                            inner_end(j)
                        for j in pair:
                            s_ = j % 2
                            row0 = e * CAP + 128 * j
                            inner(j, 3)
                            for half in range(2):
                                transpose_tile(actk[s_], r_actk[s_][half], actT[s_][:, 4 * half:4 * half + 4, :],
                                               [r_act[s_][half]], k0=4 * half, k1=4 * half + 4)
                            for cc in range(2):
                                cs = slice(cc * 512, (cc + 1) * 512)
                                bk, r_bk = pb()
                                S.group("tensor", [mk("matmul", out=bk[:], lhsT=actT[s_][:, kc, :], rhs=wd[:, kc, cs],
                                                      start=(kc == 0), stop=(kc == 7)) for kc in range(8)],
                                        reads=[r_wd] + r_act[s_], writes=[r_bk])
                                S.op("vector", mk("tensor_tensor", out=yblk[s_][:, cs], in0=bk[:], in1=mv5[:, cs], op=ALU.mult),
                                     reads=[r_bk, r_mv5], writes=[r_yblk[s_]])
                            S.dma(y_buf[row0:row0 + 128, :], yblk[s_][:], reads=[r_yblk[s_]], eng="gpsimd")
                            inner_end(j)
                        S.end_guard()
                    if e + 1 < n_experts:
                        load_wd(e + 1)
                evac_dve_only[0] = False
                S.barrier()
            st_c = st_f.enter_context(ExitStack())
            ykb = [mv4, tmpf] + [sbt(st_c, "ykb%d" % i, [128, D], F32) for i in range(4)]; r_ykb = RL(6)
            gi = 0
            for t in range(16):
                for k in range(4 if stage >= 3 else 0):
                    yk, r_yk = ykb[gi % 6], r_ykb[gi % 6]
                    gi += 1

                    def ga(e, t=t, k=k, yk=yk):
                        return e.indirect_dma_start(out=yk[:, :], out_offset=None, in_=y_buf[:, :],
                                                    in_offset=bass.IndirectOffsetOnAxis(ap=desti[:, t, k:k + 1], axis=0))
                    S.dma_fn("gpsimd", ga, reads=[r_desti[t]], writes=[r_yk])
                    S.op("vector", mk("scalar_tensor_tensor", out=x1[:, t, :], in0=yk[:], scalar=gk[:, t, k:k + 1],
                                      in1=x1[:, t, :], op0=ALU.mult, op1=ALU.add),
                         reads=[r_yk, r_gk[t]], writes=[r_x1[t]])
                ob = xb[t % 2]; r_ob = r_xb[t % 2]
                rms_tile(x1[:, t, :], r_x1[t], 48 + t, mv3[:], r_mv3, None, None, ob[:], r_ob)
                S.dma(out_d[t * 128:(t + 1) * 128, :], ob[:], reads=[r_ob])
            S.barrier()
        with nc.Block() as block:
            S.emit(block)
    return nc


_NC_CACHE = {}


def make_in_maps(inputs, cores, n_experts=NE):
    f = lambda a: np.ascontiguousarray(np.asarray(a, dtype=np.float32))
    x = f(inputs["x"]); c = f(inputs["c"])
    w_ada = f(inputs["w_ada"])[0]; b_ada = f(inputs["b_ada"])[0][None, :]
    g_mix = f(inputs["g_mix"])[0][None, :]; g_ffn = f(inputs["g_ffn"])[0][None, :]
    g_final = f(inputs["g_final"])[None, :]
    w_in = f(inputs["w_in"])[0]; w_out = f(inputs["w_out"])[0]
    conv_w = f(inputs["conv_w"])[0]; conv_b = f(inputs["conv_b"])[0]
    b_a = f(inputs["b_rg_a"])[0]; b_x = f(inputs["b_rg_x"])[0]; lam = f(inputs["lam"])[0]
    recp = np.zeros((128, 4, 8), np.float32)
    cols = [conv_w[0], conv_w[1], conv_w[2], conv_w[3], conv_b, b_a, b_x, lam]
    for j, v in enumerate(cols):
        recp[:, :, j] = v.reshape(4, 128).T
    def blockdiag(w):
        w = f(w)[0]
        o = np.zeros((128, 4, 128), np.float32)
        for blk in range(8):
            cch, hh = divmod(blk, 2)
            o[hh * 64:(hh + 1) * 64, cch, hh * 64:(hh + 1) * 64] = w[blk]
        return o
    wa_bd = blockdiag(inputs["w_rg_a"]); wx_bd = blockdiag(inputs["w_rg_x"])
    w_router = f(inputs["w_router"])[0]; b_router = f(inputs["b_router"])[0][None, :]
    w_gu = f(inputs["w_gate_up"])[0][:n_experts]; b_gu = f(inputs["b_gate_up"])[0]
    bgu_t = np.ascontiguousarray(b_gu.reshape(NE, 16, 128).transpose(2, 0, 1))
    w_dn = f(inputs["w_down"])[0][:n_experts]; b_dn = f(inputs["b_down"])[0]
    maps = []
    for core in cores:
        b, half = divmod(core, 2)
        x_all = np.zeros((TALL, D), np.float32)
        if half == 0:
            x_all[TOWN:] = x[b, :TOWN]
        else:
            x_all[:] = x[b]
        c_rep = np.ascontiguousarray(np.broadcast_to(c[b].reshape(8, 128).T[:, :, None], (128, 8, 128)))
        flag = np.full((128, 1), float(half), np.float32)
        maps.append({
            "x_all": x_all, "c_rep": c_rep, "flag": flag, "w_ada": w_ada, "b_ada": b_ada, "g_mix": g_mix,
            "g_ffn": g_ffn, "g_final": g_final, "w_in": w_in, "w_out": w_out, "recp": recp, "wa_bd": wa_bd,
            "wx_bd": wx_bd, "w_router": w_router, "b_router": b_router, "w_gate_up": w_gu, "bgu_t": bgu_t, "b_gu": b_gu,
            "w_down": w_dn, "b_down": b_dn,
        })
    return maps


def kernel(**inputs):
    if "nc" not in _NC_CACHE:
        _NC_CACHE["nc"] = build()
    nc = _NC_CACHE["nc"]
    cores = list(range(8))
    in_maps = make_in_maps(inputs, cores)
    res = run_bass_kernel_spmd(nc, in_maps, core_ids=cores)
    out = np.zeros((4, 4096, D), np.float32)
    for core in cores:
        b, half = divmod(core, 2)
        out[b, half * TOWN:(half + 1) * TOWN] = res.results[core]["out"]
    return out
```

```python
import numpy as np
from contextlib import ExitStack
import concourse.bass as bass
import concourse.mybir as mybir
from concourse.bass_utils import run_bass_kernel_spmd

F32 = mybir.dt.float32
BF16 = mybir.dt.bfloat16
AF = mybir.ActivationFunctionType
ALU = mybir.AluOpType

D = 1024
TOWN = 2048
TALL = 4096
NE = 32
EPS = 1e-6
GLU7 = float(np.float32(7.0) / (np.float32(1.0) + np.exp(np.float32(-1.702 * 7.0))))
SEM_LIMIT = 30000


class Res:
    __slots__ = ("w", "r")

    def __init__(self):
        self.w = None
        self.r = {}


def RL(n):
    return [Res() for _ in range(n)]


class Sched:
    ENGS = ("tensor", "vector", "scalar", "gpsimd", "sync")

    def __init__(self, nc, stack, n_dma_sems=8):
        self.nc = nc
        self.stack = stack
        self.ops = {e: [] for e in self.ENGS}
        self.known = {e: {} for e in self.ENGS}
        self.dom_sem = {}
        self.dom_max = {}
        self.ndom = 0
        self.cur_dom = {}
        self.cur_cnt = {}
        self.guard = None
        self.guard_snap = {}
        self.regs = {}
        for e in self.ENGS:
            self.cur_dom[e] = self._new_dom(e)
            self.cur_cnt[e] = 0
        self.dma_pool = {}
        for q in ("sync", "gpsimd"):
            self.dma_pool[q] = {"doms": [self._new_dom("dma_%s%d" % (q, i)) for i in range(n_dma_sems)],
                                "cnt": [0] * n_dma_sems, "rr": 0}

    def _new_dom(self, name):
        d = self.ndom
        self.ndom += 1
        self.dom_sem[d] = self.stack.enter_context(self.nc.semaphore("s_%s_%d" % (name, d)))
        self.dom_max[d] = 0
        return d

    def _collect(self, eng, reads, writes):
        need = {}
        for R in reads:
            if R.w is not None:
                d, v = R.w
                if need.get(d, 0) < v:
                    need[d] = v
        for R in writes:
            if R.w is not None:
                d, v = R.w
                if need.get(d, 0) < v:
                    need[d] = v
            for d, v in R.r.items():
                if need.get(d, 0) < v:
                    need[d] = v
        kn = self.known[eng]
        waits = []
        for d, v in need.items():
            if eng == "tensor" and d == self.cur_dom["tensor"]:
                continue
            if kn.get(d, 0) < v:
                kn[d] = v
                waits.append((d, v))
        return waits

    def _tick(self, eng):
        self.cur_cnt[eng] += 1
        d = self.cur_dom[eng]
        v = self.cur_cnt[eng]
        self.dom_max[d] = v
        return d, v

    def _mark(self, d, v, reads, writes):
        for R in reads:
            R.r[d] = v
        for R in writes:
            R.w = (d, v)
            R.r = {}

    def op(self, eng, fn, reads=(), writes=()):
        waits = self._collect(eng, reads, writes)
        d, v = self._tick(eng)
        self.ops[eng].append((waits, fn, self.dom_sem[d], 1, self.guard))
        self._mark(d, v, reads, writes)

    def group(self, eng, fns, reads=(), writes=()):
        waits = self._collect(eng, reads, writes)
        d, v = self._tick(eng)
        n = len(fns)
        for i, fn in enumerate(fns):
            self.ops[eng].append((waits if i == 0 else [], fn,
                                  self.dom_sem[d] if i == n - 1 else None, 1, self.guard))
        self._mark(d, v, reads, writes)

    def dma_fn(self, eng, fn, reads=(), writes=()):
        pool = self.dma_pool[eng]
        i = pool["rr"]
        pool["rr"] = (i + 1) % len(pool["doms"])
        d = pool["doms"][i]
        waits = self._collect(eng, reads, writes)
        prev = pool["cnt"][i]
        kn = self.known[eng]
        if prev > 0 and kn.get(d, 0) < prev:
            kn[d] = prev
            waits.append((d, prev))
        pool["cnt"][i] += 16
        v = pool["cnt"][i]
        self.dom_max[d] = v
        self.ops[eng].append((waits, fn, self.dom_sem[d], 16, self.guard))
        self._mark(d, v, reads, writes)

    def dma(self, out, in_, reads=(), writes=(), eng="sync"):
        def fn(e, out=out, in_=in_):
            return e.dma_start(out=out, in_=in_)
        self.dma_fn(eng, fn, reads, writes)

    def load_count(self, ap, reads):
        for eng in self.ENGS:
            waits = self._collect(eng, reads, ())
            sched = self

            def fn(e, eng=eng, ap=ap):
                return e.reg_load(sched.regs[eng], ap)
            self.ops[eng].append((waits, fn, None, 0, None))
        for R in reads:
            for eng in self.ENGS:
                pass

    def begin_guard(self, gid, thr):
        self.guard = (gid, thr, None, 0)
        self.guard_snap[gid] = dict(self.dom_max)

    def begin_inner(self, gid, thr):
        g = self.guard
        self.guard = (g[0], g[1], gid, thr)
        self.guard_snap[gid] = dict(self.dom_max)

    def end_inner(self):
        g = self.guard
        self.guard = (g[0], g[1], None, 0)

    def end_guard(self):
        self.guard = None

    def barrier(self):
        assert self.guard is None
        for eng in self.ENGS:
            kn = self.known[eng]
            waits = []
            for d, v in self.dom_max.items():
                if v > 0 and kn.get(d, 0) < v:
                    kn[d] = v
                    waits.append((d, v))
            if waits:
                self.ops[eng].append((waits, None, None, 0, None))

    def emit(self, block):
        sched = self

        def emit_one(e, item):
            waits, fn, sem, inc, _ = item
            for (d_, v) in waits:
                e.wait_ge(sched.dom_sem[d_], v)
            if fn is None:
                return
            ins = fn(e)
            if sem is not None:
                ins.then_inc(sem, inc)

        def skip_path(e, grp, snap):
            incs = []
            need = {}
            for waits, fn, sem, inc, _ in grp:
                for (d_, v) in waits:
                    v = min(v, snap.get(d_, 0))
                    if v > need.get(d_, 0):
                        need[d_] = v
            for d_, v in need.items():
                e.wait_ge(sched.dom_sem[d_], v)
            for waits, fn, sem, inc, _ in grp:
                if sem is not None:
                    for k_ in range(len(incs)):
                        if incs[k_][0] is sem:
                            incs[k_][1] += inc
                            break
                    else:
                        incs.append([sem, inc])
            e.drain()
            for sem, tot in incs:
                e.sem_inc(sem, tot)

        def emit_region(e, reg, grp):
            a = 0
            m = len(grp)
            while a < m:
                gi = grp[a][4]
                if gi[2] is None:
                    emit_one(e, grp[a])
                    a += 1
                    continue
                b = a
                while b < m and grp[b][4][2] == gi[2]:
                    b += 1
                sub = grp[a:b]
                with e.If_lt(reg, gi[3]):
                    skip_path(e, sub, sched.guard_snap[gi[2]])
                with e.Else():
                    for it in sub:
                        emit_one(e, it)
                a = b

        def emit_chain(e, reg, regions, idx):
            if idx == len(regions):
                return
            grp = regions[idx]
            g = grp[0][4]
            rest = [it for r_ in regions[idx:] for it in r_]
            with e.If_lt(reg, g[1]):
                skip_path(e, rest, sched.guard_snap[g[0]])
            with e.Else():
                emit_region(e, reg, grp)
                emit_chain(e, reg, regions, idx + 1)

        def make(engname):
            def body(e):
                ops = sched.ops[engname]
                sched.regs[engname] = e.alloc_register("cnt_" + engname)
                reg = sched.regs[engname]
                i = 0
                n = len(ops)
                while i < n:
                    g = ops[i][4]
                    if g is None:
                        emit_one(e, ops[i])
                        i += 1
                        continue
                    regions = []
                    j = i
                    while j < n and ops[j][4] is not None and ops[j][4][0][0] == g[0][0]:
                        k = j
                        while k < n and ops[k][4] is not None and ops[k][4][0] == ops[j][4][0]:
                            k += 1
                        regions.append(ops[j:k])
                        j = k
                    emit_chain(e, reg, regions, 0)
                    i = j
            return body
        for engname in self.ENGS:
            if self.ops[engname]:
                getattr(block, engname)(make(engname))


def mk(method, **kw):
    return lambda e: getattr(e, method)(**kw)


def build(debug=False, n_experts=NE, stage=3):
    nc = bass.Bass("TRN2", target_bir_lowering=False)

    def din(name, shape):
        return nc.dram_tensor(name, shape, F32, kind="ExternalInput").ap()

    x_all = din("x_all", [TALL, D])
    c_rep = din("c_rep", [128, 8, 128])
    flag_d = din("flag", [128, 1])
    w_ada = din("w_ada", [D, 6 * D])
    b_ada = din("b_ada", [1, 6 * D])
    g_mix = din("g_mix", [1, D])
    g_ffn = din("g_ffn", [1, D])
    g_final = din("g_final", [1, D])
    w_in = din("w_in", [D, 2560])
    w_out = din("w_out", [D, D])
    recp = din("recp", [128, 4, 8])
    wa_bd = din("wa_bd", [128, 4, 128])
    wx_bd = din("wx_bd", [128, 4, 128])
    w_router = din("w_router", [D, NE])
    b_router = din("b_router", [1, NE])
    w_gu = din("w_gate_up", [n_experts, D, 2 * D])
    bgu_t = din("bgu_t", [128, NE, 16])
    b_gu_d = din("b_gu", [NE, 2 * D])
    w_dn = din("w_down", [n_experts, D, D])
    b_dn = din("b_down", [NE, D])
    out_d = nc.dram_tensor("out", [TOWN, D], F32, kind="ExternalOutput").ap()
    dbg = {}
    if debug:
        def dout(name, shape):
            dbg[name] = nc.dram_tensor(name, shape, F32, kind="ExternalOutput").ap()
        dout("d_mod", [128, 6 * D])
        dout("d_hT", [128, 8 * 512])
        dout("d_mixT", [128, 8 * TOWN])
        dout("d_x1", [TOWN, D])
        dout("d_gates", [128, 16 * NE])

    with ExitStack() as st:
        S = Sched(nc, st)

        def sbt(stack, name, shape, dt):
            return stack.enter_context(nc.sbuf_tensor(name, shape, dt))

        arena = sbt(st, "arena", [128, 16384], F32)
        hT = arena[:].bitcast(BF16).rearrange("p (k t) -> p k t", k=8)
        x1 = arena[:].rearrange("p (t f) -> p t f", t=16)
        r_hT = RL(32)
        r_x1 = RL(16)
        bufA = sbt(st, "bufA", [128, 8, TOWN], BF16)
        r_bufA = [RL(16) for _ in range(8)]
        mv3 = sbt(st, "mv3", [128, D], F32); r_mv3 = Res()
        mv4 = sbt(st, "mv4", [128, D], F32); r_mv4 = Res()
        mv5 = sbt(st, "mv5", [128, D], F32); r_mv5 = Res()
        xb = [sbt(st, "xb%d" % i, [128, D], F32) for i in range(2)]; r_xb = RL(2)
        tmpf = sbt(st, "tmpf", [128, D], F32); r_tmpf = Res()
        hb = [sbt(st, "hb%d" % i, [128, D], BF16) for i in range(2)]; r_hb = RL(2)
        junk = sbt(st, "junk", [128, D], BF16)
        ss = sbt(st, "ss", [128, 64], F32); r_ss = RL(64)
        ms = sbt(st, "ms", [128, 64], F32); r_ms = RL(64)
        rstd = sbt(st, "rstd", [128, 64], F32); r_rstd = RL(64)
        ident = sbt(st, "ident", [128, 128], BF16); r_ident = Res()
        identf = sbt(st, "identf", [128, 128], F32); r_identf = Res()
        neghalf = sbt(st, "neghalf", [128, 1], F32); r_nh = Res()
        flag = sbt(st, "flag_sb", [128, 1], F32); r_flag = Res()
        flagb = sbt(st, "flagb", [128, 1], F32); r_flagb = Res()
        negmask = sbt(st, "negmask", [128, 512], BF16); r_negmask = Res()
        maskB = sbt(st, "maskB", [128, 512], BF16); r_maskB = Res()
        ones_a = sbt(st, "ones_a", [128, 128], BF16); r_ones = Res()
        ones_b = sbt(st, "ones_b", [128, 128], BF16)

        banks = [st.enter_context(nc.psum_tensor("bank%d" % i, [128, 512], F32)) for i in range(8)]
        r_bank = RL(8)
        bank_rr = [0]

        def pb():
            i = bank_rr[0]
            bank_rr[0] = (i + 1) % 8
            return banks[i], r_bank[i]

        cp_rr = [0]
        evac_dve_only = [False]

        def evac(out, in_, reads, writes):
            cp_rr[0] ^= 1
            if cp_rr[0] and not evac_dve_only[0]:
                S.op("scalar", mk("activation", out=out, in_=in_, func=AF.Copy), reads=reads, writes=writes)
            else:
                S.op("vector", mk("tensor_copy", out=out, in_=in_), reads=reads, writes=writes)

        S.dma(flag[:], flag_d, writes=[r_flag])
        S.op("gpsimd", mk("memset", ap=neghalf[:], constant=-0.5), writes=[r_nh])
        S.op("gpsimd", mk("memset", ap=ident[:], constant=1.0), writes=[r_ident])
        S.op("gpsimd", mk("affine_select", out=ident[:], in_=ident[:], pattern=[[-1, 128]],
                          compare_op=ALU.is_equal, fill=0.0, base=0, channel_multiplier=1),
             reads=[r_ident], writes=[r_ident])
        S.op("gpsimd", mk("memset", ap=identf[:], constant=1.0), writes=[r_identf])
        S.op("gpsimd", mk("affine_select", out=identf[:], in_=identf[:], pattern=[[-1, 128]],
                          compare_op=ALU.is_equal, fill=0.0, base=0, channel_multiplier=1),
             reads=[r_identf], writes=[r_identf])
        S.op("gpsimd", mk("memset", ap=negmask[:], constant=0.0), writes=[r_negmask])
        for blk in range(4):
            sl = negmask[:, blk * 128:(blk + 1) * 128]
            if blk % 2 == 0:
                S.op("gpsimd", mk("affine_select", out=sl, in_=sl, pattern=[[-1, 128]], compare_op=ALU.is_ge,
                                  fill=-30000.0, base=0, channel_multiplier=1), reads=[r_negmask], writes=[r_negmask])
            else:
                S.op("gpsimd", mk("affine_select", out=sl, in_=sl, pattern=[[1, 128]], compare_op=ALU.is_ge,
                                  fill=-30000.0, base=0, channel_multiplier=-1), reads=[r_negmask], writes=[r_negmask])
        S.op("vector", mk("tensor_scalar", out=flagb[:], in0=flag[:], scalar1=-1.0, scalar2=30000.0,
                          op0=ALU.add, op1=ALU.mult), reads=[r_flag], writes=[r_flagb])
        S.op("vector", mk("tensor_copy", out=maskB[:], in_=negmask[:]), reads=[r_negmask], writes=[r_maskB])
        for blk in (0, 2):
            sl = maskB[:, blk * 128:(blk + 1) * 128]
            S.op("vector", mk("tensor_scalar", out=sl, in0=sl, scalar1=flagb[:, 0:1], scalar2=None, op0=ALU.add),
                 reads=[r_maskB, r_flagb], writes=[r_maskB])
        S.op("gpsimd", mk("memset", ap=ones_a[:], constant=0.0), writes=[r_ones])
        S.op("gpsimd", mk("memset", ap=ones_a[:, 0:64], constant=1.0), writes=[r_ones])
        S.op("gpsimd", mk("memset", ap=ones_b[:], constant=0.0), writes=[r_ones])
        S.op("gpsimd", mk("memset", ap=ones_b[:, 64:128], constant=1.0), writes=[r_ones])

        def wview(w2d, c0, n):
            return w2d[:, c0:c0 + n].rearrange("(kc p) n -> p kc n", p=128)

        def rms_tile(src, r_src, col, A_vec, r_A, B_vec, r_B, hbt, r_hbt):
            S.op("scalar", mk("activation", out=junk[:], in_=src, func=AF.Square, accum_out=ss[:, col:col + 1]),
                 reads=[r_src], writes=[r_ss[col]])
            S.op("vector", mk("tensor_scalar", out=ms[:, col:col + 1], in0=ss[:, col:col + 1], scalar1=1.0 / D,
                              scalar2=EPS, op0=ALU.mult, op1=ALU.add), reads=[r_ss[col]], writes=[r_ms[col]])
            S.op("gpsimd", mk("tensor_tensor", out=rstd[:, col:col + 1], in0=ms[:, col:col + 1], in1=neghalf[:],
                              op=ALU.pow), reads=[r_ms[col], r_nh], writes=[r_rstd[col]])
            if B_vec is None:
                S.op("vector", mk("scalar_tensor_tensor", out=hbt, in0=src, scalar=rstd[:, col:col + 1], in1=A_vec,
                                  op0=ALU.mult, op1=ALU.mult), reads=[r_src, r_rstd[col], r_A], writes=[r_hbt])
                return
            S.op("vector", mk("scalar_tensor_tensor", out=tmpf[:], in0=src, scalar=rstd[:, col:col + 1], in1=A_vec,
                              op0=ALU.mult, op1=ALU.mult), reads=[r_src, r_rstd[col], r_A], writes=[r_tmpf])
            S.op("vector", mk("tensor_tensor", out=hbt, in0=tmpf[:], in1=B_vec, op=ALU.add),
                 reads=[r_tmpf, r_B], writes=[r_hbt])

        def transpose_tile(hbt, r_hbt, dstT, r_dst, k0=0, k1=8):
            bk, r_bk = pb()
            pv = bk[:].bitcast(BF16).rearrange("p (k t) -> p k t", k=8)
            S.group("tensor", [mk("transpose", out=pv[:, kc, :], in_=hbt[:, kc * 128:(kc + 1) * 128], identity=ident[:])
                               for kc in range(k0, k1)], reads=[r_hbt, r_ident], writes=[r_bk])
            evac(dstT, pv[:, k0:k1, :], [r_bk], r_dst)

        with ExitStack() as st_mix:
            mv2 = sbt(st_mix, "mv2", [128, D], F32); r_mv2 = Res()
            with ExitStack() as st_a:
                mv0 = sbt(st_a, "mv0", [128, D], F32); r_mv0 = Res()
                mv1 = sbt(st_a, "mv1", [128, D], F32); r_mv1 = Res()
                mvs = [mv0, mv1, mv2, mv3, mv4, mv5]
                r_mvs = [r_mv0, r_mv1, r_mv2, r_mv3, r_mv4, r_mv5]
                with ExitStack() as st0:
                    bada = sbt(st0, "bada", [128, 6 * D], F32); r_bada = Res()
                    gmix_bc = sbt(st0, "gmix_bc", [128, D], F32); r_gmix = Res()
                    gffn_bc = sbt(st0, "gffn_bc", [128, D], F32); r_gffn = Res()
                    wab = [sbt(st0, "wab%d" % i, [128, 8, 512], BF16) for i in range(2)]; r_wab = RL(2)
                    c_bf = sbt(st0, "c_bf", [128, 8, 128], BF16); r_cbf = Res()
                    S.dma(c_bf[:], c_rep, writes=[r_cbf], eng="gpsimd")
                    S.dma(bada[:], b_ada.partition_broadcast(128), writes=[r_bada])
                    S.dma(gmix_bc[:], g_mix.partition_broadcast(128), writes=[r_gmix])
                    S.dma(gffn_bc[:], g_ffn.partition_broadcast(128), writes=[r_gffn])
                    for cc in range(12):
                        wb_, r_wb_ = wab[cc % 2], r_wab[cc % 2]
                        S.dma(wb_[:], wview(w_ada, cc * 512, 512), writes=[r_wb_], eng="gpsimd")
                        bk, r_bk = pb()
                        S.group("tensor", [mk("matmul", out=bk[:], lhsT=c_bf[:, kc, :], rhs=wb_[:, kc, :],
                                              start=(kc == 0), stop=(kc == 7)) for kc in range(8)],
                                reads=[r_cbf, r_wb_], writes=[r_bk])
                        dst = mvs[cc // 2][:, (cc % 2) * 512:(cc % 2 + 1) * 512]
                        S.op("vector", mk("tensor_tensor", out=dst, in0=bk[:], in1=bada[:, cc * 512:(cc + 1) * 512],
                                          op=ALU.add), reads=[r_bk, r_bada], writes=[r_mvs[cc // 2]])
                    if debug:
                        for i in range(6):
                            S.dma(dbg["d_mod"][:, i * D:(i + 1) * D], mvs[i][:], reads=[r_mvs[i]])
                    S.op("vector", mk("scalar_tensor_tensor", out=mv1[:], in0=mv1[:], scalar=1.0, in1=gmix_bc[:],
                                      op0=ALU.add, op1=ALU.mult), reads=[r_gmix], writes=[r_mv1])
                    S.op("vector", mk("scalar_tensor_tensor", out=mv4[:], in0=mv4[:], scalar=1.0, in1=gffn_bc[:],
                                      op0=ALU.add, op1=ALU.mult), reads=[r_gffn], writes=[r_mv4])
                    S.barrier()
                for t in range(32):
                    xs, r_xs = xb[t % 2], r_xb[t % 2]
                    S.dma(xs[:], x_all[t * 128:(t + 1) * 128, :], writes=[r_xs])
                    rms_tile(xs[:], r_xs, t, mv1[:], r_mv1, mv0[:], r_mv0, hb[t % 2][:], r_hb[t % 2])
                    transpose_tile(hb[t % 2], r_hb[t % 2], hT[:, :, t * 128:(t + 1) * 128], [r_hT[t]])
                if debug:
                    S.barrier()
                    with ExitStack() as st_d:
                        dtmp = sbt(st_d, "dtmp", [128, 8 * 512], F32); r_dtmp = Res()
                        S.op("vector", mk("tensor_copy", out=dtmp[:].rearrange("p (k t) -> p k t", k=8),
                                          in_=hT[:, :, 1792:2304]), reads=r_hT, writes=[r_dtmp])
                        S.dma(dbg["d_hT"], dtmp[:], reads=[r_dtmp])
                        S.barrier()
                S.barrier()
            mixT = bufA
            with ExitStack() as st_r:
                NB = 1024
                xr = sbt(st_r, "xr", [128, NB + 3], F32); r_xr = Res()
                xc = sbt(st_r, "xc", [128, NB], F32); r_xc = Res()
                xcb = sbt(st_r, "xcb", [128, NB], BF16); r_xcb = Res()
                rg = sbt(st_r, "rg", [128, NB], F32); r_rg = Res()
                ig = sbt(st_r, "ig", [128, NB], F32); r_ig = Res()
                ag = sbt(st_r, "ag", [128, NB], F32); r_ag = Res()
                t1 = sbt(st_r, "t1", [128, NB], F32); r_t1 = Res()
                hs = sbt(st_r, "hs", [128, NB], F32); r_hs = Res()
                gg = sbt(st_r, "gg", [128, NB], F32); r_gg = Res()
                wxr = [sbt(st_r, "wxr%d" % i, [128, 8, 128], BF16) for i in range(2)]; r_wxr = RL(2)
                wgr = [sbt(st_r, "wgr%d" % i, [128, 8, 128], BF16) for i in range(2)]; r_wgr = RL(2)
                wbd = [sbt(st_r, "wbd%d" % i, [128, 4, 128], BF16) for i in range(2)]; r_wbd = RL(2)
                rp = sbt(st_r, "rp", [128, 4, 8], F32); r_rp = Res()
                clam = sbt(st_r, "clam", [128, 4], F32); r_clam = Res()
                state = sbt(st_r, "state", [128, 4], F32); r_state = Res()
                S.dma(rp[:], recp, writes=[r_rp])
                S.dma(wbd[0][:], wa_bd, writes=[r_wbd[0]], eng="gpsimd")
                S.dma(wbd[1][:], wx_bd, writes=[r_wbd[1]], eng="gpsimd")
                S.op("scalar", mk("activation", out=clam[:], in_=rp[:, :, 7], func=AF.Exp, scale=-1.0),
                     reads=[r_rp], writes=[r_clam])
                S.op("scalar", mk("activation", out=clam[:], in_=clam[:], func=AF.Ln, bias=1.0, scale=1.0),
                     reads=[r_clam], writes=[r_clam])
                S.op("vector", mk("tensor_scalar", out=clam[:], in0=clam[:], scalar1=-8.0, scalar2=None, op0=ALU.mult),
                     reads=[r_clam], writes=[r_clam])
                S.op("vector", mk("memset", ap=state[:], constant=0.0), writes=[r_state])
                for cch in range(4):
                    sl = cch % 2
                    S.dma(wxr[sl][:], wview(w_in, 1536 + cch * 128, 128), writes=[r_wxr[sl]], eng="gpsimd")
                    S.dma(wgr[sl][:], wview(w_in, 2048 + cch * 128, 128), writes=[r_wgr[sl]], eng="gpsimd")
                    S.op("vector", mk("memset", ap=xr[:, 0:3], constant=0.0), writes=[r_xr])
                    for seg in range(4):
                        t0 = seg * NB
                        rh = r_hT[seg * 8:(seg + 1) * 8]
                        if seg == 2:
                            S.op("vector", mk("tensor_scalar", out=xr[:, 0:3], in0=xr[:, 0:3], scalar1=flag[:, 0:1],
                                              scalar2=None, op0=ALU.mult), reads=[r_flag], writes=[r_xr])
                            S.op("vector", mk("tensor_scalar", out=state[:, cch:cch + 1], in0=state[:, cch:cch + 1],
                                              scalar1=flag[:, 0:1], scalar2=None, op0=ALU.mult),
                                 reads=[r_flag], writes=[r_state])
                        for h2 in range(2):
                            bk, r_bk = pb()
                            S.group("tensor", [mk("matmul", out=bk[:], lhsT=wxr[sl][:, kc, :],
                                                  rhs=hT[:, kc, t0 + h2 * 512:t0 + (h2 + 1) * 512],
                                                  start=(kc == 0), stop=(kc == 7)) for kc in range(8)],
                                    reads=[r_wxr[sl]] + rh, writes=[r_bk])
                            S.op("scalar", mk("activation", out=xr[:, 3 + h2 * 512:3 + (h2 + 1) * 512], in_=bk[:],
                                              func=AF.Copy), reads=[r_bk], writes=[r_xr])
                        S.op("vector", mk("tensor_scalar", out=xc[:], in0=xr[:, 3:NB + 3], scalar1=rp[:, cch, 3:4],
                                          scalar2=rp[:, cch, 4:5], op0=ALU.mult, op1=ALU.add),
                             reads=[r_xr, r_rp], writes=[r_xc])
                        for j in range(3):
                            S.op("vector", mk("scalar_tensor_tensor", out=xc[:], in0=xr[:, j:j + NB],
                                              scalar=rp[:, cch, j:j + 1], in1=xc[:], op0=ALU.mult, op1=ALU.add),
                                 reads=[r_xr, r_rp], writes=[r_xc])
                        S.op("vector", mk("tensor_copy", out=xr[:, 0:3], in_=xr[:, NB:NB + 3]), writes=[r_xr])
                        S.op("scalar", mk("activation", out=xcb[:], in_=xc[:], func=AF.Copy), reads=[r_xc], writes=[r_xcb])
                        for which, (dst, r_dst, bcol) in enumerate(((rg, r_rg, 5), (ig, r_ig, 6))):
                            for h2 in range(2):
                                bk, r_bk = pb()
                                S.group("tensor", [mk("matmul", out=bk[:], lhsT=wbd[which][:, cch, :],
                                                      rhs=xcb[:, h2 * 512:(h2 + 1) * 512], start=True, stop=True)],
                                        reads=[r_wbd[which], r_xcb], writes=[r_bk])
                                S.op("scalar", mk("activation", out=dst[:, h2 * 512:(h2 + 1) * 512], in_=bk[:],
                                                  func=AF.Sigmoid, bias=rp[:, cch, bcol:bcol + 1], scale=1.0),
                                     reads=[r_bk, r_rp], writes=[r_dst])
                        S.op("scalar", mk("activation", out=ag[:], in_=rg[:], func=AF.Exp, scale=clam[:, cch:cch + 1]),
                             reads=[r_rg, r_clam], writes=[r_ag])
                        S.op("vector", mk("tensor_tensor", out=t1[:], in0=ag[:], in1=ag[:], op=ALU.mult),
                             reads=[r_ag], writes=[r_t1])
                        S.op("vector", mk("tensor_scalar", out=t1[:], in0=t1[:], scalar1=-1.0, scalar2=1.0,
                                          op0=ALU.mult, op1=ALU.add), reads=[r_t1], writes=[r_t1])
                        S.op("vector", mk("tensor_scalar", out=t1[:], in0=t1[:], scalar1=1e-30, scalar2=None,
                                          op0=ALU.max), reads=[r_t1], writes=[r_t1])
                        S.op("scalar", mk("activation", out=t1[:], in_=t1[:], func=AF.Sqrt), reads=[r_t1], writes=[r_t1])
                        S.op("gpsimd", mk("tensor_tensor", out=ig[:], in0=ig[:], in1=xc[:], op=ALU.mult),
                             reads=[r_xc], writes=[r_ig])
                        S.op("gpsimd", mk("tensor_tensor", out=ig[:], in0=ig[:], in1=t1[:], op=ALU.mult),
                             reads=[r_t1], writes=[r_ig])
                        S.op("vector", mk("tensor_tensor_scan", out=hs[:], data0=ag[:], data1=ig[:],
                                          initial=state[:, cch:cch + 1], op0=ALU.mult, op1=ALU.add),
                             reads=[r_ag, r_ig, r_state], writes=[r_hs])
                        S.op("vector", mk("tensor_copy", out=state[:, cch:cch + 1], in_=hs[:, NB - 1:NB]),
                             reads=[r_hs], writes=[r_state])
                        if seg >= 2:
                            o0 = (seg - 2) * NB
                            for h2 in range(2):
                                bk, r_bk = pb()
                                S.group("tensor", [mk("matmul", out=bk[:], lhsT=wgr[sl][:, kc, :],
                                                      rhs=hT[:, kc, t0 + h2 * 512:t0 + (h2 + 1) * 512],
                                                      start=(kc == 0), stop=(kc == 7)) for kc in range(8)],
                                        reads=[r_wgr[sl]] + rh, writes=[r_bk])
                                S.op("scalar", mk("activation", out=gg[:, h2 * 512:(h2 + 1) * 512], in_=bk[:],
                                                  func=AF.Gelu_apprx_tanh), reads=[r_bk], writes=[r_gg])
                            S.op("gpsimd", mk("tensor_tensor", out=mixT[:, 4 + cch, o0:o0 + NB], in0=hs[:], in1=gg[:],
                                              op=ALU.mult), reads=[r_hs, r_gg],
                                 writes=r_bufA[4 + cch][(seg - 2) * 8:(seg - 1) * 8])
                S.barrier()
            with ExitStack() as st_at:
                wqkv = sbt(st_at, "wqkv", [128, 3, 8, 128], BF16); r_wqkv3 = RL(3)
                qTa = sbt(st_at, "qTa", [128, TOWN], BF16); r_qTa = Res()
                qTb = sbt(st_at, "qTb", [128, TOWN], BF16); r_qTb = Res()
                kT = sbt(st_at, "kT", [128, TALL], BF16); r_kT = Res()
                vT = sbt(st_at, "vT", [128, TALL], BF16); r_vT = Res()
                Vta = sbt(st_at, "Vta", [128, 32, 128], BF16); r_Vt = RL(8)
                Vtb = sbt(st_at, "Vtb", [128, 32, 128], BF16)
                accN = sbt(st_at, "accN", [128, TOWN], F32); r_accN = Res()
                accD = sbt(st_at, "accD", [128, TOWN], F32); r_accD = Res()
                PT = [sbt(st_at, "PT%d" % i, [128, 512], BF16) for i in range(2)]; r_PT = RL(2)
                pt_rr = 0
                S.op("gpsimd", mk("memset", ap=Vta[:], constant=0.0), writes=r_Vt)
                S.op("gpsimd", mk("memset", ap=Vtb[:], constant=0.0), writes=r_Vt)
                S.op("gpsimd", mk("memset", ap=qTa[:], constant=0.0), writes=[r_qTa])
                S.op("gpsimd", mk("memset", ap=qTb[:], constant=0.0), writes=[r_qTb])
                for fc in range(4):
                    for i3 in range(3):
                        S.dma(wqkv[:, i3, :, :], wview(w_in, i3 * 512 + fc * 128, 128), writes=[r_wqkv3[i3]], eng="gpsimd")
                    for tb in range(4):
                        bk, r_bk = pb()
                        c0 = TOWN + tb * 512
                        S.group("tensor", [mk("matmul", out=bk[:], lhsT=wqkv[:, 0, kc, :], rhs=hT[:, kc, c0:c0 + 512],
                                              start=(kc == 0), stop=(kc == 7)) for kc in range(8)],
                                reads=[r_wqkv3[0]] + r_hT[16 + tb * 4:16 + (tb + 1) * 4], writes=[r_bk])
                        S.op("scalar", mk("activation", out=qTa[0:64, tb * 512:(tb + 1) * 512], in_=bk[0:64, :],
                                          func=AF.Copy), reads=[r_bk], writes=[r_qTa])
                        S.op("vector", mk("tensor_copy", out=qTb[64:128, tb * 512:(tb + 1) * 512], in_=bk[64:128, :]),
                             reads=[r_bk], writes=[r_qTb])
                    for tb in range(8):
                        bk, r_bk = pb()
                        c0 = tb * 512
                        S.group("tensor", [mk("matmul", out=bk[:], lhsT=wqkv[:, 1, kc, :], rhs=hT[:, kc, c0:c0 + 512],
                                              start=(kc == 0), stop=(kc == 7)) for kc in range(8)],
                                reads=[r_wqkv3[1]] + r_hT[tb * 4:(tb + 1) * 4], writes=[r_bk])
                        evac(kT[:, c0:c0 + 512], bk[:], [r_bk], [r_kT])
                    for tb in range(8):
                        bk, r_bk = pb()
                        c0 = tb * 512
                        S.group("tensor", [mk("matmul", out=bk[:], lhsT=wqkv[:, 2, kc, :], rhs=hT[:, kc, c0:c0 + 512],
                                              start=(kc == 0), stop=(kc == 7)) for kc in range(8)],
                                reads=[r_wqkv3[2]] + r_hT[tb * 4:(tb + 1) * 4], writes=[r_bk])
                        evac(vT[:, c0:c0 + 512], bk[:], [r_bk], [r_vT])
                    for pat, d in enumerate((1, 4, 16)):
                        L = TALL // d
                        nj = L // 128
                        for g8 in range(4):
                            bk, r_bk = pb()
                            pv = bk[:].bitcast(BF16).rearrange("p (u c) -> p u c", u=8)
                            fns = []
                            for u in range(8):
                                ti = g8 * 8 + u
                                r_, j_ = divmod(ti, nj)
                                s0 = r_ + d * 128 * j_
                                fns.append(mk("transpose", out=pv[:, u, :], in_=vT[:, s0:s0 + d * 127 + 1:d], identity=ident[:]))
                            S.group("tensor", fns, reads=[r_vT, r_ident], writes=[r_bk])
                            S.op("scalar", mk("activation", out=Vta[:, g8 * 8:(g8 + 1) * 8, 0:64], in_=pv[:, :, 0:64],
                                              func=AF.Copy), reads=[r_bk], writes=[r_Vt[2 * g8], r_Vt[2 * g8 + 1]])
                            S.op("vector", mk("tensor_copy", out=Vtb[:, g8 * 8:(g8 + 1) * 8, 64:128], in_=pv[:, :, 64:128]),
                                 reads=[r_bk], writes=[r_Vt[2 * g8], r_Vt[2 * g8 + 1]])
                        for su in range(4):
                            bkN, r_bkN = pb()
                            bkD, r_bkD = pb()
                            for u in range(4):
                                if d == 1:
                                    r_, jq = 0, 16 + 4 * su + u
                                elif d == 4:
                                    r_, jq = u, 4 + su
                                else:
                                    r_, jq = 4 * su + u, 1
                                q0 = r_ + d * 128 * jq - TOWN
                                kp0 = r_ + d * 128 * (jq - 1)
                                kc0 = r_ + d * 128 * jq
                                tip = r_ * nj + jq - 1
                                tic = r_ * nj + jq
                                boundary = (jq == nj // 2)
                                span = d * 127 + 1
                                bkS, r_bkS = pb()
                                msk = maskB if boundary else negmask
                                fns = [mk("matmul", out=bkS[:], lhsT=ident[:], rhs=msk[:], start=True, stop=False)]
                                for bi, (qq, k0) in enumerate(((qTa, kp0), (qTa, kc0), (qTb, kp0), (qTb, kc0))):
                                    fns.append(mk("matmul", out=bkS[:, bi * 128:(bi + 1) * 128],
                                                  lhsT=kT[:, k0:k0 + span:d], rhs=qq[:, q0:q0 + span:d],
                                                  start=False, stop=(bi == 3)))
                                S.group("tensor", fns, reads=[r_ident, r_negmask, r_maskB, r_kT, r_qTa, r_qTb],
                                        writes=[r_bkS])
                                pt, r_pt = PT[pt_rr], r_PT[pt_rr]
                                pt_rr ^= 1
                                S.op("scalar", mk("activation", out=pt[:], in_=bkS[:], func=AF.Exp, scale=0.125),
                                     reads=[r_bkS], writes=[r_pt])
                                oc = slice(u * 128, (u + 1) * 128)
                                fnsN = [
                                    mk("matmul", out=bkN[:, oc], lhsT=Vta[:, tip, :], rhs=pt[:, 0:128], start=True, stop=False),
                                    mk("matmul", out=bkN[:, oc], lhsT=Vta[:, tic, :], rhs=pt[:, 128:256], start=False, stop=False),
                                    mk("matmul", out=bkN[:, oc], lhsT=Vtb[:, tip, :], rhs=pt[:, 256:384], start=False, stop=False),
                                    mk("matmul", out=bkN[:, oc], lhsT=Vtb[:, tic, :], rhs=pt[:, 384:512], start=False, stop=True),
                                ]
                                S.group("tensor", fnsN, reads=[r_pt, r_Vt[tip // 4], r_Vt[tic // 4]], writes=[r_bkN])
                                fnsD = [
                                    mk("matmul", out=bkD[:, oc], lhsT=ones_a[:], rhs=pt[:, 0:128], start=True, stop=False),
                                    mk("matmul", out=bkD[:, oc], lhsT=ones_a[:], rhs=pt[:, 128:256], start=False, stop=False),
                                    mk("matmul", out=bkD[:, oc], lhsT=ones_b[:], rhs=pt[:, 256:384], start=False, stop=False),
                                    mk("matmul", out=bkD[:, oc], lhsT=ones_b[:], rhs=pt[:, 384:512], start=False, stop=True),
                                ]
                                S.group("tensor", fnsD, reads=[r_pt, r_ones], writes=[r_bkD])
                            for acc, r_acc, bkX, r_bkX, eng in ((accN, r_accN, bkN, r_bkN, "vector"),
                                                               (accD, r_accD, bkD, r_bkD, "gpsimd")):
                                src = bkX[:].rearrange("p (u i) -> p u i", u=4)
                                if d == 1:
                                    dst = acc[:, su * 512:(su + 1) * 512].rearrange("p (u i) -> p u i", u=4)
                                elif d == 4:
                                    dst = acc[:, su * 512:(su + 1) * 512].rearrange("p (i r) -> p r i", r=4)
                                else:
                                    dst = acc[:].rearrange("p (i r) -> p r i", r=16)[:, 4 * su:4 * su + 4, :]
                                if d == 1:
                                    S.op("vector" if eng == "vector" else "scalar",
                                         mk("tensor_copy", out=dst, in_=src) if eng == "vector" else
                                         mk("activation", out=dst, in_=src, func=AF.Copy),
                                         reads=[r_bkX], writes=[r_acc])
                                else:
                                    S.op("vector", mk("tensor_tensor", out=dst, in0=dst, in1=src, op=ALU.add),
                                         reads=[r_bkX], writes=[r_acc])
                    S.op("vector", mk("reciprocal", out=accD[:], in_=accD[:]), reads=[r_accD], writes=[r_accD])
                    S.op("vector", mk("tensor_tensor", out=mixT[:, fc, :], in0=accN[:], in1=accD[:], op=ALU.mult),
                         reads=[r_accN, r_accD], writes=r_bufA[fc])
                S.barrier()
            if debug:
                with ExitStack() as st_d:
                    dtmp = sbt(st_d, "dtmp2", [128, 8 * 512], F32); r_dtmp = Res()
                    for q4 in range(4):
                        S.op("vector", mk("tensor_copy", out=dtmp[:].rearrange("p (k t) -> p k t", k=8),
                                          in_=mixT[:, :, q4 * 512:(q4 + 1) * 512]), reads=[], writes=[r_dtmp])
                        S.dma(dbg["d_mixT"].rearrange("p (k t) -> p k t", k=8)[:, :, q4 * 512:(q4 + 1) * 512],
                              dtmp[:].rearrange("p (k t) -> p k t", k=8), reads=[r_dtmp])
                    S.barrier()
            with ExitStack() as st_o:
                wout = sbt(st_o, "wout", [128, 8, D], BF16); r_wout2 = RL(2)
                S.dma(wout[:, 0:4, :], w_out[0:512, :].rearrange("(kc p) n -> p kc n", p=128), writes=[r_wout2[0]], eng="gpsimd")
                S.dma(wout[:, 4:8, :], w_out[512:1024, :].rearrange("(kc p) n -> p kc n", p=128), writes=[r_wout2[1]], eng="gpsimd")
                for t in range(16):
                    xs, r_xs = xb[t % 2], r_xb[t % 2]
                    S.dma(xs[:], x_all[TOWN + t * 128:TOWN + (t + 1) * 128, :], writes=[r_xs])
                    for cc in range(2):
                        bk, r_bk = pb()
                        cs = slice(cc * 512, (cc + 1) * 512)
                        S.group("tensor", [mk("matmul", out=bk[:], lhsT=mixT[:, kc, t * 128:(t + 1) * 128],
                                              rhs=wout[:, kc, cs], start=(kc == 0), stop=(kc == 7)) for kc in range(8)],
                                reads=r_wout2 + [r_bufA[kc][t] for kc in range(8)], writes=[r_bk])
                        S.op("vector", mk("tensor_tensor", out=tmpf[:, cs], in0=bk[:], in1=mv2[:, cs], op=ALU.mult),
                             reads=[r_bk, r_mv2], writes=[r_tmpf])
                        S.op("gpsimd", mk("tensor_tensor", out=x1[:, t, cs], in0=tmpf[:, cs], in1=xs[:, cs], op=ALU.add),
                             reads=[r_tmpf, r_xs], writes=[r_x1[t]])
                S.barrier()
        if debug:
            for t in range(16):
                S.dma(dbg["d_x1"][t * 128:(t + 1) * 128, :], x1[:, t, :], reads=[r_x1[t]])
        I32 = mybir.dt.int32
        CAP = TOWN
        x_buf = nc.dram_tensor("x_buf", [NE * CAP, D], BF16, kind="Internal").ap()
        y_buf = nc.dram_tensor("y_buf", [NE * CAP, D], F32, kind="Internal").ap()
        with ExitStack() as st_f:
            wgus = [bufA, sbt(st_f, "wgu1", [128, 8, 2 * D], BF16)]; r_wgus = [RL(4), RL(4)]
            wgus[0] = bufA[:].rearrange("p k t -> p (k t)").rearrange("p (k t) -> p k t", k=8)
            wd = sbt(st_f, "wd", [128, 8, D], BF16); r_wd = RL(2)
            gk = sbt(st_f, "gk", [128, 16, 4], F32); r_gk = RL(16)
            desti = sbt(st_f, "desti", [128, 16, 4], I32); r_desti = RL(16)
            cnti = sbt(st_f, "cnti", [128, NE], I32); r_cnti = Res()

            bgrow = [sbt(st_f, "bgrow%d" % i, [2, 2 * D], BF16) for i in range(2)]; r_bgrow = RL(2)
            ones_r = sbt(st_f, "ones_r", [2, 128], BF16); r_ones_r = Res()
            S.op("gpsimd", mk("memset", ap=ones_r[:], constant=1.0), writes=[r_ones_r])
            for i in range(2):
                S.op("gpsimd", mk("memset", ap=bgrow[i][:, 0:D], constant=0.0), writes=[r_bgrow[i]])
                S.op("gpsimd", mk("memset", ap=bgrow[i][:, D:2 * D], constant=1.0), writes=[r_bgrow[i]])

            def load_wgu(e):
                w_, r_w = wgus[e % 2], r_wgus[e % 2]
                for q4 in range(4):
                    S.dma(w_[:, :, q4 * 512:(q4 + 1) * 512], wview(w_gu[e], q4 * 512, 512), writes=[r_w[q4]], eng="gpsimd")
                S.dma(bgrow[e % 2][0:1, :], b_gu_d[e:e + 1, :], writes=[r_bgrow[e % 2]], eng="gpsimd")

            def load_wd(e):
                S.dma(wd[:, 0:4, :], w_dn[e][0:512, :].rearrange("(kc p) n -> p kc n", p=128), writes=[r_wd[0]], eng="gpsimd")
                S.dma(wd[:, 4:8, :], w_dn[e][512:1024, :].rearrange("(kc p) n -> p kc n", p=128), writes=[r_wd[1]], eng="gpsimd")

            load_wgu(0)
            load_wd(0)
            with ExitStack() as st_r2:
                wr = sbt(st_r2, "wr", [128, 8, NE], BF16); r_wr = Res()
                brt = sbt(st_r2, "brt", [128, NE], F32); r_brt = Res()
                lg = sbt(st_r2, "lg", [128, NE], F32); r_lg = Res()
                ex = sbt(st_r2, "ex", [128, NE], F32); r_ex = Res()
                gt = sbt(st_r2, "gt", [128, NE], F32); r_gt = Res()
                posb = sbt(st_r2, "posb", [128, NE], F32); r_posb = Res()
                scr4 = sbt(st_r2, "scr4", [128, 4, NE], F32); r_scr = Res()
                ebase = sbt(st_r2, "ebase", [128, NE], F32); r_ebase = Res()
                m8 = sbt(st_r2, "m8", [128, 8], F32); r_m8 = Res()
                e4 = sbt(st_r2, "e4", [128, 4], F32); r_e4 = Res()
                destf = sbt(st_r2, "destf", [128, 4], F32); r_destf = Res()
                nmx = sbt(st_r2, "nmx", [128, 1], F32); r_nmx = Res()
                sm = sbt(st_r2, "sm", [128, 1], F32); r_sm = Res()
                gT = sbt(st_r2, "gT", [32, 128], F32); r_gT = Res()
                bdn = sbt(st_r2, "bdn", [32, D], F32); r_bdn = Res()
                maskall = sbt(st_r2, "maskall", [128, 16, NE], BF16); r_mask = RL(16)
                ltri = sbt(st_r2, "ltri", [128, 128], BF16); r_ltri = Res()
                ones_f = sbt(st_r2, "ones_full", [128, 128], BF16); r_onesf = Res()
                h2Tt = sbt(st_r2, "h2Tt", [128, 8, 128], BF16); r_h2Tt = Res()
                S.dma(wr[:], w_router.rearrange("(kc p) n -> p kc n", p=128), writes=[r_wr], eng="gpsimd")
                S.dma(brt[:], b_router.partition_broadcast(128), writes=[r_brt])
                S.dma(bdn[:], b_dn, writes=[r_bdn])
                S.op("gpsimd", mk("iota", out=ebase[:], pattern=[[CAP, NE]], base=0, channel_multiplier=0,
                                  allow_small_or_imprecise_dtypes=True), writes=[r_ebase])
                S.op("gpsimd", mk("memset", ap=ones_f[:], constant=1.0), writes=[r_onesf])
                S.op("gpsimd", mk("memset", ap=ltri[:], constant=1.0), writes=[r_ltri])
                S.op("gpsimd", mk("affine_select", out=ltri[:], in_=ltri[:], pattern=[[1, 128]], compare_op=ALU.is_gt,
                                  fill=0.0, base=0, channel_multiplier=-1), reads=[r_ltri], writes=[r_ltri])
                for t in range(16):
                    hbt, r_hbt = hb[t % 2], r_hb[t % 2]
                    rms_tile(x1[:, t, :], r_x1[t], 32 + t, mv4[:], r_mv4, mv3[:], r_mv3, hbt[:], r_hbt)
                    transpose_tile(hbt, r_hbt, h2Tt[:], [r_h2Tt])
                    bk, r_bk = pb()
                    S.group("tensor", [mk("matmul", out=bk[:, 0:NE], lhsT=h2Tt[:, kc, :], rhs=wr[:, kc, :],
                                          start=(kc == 0), stop=(kc == 7)) for kc in range(8)],
                            reads=[r_wr, r_h2Tt], writes=[r_bk])
                    S.op("vector", mk("tensor_tensor", out=lg[:], in0=bk[:, 0:NE], in1=brt[:], op=ALU.add),
                         reads=[r_bk, r_brt], writes=[r_lg])
                    S.op("vector", mk("max", out=m8[:], in_=lg[:]), reads=[r_lg], writes=[r_m8])
                    S.op("vector", mk("tensor_scalar", out=nmx[:], in0=m8[:, 0:1], scalar1=-1.0, scalar2=None, op0=ALU.mult),
                         reads=[r_m8], writes=[r_nmx])
                    S.op("scalar", mk("activation", out=ex[:], in_=lg[:], func=AF.Exp, bias=nmx[:, 0:1], scale=1.0),
                         reads=[r_lg, r_nmx], writes=[r_ex])
                    S.op("scalar", mk("activation", out=e4[:], in_=m8[:, 0:4], func=AF.Exp, bias=nmx[:, 0:1], scale=1.0),
                         reads=[r_m8, r_nmx], writes=[r_e4])
                    S.op("vector", mk("tensor_scalar", out=maskall[:, t, :], in0=lg[:], scalar1=m8[:, 3:4], scalar2=None,
                                      op0=ALU.is_ge), reads=[r_lg, r_m8], writes=[r_mask[t]])
                    S.op("vector", mk("tensor_tensor", out=ex[:], in0=ex[:], in1=maskall[:, t, :], op=ALU.mult),
                         reads=[r_mask[t]], writes=[r_ex])
                    S.op("vector", mk("tensor_reduce", out=sm[:], in_=ex[:], axis=mybir.AxisListType.X, op=ALU.add),
                         reads=[r_ex], writes=[r_sm])
                    S.op("vector", mk("reciprocal", out=sm[:], in_=sm[:]), reads=[r_sm], writes=[r_sm])
                    S.op("vector", mk("tensor_scalar", out=gt[:], in0=ex[:], scalar1=sm[:, 0:1], scalar2=None,
                                      op0=ALU.mult), reads=[r_ex, r_sm], writes=[r_gt])
                    S.op("vector", mk("tensor_scalar", out=gk[:, t, :], in0=e4[:], scalar1=sm[:, 0:1], scalar2=None,
                                      op0=ALU.mult), reads=[r_e4, r_sm], writes=[r_gk[t]])
                    bkp, r_bkp = pb()
                    fns = [mk("matmul", out=bkp[:, 0:NE], lhsT=ones_f[:], rhs=maskall[:, tp, :], start=(tp == 0), stop=False)
                           for tp in range(t)]
                    fns.append(mk("matmul", out=bkp[:, 0:NE], lhsT=ltri[:], rhs=maskall[:, t, :], start=(t == 0), stop=True))
                    S.group("tensor", fns, reads=[r_onesf, r_ltri] + r_mask[:t + 1], writes=[r_bkp])
                    S.op("vector", mk("tensor_tensor", out=posb[:], in0=bkp[:, 0:NE], in1=ebase[:], op=ALU.add),
                         reads=[r_bkp, r_ebase], writes=[r_posb])
                    for k in range(4):
                        S.op("vector", mk("scalar_tensor_tensor", out=scr4[:, k, :], in0=lg[:], scalar=m8[:, k:k + 1], in1=posb[:],
                                          op0=ALU.is_equal, op1=ALU.mult),
                             reads=[r_lg, r_m8, r_posb], writes=[r_scr])
                    S.op("vector", mk("tensor_reduce", out=destf[:], in_=scr4[:], axis=mybir.AxisListType.X, op=ALU.add),
                         reads=[r_scr], writes=[r_destf])
                    S.op("vector", mk("tensor_scalar", out=destf[:], in0=destf[:], scalar1=0.0, scalar2=float(NE * CAP - 1),
                                      op0=ALU.max, op1=ALU.min), reads=[r_destf], writes=[r_destf])
                    S.op("vector", mk("tensor_copy", out=desti[:, t, :], in_=destf[:]), reads=[r_destf], writes=[r_desti[t]])
                    for k in range(4):
                        def sc(e, t=t, k=k, hbt=hbt):
                            return e.indirect_dma_start(out=x_buf[:, :],
                                                        out_offset=bass.IndirectOffsetOnAxis(ap=desti[:, t, k:k + 1], axis=0),
                                                        in_=hbt[:, :], in_offset=None)
                        S.dma_fn("gpsimd", sc, reads=[r_desti[t], r_hbt], writes=[])
                    bk2, r_bk2 = pb()
                    S.group("tensor", [mk("transpose", out=bk2[0:NE, 0:128], in_=gt[:], identity=identf[:])],
                            reads=[r_gt, r_identf], writes=[r_bk2])
                    S.op("vector", mk("tensor_copy", out=gT[:], in_=bk2[0:NE, 0:128]), reads=[r_bk2], writes=[r_gT])
                    for cc in range(2):
                        cs = slice(cc * 512, (cc + 1) * 512)
                        bk3, r_bk3 = pb()
                        S.group("tensor", [mk("matmul", out=bk3[:], lhsT=gT[:], rhs=bdn[:, cs], start=True, stop=True)],
                                reads=[r_gT, r_bdn], writes=[r_bk3])
                        S.op("vector", mk("tensor_tensor", out=tmpf[:, cs], in0=bk3[:], in1=mv5[:, cs], op=ALU.mult),
                             reads=[r_bk3, r_mv5], writes=[r_tmpf])
                        S.op("gpsimd", mk("tensor_tensor", out=x1[:, t, cs], in0=tmpf[:, cs], in1=x1[:, t, cs], op=ALU.add),
                             reads=[r_tmpf], writes=[r_x1[t]])
                bkc, r_bkc = pb()
                S.group("tensor", [mk("matmul", out=bkc[:, 0:NE], lhsT=ones_f[:], rhs=maskall[:, tp, :], start=(tp == 0),
                                      stop=(tp == 15)) for tp in range(16)], reads=[r_onesf] + r_mask, writes=[r_bkc])
                S.op("vector", mk("tensor_copy", out=cnti[:], in_=bkc[:, 0:NE]), reads=[r_bkc], writes=[r_cnti])
                if debug:
                    S.dma(dbg["d_gates"][:, 0:64], gk[:].rearrange("p t e -> p (t e)"), reads=r_gk)
                S.barrier()
            S.dma(mv3[:], g_final.partition_broadcast(128), writes=[r_mv3])
            with ExitStack() as st_m:
                xblk = [hb[0], hb[1]]; r_xblk = RL(2)
                xT1 = sbt(st_m, "xT1", [128, 8, 128], BF16)
                xTs = [junk[:].rearrange("p (k t) -> p k t", k=8), xT1[:]]; r_xT = RL(2)
                actT = [sbt(st_m, "actT%d" % i, [128, 8, 128], BF16) for i in range(2)]; r_act = [RL(2), RL(2)]
                yblk = [xb[0], xb[1]]; r_yblk = RL(2)
                sgx = sbt(st_m, "sgx", [128, D], F32)
                ucx = sbt(st_m, "ucx", [128, D], F32)
                gc2 = [[tmpf[:, 0:512], tmpf[:, 512:1024]], [sgx[:, 0:512], sgx[:, 512:1024]]]; r_gc2 = [RL(2), RL(2)]
                actk = [sbt(st_m, "actk%d" % i, [128, D], BF16) for i in range(2)]; r_actk = [RL(2), RL(2)]
                ucs = [[mv4[:, 0:512], mv4[:, 512:1024]], [ucx[:, 0:512], ucx[:, 512:1024]]]; r_uc = [RL(2), RL(2)]
                evac_dve_only[0] = True
                blk_rr = 0
                for e in range(n_experts if stage >= 2 else 0):
                    w_, r_w = wgus[e % 2], r_wgus[e % 2]
                    if e + 1 < n_experts:
                        load_wgu(e + 1)
                    S.load_count(cnti[0:1, e:e + 1], [r_cnti])
                    for jp in range(0, 16, 2):
                        S.begin_guard((e, jp), 128 * jp + 1)
                        pair = (jp, jp + 1)

                        NESTED = True

                        def inner(j, ph):
                            if j != jp and NESTED:
                                S.begin_inner((e, j, ph), 128 * j + 1)

                        def inner_end(j):
                            if j != jp and NESTED:
                                S.end_inner()
                        for j in pair:
                            s_ = j % 2
                            row0 = e * CAP + 128 * j
                            inner(j, 1)
                            S.dma(xblk[s_][:], x_buf[row0:row0 + 128, :], writes=[r_xblk[s_]])
                            transpose_tile(xblk[s_], r_xblk[s_], xTs[s_], [r_xT[s_]])
                            inner_end(j)
                        for j in pair:
                            s_ = j % 2
                            xT = xTs[s_]
                            inner(j, 2)
                            for half in range(2):
                                gub = []
                                for c0 in (half * 512, D + half * 512):
                                    bkX, r_bkX = pb()
                                    fns = [mk("matmul", out=bkX[:], lhsT=ones_r[0:2, :], rhs=bgrow[e % 2][0:2, c0:c0 + 512],
                                              start=True, stop=False)]
                                    for kc in range(8):
                                        fns.append(mk("matmul", out=bkX[:], lhsT=xT[:, kc, :], rhs=w_[:, kc, c0:c0 + 512],
                                                      start=False, stop=(kc == 7)))
                                    S.group("tensor", fns, reads=[r_w[c0 // 512], r_xT[s_], r_ones_r, r_bgrow[e % 2]], writes=[r_bkX])
                                    gub.append((bkX, r_bkX))
                                (bkG, r_bkG), (bkU, r_bkU) = gub
                                uc, r_uc_ = ucs[s_][half], r_uc[s_][half]
                                gc, r_gc_ = gc2[s_][half], r_gc2[s_][half]
                                S.op("scalar", mk("activation", out=gc, in_=bkG[:], func=AF.Gelu_apprx_sigmoid),
                                     reads=[r_bkG], writes=[r_gc_])
                                S.op("vector", mk("tensor_scalar", out=uc, in0=bkU[:], scalar1=8.0, scalar2=-6.0,
                                                  op0=ALU.min, op1=ALU.max), reads=[r_bkU], writes=[r_uc_])
                                S.op("vector", mk("scalar_tensor_tensor", out=actk[s_][:, half * 512:(half + 1) * 512], in0=gc,
                                                  scalar=GLU7, in1=uc, op0=ALU.min, op1=ALU.mult),
                                     reads=[r_uc_, r_gc_], writes=[r_actk[s_][half]])
                            inner_end(j)
                        for j in pair:
                            s_ = j % 2
                            row0 = e * CAP + 128 * j
                            inner(j, 3)
                            for half in range(2):
                                transpose_tile(actk[s_], r_actk[s_][half], actT[s_][:, 4 * half:4 * half + 4, :],
                                               [r_act[s_][half]], k0=4 * half, k1=4 * half + 4)
                            for cc in range(2):
                                cs = slice(cc * 512, (cc + 1) * 512)
                                bk, r_bk = pb()
                                S.group("tensor", [mk("matmul", out=bk[:], lhsT=actT[s_][:, kc, :], rhs=wd[:, kc, cs],
                                                      start=(kc == 0), stop=(kc == 7)) for kc in range(8)],
                                        reads=r_wd + r_act[s_], writes=[r_bk])
                                S.op("vector", mk("tensor_tensor", out=yblk[s_][:, cs], in0=bk[:], in1=mv5[:, cs], op=ALU.mult),
                                     reads=[r_bk, r_mv5], writes=[r_yblk[s_]])
                            S.dma(y_buf[row0:row0 + 128, :], yblk[s_][:], reads=[r_yblk[s_]], eng="gpsimd")
                            inner_end(j)
                        S.end_guard()
                    if e + 1 < n_experts:
                        load_wd(e + 1)
                evac_dve_only[0] = False
                S.barrier()
            st_c = st_f.enter_context(ExitStack())
            ykb = [mv4, tmpf] + [sbt(st_c, "ykb%d" % i, [128, D], F32) for i in range(4)]; r_ykb = RL(6)
            gi = 0
            for t in range(16):
                for k in range(4 if stage >= 3 else 0):
                    yk, r_yk = ykb[gi % 6], r_ykb[gi % 6]
                    gi += 1

                    def ga(e, t=t, k=k, yk=yk):
                        return e.indirect_dma_start(out=yk[:, :], out_offset=None, in_=y_buf[:, :],
                                                    in_offset=bass.IndirectOffsetOnAxis(ap=desti[:, t, k:k + 1], axis=0))
                    S.dma_fn("gpsimd", ga, reads=[r_desti[t]], writes=[r_yk])
                    S.op("vector", mk("scalar_tensor_tensor", out=x1[:, t, :], in0=yk[:], scalar=gk[:, t, k:k + 1],
                                      in1=x1[:, t, :], op0=ALU.mult, op1=ALU.add),
                         reads=[r_yk, r_gk[t]], writes=[r_x1[t]])
                ob = xb[t % 2]; r_ob = r_xb[t % 2]
                rms_tile(x1[:, t, :], r_x1[t], 48 + t, mv3[:], r_mv3, None, None, ob[:], r_ob)
                S.dma(out_d[t * 128:(t + 1) * 128, :], ob[:], reads=[r_ob])
            S.barrier()
        with nc.Block() as block:
            S.emit(block)
    return nc


_NC_CACHE = {}


def make_in_maps(inputs, cores, n_experts=NE):
    f = lambda a: np.ascontiguousarray(np.asarray(a, dtype=np.float32))
    x = f(inputs["x"]); c = f(inputs["c"])
    w_ada = f(inputs["w_ada"])[0]; b_ada = f(inputs["b_ada"])[0][None, :]
    g_mix = f(inputs["g_mix"])[0][None, :]; g_ffn = f(inputs["g_ffn"])[0][None, :]
    g_final = f(inputs["g_final"])[None, :]
    w_in = f(inputs["w_in"])[0]; w_out = f(inputs["w_out"])[0]
    conv_w = f(inputs["conv_w"])[0]; conv_b = f(inputs["conv_b"])[0]
    b_a = f(inputs["b_rg_a"])[0]; b_x = f(inputs["b_rg_x"])[0]; lam = f(inputs["lam"])[0]
    recp = np.zeros((128, 4, 8), np.float32)
    cols = [conv_w[0], conv_w[1], conv_w[2], conv_w[3], conv_b, b_a, b_x, lam]
    for j, v in enumerate(cols):
        recp[:, :, j] = v.reshape(4, 128).T
    def blockdiag(w):
        w = f(w)[0]
        o = np.zeros((128, 4, 128), np.float32)
        for blk in range(8):
            cch, hh = divmod(blk, 2)
            o[hh * 64:(hh + 1) * 64, cch, hh * 64:(hh + 1) * 64] = w[blk]
        return o
    wa_bd = blockdiag(inputs["w_rg_a"]); wx_bd = blockdiag(inputs["w_rg_x"])
    w_router = f(inputs["w_router"])[0]; b_router = f(inputs["b_router"])[0][None, :]
    w_gu = f(inputs["w_gate_up"])[0][:n_experts]; b_gu = f(inputs["b_gate_up"])[0]
    bgu_t = np.ascontiguousarray(b_gu.reshape(NE, 16, 128).transpose(2, 0, 1))
    w_dn = f(inputs["w_down"])[0][:n_experts]; b_dn = f(inputs["b_down"])[0]
    maps = []
    for core in cores:
        b, half = divmod(core, 2)
        x_all = np.zeros((TALL, D), np.float32)
        if half == 0:
            x_all[TOWN:] = x[b, :TOWN]
        else:
            x_all[:] = x[b]
        c_rep = np.ascontiguousarray(np.broadcast_to(c[b].reshape(8, 128).T[:, :, None], (128, 8, 128)))
        flag = np.full((128, 1), float(half), np.float32)
        maps.append({
            "x_all": x_all, "c_rep": c_rep, "flag": flag, "w_ada": w_ada, "b_ada": b_ada, "g_mix": g_mix,
            "g_ffn": g_ffn, "g_final": g_final, "w_in": w_in, "w_out": w_out, "recp": recp, "wa_bd": wa_bd,
            "wx_bd": wx_bd, "w_router": w_router, "b_router": b_router, "w_gate_up": w_gu, "bgu_t": bgu_t, "b_gu": b_gu,
            "w_down": w_dn, "b_down": b_dn,
        })
    return maps


def kernel(**inputs):
    if "nc" not in _NC_CACHE:
        _NC_CACHE["nc"] = build()
    nc = _NC_CACHE["nc"]
    cores = list(range(8))
    in_maps = make_in_maps(inputs, cores)
    res = run_bass_kernel_spmd(nc, in_maps, core_ids=cores)
    out = np.zeros((4, 4096, D), np.float32)
    for core in cores:
        b, half = divmod(core, 2)
        out[b, half * TOWN:(half + 1) * TOWN] = res.results[core]["out"]
    return out
```

```python
import numpy as np
from contextlib import ExitStack
import concourse.bass as bass
import concourse.mybir as mybir
from concourse.bass_utils import run_bass_kernel_spmd

F32 = mybir.dt.float32
BF16 = mybir.dt.bfloat16
AF = mybir.ActivationFunctionType
ALU = mybir.AluOpType

D = 1024
TOWN = 2048
TALL = 4096
NE = 32
EPS = 1e-6
GLU7 = float(np.float32(7.0) / (np.float32(1.0) + np.exp(np.float32(-1.702 * 7.0))))
SEM_LIMIT = 30000


class Res:
    __slots__ = ("w", "r")

    def __init__(self):
        self.w = None
        self.r = {}


def RL(n):
    return [Res() for _ in range(n)]


class Sched:
    ENGS = ("tensor", "vector", "scalar", "gpsimd", "sync")

    def __init__(self, nc, stack, n_dma_sems=8):
        self.nc = nc
        self.stack = stack
        self.ops = {e: [] for e in self.ENGS}
        self.known = {e: {} for e in self.ENGS}
        self.dom_sem = {}
        self.dom_max = {}
        self.ndom = 0
        self.cur_dom = {}
        self.cur_cnt = {}
        self.guard = None
        self.guard_snap = {}
        self.regs = {}
        for e in self.ENGS:
            self.cur_dom[e] = self._new_dom(e)
            self.cur_cnt[e] = 0
        self.dma_pool = {}
        for q in ("sync", "gpsimd"):
            self.dma_pool[q] = {"doms": [self._new_dom("dma_%s%d" % (q, i)) for i in range(n_dma_sems)],
                                "cnt": [0] * n_dma_sems, "rr": 0}

    def _new_dom(self, name):
        d = self.ndom
        self.ndom += 1
        self.dom_sem[d] = self.stack.enter_context(self.nc.semaphore("s_%s_%d" % (name, d)))
        self.dom_max[d] = 0
        return d

    def _collect(self, eng, reads, writes):
        need = {}
        for R in reads:
            if R.w is not None:
                d, v = R.w
                if need.get(d, 0) < v:
                    need[d] = v
        for R in writes:
            if R.w is not None:
                d, v = R.w
                if need.get(d, 0) < v:
                    need[d] = v
            for d, v in R.r.items():
                if need.get(d, 0) < v:
                    need[d] = v
        kn = self.known[eng]
        waits = []
        for d, v in need.items():
            if eng == "tensor" and d == self.cur_dom["tensor"]:
                continue
            if kn.get(d, 0) < v:
                kn[d] = v
                waits.append((d, v))
        return waits

    def _tick(self, eng):
        self.cur_cnt[eng] += 1
        d = self.cur_dom[eng]
        v = self.cur_cnt[eng]
        self.dom_max[d] = v
        return d, v

    def _mark(self, d, v, reads, writes):
        for R in reads:
            R.r[d] = v
        for R in writes:
            R.w = (d, v)
            R.r = {}

    def op(self, eng, fn, reads=(), writes=()):
        waits = self._collect(eng, reads, writes)
        d, v = self._tick(eng)
        self.ops[eng].append((waits, fn, self.dom_sem[d], 1, self.guard))
        self._mark(d, v, reads, writes)

    def group(self, eng, fns, reads=(), writes=()):
        waits = self._collect(eng, reads, writes)
        d, v = self._tick(eng)
        n = len(fns)
        for i, fn in enumerate(fns):
            self.ops[eng].append((waits if i == 0 else [], fn,
                                  self.dom_sem[d] if i == n - 1 else None, 1, self.guard))
        self._mark(d, v, reads, writes)

    def dma_fn(self, eng, fn, reads=(), writes=()):
        pool = self.dma_pool[eng]
        i = pool["rr"]
        pool["rr"] = (i + 1) % len(pool["doms"])
        d = pool["doms"][i]
        waits = self._collect(eng, reads, writes)
        prev = pool["cnt"][i]
        kn = self.known[eng]
        if prev > 0 and kn.get(d, 0) < prev:
            kn[d] = prev
            waits.append((d, prev))
        pool["cnt"][i] += 16
        v = pool["cnt"][i]
        self.dom_max[d] = v
        self.ops[eng].append((waits, fn, self.dom_sem[d], 16, self.guard))
        self._mark(d, v, reads, writes)

    def dma(self, out, in_, reads=(), writes=(), eng="sync"):
        def fn(e, out=out, in_=in_):
            return e.dma_start(out=out, in_=in_)
        self.dma_fn(eng, fn, reads, writes)

    def load_count(self, ap, reads):
        for eng in self.ENGS:
            waits = self._collect(eng, reads, ())
            sched = self

            def fn(e, eng=eng, ap=ap):
                return e.reg_load(sched.regs[eng], ap)
            self.ops[eng].append((waits, fn, None, 0, None))
        for R in reads:
            for eng in self.ENGS:
                pass

    def begin_guard(self, gid, thr):
        self.guard = (gid, thr, None, 0)
        self.guard_snap[gid] = dict(self.dom_max)

    def begin_inner(self, gid, thr):
        g = self.guard
        self.guard = (g[0], g[1], gid, thr)
        self.guard_snap[gid] = dict(self.dom_max)

    def end_inner(self):
        g = self.guard
        self.guard = (g[0], g[1], None, 0)

    def end_guard(self):
        self.guard = None

    def barrier(self):
        assert self.guard is None
        for eng in self.ENGS:
            kn = self.known[eng]
            waits = []
            for d, v in self.dom_max.items():
                if v > 0 and kn.get(d, 0) < v:
                    kn[d] = v
                    waits.append((d, v))
            if waits:
                self.ops[eng].append((waits, None, None, 0, None))

    def emit(self, block):
        sched = self

        def emit_one(e, item):
            waits, fn, sem, inc, _ = item
            for (d_, v) in waits:
                e.wait_ge(sched.dom_sem[d_], v)
            if fn is None:
                return
            ins = fn(e)
            if sem is not None:
                ins.then_inc(sem, inc)

        def skip_path(e, grp, snap):
            incs = []
            need = {}
            for waits, fn, sem, inc, _ in grp:
                for (d_, v) in waits:
                    v = min(v, snap.get(d_, 0))
                    if v > need.get(d_, 0):
                        need[d_] = v
            for d_, v in need.items():
                e.wait_ge(sched.dom_sem[d_], v)
            for waits, fn, sem, inc, _ in grp:
                if sem is not None:
                    for k_ in range(len(incs)):
                        if incs[k_][0] is sem:
                            incs[k_][1] += inc
                            break
                    else:
                        incs.append([sem, inc])
            e.drain()
            for sem, tot in incs:
                e.sem_inc(sem, tot)

        def emit_region(e, reg, grp):
            a = 0
            m = len(grp)
            while a < m:
                gi = grp[a][4]
                if gi[2] is None:
                    emit_one(e, grp[a])
                    a += 1
                    continue
                b = a
                while b < m and grp[b][4][2] == gi[2]:
                    b += 1
                sub = grp[a:b]
                with e.If_lt(reg, gi[3]):
                    skip_path(e, sub, sched.guard_snap[gi[2]])
                with e.Else():
                    for it in sub:
                        emit_one(e, it)
                a = b

        def emit_chain(e, reg, regions, idx):
            if idx == len(regions):
                return
            grp = regions[idx]
            g = grp[0][4]
            rest = [it for r_ in regions[idx:] for it in r_]
            with e.If_lt(reg, g[1]):
                skip_path(e, rest, sched.guard_snap[g[0]])
            with e.Else():
                emit_region(e, reg, grp)
                emit_chain(e, reg, regions, idx + 1)

        def make(engname):
            def body(e):
                ops = sched.ops[engname]
                sched.regs[engname] = e.alloc_register("cnt_" + engname)
                reg = sched.regs[engname]
                i = 0
                n = len(ops)
                while i < n:
                    g = ops[i][4]
                    if g is None:
                        emit_one(e, ops[i])
                        i += 1
                        continue
                    regions = []
                    j = i
                    while j < n and ops[j][4] is not None and ops[j][4][0][0] == g[0][0]:
                        k = j
                        while k < n and ops[k][4] is not None and ops[k][4][0] == ops[j][4][0]:
                            k += 1
                        regions.append(ops[j:k])
                        j = k
                    emit_chain(e, reg, regions, 0)
                    i = j
            return body
        for engname in self.ENGS:
            if self.ops[engname]:
                getattr(block, engname)(make(engname))


def mk(method, **kw):
    return lambda e: getattr(e, method)(**kw)


def build(debug=False, n_experts=NE, stage=3):
    nc = bass.Bass("TRN2", target_bir_lowering=False)

    def din(name, shape):
        return nc.dram_tensor(name, shape, F32, kind="ExternalInput").ap()

    x_all = din("x_all", [TALL, D])
    c_rep = din("c_rep", [128, 8, 128])
    flag_d = din("flag", [128, 1])
    w_ada = din("w_ada", [D, 6 * D])
    b_ada = din("b_ada", [1, 6 * D])
    g_mix = din("g_mix", [1, D])
    g_ffn = din("g_ffn", [1, D])
    g_final = din("g_final", [1, D])
    w_in = din("w_in", [D, 2560])
    w_out = din("w_out", [D, D])
    recp = din("recp", [128, 4, 8])
    wa_bd = din("wa_bd", [128, 4, 128])
    wx_bd = din("wx_bd", [128, 4, 128])
    w_router = din("w_router", [D, NE])
    b_router = din("b_router", [1, NE])
    w_gu = din("w_gate_up", [n_experts, D, 2 * D])
    bgu_t = din("bgu_t", [128, NE, 16])
    b_gu_d = din("b_gu", [NE, 2 * D])
    w_dn = din("w_down", [n_experts, D, D])
    b_dn = din("b_down", [NE, D])
    out_d = nc.dram_tensor("out", [TOWN, D], F32, kind="ExternalOutput").ap()
    dbg = {}
    if debug:
        def dout(name, shape):
            dbg[name] = nc.dram_tensor(name, shape, F32, kind="ExternalOutput").ap()
        dout("d_mod", [128, 6 * D])
        dout("d_hT", [128, 8 * 512])
        dout("d_mixT", [128, 8 * TOWN])
        dout("d_x1", [TOWN, D])
        dout("d_gates", [128, 16 * NE])

    with ExitStack() as st:
        S = Sched(nc, st)

        def sbt(stack, name, shape, dt):
            return stack.enter_context(nc.sbuf_tensor(name, shape, dt))

        arena = sbt(st, "arena", [128, 16384], F32)
        hT = arena[:].bitcast(BF16).rearrange("p (k t) -> p k t", k=8)
        x1 = arena[:].rearrange("p (t f) -> p t f", t=16)
        r_hT = RL(32)
        r_x1 = RL(16)
        bufA = sbt(st, "bufA", [128, 8, TOWN], BF16)
        r_bufA = [RL(16) for _ in range(8)]
        mv3 = sbt(st, "mv3", [128, D], F32); r_mv3 = Res()
        mv4 = sbt(st, "mv4", [128, D], F32); r_mv4 = Res()
        mv5 = sbt(st, "mv5", [128, D], F32); r_mv5 = Res()
        xb = [sbt(st, "xb%d" % i, [128, D], F32) for i in range(2)]; r_xb = RL(2)
        tmpf = sbt(st, "tmpf", [128, D], F32); r_tmpf = Res()
        hb = [sbt(st, "hb%d" % i, [128, D], BF16) for i in range(2)]; r_hb = RL(2)
        junk = sbt(st, "junk", [128, D], BF16)
        ss = sbt(st, "ss", [128, 64], F32); r_ss = RL(64)
        ms = sbt(st, "ms", [128, 64], F32); r_ms = RL(64)
        rstd = sbt(st, "rstd", [128, 64], F32); r_rstd = RL(64)
        ident = sbt(st, "ident", [128, 128], BF16); r_ident = Res()
        identf = sbt(st, "identf", [128, 128], F32); r_identf = Res()
        neghalf = sbt(st, "neghalf", [128, 1], F32); r_nh = Res()
        flag = sbt(st, "flag_sb", [128, 1], F32); r_flag = Res()
        flagb = sbt(st, "flagb", [128, 1], F32); r_flagb = Res()
        negmask = sbt(st, "negmask", [128, 512], BF16); r_negmask = Res()
        maskB = sbt(st, "maskB", [128, 512], BF16); r_maskB = Res()
        ones_a = sbt(st, "ones_a", [128, 128], BF16); r_ones = Res()
        ones_b = sbt(st, "ones_b", [128, 128], BF16)

        banks = [st.enter_context(nc.psum_tensor("bank%d" % i, [128, 512], F32)) for i in range(8)]
        r_bank = RL(8)
        bank_rr = [0]

        def pb():
            i = bank_rr[0]
            bank_rr[0] = (i + 1) % 8
            return banks[i], r_bank[i]

        cp_rr = [0]
        evac_dve_only = [False]

        def evac(out, in_, reads, writes):
            cp_rr[0] ^= 1
            if cp_rr[0] and not evac_dve_only[0]:
                S.op("scalar", mk("activation", out=out, in_=in_, func=AF.Copy), reads=reads, writes=writes)
            else:
                S.op("vector", mk("tensor_copy", out=out, in_=in_), reads=reads, writes=writes)

        S.dma(flag[:], flag_d, writes=[r_flag])
        S.op("gpsimd", mk("memset", ap=neghalf[:], constant=-0.5), writes=[r_nh])
        S.op("gpsimd", mk("memset", ap=ident[:], constant=1.0), writes=[r_ident])
        S.op("gpsimd", mk("affine_select", out=ident[:], in_=ident[:], pattern=[[-1, 128]],
                          compare_op=ALU.is_equal, fill=0.0, base=0, channel_multiplier=1),
             reads=[r_ident], writes=[r_ident])
        S.op("gpsimd", mk("memset", ap=identf[:], constant=1.0), writes=[r_identf])
        S.op("gpsimd", mk("affine_select", out=identf[:], in_=identf[:], pattern=[[-1, 128]],
                          compare_op=ALU.is_equal, fill=0.0, base=0, channel_multiplier=1),
             reads=[r_identf], writes=[r_identf])
        S.op("gpsimd", mk("memset", ap=negmask[:], constant=0.0), writes=[r_negmask])
        for blk in range(4):
            sl = negmask[:, blk * 128:(blk + 1) * 128]
            if blk % 2 == 0:
                S.op("gpsimd", mk("affine_select", out=sl, in_=sl, pattern=[[-1, 128]], compare_op=ALU.is_ge,
                                  fill=-30000.0, base=0, channel_multiplier=1), reads=[r_negmask], writes=[r_negmask])
            else:
                S.op("gpsimd", mk("affine_select", out=sl, in_=sl, pattern=[[1, 128]], compare_op=ALU.is_ge,
                                  fill=-30000.0, base=0, channel_multiplier=-1), reads=[r_negmask], writes=[r_negmask])
        S.op("vector", mk("tensor_scalar", out=flagb[:], in0=flag[:], scalar1=-1.0, scalar2=30000.0,
                          op0=ALU.add, op1=ALU.mult), reads=[r_flag], writes=[r_flagb])
        S.op("vector", mk("tensor_copy", out=maskB[:], in_=negmask[:]), reads=[r_negmask], writes=[r_maskB])
        for blk in (0, 2):
            sl = maskB[:, blk * 128:(blk + 1) * 128]
            S.op("vector", mk("tensor_scalar", out=sl, in0=sl, scalar1=flagb[:, 0:1], scalar2=None, op0=ALU.add),
                 reads=[r_maskB, r_flagb], writes=[r_maskB])
        S.op("gpsimd", mk("memset", ap=ones_a[:], constant=0.0), writes=[r_ones])
        S.op("gpsimd", mk("memset", ap=ones_a[:, 0:64], constant=1.0), writes=[r_ones])
        S.op("gpsimd", mk("memset", ap=ones_b[:], constant=0.0), writes=[r_ones])
        S.op("gpsimd", mk("memset", ap=ones_b[:, 64:128], constant=1.0), writes=[r_ones])

        def wview(w2d, c0, n):
            return w2d[:, c0:c0 + n].rearrange("(kc p) n -> p kc n", p=128)

        def rms_tile(src, r_src, col, A_vec, r_A, B_vec, r_B, hbt, r_hbt):
            S.op("scalar", mk("activation", out=junk[:], in_=src, func=AF.Square, accum_out=ss[:, col:col + 1]),
                 reads=[r_src], writes=[r_ss[col]])
            S.op("vector", mk("tensor_scalar", out=ms[:, col:col + 1], in0=ss[:, col:col + 1], scalar1=1.0 / D,
                              scalar2=EPS, op0=ALU.mult, op1=ALU.add), reads=[r_ss[col]], writes=[r_ms[col]])
            S.op("gpsimd", mk("tensor_tensor", out=rstd[:, col:col + 1], in0=ms[:, col:col + 1], in1=neghalf[:],
                              op=ALU.pow), reads=[r_ms[col], r_nh], writes=[r_rstd[col]])
            if B_vec is None:
                S.op("vector", mk("scalar_tensor_tensor", out=hbt, in0=src, scalar=rstd[:, col:col + 1], in1=A_vec,
                                  op0=ALU.mult, op1=ALU.mult), reads=[r_src, r_rstd[col], r_A], writes=[r_hbt])
                return
            S.op("vector", mk("scalar_tensor_tensor", out=tmpf[:], in0=src, scalar=rstd[:, col:col + 1], in1=A_vec,
                              op0=ALU.mult, op1=ALU.mult), reads=[r_src, r_rstd[col], r_A], writes=[r_tmpf])
            S.op("vector", mk("tensor_tensor", out=hbt, in0=tmpf[:], in1=B_vec, op=ALU.add),
                 reads=[r_tmpf, r_B], writes=[r_hbt])

        def transpose_tile(hbt, r_hbt, dstT, r_dst, k0=0, k1=8):
            bk, r_bk = pb()
            pv = bk[:].bitcast(BF16).rearrange("p (k t) -> p k t", k=8)
            S.group("tensor", [mk("transpose", out=pv[:, kc, :], in_=hbt[:, kc * 128:(kc + 1) * 128], identity=ident[:])
                               for kc in range(k0, k1)], reads=[r_hbt, r_ident], writes=[r_bk])
            evac(dstT, pv[:, k0:k1, :], [r_bk], r_dst)

        with ExitStack() as st_mix:
            mv2 = sbt(st_mix, "mv2", [128, D], F32); r_mv2 = Res()
            with ExitStack() as st_a:
                mv0 = sbt(st_a, "mv0", [128, D], F32); r_mv0 = Res()
                mv1 = sbt(st_a, "mv1", [128, D], F32); r_mv1 = Res()
                mvs = [mv0, mv1, mv2, mv3, mv4, mv5]
                r_mvs = [r_mv0, r_mv1, r_mv2, r_mv3, r_mv4, r_mv5]
                with ExitStack() as st0:
                    bada = sbt(st0, "bada", [128, 6 * D], F32); r_bada = Res()
                    gmix_bc = sbt(st0, "gmix_bc", [128, D], F32); r_gmix = Res()
                    gffn_bc = sbt(st0, "gffn_bc", [128, D], F32); r_gffn = Res()
                    wab = [sbt(st0, "wab%d" % i, [128, 8, 512], BF16) for i in range(2)]; r_wab = RL(2)
                    c_bf = sbt(st0, "c_bf", [128, 8, 128], BF16); r_cbf = Res()
                    S.dma(c_bf[:], c_rep, writes=[r_cbf], eng="gpsimd")
                    S.dma(bada[:], b_ada.partition_broadcast(128), writes=[r_bada])
                    S.dma(gmix_bc[:], g_mix.partition_broadcast(128), writes=[r_gmix])
                    S.dma(gffn_bc[:], g_ffn.partition_broadcast(128), writes=[r_gffn])
                    for cc in range(12):
                        wb_, r_wb_ = wab[cc % 2], r_wab[cc % 2]
                        S.dma(wb_[:], wview(w_ada, cc * 512, 512), writes=[r_wb_], eng="gpsimd")
                        bk, r_bk = pb()
                        S.group("tensor", [mk("matmul", out=bk[:], lhsT=c_bf[:, kc, :], rhs=wb_[:, kc, :],
                                              start=(kc == 0), stop=(kc == 7)) for kc in range(8)],
                                reads=[r_cbf, r_wb_], writes=[r_bk])
                        dst = mvs[cc // 2][:, (cc % 2) * 512:(cc % 2 + 1) * 512]
                        S.op("vector", mk("tensor_tensor", out=dst, in0=bk[:], in1=bada[:, cc * 512:(cc + 1) * 512],
                                          op=ALU.add), reads=[r_bk, r_bada], writes=[r_mvs[cc // 2]])
                    if debug:
                        for i in range(6):
                            S.dma(dbg["d_mod"][:, i * D:(i + 1) * D], mvs[i][:], reads=[r_mvs[i]])
                    S.op("vector", mk("scalar_tensor_tensor", out=mv1[:], in0=mv1[:], scalar=1.0, in1=gmix_bc[:],
                                      op0=ALU.add, op1=ALU.mult), reads=[r_gmix], writes=[r_mv1])
                    S.op("vector", mk("scalar_tensor_tensor", out=mv4[:], in0=mv4[:], scalar=1.0, in1=gffn_bc[:],
                                      op0=ALU.add, op1=ALU.mult), reads=[r_gffn], writes=[r_mv4])
                    S.barrier()
                for t in range(32):
                    xs, r_xs = xb[t % 2], r_xb[t % 2]
                    S.dma(xs[:], x_all[t * 128:(t + 1) * 128, :], writes=[r_xs])
                    rms_tile(xs[:], r_xs, t, mv1[:], r_mv1, mv0[:], r_mv0, hb[t % 2][:], r_hb[t % 2])
                    transpose_tile(hb[t % 2], r_hb[t % 2], hT[:, :, t * 128:(t + 1) * 128], [r_hT[t]])
                if debug:
                    S.barrier()
                    with ExitStack() as st_d:
                        dtmp = sbt(st_d, "dtmp", [128, 8 * 512], F32); r_dtmp = Res()
                        S.op("vector", mk("tensor_copy", out=dtmp[:].rearrange("p (k t) -> p k t", k=8),
                                          in_=hT[:, :, 1792:2304]), reads=r_hT, writes=[r_dtmp])
                        S.dma(dbg["d_hT"], dtmp[:], reads=[r_dtmp])
                        S.barrier()
                S.barrier()
            mixT = bufA
            with ExitStack() as st_r:
                NB = 1024
                xr = sbt(st_r, "xr", [128, NB + 3], F32); r_xr = Res()
                xc = sbt(st_r, "xc", [128, NB], F32); r_xc = Res()
                xcb = sbt(st_r, "xcb", [128, NB], BF16); r_xcb = Res()
                rg = sbt(st_r, "rg", [128, NB], F32); r_rg = Res()
                ig = sbt(st_r, "ig", [128, NB], F32); r_ig = Res()
                ag = sbt(st_r, "ag", [128, NB], F32); r_ag = Res()
                t1 = sbt(st_r, "t1", [128, NB], F32); r_t1 = Res()
                hs = sbt(st_r, "hs", [128, NB], F32); r_hs = Res()
                gg = sbt(st_r, "gg", [128, NB], F32); r_gg = Res()
                wxr = [sbt(st_r, "wxr%d" % i, [128, 8, 128], BF16) for i in range(2)]; r_wxr = RL(2)
                wgr = [sbt(st_r, "wgr%d" % i, [128, 8, 128], BF16) for i in range(2)]; r_wgr = RL(2)
                wbd = [sbt(st_r, "wbd%d" % i, [128, 4, 128], BF16) for i in range(2)]; r_wbd = RL(2)
                rp = sbt(st_r, "rp", [128, 4, 8], F32); r_rp = Res()
                clam = sbt(st_r, "clam", [128, 4], F32); r_clam = Res()
                state = sbt(st_r, "state", [128, 4], F32); r_state = Res()
                S.dma(rp[:], recp, writes=[r_rp])
                S.dma(wbd[0][:], wa_bd, writes=[r_wbd[0]], eng="gpsimd")
                S.dma(wbd[1][:], wx_bd, writes=[r_wbd[1]], eng="gpsimd")
                S.op("scalar", mk("activation", out=clam[:], in_=rp[:, :, 7], func=AF.Exp, scale=-1.0),
                     reads=[r_rp], writes=[r_clam])
                S.op("scalar", mk("activation", out=clam[:], in_=clam[:], func=AF.Ln, bias=1.0, scale=1.0),
                     reads=[r_clam], writes=[r_clam])
                S.op("vector", mk("tensor_scalar", out=clam[:], in0=clam[:], scalar1=-8.0, scalar2=None, op0=ALU.mult),
                     reads=[r_clam], writes=[r_clam])
                S.op("vector", mk("memset", ap=state[:], constant=0.0), writes=[r_state])
                for cch in range(4):
                    sl = cch % 2
                    S.dma(wxr[sl][:], wview(w_in, 1536 + cch * 128, 128), writes=[r_wxr[sl]], eng="gpsimd")
                    S.dma(wgr[sl][:], wview(w_in, 2048 + cch * 128, 128), writes=[r_wgr[sl]], eng="gpsimd")
                    S.op("vector", mk("memset", ap=xr[:, 0:3], constant=0.0), writes=[r_xr])
                    for seg in range(4):
                        t0 = seg * NB
                        rh = r_hT[seg * 8:(seg + 1) * 8]
                        if seg == 2:
                            S.op("vector", mk("tensor_scalar", out=xr[:, 0:3], in0=xr[:, 0:3], scalar1=flag[:, 0:1],
                                              scalar2=None, op0=ALU.mult), reads=[r_flag], writes=[r_xr])
                            S.op("vector", mk("tensor_scalar", out=state[:, cch:cch + 1], in0=state[:, cch:cch + 1],
                                              scalar1=flag[:, 0:1], scalar2=None, op0=ALU.mult),
                                 reads=[r_flag], writes=[r_state])
                        for h2 in range(2):
                            bk, r_bk = pb()
                            S.group("tensor", [mk("matmul", out=bk[:], lhsT=wxr[sl][:, kc, :],
                                                  rhs=hT[:, kc, t0 + h2 * 512:t0 + (h2 + 1) * 512],
                                                  start=(kc == 0), stop=(kc == 7)) for kc in range(8)],
                                    reads=[r_wxr[sl]] + rh, writes=[r_bk])
                            S.op("scalar", mk("activation", out=xr[:, 3 + h2 * 512:3 + (h2 + 1) * 512], in_=bk[:],
                                              func=AF.Copy), reads=[r_bk], writes=[r_xr])
                        S.op("vector", mk("tensor_scalar", out=xc[:], in0=xr[:, 3:NB + 3], scalar1=rp[:, cch, 3:4],
                                          scalar2=rp[:, cch, 4:5], op0=ALU.mult, op1=ALU.add),
                             reads=[r_xr, r_rp], writes=[r_xc])
                        for j in range(3):
                            S.op("vector", mk("scalar_tensor_tensor", out=xc[:], in0=xr[:, j:j + NB],
                                              scalar=rp[:, cch, j:j + 1], in1=xc[:], op0=ALU.mult, op1=ALU.add),
                                 reads=[r_xr, r_rp], writes=[r_xc])
                        S.op("vector", mk("tensor_copy", out=xr[:, 0:3], in_=xr[:, NB:NB + 3]), writes=[r_xr])
                        S.op("scalar", mk("activation", out=xcb[:], in_=xc[:], func=AF.Copy), reads=[r_xc], writes=[r_xcb])
                        for which, (dst, r_dst, bcol) in enumerate(((rg, r_rg, 5), (ig, r_ig, 6))):
                            for h2 in range(2):
                                bk, r_bk = pb()
                                S.group("tensor", [mk("matmul", out=bk[:], lhsT=wbd[which][:, cch, :],
                                                      rhs=xcb[:, h2 * 512:(h2 + 1) * 512], start=True, stop=True)],
                                        reads=[r_wbd[which], r_xcb], writes=[r_bk])
                                S.op("scalar", mk("activation", out=dst[:, h2 * 512:(h2 + 1) * 512], in_=bk[:],
                                                  func=AF.Sigmoid, bias=rp[:, cch, bcol:bcol + 1], scale=1.0),
                                     reads=[r_bk, r_rp], writes=[r_dst])
                        S.op("scalar", mk("activation", out=ag[:], in_=rg[:], func=AF.Exp, scale=clam[:, cch:cch + 1]),
                             reads=[r_rg, r_clam], writes=[r_ag])
                        S.op("vector", mk("tensor_tensor", out=t1[:], in0=ag[:], in1=ag[:], op=ALU.mult),
                             reads=[r_ag], writes=[r_t1])
                        S.op("vector", mk("tensor_scalar", out=t1[:], in0=t1[:], scalar1=-1.0, scalar2=1.0,
                                          op0=ALU.mult, op1=ALU.add), reads=[r_t1], writes=[r_t1])
                        S.op("vector", mk("tensor_scalar", out=t1[:], in0=t1[:], scalar1=1e-30, scalar2=None,
                                          op0=ALU.max), reads=[r_t1], writes=[r_t1])
                        S.op("scalar", mk("activation", out=t1[:], in_=t1[:], func=AF.Sqrt), reads=[r_t1], writes=[r_t1])
                        S.op("gpsimd", mk("tensor_tensor", out=ig[:], in0=ig[:], in1=xc[:], op=ALU.mult),
                             reads=[r_xc], writes=[r_ig])
                        S.op("gpsimd", mk("tensor_tensor", out=ig[:], in0=ig[:], in1=t1[:], op=ALU.mult),
                             reads=[r_t1], writes=[r_ig])
                        S.op("vector", mk("tensor_tensor_scan", out=hs[:], data0=ag[:], data1=ig[:],
                                          initial=state[:, cch:cch + 1], op0=ALU.mult, op1=ALU.add),
                             reads=[r_ag, r_ig, r_state], writes=[r_hs])
                        S.op("vector", mk("tensor_copy", out=state[:, cch:cch + 1], in_=hs[:, NB - 1:NB]),
                             reads=[r_hs], writes=[r_state])
                        if seg >= 2:
                            o0 = (seg - 2) * NB
                            for h2 in range(2):
                                bk, r_bk = pb()
                                S.group("tensor", [mk("matmul", out=bk[:], lhsT=wgr[sl][:, kc, :],
                                                      rhs=hT[:, kc, t0 + h2 * 512:t0 + (h2 + 1) * 512],
                                                      start=(kc == 0), stop=(kc == 7)) for kc in range(8)],
                                        reads=[r_wgr[sl]] + rh, writes=[r_bk])
                                S.op("scalar", mk("activation", out=gg[:, h2 * 512:(h2 + 1) * 512], in_=bk[:],
                                                  func=AF.Gelu_apprx_tanh), reads=[r_bk], writes=[r_gg])
                            S.op("gpsimd", mk("tensor_tensor", out=mixT[:, 4 + cch, o0:o0 + NB], in0=hs[:], in1=gg[:],
                                              op=ALU.mult), reads=[r_hs, r_gg],
                                 writes=r_bufA[4 + cch][(seg - 2) * 8:(seg - 1) * 8])
                S.barrier()
            with ExitStack() as st_at:
                wqkv = sbt(st_at, "wqkv", [128, 3, 8, 128], BF16); r_wqkv3 = RL(3)
                qTa = sbt(st_at, "qTa", [128, TOWN], BF16); r_qTa = Res()
                qTb = sbt(st_at, "qTb", [128, TOWN], BF16); r_qTb = Res()
                kT = sbt(st_at, "kT", [128, TALL], BF16); r_kT = Res()
                vT = sbt(st_at, "vT", [128, TALL], BF16); r_vT = Res()
                Vta = sbt(st_at, "Vta", [128, 32, 128], BF16); r_Vt = RL(8)
                Vtb = sbt(st_at, "Vtb", [128, 32, 128], BF16)
                accN = sbt(st_at, "accN", [128, TOWN], F32); r_accN = Res()
                accD = sbt(st_at, "accD", [128, TOWN], F32); r_accD = Res()
                PT = [sbt(st_at, "PT%d" % i, [128, 512], BF16) for i in range(2)]; r_PT = RL(2)
                pt_rr = 0
                S.op("gpsimd", mk("memset", ap=Vta[:], constant=0.0), writes=r_Vt)
                S.op("gpsimd", mk("memset", ap=Vtb[:], constant=0.0), writes=r_Vt)
                S.op("gpsimd", mk("memset", ap=qTa[:], constant=0.0), writes=[r_qTa])
                S.op("gpsimd", mk("memset", ap=qTb[:], constant=0.0), writes=[r_qTb])
                for fc in range(4):
                    for i3 in range(3):
                        S.dma(wqkv[:, i3, :, :], wview(w_in, i3 * 512 + fc * 128, 128), writes=[r_wqkv3[i3]], eng="gpsimd")
                    for tb in range(4):
                        bk, r_bk = pb()
                        c0 = TOWN + tb * 512
                        S.group("tensor", [mk("matmul", out=bk[:], lhsT=wqkv[:, 0, kc, :], rhs=hT[:, kc, c0:c0 + 512],
                                              start=(kc == 0), stop=(kc == 7)) for kc in range(8)],
                                reads=[r_wqkv3[0]] + r_hT[16 + tb * 4:16 + (tb + 1) * 4], writes=[r_bk])
                        S.op("scalar", mk("activation", out=qTa[0:64, tb * 512:(tb + 1) * 512], in_=bk[0:64, :],
                                          func=AF.Copy), reads=[r_bk], writes=[r_qTa])
                        S.op("vector", mk("tensor_copy", out=qTb[64:128, tb * 512:(tb + 1) * 512], in_=bk[64:128, :]),
                             reads=[r_bk], writes=[r_qTb])
                    for tb in range(8):
                        bk, r_bk = pb()
                        c0 = tb * 512
                        S.group("tensor", [mk("matmul", out=bk[:], lhsT=wqkv[:, 1, kc, :], rhs=hT[:, kc, c0:c0 + 512],
                                              start=(kc == 0), stop=(kc == 7)) for kc in range(8)],
                                reads=[r_wqkv3[1]] + r_hT[tb * 4:(tb + 1) * 4], writes=[r_bk])
                        evac(kT[:, c0:c0 + 512], bk[:], [r_bk], [r_kT])
                    for tb in range(8):
                        bk, r_bk = pb()
                        c0 = tb * 512
                        S.group("tensor", [mk("matmul", out=bk[:], lhsT=wqkv[:, 2, kc, :], rhs=hT[:, kc, c0:c0 + 512],
                                              start=(kc == 0), stop=(kc == 7)) for kc in range(8)],
                                reads=[r_wqkv3[2]] + r_hT[tb * 4:(tb + 1) * 4], writes=[r_bk])
                        evac(vT[:, c0:c0 + 512], bk[:], [r_bk], [r_vT])
                    for pat, d in enumerate((1, 4, 16)):
                        L = TALL // d
                        nj = L // 128
                        for g8 in range(4):
                            bk, r_bk = pb()
                            pv = bk[:].bitcast(BF16).rearrange("p (u c) -> p u c", u=8)
                            fns = []
                            for u in range(8):
                                ti = g8 * 8 + u
                                r_, j_ = divmod(ti, nj)
                                s0 = r_ + d * 128 * j_
                                fns.append(mk("transpose", out=pv[:, u, :], in_=vT[:, s0:s0 + d * 127 + 1:d], identity=ident[:]))
                            S.group("tensor", fns, reads=[r_vT, r_ident], writes=[r_bk])
                            S.op("scalar", mk("activation", out=Vta[:, g8 * 8:(g8 + 1) * 8, 0:64], in_=pv[:, :, 0:64],
                                              func=AF.Copy), reads=[r_bk], writes=[r_Vt[2 * g8], r_Vt[2 * g8 + 1]])
                            S.op("vector", mk("tensor_copy", out=Vtb[:, g8 * 8:(g8 + 1) * 8, 64:128], in_=pv[:, :, 64:128]),
                                 reads=[r_bk], writes=[r_Vt[2 * g8], r_Vt[2 * g8 + 1]])
                        for su in range(4):
                            bkN, r_bkN = pb()
                            bkD, r_bkD = pb()
                            for u in range(4):
                                if d == 1:
                                    r_, jq = 0, 16 + 4 * su + u
                                elif d == 4:
                                    r_, jq = u, 4 + su
                                else:
                                    r_, jq = 4 * su + u, 1
                                q0 = r_ + d * 128 * jq - TOWN
                                kp0 = r_ + d * 128 * (jq - 1)
                                kc0 = r_ + d * 128 * jq
                                tip = r_ * nj + jq - 1
                                tic = r_ * nj + jq
                                boundary = (jq == nj // 2)
                                span = d * 127 + 1
                                bkS, r_bkS = pb()
                                msk = maskB if boundary else negmask
                                fns = [mk("matmul", out=bkS[:], lhsT=ident[:], rhs=msk[:], start=True, stop=False)]
                                for bi, (qq, k0) in enumerate(((qTa, kp0), (qTa, kc0), (qTb, kp0), (qTb, kc0))):
                                    fns.append(mk("matmul", out=bkS[:, bi * 128:(bi + 1) * 128],
                                                  lhsT=kT[:, k0:k0 + span:d], rhs=qq[:, q0:q0 + span:d],
                                                  start=False, stop=(bi == 3)))
                                S.group("tensor", fns, reads=[r_ident, r_negmask, r_maskB, r_kT, r_qTa, r_qTb],
                                        writes=[r_bkS])
                                pt, r_pt = PT[pt_rr], r_PT[pt_rr]
                                pt_rr ^= 1
                                S.op("scalar", mk("activation", out=pt[:], in_=bkS[:], func=AF.Exp, scale=0.125),
                                     reads=[r_bkS], writes=[r_pt])
                                oc = slice(u * 128, (u + 1) * 128)
                                fnsN = [
                                    mk("matmul", out=bkN[:, oc], lhsT=Vta[:, tip, :], rhs=pt[:, 0:128], start=True, stop=False),
                                    mk("matmul", out=bkN[:, oc], lhsT=Vta[:, tic, :], rhs=pt[:, 128:256], start=False, stop=False),
                                    mk("matmul", out=bkN[:, oc], lhsT=Vtb[:, tip, :], rhs=pt[:, 256:384], start=False, stop=False),
                                    mk("matmul", out=bkN[:, oc], lhsT=Vtb[:, tic, :], rhs=pt[:, 384:512], start=False, stop=True),
                                ]
                                S.group("tensor", fnsN, reads=[r_pt, r_Vt[tip // 4], r_Vt[tic // 4]], writes=[r_bkN])
                                fnsD = [
                                    mk("matmul", out=bkD[:, oc], lhsT=ones_a[:], rhs=pt[:, 0:128], start=True, stop=False),
                                    mk("matmul", out=bkD[:, oc], lhsT=ones_a[:], rhs=pt[:, 128:256], start=False, stop=False),
                                    mk("matmul", out=bkD[:, oc], lhsT=ones_b[:], rhs=pt[:, 256:384], start=False, stop=False),
                                    mk("matmul", out=bkD[:, oc], lhsT=ones_b[:], rhs=pt[:, 384:512], start=False, stop=True),
                                ]
                                S.group("tensor", fnsD, reads=[r_pt, r_ones], writes=[r_bkD])
                            for acc, r_acc, bkX, r_bkX, eng in ((accN, r_accN, bkN, r_bkN, "vector"),
                                                               (accD, r_accD, bkD, r_bkD, "gpsimd")):
                                src = bkX[:].rearrange("p (u i) -> p u i", u=4)
                                if d == 1:
                                    dst = acc[:, su * 512:(su + 1) * 512].rearrange("p (u i) -> p u i", u=4)
                                elif d == 4:
                                    dst = acc[:, su * 512:(su + 1) * 512].rearrange("p (i r) -> p r i", r=4)
                                else:
                                    dst = acc[:].rearrange("p (i r) -> p r i", r=16)[:, 4 * su:4 * su + 4, :]
                                if d == 1:
                                    S.op("vector" if eng == "vector" else "scalar",
                                         mk("tensor_copy", out=dst, in_=src) if eng == "vector" else
                                         mk("activation", out=dst, in_=src, func=AF.Copy),
                                         reads=[r_bkX], writes=[r_acc])
                                else:
                                    S.op("vector", mk("tensor_tensor", out=dst, in0=dst, in1=src, op=ALU.add),
                                         reads=[r_bkX], writes=[r_acc])
                    S.op("vector", mk("reciprocal", out=accD[:], in_=accD[:]), reads=[r_accD], writes=[r_accD])
                    S.op("vector", mk("tensor_tensor", out=mixT[:, fc, :], in0=accN[:], in1=accD[:], op=ALU.mult),
                         reads=[r_accN, r_accD], writes=r_bufA[fc])
                S.barrier()
            if debug:
                with ExitStack() as st_d:
                    dtmp = sbt(st_d, "dtmp2", [128, 8 * 512], F32); r_dtmp = Res()
                    for q4 in range(4):
                        S.op("vector", mk("tensor_copy", out=dtmp[:].rearrange("p (k t) -> p k t", k=8),
                                          in_=mixT[:, :, q4 * 512:(q4 + 1) * 512]), reads=[], writes=[r_dtmp])
                        S.dma(dbg["d_mixT"].rearrange("p (k t) -> p k t", k=8)[:, :, q4 * 512:(q4 + 1) * 512],
                              dtmp[:].rearrange("p (k t) -> p k t", k=8), reads=[r_dtmp])
                    S.barrier()
            with ExitStack() as st_o:
                wout = sbt(st_o, "wout", [128, 8, D], BF16); r_wout2 = RL(2)
                S.dma(wout[:, 0:4, :], w_out[0:512, :].rearrange("(kc p) n -> p kc n", p=128), writes=[r_wout2[0]], eng="gpsimd")
                S.dma(wout[:, 4:8, :], w_out[512:1024, :].rearrange("(kc p) n -> p kc n", p=128), writes=[r_wout2[1]], eng="gpsimd")
                for t in range(16):
                    xs, r_xs = xb[t % 2], r_xb[t % 2]
                    S.dma(xs[:], x_all[TOWN + t * 128:TOWN + (t + 1) * 128, :], writes=[r_xs])
                    for cc in range(2):
                        bk, r_bk = pb()
                        cs = slice(cc * 512, (cc + 1) * 512)
                        S.group("tensor", [mk("matmul", out=bk[:], lhsT=mixT[:, kc, t * 128:(t + 1) * 128],
                                              rhs=wout[:, kc, cs], start=(kc == 0), stop=(kc == 7)) for kc in range(8)],
                                reads=r_wout2 + [r_bufA[kc][t] for kc in range(8)], writes=[r_bk])
                        S.op("vector", mk("tensor_tensor", out=tmpf[:, cs], in0=bk[:], in1=mv2[:, cs], op=ALU.mult),
                             reads=[r_bk, r_mv2], writes=[r_tmpf])
                        S.op("gpsimd", mk("tensor_tensor", out=x1[:, t, cs], in0=tmpf[:, cs], in1=xs[:, cs], op=ALU.add),
                             reads=[r_tmpf, r_xs], writes=[r_x1[t]])
                S.barrier()
        if debug:
            for t in range(16):
                S.dma(dbg["d_x1"][t * 128:(t + 1) * 128, :], x1[:, t, :], reads=[r_x1[t]])
        I32 = mybir.dt.int32
        CAP = TOWN
        x_buf = nc.dram_tensor("x_buf", [NE * CAP, D], BF16, kind="Internal").ap()
        y_buf = nc.dram_tensor("y_buf", [NE * CAP, D], F32, kind="Internal").ap()
        with ExitStack() as st_f:
            wgus = [bufA, sbt(st_f, "wgu1", [128, 8, 2 * D], BF16)]; r_wgus = [RL(4), RL(4)]
            wgus[0] = bufA[:].rearrange("p k t -> p (k t)").rearrange("p (k t) -> p k t", k=8)
            wd = sbt(st_f, "wd", [128, 8, D], BF16); r_wd = RL(2)
            gk = sbt(st_f, "gk", [128, 16, 4], F32); r_gk = RL(16)
            desti = sbt(st_f, "desti", [128, 16, 4], I32); r_desti = RL(16)
            cnti = sbt(st_f, "cnti", [128, NE], I32); r_cnti = Res()

            bgrow = [sbt(st_f, "bgrow%d" % i, [2, 2 * D], BF16) for i in range(2)]; r_bgrow = RL(2)
            ones_r = sbt(st_f, "ones_r", [2, 128], BF16); r_ones_r = Res()
            S.op("gpsimd", mk("memset", ap=ones_r[:], constant=1.0), writes=[r_ones_r])
            for i in range(2):
                S.op("gpsimd", mk("memset", ap=bgrow[i][:, 0:D], constant=0.0), writes=[r_bgrow[i]])
                S.op("gpsimd", mk("memset", ap=bgrow[i][:, D:2 * D], constant=1.0), writes=[r_bgrow[i]])

            def load_wgu(e):
                w_, r_w = wgus[e % 2], r_wgus[e % 2]
                for q4 in range(4):
                    S.dma(w_[:, :, q4 * 512:(q4 + 1) * 512], wview(w_gu[e], q4 * 512, 512), writes=[r_w[q4]], eng="gpsimd")
                S.dma(bgrow[e % 2][0:1, :], b_gu_d[e:e + 1, :], writes=[r_bgrow[e % 2]], eng="gpsimd")

            def load_wd(e):
                S.dma(wd[:, 0:4, :], w_dn[e][0:512, :].rearrange("(kc p) n -> p kc n", p=128), writes=[r_wd[0]], eng="gpsimd")
                S.dma(wd[:, 4:8, :], w_dn[e][512:1024, :].rearrange("(kc p) n -> p kc n", p=128), writes=[r_wd[1]], eng="gpsimd")

            load_wgu(0)
            load_wd(0)
            with ExitStack() as st_r2:
                wr = sbt(st_r2, "wr", [128, 8, NE], BF16); r_wr = Res()
                brt = sbt(st_r2, "brt", [128, NE], F32); r_brt = Res()
                lg_2 = [sbt(st_r2, "lg_%d" % i_, [128, NE], F32) for i_ in range(2)]; r_lg_2 = RL(2)
                ex_2 = [sbt(st_r2, "ex_%d" % i_, [128, NE], F32) for i_ in range(2)]; r_ex_2 = RL(2)
                gt_2 = [sbt(st_r2, "gt_%d" % i_, [128, NE], F32) for i_ in range(2)]; r_gt_2 = RL(2)
                posb_2 = [sbt(st_r2, "posb_%d" % i_, [128, NE], F32) for i_ in range(2)]; r_posb_2 = RL(2)
                scr4_2 = [sbt(st_r2, "scr4_%d" % i_, [128, 4, NE], F32) for i_ in range(2)]; r_scr_2 = RL(2)
                ebase = sbt(st_r2, "ebase", [128, NE], F32); r_ebase = Res()
                m8_2 = [sbt(st_r2, "m8_%d" % i_, [128, 8], F32) for i_ in range(2)]; r_m8_2 = RL(2)
                e4_2 = [sbt(st_r2, "e4_%d" % i_, [128, 4], F32) for i_ in range(2)]; r_e4_2 = RL(2)
                destf_2 = [sbt(st_r2, "destf_%d" % i_, [128, 4], F32) for i_ in range(2)]; r_destf_2 = RL(2)
                nmx_2 = [sbt(st_r2, "nmx_%d" % i_, [128, 1], F32) for i_ in range(2)]; r_nmx_2 = RL(2)
                sm_2 = [sbt(st_r2, "sm_%d" % i_, [128, 1], F32) for i_ in range(2)]; r_sm_2 = RL(2)
                gT_2 = [sbt(st_r2, "gT_%d" % i_, [32, 128], F32) for i_ in range(2)]; r_gT_2 = RL(2)
                bdn = sbt(st_r2, "bdn", [32, D], F32); r_bdn = Res()
                maskall = sbt(st_r2, "maskall", [128, 16, NE], BF16); r_mask = RL(16)
                ltri = sbt(st_r2, "ltri", [128, 128], BF16); r_ltri = Res()
                ones_f = sbt(st_r2, "ones_full", [128, 128], BF16); r_onesf = Res()
                h2Tt_2 = [sbt(st_r2, "h2Tt_%d" % i_, [128, 8, 128], BF16) for i_ in range(2)]; r_h2Tt_2 = RL(2)
                S.dma(wr[:], w_router.rearrange("(kc p) n -> p kc n", p=128), writes=[r_wr], eng="gpsimd")
                S.dma(brt[:], b_router.partition_broadcast(128), writes=[r_brt])
                S.dma(bdn[:], b_dn, writes=[r_bdn])
                S.op("gpsimd", mk("iota", out=ebase[:], pattern=[[CAP, NE]], base=0, channel_multiplier=0,
                                  allow_small_or_imprecise_dtypes=True), writes=[r_ebase])
                S.op("gpsimd", mk("memset", ap=ones_f[:], constant=1.0), writes=[r_onesf])
                S.op("gpsimd", mk("memset", ap=ltri[:], constant=1.0), writes=[r_ltri])
                S.op("gpsimd", mk("affine_select", out=ltri[:], in_=ltri[:], pattern=[[1, 128]], compare_op=ALU.is_gt,
                                  fill=0.0, base=0, channel_multiplier=-1), reads=[r_ltri], writes=[r_ltri])
                for t in range(16):
                    hbt, r_hbt = hb[t % 2], r_hb[t % 2]
                    lg, r_lg = lg_2[t % 2], r_lg_2[t % 2]
                    ex, r_ex = ex_2[t % 2], r_ex_2[t % 2]
                    gt, r_gt = gt_2[t % 2], r_gt_2[t % 2]
                    posb, r_posb = posb_2[t % 2], r_posb_2[t % 2]
                    scr4, r_scr = scr4_2[t % 2], r_scr_2[t % 2]
                    m8, r_m8 = m8_2[t % 2], r_m8_2[t % 2]
                    e4, r_e4 = e4_2[t % 2], r_e4_2[t % 2]
                    destf, r_destf = destf_2[t % 2], r_destf_2[t % 2]
                    nmx, r_nmx = nmx_2[t % 2], r_nmx_2[t % 2]
                    sm, r_sm = sm_2[t % 2], r_sm_2[t % 2]
                    gT, r_gT = gT_2[t % 2], r_gT_2[t % 2]
                    h2Tt, r_h2Tt = h2Tt_2[t % 2], r_h2Tt_2[t % 2]
                    rms_tile(x1[:, t, :], r_x1[t], 32 + t, mv4[:], r_mv4, mv3[:], r_mv3, hbt[:], r_hbt)
                    transpose_tile(hbt, r_hbt, h2Tt[:], [r_h2Tt])
                    bk, r_bk = pb()
                    S.group("tensor", [mk("matmul", out=bk[:, 0:NE], lhsT=h2Tt[:, kc, :], rhs=wr[:, kc, :],
                                          start=(kc == 0), stop=(kc == 7)) for kc in range(8)],
                            reads=[r_wr, r_h2Tt], writes=[r_bk])
                    S.op("vector", mk("tensor_tensor", out=lg[:], in0=bk[:, 0:NE], in1=brt[:], op=ALU.add),
                         reads=[r_bk, r_brt], writes=[r_lg])
                    S.op("vector", mk("max", out=m8[:], in_=lg[:]), reads=[r_lg], writes=[r_m8])
                    S.op("vector", mk("tensor_scalar", out=nmx[:], in0=m8[:, 0:1], scalar1=-1.0, scalar2=None, op0=ALU.mult),
                         reads=[r_m8], writes=[r_nmx])
                    S.op("scalar", mk("activation", out=ex[:], in_=lg[:], func=AF.Exp, bias=nmx[:, 0:1], scale=1.0),
                         reads=[r_lg, r_nmx], writes=[r_ex])
                    S.op("scalar", mk("activation", out=e4[:], in_=m8[:, 0:4], func=AF.Exp, bias=nmx[:, 0:1], scale=1.0),
                         reads=[r_m8, r_nmx], writes=[r_e4])
                    S.op("vector", mk("tensor_scalar", out=maskall[:, t, :], in0=lg[:], scalar1=m8[:, 3:4], scalar2=None,
                                      op0=ALU.is_ge), reads=[r_lg, r_m8], writes=[r_mask[t]])
                    S.op("vector", mk("tensor_tensor", out=ex[:], in0=ex[:], in1=maskall[:, t, :], op=ALU.mult),
                         reads=[r_mask[t]], writes=[r_ex])
                    S.op("vector", mk("tensor_reduce", out=sm[:], in_=ex[:], axis=mybir.AxisListType.X, op=ALU.add),
                         reads=[r_ex], writes=[r_sm])
                    S.op("vector", mk("reciprocal", out=sm[:], in_=sm[:]), reads=[r_sm], writes=[r_sm])
                    S.op("vector", mk("tensor_scalar", out=gt[:], in0=ex[:], scalar1=sm[:, 0:1], scalar2=None,
                                      op0=ALU.mult), reads=[r_ex, r_sm], writes=[r_gt])
                    S.op("vector", mk("tensor_scalar", out=gk[:, t, :], in0=e4[:], scalar1=sm[:, 0:1], scalar2=None,
                                      op0=ALU.mult), reads=[r_e4, r_sm], writes=[r_gk[t]])
                    bkp, r_bkp = pb()
                    fns = [mk("matmul", out=bkp[:, 0:NE], lhsT=ones_f[:], rhs=maskall[:, tp, :], start=(tp == 0), stop=False)
                           for tp in range(t)]
                    fns.append(mk("matmul", out=bkp[:, 0:NE], lhsT=ltri[:], rhs=maskall[:, t, :], start=(t == 0), stop=True))
                    S.group("tensor", fns, reads=[r_onesf, r_ltri] + r_mask[:t + 1], writes=[r_bkp])
                    S.op("vector", mk("tensor_tensor", out=posb[:], in0=bkp[:, 0:NE], in1=ebase[:], op=ALU.add),
                         reads=[r_bkp, r_ebase], writes=[r_posb])
                    for k in range(4):
                        S.op("vector", mk("scalar_tensor_tensor", out=scr4[:, k, :], in0=lg[:], scalar=m8[:, k:k + 1], in1=posb[:],
                                          op0=ALU.is_equal, op1=ALU.mult),
                             reads=[r_lg, r_m8, r_posb], writes=[r_scr])
                    S.op("vector", mk("tensor_reduce", out=destf[:], in_=scr4[:], axis=mybir.AxisListType.X, op=ALU.add),
                         reads=[r_scr], writes=[r_destf])
                    S.op("vector", mk("tensor_scalar", out=destf[:], in0=destf[:], scalar1=0.0, scalar2=float(NE * CAP - 1),
                                      op0=ALU.max, op1=ALU.min), reads=[r_destf], writes=[r_destf])
                    S.op("vector", mk("tensor_copy", out=desti[:, t, :], in_=destf[:]), reads=[r_destf], writes=[r_desti[t]])
                    for k in range(4):
                        def sc(e, t=t, k=k, hbt=hbt):
                            return e.indirect_dma_start(out=x_buf[:, :],
                                                        out_offset=bass.IndirectOffsetOnAxis(ap=desti[:, t, k:k + 1], axis=0),
                                                        in_=hbt[:, :], in_offset=None)
                        S.dma_fn("gpsimd", sc, reads=[r_desti[t], r_hbt], writes=[])
                    bk2, r_bk2 = pb()
                    S.group("tensor", [mk("transpose", out=bk2[0:NE, 0:128], in_=gt[:], identity=identf[:])],
                            reads=[r_gt, r_identf], writes=[r_bk2])
                    S.op("vector", mk("tensor_copy", out=gT[:], in_=bk2[0:NE, 0:128]), reads=[r_bk2], writes=[r_gT])
                    for cc in range(2):
                        cs = slice(cc * 512, (cc + 1) * 512)
                        bk3, r_bk3 = pb()
                        S.group("tensor", [mk("matmul", out=bk3[:], lhsT=gT[:], rhs=bdn[:, cs], start=True, stop=True)],
                                reads=[r_gT, r_bdn], writes=[r_bk3])
                        S.op("vector", mk("tensor_tensor", out=tmpf[:, cs], in0=bk3[:], in1=mv5[:, cs], op=ALU.mult),
                             reads=[r_bk3, r_mv5], writes=[r_tmpf])
                        S.op("gpsimd", mk("tensor_tensor", out=x1[:, t, cs], in0=tmpf[:, cs], in1=x1[:, t, cs], op=ALU.add),
                             reads=[r_tmpf], writes=[r_x1[t]])
                bkc, r_bkc = pb()
                S.group("tensor", [mk("matmul", out=bkc[:, 0:NE], lhsT=ones_f[:], rhs=maskall[:, tp, :], start=(tp == 0),
                                      stop=(tp == 15)) for tp in range(16)], reads=[r_onesf] + r_mask, writes=[r_bkc])
                S.op("vector", mk("tensor_copy", out=cnti[:], in_=bkc[:, 0:NE]), reads=[r_bkc], writes=[r_cnti])
                if debug:
                    S.dma(dbg["d_gates"][:, 0:64], gk[:].rearrange("p t e -> p (t e)"), reads=r_gk)
                S.barrier()
            S.dma(mv3[:], g_final.partition_broadcast(128), writes=[r_mv3])
            with ExitStack() as st_m:
                xblk = [hb[0], hb[1]]; r_xblk = RL(2)
                xT1 = sbt(st_m, "xT1", [128, 8, 128], BF16)
                xTs = [junk[:].rearrange("p (k t) -> p k t", k=8), xT1[:]]; r_xT = RL(2)
                actT = [sbt(st_m, "actT%d" % i, [128, 8, 128], BF16) for i in range(2)]; r_act = [RL(2), RL(2)]
                yblk = [xb[0], xb[1]]; r_yblk = RL(2)
                sgx = sbt(st_m, "sgx", [128, D], F32)
                ucx = sbt(st_m, "ucx", [128, D], F32)
                gc2 = [[tmpf[:, 0:512], tmpf[:, 512:1024]], [sgx[:, 0:512], sgx[:, 512:1024]]]; r_gc2 = [RL(2), RL(2)]
                actk = [sbt(st_m, "actk%d" % i, [128, D], BF16) for i in range(2)]; r_actk = [RL(2), RL(2)]
                ucs = [[mv4[:, 0:512], mv4[:, 512:1024]], [ucx[:, 0:512], ucx[:, 512:1024]]]; r_uc = [RL(2), RL(2)]
                evac_dve_only[0] = True
                blk_rr = 0
                for e in range(n_experts if stage >= 2 else 0):
                    w_, r_w = wgus[e % 2], r_wgus[e % 2]
                    if e + 1 < n_experts:
                        load_wgu(e + 1)
                    S.load_count(cnti[0:1, e:e + 1], [r_cnti])
                    for jp in range(0, 16, 2):
                        S.begin_guard((e, jp), 128 * jp + 1)
                        pair = (jp, jp + 1)

                        NESTED = True

                        def inner(j, ph):
                            if j != jp and NESTED:
                                S.begin_inner((e, j, ph), 128 * j + 1)

                        def inner_end(j):
                            if j != jp and NESTED:
                                S.end_inner()
                        for j in pair:
                            s_ = j % 2
                            row0 = e * CAP + 128 * j
                            inner(j, 1)
                            S.dma(xblk[s_][:], x_buf[row0:row0 + 128, :], writes=[r_xblk[s_]])
                            transpose_tile(xblk[s_], r_xblk[s_], xTs[s_], [r_xT[s_]])
                            inner_end(j)
                        for j in pair:
                            s_ = j % 2
                            xT = xTs[s_]
                            inner(j, 2)
                            for half in range(2):
                                gub = []
                                for c0 in (half * 512, D + half * 512):
                                    bkX, r_bkX = pb()
                                    fns = [mk("matmul", out=bkX[:], lhsT=ones_r[0:2, :], rhs=bgrow[e % 2][0:2, c0:c0 + 512],
                                              start=True, stop=False)]
                                    for kc in range(8):
                                        fns.append(mk("matmul", out=bkX[:], lhsT=xT[:, kc, :], rhs=w_[:, kc, c0:c0 + 512],
                                                      start=False, stop=(kc == 7)))
                                    S.group("tensor", fns, reads=[r_w[c0 // 512], r_xT[s_], r_ones_r, r_bgrow[e % 2]], writes=[r_bkX])
                                    gub.append((bkX, r_bkX))
                                (bkG, r_bkG), (bkU, r_bkU) = gub
                                uc, r_uc_ = ucs[s_][half], r_uc[s_][half]
                                gc, r_gc_ = gc2[s_][half], r_gc2[s_][half]
                                S.op("scalar", mk("activation", out=gc, in_=bkG[:], func=AF.Gelu_apprx_sigmoid),
                                     reads=[r_bkG], writes=[r_gc_])
                                S.op("vector", mk("tensor_scalar", out=uc, in0=bkU[:], scalar1=8.0, scalar2=-6.0,
                                                  op0=ALU.min, op1=ALU.max), reads=[r_bkU], writes=[r_uc_])
                                S.op("vector", mk("scalar_tensor_tensor", out=actk[s_][:, half * 512:(half + 1) * 512], in0=gc,
                                                  scalar=GLU7, in1=uc, op0=ALU.min, op1=ALU.mult),
                                     reads=[r_uc_, r_gc_], writes=[r_actk[s_][half]])
                            inner_end(j)
                        for j in pair:
                            s_ = j % 2
                            row0 = e * CAP + 128 * j
                            inner(j, 3)
                            for half in range(2):
                                transpose_tile(actk[s_], r_actk[s_][half], actT[s_][:, 4 * half:4 * half + 4, :],
                                               [r_act[s_][half]], k0=4 * half, k1=4 * half + 4)
                            for cc in range(2):
                                cs = slice(cc * 512, (cc + 1) * 512)
                                bk, r_bk = pb()
                                S.group("tensor", [mk("matmul", out=bk[:], lhsT=actT[s_][:, kc, :], rhs=wd[:, kc, cs],
                                                      start=(kc == 0), stop=(kc == 7)) for kc in range(8)],
                                        reads=r_wd + r_act[s_], writes=[r_bk])
                                S.op("vector", mk("tensor_tensor", out=yblk[s_][:, cs], in0=bk[:], in1=mv5[:, cs], op=ALU.mult),
                                     reads=[r_bk, r_mv5], writes=[r_yblk[s_]])
                            S.dma(y_buf[row0:row0 + 128, :], yblk[s_][:], reads=[r_yblk[s_]], eng="gpsimd")
                            inner_end(j)
                        S.end_guard()
                    if e + 1 < n_experts:
                        load_wd(e + 1)
                evac_dve_only[0] = False
                S.barrier()
            st_c = st_f.enter_context(ExitStack())
            ykb = [mv4, tmpf] + [sbt(st_c, "ykb%d" % i, [128, D], F32) for i in range(4)]; r_ykb = RL(6)
            gi = 0
            for t in range(16):
                for k in range(4 if stage >= 3 else 0):
                    yk, r_yk = ykb[gi % 6], r_ykb[gi % 6]
                    gi += 1

                    def ga(e, t=t, k=k, yk=yk):
                        return e.indirect_dma_start(out=yk[:, :], out_offset=None, in_=y_buf[:, :],
                                                    in_offset=bass.IndirectOffsetOnAxis(ap=desti[:, t, k:k + 1], axis=0))
                    S.dma_fn("gpsimd", ga, reads=[r_desti[t]], writes=[r_yk])
                    S.op("vector", mk("scalar_tensor_tensor", out=x1[:, t, :], in0=yk[:], scalar=gk[:, t, k:k + 1],
                                      in1=x1[:, t, :], op0=ALU.mult, op1=ALU.add),
                         reads=[r_yk, r_gk[t]], writes=[r_x1[t]])
                ob = xb[t % 2]; r_ob = r_xb[t % 2]
                rms_tile(x1[:, t, :], r_x1[t], 48 + t, mv3[:], r_mv3, None, None, ob[:], r_ob)
                S.dma(out_d[t * 128:(t + 1) * 128, :], ob[:], reads=[r_ob])
            S.barrier()
        with nc.Block() as block:
            S.emit(block)
    return nc


_NC_CACHE = {}


def make_in_maps(inputs, cores, n_experts=NE):
    f = lambda a: np.ascontiguousarray(np.asarray(a, dtype=np.float32))
    x = f(inputs["x"]); c = f(inputs["c"])
    w_ada = f(inputs["w_ada"])[0]; b_ada = f(inputs["b_ada"])[0][None, :]
    g_mix = f(inputs["g_mix"])[0][None, :]; g_ffn = f(inputs["g_ffn"])[0][None, :]
    g_final = f(inputs["g_final"])[None, :]
    w_in = f(inputs["w_in"])[0]; w_out = f(inputs["w_out"])[0]
    conv_w = f(inputs["conv_w"])[0]; conv_b = f(inputs["conv_b"])[0]
    b_a = f(inputs["b_rg_a"])[0]; b_x = f(inputs["b_rg_x"])[0]; lam = f(inputs["lam"])[0]
    recp = np.zeros((128, 4, 8), np.float32)
    cols = [conv_w[0], conv_w[1], conv_w[2], conv_w[3], conv_b, b_a, b_x, lam]
    for j, v in enumerate(cols):
        recp[:, :, j] = v.reshape(4, 128).T
    def blockdiag(w):
        w = f(w)[0]
        o = np.zeros((128, 4, 128), np.float32)
        for blk in range(8):
            cch, hh = divmod(blk, 2)
            o[hh * 64:(hh + 1) * 64, cch, hh * 64:(hh + 1) * 64] = w[blk]
        return o
    wa_bd = blockdiag(inputs["w_rg_a"]); wx_bd = blockdiag(inputs["w_rg_x"])
    w_router = f(inputs["w_router"])[0]; b_router = f(inputs["b_router"])[0][None, :]
    w_gu = f(inputs["w_gate_up"])[0][:n_experts]; b_gu = f(inputs["b_gate_up"])[0]
    bgu_t = np.ascontiguousarray(b_gu.reshape(NE, 16, 128).transpose(2, 0, 1))
    w_dn = f(inputs["w_down"])[0][:n_experts]; b_dn = f(inputs["b_down"])[0]
    maps = []
    for core in cores:
        b, half = divmod(core, 2)
        x_all = np.zeros((TALL, D), np.float32)
        if half == 0:
            x_all[TOWN:] = x[b, :TOWN]
        else:
            x_all[:] = x[b]
        c_rep = np.ascontiguousarray(np.broadcast_to(c[b].reshape(8, 128).T[:, :, None], (128, 8, 128)))
        flag = np.full((128, 1), float(half), np.float32)
        maps.append({
            "x_all": x_all, "c_rep": c_rep, "flag": flag, "w_ada": w_ada, "b_ada": b_ada, "g_mix": g_mix,
            "g_ffn": g_ffn, "g_final": g_final, "w_in": w_in, "w_out": w_out, "recp": recp, "wa_bd": wa_bd,
            "wx_bd": wx_bd, "w_router": w_router, "b_router": b_router, "w_gate_up": w_gu, "bgu_t": bgu_t, "b_gu": b_gu,
            "w_down": w_dn, "b_down": b_dn,
        })
    return maps


def kernel(**inputs):
    if "nc" not in _NC_CACHE:
        _NC_CACHE["nc"] = build()
    nc = _NC_CACHE["nc"]
    cores = list(range(8))
    in_maps = make_in_maps(inputs, cores)
    res = run_bass_kernel_spmd(nc, in_maps, core_ids=cores)
    out = np.zeros((4, 4096, D), np.float32)
    for core in cores:
        b, half = divmod(core, 2)
        out[b, half * TOWN:(half + 1) * TOWN] = res.results[core]["out"]
    return out
```

```python
import numpy as np
from contextlib import ExitStack
import concourse.bass as bass
import concourse.mybir as mybir
from concourse.bass_utils import run_bass_kernel_spmd

F32 = mybir.dt.float32
BF16 = mybir.dt.bfloat16
AF = mybir.ActivationFunctionType
ALU = mybir.AluOpType

D = 1024
TOWN = 2048
TALL = 4096
NE = 32
EPS = 1e-6
GLU7 = float(np.float32(7.0) / (np.float32(1.0) + np.exp(np.float32(-1.702 * 7.0))))
SEM_LIMIT = 30000


class Res:
    __slots__ = ("w", "r")

    def __init__(self):
        self.w = None
        self.r = {}


def RL(n):
    return [Res() for _ in range(n)]


class Sched:
    ENGS = ("tensor", "vector", "scalar", "gpsimd", "sync")

    def __init__(self, nc, stack, n_dma_sems=8):
        self.nc = nc
        self.stack = stack
        self.ops = {e: [] for e in self.ENGS}
        self.known = {e: {} for e in self.ENGS}
        self.dom_sem = {}
        self.dom_max = {}
        self.ndom = 0
        self.cur_dom = {}
        self.cur_cnt = {}
        self.guard = None
        self.guard_snap = {}
        self.regs = {}
        for e in self.ENGS:
            self.cur_dom[e] = self._new_dom(e)
            self.cur_cnt[e] = 0
        self.dma_pool = {}
        for q in ("sync", "gpsimd"):
            self.dma_pool[q] = {"doms": [self._new_dom("dma_%s%d" % (q, i)) for i in range(n_dma_sems)],
                                "cnt": [0] * n_dma_sems, "rr": 0}

    def _new_dom(self, name):
        d = self.ndom
        self.ndom += 1
        self.dom_sem[d] = self.stack.enter_context(self.nc.semaphore("s_%s_%d" % (name, d)))
        self.dom_max[d] = 0
        return d

    def _collect(self, eng, reads, writes):
        need = {}
        for R in reads:
            if R.w is not None:
                d, v = R.w
                if need.get(d, 0) < v:
                    need[d] = v
        for R in writes:
            if R.w is not None:
                d, v = R.w
                if need.get(d, 0) < v:
                    need[d] = v
            for d, v in R.r.items():
                if need.get(d, 0) < v:
                    need[d] = v
        kn = self.known[eng]
        waits = []
        for d, v in need.items():
            if eng == "tensor" and d == self.cur_dom["tensor"]:
                continue
            if kn.get(d, 0) < v:
                kn[d] = v
                waits.append((d, v))
        return waits

    def _tick(self, eng):
        self.cur_cnt[eng] += 1
        d = self.cur_dom[eng]
        v = self.cur_cnt[eng]
        self.dom_max[d] = v
        return d, v

    def _mark(self, d, v, reads, writes):
        for R in reads:
            R.r[d] = v
        for R in writes:
            R.w = (d, v)
            R.r = {}

    def op(self, eng, fn, reads=(), writes=()):
        waits = self._collect(eng, reads, writes)
        d, v = self._tick(eng)
        self.ops[eng].append((waits, fn, self.dom_sem[d], 1, self.guard))
        self._mark(d, v, reads, writes)

    def group(self, eng, fns, reads=(), writes=()):
        waits = self._collect(eng, reads, writes)
        d, v = self._tick(eng)
        n = len(fns)
        for i, fn in enumerate(fns):
            self.ops[eng].append((waits if i == 0 else [], fn,
                                  self.dom_sem[d] if i == n - 1 else None, 1, self.guard))
        self._mark(d, v, reads, writes)

    def dma_fn(self, eng, fn, reads=(), writes=()):
        pool = self.dma_pool[eng]
        i = pool["rr"]
        pool["rr"] = (i + 1) % len(pool["doms"])
        d = pool["doms"][i]
        waits = self._collect(eng, reads, writes)
        prev = pool["cnt"][i]
        kn = self.known[eng]
        if prev > 0 and kn.get(d, 0) < prev:
            kn[d] = prev
            waits.append((d, prev))
        pool["cnt"][i] += 16
        v = pool["cnt"][i]
        self.dom_max[d] = v
        self.ops[eng].append((waits, fn, self.dom_sem[d], 16, self.guard))
        self._mark(d, v, reads, writes)

    def dma(self, out, in_, reads=(), writes=(), eng="sync"):
        def fn(e, out=out, in_=in_):
            return e.dma_start(out=out, in_=in_)
        self.dma_fn(eng, fn, reads, writes)

    def load_count(self, ap, reads):
        for eng in self.ENGS:
            waits = self._collect(eng, reads, ())
            sched = self

            def fn(e, eng=eng, ap=ap):
                return e.reg_load(sched.regs[eng], ap)
            self.ops[eng].append((waits, fn, None, 0, None))
        for R in reads:
            for eng in self.ENGS:
                pass

    def begin_guard(self, gid, thr):
        self.guard = (gid, thr, None, 0)
        self.guard_snap[gid] = dict(self.dom_max)

    def begin_inner(self, gid, thr):
        g = self.guard
        self.guard = (g[0], g[1], gid, thr)
        self.guard_snap[gid] = dict(self.dom_max)

    def end_inner(self):
        g = self.guard
        self.guard = (g[0], g[1], None, 0)

    def end_guard(self):
        self.guard = None

    def barrier(self):
        assert self.guard is None
        for eng in self.ENGS:
            kn = self.known[eng]
            waits = []
            for d, v in self.dom_max.items():
                if v > 0 and kn.get(d, 0) < v:
                    kn[d] = v
                    waits.append((d, v))
            if waits:
                self.ops[eng].append((waits, None, None, 0, None))

    def emit(self, block):
        sched = self

        def emit_one(e, item):
            waits, fn, sem, inc, _ = item
            for (d_, v) in waits:
                e.wait_ge(sched.dom_sem[d_], v)
            if fn is None:
                return
            ins = fn(e)
            if sem is not None:
                ins.then_inc(sem, inc)

        def skip_path(e, grp, snap):
            incs = []
            need = {}
            for waits, fn, sem, inc, _ in grp:
                for (d_, v) in waits:
                    v = min(v, snap.get(d_, 0))
                    if v > need.get(d_, 0):
                        need[d_] = v
            for d_, v in need.items():
                e.wait_ge(sched.dom_sem[d_], v)
            for waits, fn, sem, inc, _ in grp:
                if sem is not None:
                    for k_ in range(len(incs)):
                        if incs[k_][0] is sem:
                            incs[k_][1] += inc
                            break
                    else:
                        incs.append([sem, inc])
            e.drain()
            for sem, tot in incs:
                e.sem_inc(sem, tot)

        def emit_region(e, reg, grp):
            a = 0
            m = len(grp)
            while a < m:
                gi = grp[a][4]
                if gi[2] is None:
                    emit_one(e, grp[a])
                    a += 1
                    continue
                b = a
                while b < m and grp[b][4][2] == gi[2]:
                    b += 1
                sub = grp[a:b]
                with e.If_lt(reg, gi[3]):
                    skip_path(e, sub, sched.guard_snap[gi[2]])
                with e.Else():
                    for it in sub:
                        emit_one(e, it)
                a = b

        def emit_chain(e, reg, regions, idx):
            if idx == len(regions):
                return
            grp = regions[idx]
            g = grp[0][4]
            rest = [it for r_ in regions[idx:] for it in r_]
            with e.If_lt(reg, g[1]):
                skip_path(e, rest, sched.guard_snap[g[0]])
            with e.Else():
                emit_region(e, reg, grp)
                emit_chain(e, reg, regions, idx + 1)

        def make(engname):
            def body(e):
                ops = sched.ops[engname]
                sched.regs[engname] = e.alloc_register("cnt_" + engname)
                reg = sched.regs[engname]
                i = 0
                n = len(ops)
                while i < n:
                    g = ops[i][4]
                    if g is None:
                        emit_one(e, ops[i])
                        i += 1
                        continue
                    regions = []
                    j = i
                    while j < n and ops[j][4] is not None and ops[j][4][0][0] == g[0][0]:
                        k = j
                        while k < n and ops[k][4] is not None and ops[k][4][0] == ops[j][4][0]:
                            k += 1
                        regions.append(ops[j:k])
                        j = k
                    emit_chain(e, reg, regions, 0)
                    i = j
            return body
        for engname in self.ENGS:
            if self.ops[engname]:
                getattr(block, engname)(make(engname))


def mk(method, **kw):
    return lambda e: getattr(e, method)(**kw)


def build(debug=False, n_experts=NE, stage=3):
    nc = bass.Bass("TRN2", target_bir_lowering=False)

    def din(name, shape):
        return nc.dram_tensor(name, shape, F32, kind="ExternalInput").ap()

    x_all = din("x_all", [TALL, D])
    c_rep = din("c_rep", [128, 8, 128])
    flag_d = din("flag", [128, 1])
    w_ada = din("w_ada", [D, 6 * D])
    b_ada = din("b_ada", [1, 6 * D])
    g_mix = din("g_mix", [1, D])
    g_ffn = din("g_ffn", [1, D])
    g_final = din("g_final", [1, D])
    w_in = din("w_in", [D, 2560])
    w_out = din("w_out", [D, D])
    recp = din("recp", [128, 4, 8])
    wa_bd = din("wa_bd", [128, 4, 128])
    wx_bd = din("wx_bd", [128, 4, 128])
    w_router = din("w_router", [D, NE])
    b_router = din("b_router", [1, NE])
    w_gu = din("w_gate_up", [n_experts, D, 2 * D])
    bgu_t = din("bgu_t", [128, NE, 16])
    b_gu_d = din("b_gu", [NE, 2 * D])
    w_dn = din("w_down", [n_experts, D, D])
    b_dn = din("b_down", [NE, D])
    out_d = nc.dram_tensor("out", [TOWN, D], F32, kind="ExternalOutput").ap()
    dbg = {}
    if debug:
        def dout(name, shape):
            dbg[name] = nc.dram_tensor(name, shape, F32, kind="ExternalOutput").ap()
        dout("d_mod", [128, 6 * D])
        dout("d_hT", [128, 8 * 512])
        dout("d_mixT", [128, 8 * TOWN])
        dout("d_x1", [TOWN, D])
        dout("d_gates", [128, 16 * NE])

    with ExitStack() as st:
        S = Sched(nc, st)

        def sbt(stack, name, shape, dt):
            return stack.enter_context(nc.sbuf_tensor(name, shape, dt))

        arena = sbt(st, "arena", [128, 16384], F32)
        hT = arena[:].bitcast(BF16).rearrange("p (k t) -> p k t", k=8)
        x1 = arena[:].rearrange("p (t f) -> p t f", t=16)
        r_hT = RL(32)
        r_x1 = RL(16)
        bufA = sbt(st, "bufA", [128, 8, TOWN], BF16)
        r_bufA = [RL(16) for _ in range(8)]
        mv3 = sbt(st, "mv3", [128, D], F32); r_mv3 = Res()
        mv4 = sbt(st, "mv4", [128, D], F32); r_mv4 = Res()
        mv5 = sbt(st, "mv5", [128, D], F32); r_mv5 = Res()
        xb = [sbt(st, "xb%d" % i, [128, D], F32) for i in range(2)]; r_xb = RL(2)
        tmpf = sbt(st, "tmpf", [128, D], F32); r_tmpf = Res()
        hb = [sbt(st, "hb%d" % i, [128, D], BF16) for i in range(2)]; r_hb = RL(2)
        junk = sbt(st, "junk", [128, D], BF16)
        ss = sbt(st, "ss", [128, 64], F32); r_ss = RL(64)
        ms = sbt(st, "ms", [128, 64], F32); r_ms = RL(64)
        rstd = sbt(st, "rstd", [128, 64], F32); r_rstd = RL(64)
        ident = sbt(st, "ident", [128, 128], BF16); r_ident = Res()
        identf = sbt(st, "identf", [128, 128], F32); r_identf = Res()
        neghalf = sbt(st, "neghalf", [128, 1], F32); r_nh = Res()
        flag = sbt(st, "flag_sb", [128, 1], F32); r_flag = Res()
        flagb = sbt(st, "flagb", [128, 1], F32); r_flagb = Res()
        negmask = sbt(st, "negmask", [128, 512], BF16); r_negmask = Res()
        maskB = sbt(st, "maskB", [128, 512], BF16); r_maskB = Res()
        ones_a = sbt(st, "ones_a", [128, 128], BF16); r_ones = Res()
        ones_b = sbt(st, "ones_b", [128, 128], BF16)

        banks = [st.enter_context(nc.psum_tensor("bank%d" % i, [128, 512], F32)) for i in range(8)]
        r_bank = RL(8)
        bank_rr = [0]

        def pb():
            i = bank_rr[0]
            bank_rr[0] = (i + 1) % 8
            return banks[i], r_bank[i]

        cp_rr = [0]
        evac_dve_only = [False]

        def evac(out, in_, reads, writes):
            cp_rr[0] ^= 1
            if cp_rr[0] and not evac_dve_only[0]:
                S.op("scalar", mk("activation", out=out, in_=in_, func=AF.Copy), reads=reads, writes=writes)
            else:
                S.op("vector", mk("tensor_copy", out=out, in_=in_), reads=reads, writes=writes)

        S.dma(flag[:], flag_d, writes=[r_flag])
        S.op("gpsimd", mk("memset", ap=neghalf[:], constant=-0.5), writes=[r_nh])
        S.op("gpsimd", mk("memset", ap=ident[:], constant=1.0), writes=[r_ident])
        S.op("gpsimd", mk("affine_select", out=ident[:], in_=ident[:], pattern=[[-1, 128]],
                          compare_op=ALU.is_equal, fill=0.0, base=0, channel_multiplier=1),
             reads=[r_ident], writes=[r_ident])
        S.op("gpsimd", mk("memset", ap=identf[:], constant=1.0), writes=[r_identf])
        S.op("gpsimd", mk("affine_select", out=identf[:], in_=identf[:], pattern=[[-1, 128]],
                          compare_op=ALU.is_equal, fill=0.0, base=0, channel_multiplier=1),
             reads=[r_identf], writes=[r_identf])
        S.op("gpsimd", mk("memset", ap=negmask[:], constant=0.0), writes=[r_negmask])
        for blk in range(4):
            sl = negmask[:, blk * 128:(blk + 1) * 128]
            if blk % 2 == 0:
                S.op("gpsimd", mk("affine_select", out=sl, in_=sl, pattern=[[-1, 128]], compare_op=ALU.is_ge,
                                  fill=-30000.0, base=0, channel_multiplier=1), reads=[r_negmask], writes=[r_negmask])
            else:
                S.op("gpsimd", mk("affine_select", out=sl, in_=sl, pattern=[[1, 128]], compare_op=ALU.is_ge,
                                  fill=-30000.0, base=0, channel_multiplier=-1), reads=[r_negmask], writes=[r_negmask])
        S.op("vector", mk("tensor_scalar", out=flagb[:], in0=flag[:], scalar1=-1.0, scalar2=30000.0,
                          op0=ALU.add, op1=ALU.mult), reads=[r_flag], writes=[r_flagb])
        S.op("vector", mk("tensor_copy", out=maskB[:], in_=negmask[:]), reads=[r_negmask], writes=[r_maskB])
        for blk in (0, 2):
            sl = maskB[:, blk * 128:(blk + 1) * 128]
            S.op("vector", mk("tensor_scalar", out=sl, in0=sl, scalar1=flagb[:, 0:1], scalar2=None, op0=ALU.add),
                 reads=[r_maskB, r_flagb], writes=[r_maskB])
        S.op("gpsimd", mk("memset", ap=ones_a[:], constant=0.0), writes=[r_ones])
        S.op("gpsimd", mk("memset", ap=ones_a[:, 0:64], constant=1.0), writes=[r_ones])
        S.op("gpsimd", mk("memset", ap=ones_b[:], constant=0.0), writes=[r_ones])
        S.op("gpsimd", mk("memset", ap=ones_b[:, 64:128], constant=1.0), writes=[r_ones])

        def wview(w2d, c0, n):
            return w2d[:, c0:c0 + n].rearrange("(kc p) n -> p kc n", p=128)

        r_junk = Res()

        def rms_stats(src, r_src, col):
            S.op("scalar", mk("activation", out=junk[:], in_=src, func=AF.Square, accum_out=ss[:, col:col + 1]),
                 reads=[r_src], writes=[r_ss[col], r_junk])
            S.op("vector", mk("tensor_scalar", out=ms[:, col:col + 1], in0=ss[:, col:col + 1], scalar1=1.0 / D,
                              scalar2=EPS, op0=ALU.mult, op1=ALU.add), reads=[r_ss[col]], writes=[r_ms[col]])
            S.op("gpsimd", mk("tensor_tensor", out=rstd[:, col:col + 1], in0=ms[:, col:col + 1], in1=neghalf[:],
                              op=ALU.pow), reads=[r_ms[col], r_nh], writes=[r_rstd[col]])

        def rms_tile(src, r_src, col, A_vec, r_A, B_vec, r_B, hbt, r_hbt, stats=True):
            if stats:
                rms_stats(src, r_src, col)
            if B_vec is None:
                S.op("vector", mk("scalar_tensor_tensor", out=hbt, in0=src, scalar=rstd[:, col:col + 1], in1=A_vec,
                                  op0=ALU.mult, op1=ALU.mult), reads=[r_src, r_rstd[col], r_A], writes=[r_hbt])
                return
            S.op("vector", mk("scalar_tensor_tensor", out=tmpf[:], in0=src, scalar=rstd[:, col:col + 1], in1=A_vec,
                              op0=ALU.mult, op1=ALU.mult), reads=[r_src, r_rstd[col], r_A], writes=[r_tmpf])
            S.op("vector", mk("tensor_tensor", out=hbt, in0=tmpf[:], in1=B_vec, op=ALU.add),
                 reads=[r_tmpf, r_B], writes=[r_hbt])

        def transpose_tile(hbt, r_hbt, dstT, r_dst, k0=0, k1=8):
            bk, r_bk = pb()
            pv = bk[:].bitcast(BF16).rearrange("p (k t) -> p k t", k=8)
            S.group("tensor", [mk("transpose", out=pv[:, kc, :], in_=hbt[:, kc * 128:(kc + 1) * 128], identity=ident[:])
                               for kc in range(k0, k1)], reads=[r_hbt, r_ident], writes=[r_bk])
            evac(dstT, pv[:, k0:k1, :], [r_bk], r_dst)

        with ExitStack() as st_mix:
            mv2 = sbt(st_mix, "mv2", [128, D], F32); r_mv2 = Res()
            with ExitStack() as st_a:
                mv0 = sbt(st_a, "mv0", [128, D], F32); r_mv0 = Res()
                mv1 = sbt(st_a, "mv1", [128, D], F32); r_mv1 = Res()
                mvs = [mv0, mv1, mv2, mv3, mv4, mv5]
                r_mvs = [r_mv0, r_mv1, r_mv2, r_mv3, r_mv4, r_mv5]
                with ExitStack() as st0:
                    bada = sbt(st0, "bada", [128, 6 * D], F32); r_bada = Res()
                    gmix_bc = sbt(st0, "gmix_bc", [128, D], F32); r_gmix = Res()
                    gffn_bc = sbt(st0, "gffn_bc", [128, D], F32); r_gffn = Res()
                    wab = [sbt(st0, "wab%d" % i, [128, 8, 512], BF16) for i in range(2)]; r_wab = RL(2)
                    c_bf = sbt(st0, "c_bf", [128, 8, 128], BF16); r_cbf = Res()
                    S.dma(c_bf[:], c_rep, writes=[r_cbf], eng="gpsimd")
                    S.dma(bada[:], b_ada.partition_broadcast(128), writes=[r_bada])
                    S.dma(gmix_bc[:], g_mix.partition_broadcast(128), writes=[r_gmix])
                    S.dma(gffn_bc[:], g_ffn.partition_broadcast(128), writes=[r_gffn])
                    for cc in range(12):
                        wb_, r_wb_ = wab[cc % 2], r_wab[cc % 2]
                        S.dma(wb_[:], wview(w_ada, cc * 512, 512), writes=[r_wb_], eng="gpsimd")
                        bk, r_bk = pb()
                        S.group("tensor", [mk("matmul", out=bk[:], lhsT=c_bf[:, kc, :], rhs=wb_[:, kc, :],
                                              start=(kc == 0), stop=(kc == 7)) for kc in range(8)],
                                reads=[r_cbf, r_wb_], writes=[r_bk])
                        dst = mvs[cc // 2][:, (cc % 2) * 512:(cc % 2 + 1) * 512]
                        S.op("vector", mk("tensor_tensor", out=dst, in0=bk[:], in1=bada[:, cc * 512:(cc + 1) * 512],
                                          op=ALU.add), reads=[r_bk, r_bada], writes=[r_mvs[cc // 2]])
                    if debug:
                        for i in range(6):
                            S.dma(dbg["d_mod"][:, i * D:(i + 1) * D], mvs[i][:], reads=[r_mvs[i]])
                    S.op("vector", mk("scalar_tensor_tensor", out=mv1[:], in0=mv1[:], scalar=1.0, in1=gmix_bc[:],
                                      op0=ALU.add, op1=ALU.mult), reads=[r_gmix], writes=[r_mv1])
                    S.op("vector", mk("scalar_tensor_tensor", out=mv4[:], in0=mv4[:], scalar=1.0, in1=gffn_bc[:],
                                      op0=ALU.add, op1=ALU.mult), reads=[r_gffn], writes=[r_mv4])
                    S.barrier()
                for t in range(32):
                    xs, r_xs = xb[t % 2], r_xb[t % 2]
                    S.dma(xs[:], x_all[t * 128:(t + 1) * 128, :], writes=[r_xs])
                    rms_tile(xs[:], r_xs, t, mv1[:], r_mv1, mv0[:], r_mv0, hb[t % 2][:], r_hb[t % 2])
                    transpose_tile(hb[t % 2], r_hb[t % 2], hT[:, :, t * 128:(t + 1) * 128], [r_hT[t]])
                if debug:
                    S.barrier()
                    with ExitStack() as st_d:
                        dtmp = sbt(st_d, "dtmp", [128, 8 * 512], F32); r_dtmp = Res()
                        S.op("vector", mk("tensor_copy", out=dtmp[:].rearrange("p (k t) -> p k t", k=8),
                                          in_=hT[:, :, 1792:2304]), reads=r_hT, writes=[r_dtmp])
                        S.dma(dbg["d_hT"], dtmp[:], reads=[r_dtmp])
                        S.barrier()
                S.barrier()
            mixT = bufA
            with ExitStack() as st_r:
                NB = 1024
                xr = sbt(st_r, "xr", [128, NB + 3], F32); r_xr = Res()
                xc = sbt(st_r, "xc", [128, NB], F32); r_xc = Res()
                xcb = sbt(st_r, "xcb", [128, NB], BF16); r_xcb = Res()
                rg = sbt(st_r, "rg", [128, NB], F32); r_rg = Res()
                ig = sbt(st_r, "ig", [128, NB], F32); r_ig = Res()
                ag = sbt(st_r, "ag", [128, NB], F32); r_ag = Res()
                t1 = sbt(st_r, "t1", [128, NB], F32); r_t1 = Res()
                hs = sbt(st_r, "hs", [128, NB], F32); r_hs = Res()
                gg = sbt(st_r, "gg", [128, NB], F32); r_gg = Res()
                wxr = [sbt(st_r, "wxr%d" % i, [128, 8, 128], BF16) for i in range(2)]; r_wxr = RL(2)
                wgr = [sbt(st_r, "wgr%d" % i, [128, 8, 128], BF16) for i in range(2)]; r_wgr = RL(2)
                wbd = [sbt(st_r, "wbd%d" % i, [128, 4, 128], BF16) for i in range(2)]; r_wbd = RL(2)
                rp = sbt(st_r, "rp", [128, 4, 8], F32); r_rp = Res()
                clam = sbt(st_r, "clam", [128, 4], F32); r_clam = Res()
                state = sbt(st_r, "state", [128, 4], F32); r_state = Res()
                S.dma(rp[:], recp, writes=[r_rp])
                S.dma(wbd[0][:], wa_bd, writes=[r_wbd[0]], eng="gpsimd")
                S.dma(wbd[1][:], wx_bd, writes=[r_wbd[1]], eng="gpsimd")
                S.op("scalar", mk("activation", out=clam[:], in_=rp[:, :, 7], func=AF.Exp, scale=-1.0),
                     reads=[r_rp], writes=[r_clam])
                S.op("scalar", mk("activation", out=clam[:], in_=clam[:], func=AF.Ln, bias=1.0, scale=1.0),
                     reads=[r_clam], writes=[r_clam])
                S.op("vector", mk("tensor_scalar", out=clam[:], in0=clam[:], scalar1=-8.0, scalar2=None, op0=ALU.mult),
                     reads=[r_clam], writes=[r_clam])
                S.op("vector", mk("memset", ap=state[:], constant=0.0), writes=[r_state])
                for cch in range(4):
                    sl = cch % 2
                    S.dma(wxr[sl][:], wview(w_in, 1536 + cch * 128, 128), writes=[r_wxr[sl]], eng="gpsimd")
                    S.dma(wgr[sl][:], wview(w_in, 2048 + cch * 128, 128), writes=[r_wgr[sl]], eng="gpsimd")
                    S.op("vector", mk("memset", ap=xr[:, 0:3], constant=0.0), writes=[r_xr])
                    for seg in range(4):
                        t0 = seg * NB
                        rh = r_hT[seg * 8:(seg + 1) * 8]
                        if seg == 2:
                            S.op("vector", mk("tensor_scalar", out=xr[:, 0:3], in0=xr[:, 0:3], scalar1=flag[:, 0:1],
                                              scalar2=None, op0=ALU.mult), reads=[r_flag], writes=[r_xr])
                            S.op("vector", mk("tensor_scalar", out=state[:, cch:cch + 1], in0=state[:, cch:cch + 1],
                                              scalar1=flag[:, 0:1], scalar2=None, op0=ALU.mult),
                                 reads=[r_flag], writes=[r_state])
                        for h2 in range(2):
                            bk, r_bk = pb()
                            S.group("tensor", [mk("matmul", out=bk[:], lhsT=wxr[sl][:, kc, :],
                                                  rhs=hT[:, kc, t0 + h2 * 512:t0 + (h2 + 1) * 512],
                                                  start=(kc == 0), stop=(kc == 7)) for kc in range(8)],
                                    reads=[r_wxr[sl]] + rh, writes=[r_bk])
                            S.op("scalar", mk("activation", out=xr[:, 3 + h2 * 512:3 + (h2 + 1) * 512], in_=bk[:],
                                              func=AF.Copy), reads=[r_bk], writes=[r_xr])
                        S.op("vector", mk("tensor_scalar", out=xc[:], in0=xr[:, 3:NB + 3], scalar1=rp[:, cch, 3:4],
                                          scalar2=rp[:, cch, 4:5], op0=ALU.mult, op1=ALU.add),
                             reads=[r_xr, r_rp], writes=[r_xc])
                        for j in range(3):
                            S.op("vector", mk("scalar_tensor_tensor", out=xc[:], in0=xr[:, j:j + NB],
                                              scalar=rp[:, cch, j:j + 1], in1=xc[:], op0=ALU.mult, op1=ALU.add),
                                 reads=[r_xr, r_rp], writes=[r_xc])
                        S.op("vector", mk("tensor_copy", out=xr[:, 0:3], in_=xr[:, NB:NB + 3]), writes=[r_xr])
                        S.op("scalar", mk("activation", out=xcb[:], in_=xc[:], func=AF.Copy), reads=[r_xc], writes=[r_xcb])
                        for which, (dst, r_dst, bcol) in enumerate(((rg, r_rg, 5), (ig, r_ig, 6))):
                            for h2 in range(2):
                                bk, r_bk = pb()
                                S.group("tensor", [mk("matmul", out=bk[:], lhsT=wbd[which][:, cch, :],
                                                      rhs=xcb[:, h2 * 512:(h2 + 1) * 512], start=True, stop=True)],
                                        reads=[r_wbd[which], r_xcb], writes=[r_bk])
                                S.op("scalar", mk("activation", out=dst[:, h2 * 512:(h2 + 1) * 512], in_=bk[:],
                                                  func=AF.Sigmoid, bias=rp[:, cch, bcol:bcol + 1], scale=1.0),
                                     reads=[r_bk, r_rp], writes=[r_dst])
                        S.op("scalar", mk("activation", out=ag[:], in_=rg[:], func=AF.Exp, scale=clam[:, cch:cch + 1]),
                             reads=[r_rg, r_clam], writes=[r_ag])
                        S.op("vector", mk("tensor_tensor", out=t1[:], in0=ag[:], in1=ag[:], op=ALU.mult),
                             reads=[r_ag], writes=[r_t1])
                        S.op("vector", mk("tensor_scalar", out=t1[:], in0=t1[:], scalar1=-1.0, scalar2=1.0,
                                          op0=ALU.mult, op1=ALU.add), reads=[r_t1], writes=[r_t1])
                        S.op("vector", mk("tensor_scalar", out=t1[:], in0=t1[:], scalar1=1e-30, scalar2=None,
                                          op0=ALU.max), reads=[r_t1], writes=[r_t1])
                        S.op("scalar", mk("activation", out=t1[:], in_=t1[:], func=AF.Sqrt), reads=[r_t1], writes=[r_t1])
                        S.op("gpsimd", mk("tensor_tensor", out=ig[:], in0=ig[:], in1=xc[:], op=ALU.mult),
                             reads=[r_xc], writes=[r_ig])
                        S.op("gpsimd", mk("tensor_tensor", out=ig[:], in0=ig[:], in1=t1[:], op=ALU.mult),
                             reads=[r_t1], writes=[r_ig])
                        S.op("vector", mk("tensor_tensor_scan", out=hs[:], data0=ag[:], data1=ig[:],
                                          initial=state[:, cch:cch + 1], op0=ALU.mult, op1=ALU.add),
                             reads=[r_ag, r_ig, r_state], writes=[r_hs])
                        S.op("vector", mk("tensor_copy", out=state[:, cch:cch + 1], in_=hs[:, NB - 1:NB]),
                             reads=[r_hs], writes=[r_state])
                        if seg >= 2:
                            o0 = (seg - 2) * NB
                            for h2 in range(2):
                                bk, r_bk = pb()
                                S.group("tensor", [mk("matmul", out=bk[:], lhsT=wgr[sl][:, kc, :],
                                                      rhs=hT[:, kc, t0 + h2 * 512:t0 + (h2 + 1) * 512],
                                                      start=(kc == 0), stop=(kc == 7)) for kc in range(8)],
                                        reads=[r_wgr[sl]] + rh, writes=[r_bk])
                                S.op("scalar", mk("activation", out=gg[:, h2 * 512:(h2 + 1) * 512], in_=bk[:],
                                                  func=AF.Gelu_apprx_tanh), reads=[r_bk], writes=[r_gg])
                            S.op("gpsimd", mk("tensor_tensor", out=mixT[:, 4 + cch, o0:o0 + NB], in0=hs[:], in1=gg[:],
                                              op=ALU.mult), reads=[r_hs, r_gg],
                                 writes=r_bufA[4 + cch][(seg - 2) * 8:(seg - 1) * 8])
                S.barrier()
            with ExitStack() as st_at:
                wqkv = sbt(st_at, "wqkv", [128, 3, 8, 128], BF16); r_wqkv3 = RL(3)
                qTa = sbt(st_at, "qTa", [128, TOWN], BF16); r_qTa = Res()
                qTb = sbt(st_at, "qTb", [128, TOWN], BF16); r_qTb = Res()
                kT = sbt(st_at, "kT", [128, TALL], BF16); r_kT = Res()
                vT = sbt(st_at, "vT", [128, TALL], BF16); r_vT = Res()
                Vta = sbt(st_at, "Vta", [128, 32, 128], BF16); r_Vt = RL(8)
                Vtb = sbt(st_at, "Vtb", [128, 32, 128], BF16)
                accN = sbt(st_at, "accN", [128, TOWN], F32); r_accN = Res()
                accD = sbt(st_at, "accD", [128, TOWN], F32); r_accD = Res()
                PT = [sbt(st_at, "PT%d" % i, [128, 512], BF16) for i in range(2)]; r_PT = RL(2)
                pt_rr = 0
                S.op("gpsimd", mk("memset", ap=Vta[:], constant=0.0), writes=r_Vt)
                S.op("gpsimd", mk("memset", ap=Vtb[:], constant=0.0), writes=r_Vt)
                S.op("gpsimd", mk("memset", ap=qTa[:], constant=0.0), writes=[r_qTa])
                S.op("gpsimd", mk("memset", ap=qTb[:], constant=0.0), writes=[r_qTb])
                for fc in range(4):
                    for i3 in range(3):
                        S.dma(wqkv[:, i3, :, :], wview(w_in, i3 * 512 + fc * 128, 128), writes=[r_wqkv3[i3]], eng="gpsimd")
                    for tb in range(4):
                        bk, r_bk = pb()
                        c0 = TOWN + tb * 512
                        S.group("tensor", [mk("matmul", out=bk[:], lhsT=wqkv[:, 0, kc, :], rhs=hT[:, kc, c0:c0 + 512],
                                              start=(kc == 0), stop=(kc == 7)) for kc in range(8)],
                                reads=[r_wqkv3[0]] + r_hT[16 + tb * 4:16 + (tb + 1) * 4], writes=[r_bk])
                        S.op("scalar", mk("activation", out=qTa[0:64, tb * 512:(tb + 1) * 512], in_=bk[0:64, :],
                                          func=AF.Copy), reads=[r_bk], writes=[r_qTa])
                        S.op("vector", mk("tensor_copy", out=qTb[64:128, tb * 512:(tb + 1) * 512], in_=bk[64:128, :]),
                             reads=[r_bk], writes=[r_qTb])
                    for tb in range(8):
                        bk, r_bk = pb()
                        c0 = tb * 512
                        S.group("tensor", [mk("matmul", out=bk[:], lhsT=wqkv[:, 1, kc, :], rhs=hT[:, kc, c0:c0 + 512],
                                              start=(kc == 0), stop=(kc == 7)) for kc in range(8)],
                                reads=[r_wqkv3[1]] + r_hT[tb * 4:(tb + 1) * 4], writes=[r_bk])
                        evac(kT[:, c0:c0 + 512], bk[:], [r_bk], [r_kT])
                    for tb in range(8):
                        bk, r_bk = pb()
                        c0 = tb * 512
                        S.group("tensor", [mk("matmul", out=bk[:], lhsT=wqkv[:, 2, kc, :], rhs=hT[:, kc, c0:c0 + 512],
                                              start=(kc == 0), stop=(kc == 7)) for kc in range(8)],
                                reads=[r_wqkv3[2]] + r_hT[tb * 4:(tb + 1) * 4], writes=[r_bk])
                        evac(vT[:, c0:c0 + 512], bk[:], [r_bk], [r_vT])
                    for pat, d in enumerate((1, 4, 16)):
                        L = TALL // d
                        nj = L // 128
                        for g8 in range(4):
                            bk, r_bk = pb()
                            pv = bk[:].bitcast(BF16).rearrange("p (u c) -> p u c", u=8)
                            fns = []
                            for u in range(8):
                                ti = g8 * 8 + u
                                r_, j_ = divmod(ti, nj)
                                s0 = r_ + d * 128 * j_
                                fns.append(mk("transpose", out=pv[:, u, :], in_=vT[:, s0:s0 + d * 127 + 1:d], identity=ident[:]))
                            S.group("tensor", fns, reads=[r_vT, r_ident], writes=[r_bk])
                            S.op("scalar", mk("activation", out=Vta[:, g8 * 8:(g8 + 1) * 8, 0:64], in_=pv[:, :, 0:64],
                                              func=AF.Copy), reads=[r_bk], writes=[r_Vt[2 * g8], r_Vt[2 * g8 + 1]])
                            S.op("vector", mk("tensor_copy", out=Vtb[:, g8 * 8:(g8 + 1) * 8, 64:128], in_=pv[:, :, 64:128]),
                                 reads=[r_bk], writes=[r_Vt[2 * g8], r_Vt[2 * g8 + 1]])
                        for su in range(4):
                            bkN, r_bkN = pb()
                            bkD, r_bkD = pb()
                            for u in range(4):
                                if d == 1:
                                    r_, jq = 0, 16 + 4 * su + u
                                elif d == 4:
                                    r_, jq = u, 4 + su
                                else:
                                    r_, jq = 4 * su + u, 1
                                q0 = r_ + d * 128 * jq - TOWN
                                kp0 = r_ + d * 128 * (jq - 1)
                                kc0 = r_ + d * 128 * jq
                                tip = r_ * nj + jq - 1
                                tic = r_ * nj + jq
                                boundary = (jq == nj // 2)
                                span = d * 127 + 1
                                bkS, r_bkS = pb()
                                msk = maskB if boundary else negmask
                                fns = [mk("matmul", out=bkS[:], lhsT=ident[:], rhs=msk[:], start=True, stop=False)]
                                for bi, (qq, k0) in enumerate(((qTa, kp0), (qTa, kc0), (qTb, kp0), (qTb, kc0))):
                                    fns.append(mk("matmul", out=bkS[:, bi * 128:(bi + 1) * 128],
                                                  lhsT=kT[:, k0:k0 + span:d], rhs=qq[:, q0:q0 + span:d],
                                                  start=False, stop=(bi == 3)))
                                S.group("tensor", fns, reads=[r_ident, r_negmask, r_maskB, r_kT, r_qTa, r_qTb],
                                        writes=[r_bkS])
                                pt, r_pt = PT[pt_rr], r_PT[pt_rr]
                                pt_rr ^= 1
                                S.op("scalar", mk("activation", out=pt[:], in_=bkS[:], func=AF.Exp, scale=0.125),
                                     reads=[r_bkS], writes=[r_pt])
                                oc = slice(u * 128, (u + 1) * 128)
                                fnsN = [
                                    mk("matmul", out=bkN[:, oc], lhsT=Vta[:, tip, :], rhs=pt[:, 0:128], start=True, stop=False),
                                    mk("matmul", out=bkN[:, oc], lhsT=Vta[:, tic, :], rhs=pt[:, 128:256], start=False, stop=False),
                                    mk("matmul", out=bkN[:, oc], lhsT=Vtb[:, tip, :], rhs=pt[:, 256:384], start=False, stop=False),
                                    mk("matmul", out=bkN[:, oc], lhsT=Vtb[:, tic, :], rhs=pt[:, 384:512], start=False, stop=True),
                                ]
                                S.group("tensor", fnsN, reads=[r_pt, r_Vt[tip // 4], r_Vt[tic // 4]], writes=[r_bkN])
                                fnsD = [
                                    mk("matmul", out=bkD[:, oc], lhsT=ones_a[:], rhs=pt[:, 0:128], start=True, stop=False),
                                    mk("matmul", out=bkD[:, oc], lhsT=ones_a[:], rhs=pt[:, 128:256], start=False, stop=False),
                                    mk("matmul", out=bkD[:, oc], lhsT=ones_b[:], rhs=pt[:, 256:384], start=False, stop=False),
                                    mk("matmul", out=bkD[:, oc], lhsT=ones_b[:], rhs=pt[:, 384:512], start=False, stop=True),
                                ]
                                S.group("tensor", fnsD, reads=[r_pt, r_ones], writes=[r_bkD])
                            for acc, r_acc, bkX, r_bkX, eng in ((accN, r_accN, bkN, r_bkN, "vector"),
                                                               (accD, r_accD, bkD, r_bkD, "gpsimd")):
                                src = bkX[:].rearrange("p (u i) -> p u i", u=4)
                                if d == 1:
                                    dst = acc[:, su * 512:(su + 1) * 512].rearrange("p (u i) -> p u i", u=4)
                                elif d == 4:
                                    dst = acc[:, su * 512:(su + 1) * 512].rearrange("p (i r) -> p r i", r=4)
                                else:
                                    dst = acc[:].rearrange("p (i r) -> p r i", r=16)[:, 4 * su:4 * su + 4, :]
                                if d == 1:
                                    S.op("vector" if eng == "vector" else "scalar",
                                         mk("tensor_copy", out=dst, in_=src) if eng == "vector" else
                                         mk("activation", out=dst, in_=src, func=AF.Copy),
                                         reads=[r_bkX], writes=[r_acc])
                                else:
                                    S.op("vector", mk("tensor_tensor", out=dst, in0=dst, in1=src, op=ALU.add),
                                         reads=[r_bkX], writes=[r_acc])
                    S.op("vector", mk("reciprocal", out=accD[:], in_=accD[:]), reads=[r_accD], writes=[r_accD])
                    S.op("vector", mk("tensor_tensor", out=mixT[:, fc, :], in0=accN[:], in1=accD[:], op=ALU.mult),
                         reads=[r_accN, r_accD], writes=r_bufA[fc])
                S.barrier()
            if debug:
                with ExitStack() as st_d:
                    dtmp = sbt(st_d, "dtmp2", [128, 8 * 512], F32); r_dtmp = Res()
                    for q4 in range(4):
                        S.op("vector", mk("tensor_copy", out=dtmp[:].rearrange("p (k t) -> p k t", k=8),
                                          in_=mixT[:, :, q4 * 512:(q4 + 1) * 512]), reads=[], writes=[r_dtmp])
                        S.dma(dbg["d_mixT"].rearrange("p (k t) -> p k t", k=8)[:, :, q4 * 512:(q4 + 1) * 512],
                              dtmp[:].rearrange("p (k t) -> p k t", k=8), reads=[r_dtmp])
                    S.barrier()
            with ExitStack() as st_o:
                wout = sbt(st_o, "wout", [128, 8, D], BF16); r_wout2 = RL(2)
                S.dma(wout[:, 0:4, :], w_out[0:512, :].rearrange("(kc p) n -> p kc n", p=128), writes=[r_wout2[0]], eng="gpsimd")
                S.dma(wout[:, 4:8, :], w_out[512:1024, :].rearrange("(kc p) n -> p kc n", p=128), writes=[r_wout2[1]], eng="gpsimd")
                for t in range(16):
                    xs, r_xs = xb[t % 2], r_xb[t % 2]
                    S.dma(xs[:], x_all[TOWN + t * 128:TOWN + (t + 1) * 128, :], writes=[r_xs])
                    for cc in range(2):
                        bk, r_bk = pb()
                        cs = slice(cc * 512, (cc + 1) * 512)
                        S.group("tensor", [mk("matmul", out=bk[:], lhsT=mixT[:, kc, t * 128:(t + 1) * 128],
                                              rhs=wout[:, kc, cs], start=(kc == 0), stop=(kc == 7)) for kc in range(8)],
                                reads=r_wout2 + [r_bufA[kc][t] for kc in range(8)], writes=[r_bk])
                        S.op("vector", mk("tensor_tensor", out=tmpf[:, cs], in0=bk[:], in1=mv2[:, cs], op=ALU.mult),
                             reads=[r_bk, r_mv2], writes=[r_tmpf])
                        S.op("gpsimd", mk("tensor_tensor", out=x1[:, t, cs], in0=tmpf[:, cs], in1=xs[:, cs], op=ALU.add),
                             reads=[r_tmpf, r_xs], writes=[r_x1[t]])
                S.barrier()
        if debug:
            for t in range(16):
                S.dma(dbg["d_x1"][t * 128:(t + 1) * 128, :], x1[:, t, :], reads=[r_x1[t]])
        I32 = mybir.dt.int32
        CAP = TOWN
        x_buf = nc.dram_tensor("x_buf", [NE * CAP, D], BF16, kind="Internal").ap()
        y_buf = nc.dram_tensor("y_buf", [NE * CAP, D], F32, kind="Internal").ap()
        with ExitStack() as st_f:
            wgus = [bufA, sbt(st_f, "wgu1", [128, 8, 2 * D], BF16)]; r_wgus = [RL(4), RL(4)]
            wgus[0] = bufA[:].rearrange("p k t -> p (k t)").rearrange("p (k t) -> p k t", k=8)
            wd = sbt(st_f, "wd", [128, 8, D], BF16); r_wd = RL(2)
            gk = sbt(st_f, "gk", [128, 16, 4], F32); r_gk = RL(16)
            desti = sbt(st_f, "desti", [128, 16, 4], I32); r_desti = RL(16)
            cnti = sbt(st_f, "cnti", [128, NE], I32); r_cnti = Res()

            bgrow = [sbt(st_f, "bgrow%d" % i, [2, 2 * D], BF16) for i in range(2)]; r_bgrow = RL(2)
            ones_r = sbt(st_f, "ones_r", [2, 128], BF16); r_ones_r = Res()
            S.op("gpsimd", mk("memset", ap=ones_r[:], constant=1.0), writes=[r_ones_r])
            for i in range(2):
                S.op("gpsimd", mk("memset", ap=bgrow[i][:, 0:D], constant=0.0), writes=[r_bgrow[i]])
                S.op("gpsimd", mk("memset", ap=bgrow[i][:, D:2 * D], constant=1.0), writes=[r_bgrow[i]])

            def load_wgu(e):
                w_, r_w = wgus[e % 2], r_wgus[e % 2]
                for q4 in range(4):
                    S.dma(w_[:, :, q4 * 512:(q4 + 1) * 512], wview(w_gu[e], q4 * 512, 512), writes=[r_w[q4]], eng="gpsimd")
                S.dma(bgrow[e % 2][0:1, :], b_gu_d[e:e + 1, :], writes=[r_bgrow[e % 2]], eng="gpsimd")

            def load_wd(e):
                S.dma(wd[:, 0:4, :], w_dn[e][0:512, :].rearrange("(kc p) n -> p kc n", p=128), writes=[r_wd[0]], eng="gpsimd")
                S.dma(wd[:, 4:8, :], w_dn[e][512:1024, :].rearrange("(kc p) n -> p kc n", p=128), writes=[r_wd[1]], eng="gpsimd")

            load_wgu(0)
            load_wd(0)
            with ExitStack() as st_r2:
                wr = sbt(st_r2, "wr", [128, 8, NE], BF16); r_wr = Res()
                brt = sbt(st_r2, "brt", [128, NE], F32); r_brt = Res()
                lg_2 = [sbt(st_r2, "lg_%d" % i_, [128, NE], F32) for i_ in range(2)]; r_lg_2 = RL(2)
                ex_2 = [sbt(st_r2, "ex_%d" % i_, [128, NE], F32) for i_ in range(2)]; r_ex_2 = RL(2)
                gt_2 = [sbt(st_r2, "gt_%d" % i_, [128, NE], F32) for i_ in range(2)]; r_gt_2 = RL(2)
                posb_2 = [sbt(st_r2, "posb_%d" % i_, [128, NE], F32) for i_ in range(2)]; r_posb_2 = RL(2)
                scr4_2 = [sbt(st_r2, "scr4_%d" % i_, [128, 4, NE], F32) for i_ in range(2)]; r_scr_2 = RL(2)
                ebase = sbt(st_r2, "ebase", [128, NE], F32); r_ebase = Res()
                m8_2 = [sbt(st_r2, "m8_%d" % i_, [128, 8], F32) for i_ in range(2)]; r_m8_2 = RL(2)
                e4_2 = [sbt(st_r2, "e4_%d" % i_, [128, 4], F32) for i_ in range(2)]; r_e4_2 = RL(2)
                destf_2 = [sbt(st_r2, "destf_%d" % i_, [128, 4], F32) for i_ in range(2)]; r_destf_2 = RL(2)
                nmx_2 = [sbt(st_r2, "nmx_%d" % i_, [128, 1], F32) for i_ in range(2)]; r_nmx_2 = RL(2)
                sm_2 = [sbt(st_r2, "sm_%d" % i_, [128, 1], F32) for i_ in range(2)]; r_sm_2 = RL(2)
                gT_2 = [sbt(st_r2, "gT_%d" % i_, [32, 128], F32) for i_ in range(2)]; r_gT_2 = RL(2)
                bdn = sbt(st_r2, "bdn", [32, D], F32); r_bdn = Res()
                maskall = sbt(st_r2, "maskall", [128, 16, NE], BF16); r_mask = RL(16)
                ltri = sbt(st_r2, "ltri", [128, 128], BF16); r_ltri = Res()
                ones_f = sbt(st_r2, "ones_full", [128, 128], BF16); r_onesf = Res()
                h2Tt_2 = [sbt(st_r2, "h2Tt_%d" % i_, [128, 8, 128], BF16) for i_ in range(2)]; r_h2Tt_2 = RL(2)
                S.dma(wr[:], w_router.rearrange("(kc p) n -> p kc n", p=128), writes=[r_wr], eng="gpsimd")
                S.dma(brt[:], b_router.partition_broadcast(128), writes=[r_brt])
                S.dma(bdn[:], b_dn, writes=[r_bdn])
                S.op("gpsimd", mk("iota", out=ebase[:], pattern=[[CAP, NE]], base=0, channel_multiplier=0,
                                  allow_small_or_imprecise_dtypes=True), writes=[r_ebase])
                S.op("gpsimd", mk("memset", ap=ones_f[:], constant=1.0), writes=[r_onesf])
                S.op("gpsimd", mk("memset", ap=ltri[:], constant=1.0), writes=[r_ltri])
                S.op("gpsimd", mk("affine_select", out=ltri[:], in_=ltri[:], pattern=[[1, 128]], compare_op=ALU.is_gt,
                                  fill=0.0, base=0, channel_multiplier=-1), reads=[r_ltri], writes=[r_ltri])
                for t in range(16):
                    rms_stats(x1[:, t, :], r_x1[t], 32 + t)
                for t in range(16):
                    hbt, r_hbt = hb[t % 2], r_hb[t % 2]
                    lg, r_lg = lg_2[t % 2], r_lg_2[t % 2]
                    ex, r_ex = ex_2[t % 2], r_ex_2[t % 2]
                    gt, r_gt = gt_2[t % 2], r_gt_2[t % 2]
                    posb, r_posb = posb_2[t % 2], r_posb_2[t % 2]
                    scr4, r_scr = scr4_2[t % 2], r_scr_2[t % 2]
                    m8, r_m8 = m8_2[t % 2], r_m8_2[t % 2]
                    e4, r_e4 = e4_2[t % 2], r_e4_2[t % 2]
                    destf, r_destf = destf_2[t % 2], r_destf_2[t % 2]
                    nmx, r_nmx = nmx_2[t % 2], r_nmx_2[t % 2]
                    sm, r_sm = sm_2[t % 2], r_sm_2[t % 2]
                    gT, r_gT = gT_2[t % 2], r_gT_2[t % 2]
                    h2Tt, r_h2Tt = h2Tt_2[t % 2], r_h2Tt_2[t % 2]
                    rms_tile(x1[:, t, :], r_x1[t], 32 + t, mv4[:], r_mv4, mv3[:], r_mv3, hbt[:], r_hbt, stats=False)
                    transpose_tile(hbt, r_hbt, h2Tt[:], [r_h2Tt])
                    bk, r_bk = pb()
                    S.group("tensor", [mk("matmul", out=bk[:, 0:NE], lhsT=h2Tt[:, kc, :], rhs=wr[:, kc, :],
                                          start=(kc == 0), stop=(kc == 7)) for kc in range(8)],
                            reads=[r_wr, r_h2Tt], writes=[r_bk])
                    S.op("vector", mk("tensor_tensor", out=lg[:], in0=bk[:, 0:NE], in1=brt[:], op=ALU.add),
                         reads=[r_bk, r_brt], writes=[r_lg])
                    S.op("vector", mk("max", out=m8[:], in_=lg[:]), reads=[r_lg], writes=[r_m8])
                    S.op("vector", mk("tensor_scalar", out=nmx[:], in0=m8[:, 0:1], scalar1=-1.0, scalar2=None, op0=ALU.mult),
                         reads=[r_m8], writes=[r_nmx])
                    S.op("scalar", mk("activation", out=ex[:], in_=lg[:], func=AF.Exp, bias=nmx[:, 0:1], scale=1.0),
                         reads=[r_lg, r_nmx], writes=[r_ex])
                    S.op("scalar", mk("activation", out=e4[:], in_=m8[:, 0:4], func=AF.Exp, bias=nmx[:, 0:1], scale=1.0),
                         reads=[r_m8, r_nmx], writes=[r_e4])
                    S.op("vector", mk("tensor_scalar", out=maskall[:, t, :], in0=lg[:], scalar1=m8[:, 3:4], scalar2=None,
                                      op0=ALU.is_ge), reads=[r_lg, r_m8], writes=[r_mask[t]])
                    S.op("vector", mk("tensor_tensor", out=ex[:], in0=ex[:], in1=maskall[:, t, :], op=ALU.mult),
                         reads=[r_mask[t]], writes=[r_ex])
                    S.op("vector", mk("tensor_reduce", out=sm[:], in_=ex[:], axis=mybir.AxisListType.X, op=ALU.add),
                         reads=[r_ex], writes=[r_sm])
                    S.op("vector", mk("reciprocal", out=sm[:], in_=sm[:]), reads=[r_sm], writes=[r_sm])
                    S.op("vector", mk("tensor_scalar", out=gt[:], in0=ex[:], scalar1=sm[:, 0:1], scalar2=None,
                                      op0=ALU.mult), reads=[r_ex, r_sm], writes=[r_gt])
                    S.op("vector", mk("tensor_scalar", out=gk[:, t, :], in0=e4[:], scalar1=sm[:, 0:1], scalar2=None,
                                      op0=ALU.mult), reads=[r_e4, r_sm], writes=[r_gk[t]])
                    bkp, r_bkp = pb()
                    fns = [mk("matmul", out=bkp[:, 0:NE], lhsT=ones_f[:], rhs=maskall[:, tp, :], start=(tp == 0), stop=False)
                           for tp in range(t)]
                    fns.append(mk("matmul", out=bkp[:, 0:NE], lhsT=ltri[:], rhs=maskall[:, t, :], start=(t == 0), stop=True))
                    S.group("tensor", fns, reads=[r_onesf, r_ltri] + r_mask[:t + 1], writes=[r_bkp])
                    S.op("vector", mk("tensor_tensor", out=posb[:], in0=bkp[:, 0:NE], in1=ebase[:], op=ALU.add),
                         reads=[r_bkp, r_ebase], writes=[r_posb])
                    for k in range(4):
                        S.op("vector", mk("scalar_tensor_tensor", out=scr4[:, k, :], in0=lg[:], scalar=m8[:, k:k + 1], in1=posb[:],
                                          op0=ALU.is_equal, op1=ALU.mult),
                             reads=[r_lg, r_m8, r_posb], writes=[r_scr])
                    S.op("vector", mk("tensor_reduce", out=destf[:], in_=scr4[:], axis=mybir.AxisListType.X, op=ALU.add),
                         reads=[r_scr], writes=[r_destf])
                    S.op("vector", mk("tensor_scalar", out=destf[:], in0=destf[:], scalar1=0.0, scalar2=float(NE * CAP - 1),
                                      op0=ALU.max, op1=ALU.min), reads=[r_destf], writes=[r_destf])
                    S.op("vector", mk("tensor_copy", out=desti[:, t, :], in_=destf[:]), reads=[r_destf], writes=[r_desti[t]])
                    for k in range(4):
                        def sc(e, t=t, k=k, hbt=hbt):
                            return e.indirect_dma_start(out=x_buf[:, :],
                                                        out_offset=bass.IndirectOffsetOnAxis(ap=desti[:, t, k:k + 1], axis=0),
                                                        in_=hbt[:, :], in_offset=None)
                        S.dma_fn("gpsimd", sc, reads=[r_desti[t], r_hbt], writes=[])
                    bk2, r_bk2 = pb()
                    S.group("tensor", [mk("transpose", out=bk2[0:NE, 0:128], in_=gt[:], identity=identf[:])],
                            reads=[r_gt, r_identf], writes=[r_bk2])
                    S.op("vector", mk("tensor_copy", out=gT[:], in_=bk2[0:NE, 0:128]), reads=[r_bk2], writes=[r_gT])
                    for cc in range(2):
                        cs = slice(cc * 512, (cc + 1) * 512)
                        bk3, r_bk3 = pb()
                        S.group("tensor", [mk("matmul", out=bk3[:], lhsT=gT[:], rhs=bdn[:, cs], start=True, stop=True)],
                                reads=[r_gT, r_bdn], writes=[r_bk3])
                        S.op("vector", mk("tensor_tensor", out=tmpf[:, cs], in0=bk3[:], in1=mv5[:, cs], op=ALU.mult),
                             reads=[r_bk3, r_mv5], writes=[r_tmpf])
                        S.op("gpsimd", mk("tensor_tensor", out=x1[:, t, cs], in0=tmpf[:, cs], in1=x1[:, t, cs], op=ALU.add),
                             reads=[r_tmpf], writes=[r_x1[t]])
                bkc, r_bkc = pb()
                S.group("tensor", [mk("matmul", out=bkc[:, 0:NE], lhsT=ones_f[:], rhs=maskall[:, tp, :], start=(tp == 0),
                                      stop=(tp == 15)) for tp in range(16)], reads=[r_onesf] + r_mask, writes=[r_bkc])
                S.op("vector", mk("tensor_copy", out=cnti[:], in_=bkc[:, 0:NE]), reads=[r_bkc], writes=[r_cnti])
                if debug:
                    S.dma(dbg["d_gates"][:, 0:64], gk[:].rearrange("p t e -> p (t e)"), reads=r_gk)
                S.barrier()
            S.dma(mv3[:], g_final.partition_broadcast(128), writes=[r_mv3])
            with ExitStack() as st_m:
                xblk = [hb[0], hb[1]]; r_xblk = RL(2)
                xT1 = sbt(st_m, "xT1", [128, 8, 128], BF16)
                xTs = [junk[:].rearrange("p (k t) -> p k t", k=8), xT1[:]]; r_xT = RL(2)
                actT = [sbt(st_m, "actT%d" % i, [128, 8, 128], BF16) for i in range(2)]; r_act = [RL(2), RL(2)]
                yblk = [xb[0], xb[1]]; r_yblk = RL(2)
                sgx = sbt(st_m, "sgx", [128, D], F32)
                ucx = sbt(st_m, "ucx", [128, D], F32)
                gc2 = [[tmpf[:, 0:512], tmpf[:, 512:1024]], [sgx[:, 0:512], sgx[:, 512:1024]]]; r_gc2 = [RL(2), RL(2)]
                actk = [sbt(st_m, "actk%d" % i, [128, D], BF16) for i in range(2)]; r_actk = [RL(2), RL(2)]
                ucs = [[mv4[:, 0:512], mv4[:, 512:1024]], [ucx[:, 0:512], ucx[:, 512:1024]]]; r_uc = [RL(2), RL(2)]
                evac_dve_only[0] = True
                blk_rr = 0
                for e in range(n_experts if stage >= 2 else 0):
                    w_, r_w = wgus[e % 2], r_wgus[e % 2]
                    if e + 1 < n_experts:
                        load_wgu(e + 1)
                    S.load_count(cnti[0:1, e:e + 1], [r_cnti])
                    for jp in range(0, 16, 2):
                        S.begin_guard((e, jp), 128 * jp + 1)
                        pair = (jp, jp + 1)

                        NESTED = True

                        def inner(j, ph):
                            if j != jp and NESTED:
                                S.begin_inner((e, j, ph), 128 * j + 1)

                        def inner_end(j):
                            if j != jp and NESTED:
                                S.end_inner()
                        for j in pair:
                            s_ = j % 2
                            row0 = e * CAP + 128 * j
                            inner(j, 1)
                            S.dma(xblk[s_][:], x_buf[row0:row0 + 128, :], writes=[r_xblk[s_]])
                            transpose_tile(xblk[s_], r_xblk[s_], xTs[s_], [r_xT[s_]])
                            inner_end(j)
                        for j in pair:
                            s_ = j % 2
                            xT = xTs[s_]
                            inner(j, 2)
                            for half in range(2):
                                gub = []
                                for c0 in (half * 512, D + half * 512):
                                    bkX, r_bkX = pb()
                                    fns = [mk("matmul", out=bkX[:], lhsT=ones_r[0:2, :], rhs=bgrow[e % 2][0:2, c0:c0 + 512],
                                              start=True, stop=False)]
                                    for kc in range(8):
                                        fns.append(mk("matmul", out=bkX[:], lhsT=xT[:, kc, :], rhs=w_[:, kc, c0:c0 + 512],
                                                      start=False, stop=(kc == 7)))
                                    S.group("tensor", fns, reads=[r_w[c0 // 512], r_xT[s_], r_ones_r, r_bgrow[e % 2]], writes=[r_bkX])
                                    gub.append((bkX, r_bkX))
                                (bkG, r_bkG), (bkU, r_bkU) = gub
                                uc, r_uc_ = ucs[s_][half], r_uc[s_][half]
                                gc, r_gc_ = gc2[s_][half], r_gc2[s_][half]
                                S.op("scalar", mk("activation", out=gc, in_=bkG[:], func=AF.Gelu_apprx_sigmoid),
                                     reads=[r_bkG], writes=[r_gc_])
                                S.op("vector", mk("tensor_scalar", out=uc, in0=bkU[:], scalar1=8.0, scalar2=-6.0,
                                                  op0=ALU.min, op1=ALU.max), reads=[r_bkU], writes=[r_uc_])
                                S.op("vector", mk("scalar_tensor_tensor", out=actk[s_][:, half * 512:(half + 1) * 512], in0=gc,
                                                  scalar=GLU7, in1=uc, op0=ALU.min, op1=ALU.mult),
                                     reads=[r_uc_, r_gc_], writes=[r_actk[s_][half]])
                            inner_end(j)
                        for j in pair:
                            s_ = j % 2
                            row0 = e * CAP + 128 * j
                            inner(j, 3)
                            for half in range(2):
                                transpose_tile(actk[s_], r_actk[s_][half], actT[s_][:, 4 * half:4 * half + 4, :],
                                               [r_act[s_][half]], k0=4 * half, k1=4 * half + 4)
                            for cc in range(2):
                                cs = slice(cc * 512, (cc + 1) * 512)
                                bk, r_bk = pb()
                                S.group("tensor", [mk("matmul", out=bk[:], lhsT=actT[s_][:, kc, :], rhs=wd[:, kc, cs],
                                                      start=(kc == 0), stop=(kc == 7)) for kc in range(8)],
                                        reads=r_wd + r_act[s_], writes=[r_bk])
                                S.op("vector", mk("tensor_tensor", out=yblk[s_][:, cs], in0=bk[:], in1=mv5[:, cs], op=ALU.mult),
                                     reads=[r_bk, r_mv5], writes=[r_yblk[s_]])
                            S.dma(y_buf[row0:row0 + 128, :], yblk[s_][:], reads=[r_yblk[s_]], eng="gpsimd")
                            inner_end(j)
                        S.end_guard()
                    if e + 1 < n_experts:
                        load_wd(e + 1)
                evac_dve_only[0] = False
                S.barrier()
            st_c = st_f.enter_context(ExitStack())
            ykb = [mv4, tmpf] + [sbt(st_c, "ykb%d" % i, [128, D], F32) for i in range(4)]; r_ykb = RL(6)
            gi = 0
            for t in range(16):
                for k in range(4 if stage >= 3 else 0):
                    yk, r_yk = ykb[gi % 6], r_ykb[gi % 6]
                    gi += 1

                    def ga(e, t=t, k=k, yk=yk):
                        return e.indirect_dma_start(out=yk[:, :], out_offset=None, in_=y_buf[:, :],
                                                    in_offset=bass.IndirectOffsetOnAxis(ap=desti[:, t, k:k + 1], axis=0))
                    S.dma_fn("gpsimd", ga, reads=[r_desti[t]], writes=[r_yk])
                    S.op("vector", mk("scalar_tensor_tensor", out=x1[:, t, :], in0=yk[:], scalar=gk[:, t, k:k + 1],
                                      in1=x1[:, t, :], op0=ALU.mult, op1=ALU.add),
                         reads=[r_yk, r_gk[t]], writes=[r_x1[t]])
            for t in range(16):
                ob = xb[t % 2]; r_ob = r_xb[t % 2]
                rms_tile(x1[:, t, :], r_x1[t], 48 + t, mv3[:], r_mv3, None, None, ob[:], r_ob)
                S.dma(out_d[t * 128:(t + 1) * 128, :], ob[:], reads=[r_ob])
            S.barrier()
        with nc.Block() as block:
            S.emit(block)
    return nc


_NC_CACHE = {}


def make_in_maps(inputs, cores, n_experts=NE):
    f = lambda a: np.ascontiguousarray(np.asarray(a, dtype=np.float32))
    x = f(inputs["x"]); c = f(inputs["c"])
    w_ada = f(inputs["w_ada"])[0]; b_ada = f(inputs["b_ada"])[0][None, :]
    g_mix = f(inputs["g_mix"])[0][None, :]; g_ffn = f(inputs["g_ffn"])[0][None, :]
    g_final = f(inputs["g_final"])[None, :]
    w_in = f(inputs["w_in"])[0]; w_out = f(inputs["w_out"])[0]
    conv_w = f(inputs["conv_w"])[0]; conv_b = f(inputs["conv_b"])[0]
    b_a = f(inputs["b_rg_a"])[0]; b_x = f(inputs["b_rg_x"])[0]; lam = f(inputs["lam"])[0]
    recp = np.zeros((128, 4, 8), np.float32)
    cols = [conv_w[0], conv_w[1], conv_w[2], conv_w[3], conv_b, b_a, b_x, lam]
    for j, v in enumerate(cols):
        recp[:, :, j] = v.reshape(4, 128).T
    def blockdiag(w):
        w = f(w)[0]
        o = np.zeros((128, 4, 128), np.float32)
        for blk in range(8):
            cch, hh = divmod(blk, 2)
            o[hh * 64:(hh + 1) * 64, cch, hh * 64:(hh + 1) * 64] = w[blk]
        return o
    wa_bd = blockdiag(inputs["w_rg_a"]); wx_bd = blockdiag(inputs["w_rg_x"])
    w_router = f(inputs["w_router"])[0]; b_router = f(inputs["b_router"])[0][None, :]
    w_gu = f(inputs["w_gate_up"])[0][:n_experts]; b_gu = f(inputs["b_gate_up"])[0]
    bgu_t = np.ascontiguousarray(b_gu.reshape(NE, 16, 128).transpose(2, 0, 1))
    w_dn = f(inputs["w_down"])[0][:n_experts]; b_dn = f(inputs["b_down"])[0]
    maps = []
    for core in cores:
        b, half = divmod(core, 2)
        x_all = np.zeros((TALL, D), np.float32)
        if half == 0:
            x_all[TOWN:] = x[b, :TOWN]
        else:
            x_all[:] = x[b]
        c_rep = np.ascontiguousarray(np.broadcast_to(c[b].reshape(8, 128).T[:, :, None], (128, 8, 128)))
        flag = np.full((128, 1), float(half), np.float32)
        maps.append({
            "x_all": x_all, "c_rep": c_rep, "flag": flag, "w_ada": w_ada, "b_ada": b_ada, "g_mix": g_mix,
            "g_ffn": g_ffn, "g_final": g_final, "w_in": w_in, "w_out": w_out, "recp": recp, "wa_bd": wa_bd,
            "wx_bd": wx_bd, "w_router": w_router, "b_router": b_router, "w_gate_up": w_gu, "bgu_t": bgu_t, "b_gu": b_gu,
            "w_down": w_dn, "b_down": b_dn,
        })
    return maps


def kernel(**inputs):
    if "nc" not in _NC_CACHE:
        _NC_CACHE["nc"] = build()
    nc = _NC_CACHE["nc"]
    cores = list(range(8))
    in_maps = make_in_maps(inputs, cores)
    res = run_bass_kernel_spmd(nc, in_maps, core_ids=cores)
    out = np.zeros((4, 4096, D), np.float32)
    for core in cores:
        b, half = divmod(core, 2)
        out[b, half * TOWN:(half + 1) * TOWN] = res.results[core]["out"]
    return out
```

```python
import numpy as np
from contextlib import ExitStack
import concourse.bass as bass
import concourse.mybir as mybir
from concourse.bass_utils import run_bass_kernel_spmd

F32 = mybir.dt.float32
BF16 = mybir.dt.bfloat16
AF = mybir.ActivationFunctionType
ALU = mybir.AluOpType

D = 1024
TOWN = 2048
TALL = 4096
NE = 32
EPS = 1e-6
GLU7 = float(np.float32(7.0) / (np.float32(1.0) + np.exp(np.float32(-1.702 * 7.0))))
SEM_LIMIT = 30000


class Res:
    __slots__ = ("w", "r")

    def __init__(self):
        self.w = None
        self.r = {}


def RL(n):
    return [Res() for _ in range(n)]


class Sched:
    ENGS = ("tensor", "vector", "scalar", "gpsimd", "sync")

    def __init__(self, nc, stack, n_dma_sems=8):
        self.nc = nc
        self.stack = stack
        self.ops = {e: [] for e in self.ENGS}
        self.known = {e: {} for e in self.ENGS}
        self.dom_sem = {}
        self.dom_max = {}
        self.ndom = 0
        self.cur_dom = {}
        self.cur_cnt = {}
        self.guard = None
        self.guard_snap = {}
        self.regs = {}
        for e in self.ENGS:
            self.cur_dom[e] = self._new_dom(e)
            self.cur_cnt[e] = 0
        self.dma_pool = {}
        for q in ("sync", "gpsimd"):
            self.dma_pool[q] = {"doms": [self._new_dom("dma_%s%d" % (q, i)) for i in range(n_dma_sems)],
                                "cnt": [0] * n_dma_sems, "rr": 0}

    def _new_dom(self, name):
        d = self.ndom
        self.ndom += 1
        self.dom_sem[d] = self.stack.enter_context(self.nc.semaphore("s_%s_%d" % (name, d)))
        self.dom_max[d] = 0
        return d

    def _collect(self, eng, reads, writes):
        need = {}
        for R in reads:
            if R.w is not None:
                d, v = R.w
                if need.get(d, 0) < v:
                    need[d] = v
        for R in writes:
            if R.w is not None:
                d, v = R.w
                if need.get(d, 0) < v:
                    need[d] = v
            for d, v in R.r.items():
                if need.get(d, 0) < v:
                    need[d] = v
        kn = self.known[eng]
        waits = []
        for d, v in need.items():
            if eng == "tensor" and d == self.cur_dom["tensor"]:
                continue
            if kn.get(d, 0) < v:
                kn[d] = v
                waits.append((d, v))
        return waits

    def _tick(self, eng):
        self.cur_cnt[eng] += 1
        d = self.cur_dom[eng]
        v = self.cur_cnt[eng]
        self.dom_max[d] = v
        return d, v

    def _mark(self, d, v, reads, writes):
        for R in reads:
            R.r[d] = v
        for R in writes:
            R.w = (d, v)
            R.r = {}

    def op(self, eng, fn, reads=(), writes=()):
        waits = self._collect(eng, reads, writes)
        d, v = self._tick(eng)
        self.ops[eng].append((waits, fn, self.dom_sem[d], 1, self.guard))
        self._mark(d, v, reads, writes)

    def group(self, eng, fns, reads=(), writes=()):
        waits = self._collect(eng, reads, writes)
        d, v = self._tick(eng)
        n = len(fns)
        for i, fn in enumerate(fns):
            self.ops[eng].append((waits if i == 0 else [], fn,
                                  self.dom_sem[d] if i == n - 1 else None, 1, self.guard))
        self._mark(d, v, reads, writes)

    def dma_fn(self, eng, fn, reads=(), writes=()):
        pool = self.dma_pool[eng]
        i = pool["rr"]
        pool["rr"] = (i + 1) % len(pool["doms"])
        d = pool["doms"][i]
        waits = self._collect(eng, reads, writes)
        prev = pool["cnt"][i]
        kn = self.known[eng]
        if prev > 0 and kn.get(d, 0) < prev:
            kn[d] = prev
            waits.append((d, prev))
        pool["cnt"][i] += 16
        v = pool["cnt"][i]
        self.dom_max[d] = v
        self.ops[eng].append((waits, fn, self.dom_sem[d], 16, self.guard))
        self._mark(d, v, reads, writes)

    def dma(self, out, in_, reads=(), writes=(), eng="sync"):
        def fn(e, out=out, in_=in_):
            return e.dma_start(out=out, in_=in_)
        self.dma_fn(eng, fn, reads, writes)

    def load_counts(self, ap_of, n, reads):
        self.n_count_regs = n
        for eng in self.ENGS:
            waits = self._collect(eng, reads, ())
            sched = self
            for k in range(n):
                def fn(e, eng=eng, k=k):
                    return e.reg_load(sched.regs[eng][k], ap_of(k))
                self.ops[eng].append((waits if k == 0 else [], fn, None, 0, None))

    def begin_guard(self, gid, thr):
        self.guard = (gid, thr, None, 0)
        self.guard_snap[gid] = dict(self.dom_max)

    def begin_inner(self, gid, thr):
        g = self.guard
        self.guard = (g[0], g[1], gid, thr)
        self.guard_snap[gid] = dict(self.dom_max)

    def end_inner(self):
        g = self.guard
        self.guard = (g[0], g[1], None, 0)

    def end_guard(self):
        self.guard = None

    def barrier(self):
        assert self.guard is None
        for eng in self.ENGS:
            kn = self.known[eng]
            waits = []
            for d, v in self.dom_max.items():
                if v > 0 and kn.get(d, 0) < v:
                    kn[d] = v
                    waits.append((d, v))
            if waits:
                self.ops[eng].append((waits, None, None, 0, None))

    def emit(self, block):
        sched = self

        def emit_one(e, item):
            waits, fn, sem, inc, _ = item
            for (d_, v) in waits:
                e.wait_ge(sched.dom_sem[d_], v)
            if fn is None:
                return
            ins = fn(e)
            if sem is not None:
                ins.then_inc(sem, inc)

        def skip_path(e, grp, snap):
            incs = []
            need = {}
            for waits, fn, sem, inc, _ in grp:
                for (d_, v) in waits:
                    v = min(v, snap.get(d_, 0))
                    if v > need.get(d_, 0):
                        need[d_] = v
            for d_, v in need.items():
                e.wait_ge(sched.dom_sem[d_], v)
            for waits, fn, sem, inc, _ in grp:
                if sem is not None:
                    for k_ in range(len(incs)):
                        if incs[k_][0] is sem:
                            incs[k_][1] += inc
                            break
                    else:
                        incs.append([sem, inc])
            e.drain()
            for sem, tot in incs:
                e.sem_inc(sem, tot)

        def emit_region(e, reg, grp):
            a = 0
            m = len(grp)
            while a < m:
                gi = grp[a][4]
                if gi[2] is None:
                    emit_one(e, grp[a])
                    a += 1
                    continue
                b = a
                while b < m and grp[b][4][2] == gi[2]:
                    b += 1
                sub = grp[a:b]
                with e.If_lt(reg, gi[3]):
                    skip_path(e, sub, sched.guard_snap[gi[2]])
                with e.Else():
                    for it in sub:
                        emit_one(e, it)
                a = b

        def emit_chain(e, reg, regions, idx):
            if idx == len(regions):
                return
            grp = regions[idx]
            g = grp[0][4]
            rest = [it for r_ in regions[idx:] for it in r_]
            with e.If_lt(reg, g[1]):
                skip_path(e, rest, sched.guard_snap[g[0]])
            with e.Else():
                emit_region(e, reg, grp)
                emit_chain(e, reg, regions, idx + 1)

        def make(engname):
            def body(e):
                ops = sched.ops[engname]
                sched.regs[engname] = [e.alloc_register("cnt%d_%s" % (k_, engname))
                                       for k_ in range(getattr(sched, "n_count_regs", 1))]
                i = 0
                n = len(ops)
                while i < n:
                    g = ops[i][4]
                    if g is None:
                        emit_one(e, ops[i])
                        i += 1
                        continue
                    regions = []
                    j = i
                    while j < n and ops[j][4] is not None and ops[j][4][0][0] == g[0][0]:
                        k = j
                        while k < n and ops[k][4] is not None and ops[k][4][0] == ops[j][4][0]:
                            k += 1
                        regions.append(ops[j:k])
                        j = k
                    emit_chain(e, sched.regs[engname][g[0][0]], regions, 0)
                    i = j
            return body
        for engname in self.ENGS:
            if self.ops[engname]:
                getattr(block, engname)(make(engname))


def mk(method, **kw):
    return lambda e: getattr(e, method)(**kw)


def build(debug=False, n_experts=NE, stage=3):
    nc = bass.Bass("TRN2", target_bir_lowering=False)

    def din(name, shape):
        return nc.dram_tensor(name, shape, F32, kind="ExternalInput").ap()

    x_all = din("x_all", [TALL, D])
    c_rep = din("c_rep", [128, 8, 128])
    flag_d = din("flag", [128, 1])
    w_ada = din("w_ada", [D, 6 * D])
    b_ada = din("b_ada", [1, 6 * D])
    g_mix = din("g_mix", [1, D])
    g_ffn = din("g_ffn", [1, D])
    g_final = din("g_final", [1, D])
    w_in = din("w_in", [D, 2560])
    w_out = din("w_out", [D, D])
    recp = din("recp", [128, 4, 8])
    wa_bd = din("wa_bd", [128, 4, 128])
    wx_bd = din("wx_bd", [128, 4, 128])
    w_router = din("w_router", [D, NE])
    b_router = din("b_router", [1, NE])
    w_gu = din("w_gate_up", [n_experts, D, 2 * D])
    bgu_t = din("bgu_t", [128, NE, 16])
    b_gu_d = din("b_gu", [NE, 2 * D])
    w_dn = din("w_down", [n_experts, D, D])
    b_dn = din("b_down", [NE, D])
    out_d = nc.dram_tensor("out", [TOWN, D], F32, kind="ExternalOutput").ap()
    dbg = {}
    if debug:
        def dout(name, shape):
            dbg[name] = nc.dram_tensor(name, shape, F32, kind="ExternalOutput").ap()
        dout("d_mod", [128, 6 * D])
        dout("d_hT", [128, 8 * 512])
        dout("d_mixT", [128, 8 * TOWN])
        dout("d_x1", [TOWN, D])
        dout("d_gates", [128, 16 * NE])

    with ExitStack() as st:
        S = Sched(nc, st)

        def sbt(stack, name, shape, dt):
            return stack.enter_context(nc.sbuf_tensor(name, shape, dt))

        arena = sbt(st, "arena", [128, 16384], F32)
        hT = arena[:].bitcast(BF16).rearrange("p (k t) -> p k t", k=8)
        x1 = arena[:].rearrange("p (t f) -> p t f", t=16)
        r_hT = RL(32)
        r_x1 = RL(16)
        bufA = sbt(st, "bufA", [128, 8, TOWN], BF16)
        r_bufA = [RL(16) for _ in range(8)]
        mv3 = sbt(st, "mv3", [128, D], F32); r_mv3 = Res()
        mv4 = sbt(st, "mv4", [128, D], F32); r_mv4 = Res()
        mv5 = sbt(st, "mv5", [128, D], F32); r_mv5 = Res()
        xb = [sbt(st, "xb%d" % i, [128, D], F32) for i in range(2)]; r_xb = RL(2)
        tmpf = sbt(st, "tmpf", [128, D], F32); r_tmpf = Res()
        hb = [sbt(st, "hb%d" % i, [128, D], BF16) for i in range(2)]; r_hb = RL(2)
        junk = sbt(st, "junk", [128, D], BF16)
        ss = sbt(st, "ss", [128, 64], F32); r_ss = RL(64)
        ms = sbt(st, "ms", [128, 64], F32); r_ms = RL(64)
        rstd = sbt(st, "rstd", [128, 64], F32); r_rstd = RL(64)
        ident = sbt(st, "ident", [128, 128], BF16); r_ident = Res()
        identf = sbt(st, "identf", [128, 128], F32); r_identf = Res()
        neghalf = sbt(st, "neghalf", [128, 1], F32); r_nh = Res()
        flag = sbt(st, "flag_sb", [128, 1], F32); r_flag = Res()
        flagb = sbt(st, "flagb", [128, 1], F32); r_flagb = Res()
        negmask = sbt(st, "negmask", [128, 512], BF16); r_negmask = Res()
        maskB = sbt(st, "maskB", [128, 512], BF16); r_maskB = Res()
        ones_a = sbt(st, "ones_a", [128, 128], BF16); r_ones = Res()
        ones_b = sbt(st, "ones_b", [128, 128], BF16)

        banks = [st.enter_context(nc.psum_tensor("bank%d" % i, [128, 512], F32)) for i in range(8)]
        r_bank = RL(8)
        bank_rr = [0]

        def pb():
            i = bank_rr[0]
            bank_rr[0] = (i + 1) % 8
            return banks[i], r_bank[i]

        cp_rr = [0]
        evac_dve_only = [False]

        def evac(out, in_, reads, writes):
            cp_rr[0] ^= 1
            if cp_rr[0] and not evac_dve_only[0]:
                S.op("scalar", mk("activation", out=out, in_=in_, func=AF.Copy), reads=reads, writes=writes)
            else:
                S.op("vector", mk("tensor_copy", out=out, in_=in_), reads=reads, writes=writes)

        S.dma(flag[:], flag_d, writes=[r_flag])
        S.op("gpsimd", mk("memset", ap=neghalf[:], constant=-0.5), writes=[r_nh])
        S.op("gpsimd", mk("memset", ap=ident[:], constant=1.0), writes=[r_ident])
        S.op("gpsimd", mk("affine_select", out=ident[:], in_=ident[:], pattern=[[-1, 128]],
                          compare_op=ALU.is_equal, fill=0.0, base=0, channel_multiplier=1),
             reads=[r_ident], writes=[r_ident])
        S.op("gpsimd", mk("memset", ap=identf[:], constant=1.0), writes=[r_identf])
        S.op("gpsimd", mk("affine_select", out=identf[:], in_=identf[:], pattern=[[-1, 128]],
                          compare_op=ALU.is_equal, fill=0.0, base=0, channel_multiplier=1),
             reads=[r_identf], writes=[r_identf])
        S.op("gpsimd", mk("memset", ap=negmask[:], constant=0.0), writes=[r_negmask])
        for blk in range(4):
            sl = negmask[:, blk * 128:(blk + 1) * 128]
            if blk % 2 == 0:
                S.op("gpsimd", mk("affine_select", out=sl, in_=sl, pattern=[[-1, 128]], compare_op=ALU.is_ge,
                                  fill=-30000.0, base=0, channel_multiplier=1), reads=[r_negmask], writes=[r_negmask])
            else:
                S.op("gpsimd", mk("affine_select", out=sl, in_=sl, pattern=[[1, 128]], compare_op=ALU.is_ge,
                                  fill=-30000.0, base=0, channel_multiplier=-1), reads=[r_negmask], writes=[r_negmask])
        S.op("vector", mk("tensor_scalar", out=flagb[:], in0=flag[:], scalar1=-1.0, scalar2=30000.0,
                          op0=ALU.add, op1=ALU.mult), reads=[r_flag], writes=[r_flagb])
        S.op("vector", mk("tensor_copy", out=maskB[:], in_=negmask[:]), reads=[r_negmask], writes=[r_maskB])
        for blk in (0, 2):
            sl = maskB[:, blk * 128:(blk + 1) * 128]
            S.op("vector", mk("tensor_scalar", out=sl, in0=sl, scalar1=flagb[:, 0:1], scalar2=None, op0=ALU.add),
                 reads=[r_maskB, r_flagb], writes=[r_maskB])
        S.op("gpsimd", mk("memset", ap=ones_a[:], constant=0.0), writes=[r_ones])
        S.op("gpsimd", mk("memset", ap=ones_a[:, 0:64], constant=1.0), writes=[r_ones])
        S.op("gpsimd", mk("memset", ap=ones_b[:], constant=0.0), writes=[r_ones])
        S.op("gpsimd", mk("memset", ap=ones_b[:, 64:128], constant=1.0), writes=[r_ones])

        def wview(w2d, c0, n):
            return w2d[:, c0:c0 + n].rearrange("(kc p) n -> p kc n", p=128)

        r_junk = Res()

        def rms_stats(src, r_src, col):
            S.op("scalar", mk("activation", out=junk[:], in_=src, func=AF.Square, accum_out=ss[:, col:col + 1]),
                 reads=[r_src], writes=[r_ss[col], r_junk])
            S.op("vector", mk("tensor_scalar", out=ms[:, col:col + 1], in0=ss[:, col:col + 1], scalar1=1.0 / D,
                              scalar2=EPS, op0=ALU.mult, op1=ALU.add), reads=[r_ss[col]], writes=[r_ms[col]])
            S.op("gpsimd", mk("tensor_tensor", out=rstd[:, col:col + 1], in0=ms[:, col:col + 1], in1=neghalf[:],
                              op=ALU.pow), reads=[r_ms[col], r_nh], writes=[r_rstd[col]])

        def rms_tile(src, r_src, col, A_vec, r_A, B_vec, r_B, hbt, r_hbt, stats=True):
            if stats:
                rms_stats(src, r_src, col)
            if B_vec is None:
                S.op("vector", mk("scalar_tensor_tensor", out=hbt, in0=src, scalar=rstd[:, col:col + 1], in1=A_vec,
                                  op0=ALU.mult, op1=ALU.mult), reads=[r_src, r_rstd[col], r_A], writes=[r_hbt])
                return
            S.op("vector", mk("scalar_tensor_tensor", out=tmpf[:], in0=src, scalar=rstd[:, col:col + 1], in1=A_vec,
                              op0=ALU.mult, op1=ALU.mult), reads=[r_src, r_rstd[col], r_A], writes=[r_tmpf])
            S.op("vector", mk("tensor_tensor", out=hbt, in0=tmpf[:], in1=B_vec, op=ALU.add),
                 reads=[r_tmpf, r_B], writes=[r_hbt])

        def transpose_tile(hbt, r_hbt, dstT, r_dst, k0=0, k1=8):
            bk, r_bk = pb()
            pv = bk[:].bitcast(BF16).rearrange("p (k t) -> p k t", k=8)
            S.group("tensor", [mk("transpose", out=pv[:, kc, :], in_=hbt[:, kc * 128:(kc + 1) * 128], identity=ident[:])
                               for kc in range(k0, k1)], reads=[r_hbt, r_ident], writes=[r_bk])
            evac(dstT, pv[:, k0:k1, :], [r_bk], r_dst)

        with ExitStack() as st_mix:
            mv2 = sbt(st_mix, "mv2", [128, D], F32); r_mv2 = Res()
            with ExitStack() as st_a:
                mv0 = sbt(st_a, "mv0", [128, D], F32); r_mv0 = Res()
                mv1 = sbt(st_a, "mv1", [128, D], F32); r_mv1 = Res()
                mvs = [mv0, mv1, mv2, mv3, mv4, mv5]
                r_mvs = [r_mv0, r_mv1, r_mv2, r_mv3, r_mv4, r_mv5]
                with ExitStack() as st0:
                    bada = sbt(st0, "bada", [128, 6 * D], F32); r_bada = Res()
                    gmix_bc = sbt(st0, "gmix_bc", [128, D], F32); r_gmix = Res()
                    gffn_bc = sbt(st0, "gffn_bc", [128, D], F32); r_gffn = Res()
                    wab = [sbt(st0, "wab%d" % i, [128, 8, 512], BF16) for i in range(2)]; r_wab = RL(2)
                    c_bf = sbt(st0, "c_bf", [128, 8, 128], BF16); r_cbf = Res()
                    S.dma(c_bf[:], c_rep, writes=[r_cbf], eng="gpsimd")
                    S.dma(bada[:], b_ada.partition_broadcast(128), writes=[r_bada])
                    S.dma(gmix_bc[:], g_mix.partition_broadcast(128), writes=[r_gmix])
                    S.dma(gffn_bc[:], g_ffn.partition_broadcast(128), writes=[r_gffn])
                    for cc in range(12):
                        wb_, r_wb_ = wab[cc % 2], r_wab[cc % 2]
                        S.dma(wb_[:], wview(w_ada, cc * 512, 512), writes=[r_wb_], eng="gpsimd")
                        bk, r_bk = pb()
                        S.group("tensor", [mk("matmul", out=bk[:], lhsT=c_bf[:, kc, :], rhs=wb_[:, kc, :],
                                              start=(kc == 0), stop=(kc == 7)) for kc in range(8)],
                                reads=[r_cbf, r_wb_], writes=[r_bk])
                        dst = mvs[cc // 2][:, (cc % 2) * 512:(cc % 2 + 1) * 512]
                        S.op("vector", mk("tensor_tensor", out=dst, in0=bk[:], in1=bada[:, cc * 512:(cc + 1) * 512],
                                          op=ALU.add), reads=[r_bk, r_bada], writes=[r_mvs[cc // 2]])
                    if debug:
                        for i in range(6):
                            S.dma(dbg["d_mod"][:, i * D:(i + 1) * D], mvs[i][:], reads=[r_mvs[i]])
                    S.op("vector", mk("scalar_tensor_tensor", out=mv1[:], in0=mv1[:], scalar=1.0, in1=gmix_bc[:],
                                      op0=ALU.add, op1=ALU.mult), reads=[r_gmix], writes=[r_mv1])
                    S.op("vector", mk("scalar_tensor_tensor", out=mv4[:], in0=mv4[:], scalar=1.0, in1=gffn_bc[:],
                                      op0=ALU.add, op1=ALU.mult), reads=[r_gffn], writes=[r_mv4])
                    S.barrier()
                for t in range(32):
                    xs, r_xs = xb[t % 2], r_xb[t % 2]
                    S.dma(xs[:], x_all[t * 128:(t + 1) * 128, :], writes=[r_xs])
                    rms_tile(xs[:], r_xs, t, mv1[:], r_mv1, mv0[:], r_mv0, hb[t % 2][:], r_hb[t % 2])
                    transpose_tile(hb[t % 2], r_hb[t % 2], hT[:, :, t * 128:(t + 1) * 128], [r_hT[t]])
                if debug:
                    S.barrier()
                    with ExitStack() as st_d:
                        dtmp = sbt(st_d, "dtmp", [128, 8 * 512], F32); r_dtmp = Res()
                        S.op("vector", mk("tensor_copy", out=dtmp[:].rearrange("p (k t) -> p k t", k=8),
                                          in_=hT[:, :, 1792:2304]), reads=r_hT, writes=[r_dtmp])
                        S.dma(dbg["d_hT"], dtmp[:], reads=[r_dtmp])
                        S.barrier()
                S.barrier()
            mixT = bufA
            with ExitStack() as st_r:
                NB = 1024
                xr = sbt(st_r, "xr", [128, NB + 3], F32); r_xr = Res()
                xc = sbt(st_r, "xc", [128, NB], F32); r_xc = Res()
                xcb = sbt(st_r, "xcb", [128, NB], BF16); r_xcb = Res()
                rg = sbt(st_r, "rg", [128, NB], F32); r_rg = Res()
                ig = sbt(st_r, "ig", [128, NB], F32); r_ig = Res()
                ag = sbt(st_r, "ag", [128, NB], F32); r_ag = Res()
                t1 = sbt(st_r, "t1", [128, NB], F32); r_t1 = Res()
                hs = sbt(st_r, "hs", [128, NB], F32); r_hs = Res()
                gg = sbt(st_r, "gg", [128, NB], F32); r_gg = Res()
                wxr = [sbt(st_r, "wxr%d" % i, [128, 8, 128], BF16) for i in range(2)]; r_wxr = RL(2)
                wgr = [sbt(st_r, "wgr%d" % i, [128, 8, 128], BF16) for i in range(2)]; r_wgr = RL(2)
                wbd = [sbt(st_r, "wbd%d" % i, [128, 4, 128], BF16) for i in range(2)]; r_wbd = RL(2)
                rp = sbt(st_r, "rp", [128, 4, 8], F32); r_rp = Res()
                clam = sbt(st_r, "clam", [128, 4], F32); r_clam = Res()
                state = sbt(st_r, "state", [128, 4], F32); r_state = Res()
                S.dma(rp[:], recp, writes=[r_rp])
                S.dma(wbd[0][:], wa_bd, writes=[r_wbd[0]], eng="gpsimd")
                S.dma(wbd[1][:], wx_bd, writes=[r_wbd[1]], eng="gpsimd")
                S.op("scalar", mk("activation", out=clam[:], in_=rp[:, :, 7], func=AF.Exp, scale=-1.0),
                     reads=[r_rp], writes=[r_clam])
                S.op("scalar", mk("activation", out=clam[:], in_=clam[:], func=AF.Ln, bias=1.0, scale=1.0),
                     reads=[r_clam], writes=[r_clam])
                S.op("vector", mk("tensor_scalar", out=clam[:], in0=clam[:], scalar1=-8.0, scalar2=None, op0=ALU.mult),
                     reads=[r_clam], writes=[r_clam])
                S.op("vector", mk("memset", ap=state[:], constant=0.0), writes=[r_state])
                for cch in range(4):
                    sl = cch % 2
                    S.dma(wxr[sl][:], wview(w_in, 1536 + cch * 128, 128), writes=[r_wxr[sl]], eng="gpsimd")
                    S.dma(wgr[sl][:], wview(w_in, 2048 + cch * 128, 128), writes=[r_wgr[sl]], eng="gpsimd")
                    S.op("vector", mk("memset", ap=xr[:, 0:3], constant=0.0), writes=[r_xr])
                    for seg in range(4):
                        t0 = seg * NB
                        rh = r_hT[seg * 8:(seg + 1) * 8]
                        if seg == 2:
                            S.op("vector", mk("tensor_scalar", out=xr[:, 0:3], in0=xr[:, 0:3], scalar1=flag[:, 0:1],
                                              scalar2=None, op0=ALU.mult), reads=[r_flag], writes=[r_xr])
                            S.op("vector", mk("tensor_scalar", out=state[:, cch:cch + 1], in0=state[:, cch:cch + 1],
                                              scalar1=flag[:, 0:1], scalar2=None, op0=ALU.mult),
                                 reads=[r_flag], writes=[r_state])
                        for h2 in range(2):
                            bk, r_bk = pb()
                            S.group("tensor", [mk("matmul", out=bk[:], lhsT=wxr[sl][:, kc, :],
                                                  rhs=hT[:, kc, t0 + h2 * 512:t0 + (h2 + 1) * 512],
                                                  start=(kc == 0), stop=(kc == 7)) for kc in range(8)],
                                    reads=[r_wxr[sl]] + rh, writes=[r_bk])
                            S.op("scalar", mk("activation", out=xr[:, 3 + h2 * 512:3 + (h2 + 1) * 512], in_=bk[:],
                                              func=AF.Copy), reads=[r_bk], writes=[r_xr])
                        S.op("vector", mk("tensor_scalar", out=xc[:], in0=xr[:, 3:NB + 3], scalar1=rp[:, cch, 3:4],
                                          scalar2=rp[:, cch, 4:5], op0=ALU.mult, op1=ALU.add),
                             reads=[r_xr, r_rp], writes=[r_xc])
                        for j in range(3):
                            S.op("vector", mk("scalar_tensor_tensor", out=xc[:], in0=xr[:, j:j + NB],
                                              scalar=rp[:, cch, j:j + 1], in1=xc[:], op0=ALU.mult, op1=ALU.add),
                                 reads=[r_xr, r_rp], writes=[r_xc])
                        S.op("vector", mk("tensor_copy", out=xr[:, 0:3], in_=xr[:, NB:NB + 3]), writes=[r_xr])
                        S.op("scalar", mk("activation", out=xcb[:], in_=xc[:], func=AF.Copy), reads=[r_xc], writes=[r_xcb])
                        for which, (dst, r_dst, bcol) in enumerate(((rg, r_rg, 5), (ig, r_ig, 6))):
                            for h2 in range(2):
                                bk, r_bk = pb()
                                S.group("tensor", [mk("matmul", out=bk[:], lhsT=wbd[which][:, cch, :],
                                                      rhs=xcb[:, h2 * 512:(h2 + 1) * 512], start=True, stop=True)],
                                        reads=[r_wbd[which], r_xcb], writes=[r_bk])
                                S.op("scalar", mk("activation", out=dst[:, h2 * 512:(h2 + 1) * 512], in_=bk[:],
                                                  func=AF.Sigmoid, bias=rp[:, cch, bcol:bcol + 1], scale=1.0),
                                     reads=[r_bk, r_rp], writes=[r_dst])
                        S.op("scalar", mk("activation", out=ag[:], in_=rg[:], func=AF.Exp, scale=clam[:, cch:cch + 1]),
                             reads=[r_rg, r_clam], writes=[r_ag])
                        S.op("vector", mk("tensor_tensor", out=t1[:], in0=ag[:], in1=ag[:], op=ALU.mult),
                             reads=[r_ag], writes=[r_t1])
                        S.op("vector", mk("tensor_scalar", out=t1[:], in0=t1[:], scalar1=-1.0, scalar2=1.0,
                                          op0=ALU.mult, op1=ALU.add), reads=[r_t1], writes=[r_t1])
                        S.op("vector", mk("tensor_scalar", out=t1[:], in0=t1[:], scalar1=1e-30, scalar2=None,
                                          op0=ALU.max), reads=[r_t1], writes=[r_t1])
                        S.op("scalar", mk("activation", out=t1[:], in_=t1[:], func=AF.Sqrt), reads=[r_t1], writes=[r_t1])
                        S.op("gpsimd", mk("tensor_tensor", out=ig[:], in0=ig[:], in1=xc[:], op=ALU.mult),
                             reads=[r_xc], writes=[r_ig])
                        S.op("gpsimd", mk("tensor_tensor", out=ig[:], in0=ig[:], in1=t1[:], op=ALU.mult),
                             reads=[r_t1], writes=[r_ig])
                        S.op("vector", mk("tensor_tensor_scan", out=hs[:], data0=ag[:], data1=ig[:],
                                          initial=state[:, cch:cch + 1], op0=ALU.mult, op1=ALU.add),
                             reads=[r_ag, r_ig, r_state], writes=[r_hs])
                        S.op("vector", mk("tensor_copy", out=state[:, cch:cch + 1], in_=hs[:, NB - 1:NB]),
                             reads=[r_hs], writes=[r_state])
                        if seg >= 2:
                            o0 = (seg - 2) * NB
                            for h2 in range(2):
                                bk, r_bk = pb()
                                S.group("tensor", [mk("matmul", out=bk[:], lhsT=wgr[sl][:, kc, :],
                                                      rhs=hT[:, kc, t0 + h2 * 512:t0 + (h2 + 1) * 512],
                                                      start=(kc == 0), stop=(kc == 7)) for kc in range(8)],
                                        reads=[r_wgr[sl]] + rh, writes=[r_bk])
                                S.op("scalar", mk("activation", out=gg[:, h2 * 512:(h2 + 1) * 512], in_=bk[:],
                                                  func=AF.Gelu_apprx_tanh), reads=[r_bk], writes=[r_gg])
                            S.op("gpsimd", mk("tensor_tensor", out=mixT[:, 4 + cch, o0:o0 + NB], in0=hs[:], in1=gg[:],
                                              op=ALU.mult), reads=[r_hs, r_gg],
                                 writes=r_bufA[4 + cch][(seg - 2) * 8:(seg - 1) * 8])
                S.barrier()
            with ExitStack() as st_at:
                wqkv = sbt(st_at, "wqkv", [128, 3, 8, 128], BF16); r_wqkv3 = RL(3)
                qTa = sbt(st_at, "qTa", [128, TOWN], BF16); r_qTa = Res()
                qTb = sbt(st_at, "qTb", [128, TOWN], BF16); r_qTb = Res()
                kT = sbt(st_at, "kT", [128, TALL], BF16); r_kT = Res()
                vT = sbt(st_at, "vT", [128, TALL], BF16); r_vT = Res()
                Vta = sbt(st_at, "Vta", [128, 32, 128], BF16); r_Vt = RL(8)
                Vtb = sbt(st_at, "Vtb", [128, 32, 128], BF16)
                accN = sbt(st_at, "accN", [128, TOWN], F32); r_accN = Res()
                accD = sbt(st_at, "accD", [128, TOWN], F32); r_accD = Res()
                PT = [sbt(st_at, "PT%d" % i, [128, 512], BF16) for i in range(2)]; r_PT = RL(2)
                pt_rr = 0
                S.op("gpsimd", mk("memset", ap=Vta[:], constant=0.0), writes=r_Vt)
                S.op("gpsimd", mk("memset", ap=Vtb[:], constant=0.0), writes=r_Vt)
                S.op("gpsimd", mk("memset", ap=qTa[:], constant=0.0), writes=[r_qTa])
                S.op("gpsimd", mk("memset", ap=qTb[:], constant=0.0), writes=[r_qTb])
                for fc in range(4):
                    for i3 in range(3):
                        S.dma(wqkv[:, i3, :, :], wview(w_in, i3 * 512 + fc * 128, 128), writes=[r_wqkv3[i3]], eng="gpsimd")
                    for tb in range(4):
                        bk, r_bk = pb()
                        c0 = TOWN + tb * 512
                        S.group("tensor", [mk("matmul", out=bk[:], lhsT=wqkv[:, 0, kc, :], rhs=hT[:, kc, c0:c0 + 512],
                                              start=(kc == 0), stop=(kc == 7)) for kc in range(8)],
                                reads=[r_wqkv3[0]] + r_hT[16 + tb * 4:16 + (tb + 1) * 4], writes=[r_bk])
                        S.op("scalar", mk("activation", out=qTa[0:64, tb * 512:(tb + 1) * 512], in_=bk[0:64, :],
                                          func=AF.Copy), reads=[r_bk], writes=[r_qTa])
                        S.op("vector", mk("tensor_copy", out=qTb[64:128, tb * 512:(tb + 1) * 512], in_=bk[64:128, :]),
                             reads=[r_bk], writes=[r_qTb])
                    for tb in range(8):
                        bk, r_bk = pb()
                        c0 = tb * 512
                        S.group("tensor", [mk("matmul", out=bk[:], lhsT=wqkv[:, 1, kc, :], rhs=hT[:, kc, c0:c0 + 512],
                                              start=(kc == 0), stop=(kc == 7)) for kc in range(8)],
                                reads=[r_wqkv3[1]] + r_hT[tb * 4:(tb + 1) * 4], writes=[r_bk])
                        evac(kT[:, c0:c0 + 512], bk[:], [r_bk], [r_kT])
                    for tb in range(8):
                        bk, r_bk = pb()
                        c0 = tb * 512
                        S.group("tensor", [mk("matmul", out=bk[:], lhsT=wqkv[:, 2, kc, :], rhs=hT[:, kc, c0:c0 + 512],
                                              start=(kc == 0), stop=(kc == 7)) for kc in range(8)],
                                reads=[r_wqkv3[2]] + r_hT[tb * 4:(tb + 1) * 4], writes=[r_bk])
                        evac(vT[:, c0:c0 + 512], bk[:], [r_bk], [r_vT])
                    for pat, d in enumerate((1, 4, 16)):
                        L = TALL // d
                        nj = L // 128
                        for g8 in range(4):
                            bk, r_bk = pb()
                            pv = bk[:].bitcast(BF16).rearrange("p (u c) -> p u c", u=8)
                            fns = []
                            for u in range(8):
                                ti = g8 * 8 + u
                                r_, j_ = divmod(ti, nj)
                                s0 = r_ + d * 128 * j_
                                fns.append(mk("transpose", out=pv[:, u, :], in_=vT[:, s0:s0 + d * 127 + 1:d], identity=ident[:]))
                            S.group("tensor", fns, reads=[r_vT, r_ident], writes=[r_bk])
                            S.op("scalar", mk("activation", out=Vta[:, g8 * 8:(g8 + 1) * 8, 0:64], in_=pv[:, :, 0:64],
                                              func=AF.Copy), reads=[r_bk], writes=[r_Vt[2 * g8], r_Vt[2 * g8 + 1]])
                            S.op("vector", mk("tensor_copy", out=Vtb[:, g8 * 8:(g8 + 1) * 8, 64:128], in_=pv[:, :, 64:128]),
                                 reads=[r_bk], writes=[r_Vt[2 * g8], r_Vt[2 * g8 + 1]])
                        for su in range(4):
                            bkN, r_bkN = pb()
                            bkD, r_bkD = pb()
                            for u in range(4):
                                if d == 1:
                                    r_, jq = 0, 16 + 4 * su + u
                                elif d == 4:
                                    r_, jq = u, 4 + su
                                else:
                                    r_, jq = 4 * su + u, 1
                                q0 = r_ + d * 128 * jq - TOWN
                                kp0 = r_ + d * 128 * (jq - 1)
                                kc0 = r_ + d * 128 * jq
                                tip = r_ * nj + jq - 1
                                tic = r_ * nj + jq
                                boundary = (jq == nj // 2)
                                span = d * 127 + 1
                                bkS, r_bkS = pb()
                                msk = maskB if boundary else negmask
                                fns = [mk("matmul", out=bkS[:], lhsT=ident[:], rhs=msk[:], start=True, stop=False)]
                                for bi, (qq, k0) in enumerate(((qTa, kp0), (qTa, kc0), (qTb, kp0), (qTb, kc0))):
                                    fns.append(mk("matmul", out=bkS[:, bi * 128:(bi + 1) * 128],
                                                  lhsT=kT[:, k0:k0 + span:d], rhs=qq[:, q0:q0 + span:d],
                                                  start=False, stop=(bi == 3)))
                                S.group("tensor", fns, reads=[r_ident, r_negmask, r_maskB, r_kT, r_qTa, r_qTb],
                                        writes=[r_bkS])
                                pt, r_pt = PT[pt_rr], r_PT[pt_rr]
                                pt_rr ^= 1
                                S.op("scalar", mk("activation", out=pt[:], in_=bkS[:], func=AF.Exp, scale=0.125),
                                     reads=[r_bkS], writes=[r_pt])
                                oc = slice(u * 128, (u + 1) * 128)
                                fnsN = [
                                    mk("matmul", out=bkN[:, oc], lhsT=Vta[:, tip, :], rhs=pt[:, 0:128], start=True, stop=False),
                                    mk("matmul", out=bkN[:, oc], lhsT=Vta[:, tic, :], rhs=pt[:, 128:256], start=False, stop=False),
                                    mk("matmul", out=bkN[:, oc], lhsT=Vtb[:, tip, :], rhs=pt[:, 256:384], start=False, stop=False),
                                    mk("matmul", out=bkN[:, oc], lhsT=Vtb[:, tic, :], rhs=pt[:, 384:512], start=False, stop=True),
                                ]
                                S.group("tensor", fnsN, reads=[r_pt, r_Vt[tip // 4], r_Vt[tic // 4]], writes=[r_bkN])
                                fnsD = [
                                    mk("matmul", out=bkD[:, oc], lhsT=ones_a[:], rhs=pt[:, 0:128], start=True, stop=False),
                                    mk("matmul", out=bkD[:, oc], lhsT=ones_a[:], rhs=pt[:, 128:256], start=False, stop=False),
                                    mk("matmul", out=bkD[:, oc], lhsT=ones_b[:], rhs=pt[:, 256:384], start=False, stop=False),
                                    mk("matmul", out=bkD[:, oc], lhsT=ones_b[:], rhs=pt[:, 384:512], start=False, stop=True),
                                ]
                                S.group("tensor", fnsD, reads=[r_pt, r_ones], writes=[r_bkD])
                            for acc, r_acc, bkX, r_bkX, eng in ((accN, r_accN, bkN, r_bkN, "vector"),
                                                               (accD, r_accD, bkD, r_bkD, "gpsimd")):
                                src = bkX[:].rearrange("p (u i) -> p u i", u=4)
                                if d == 1:
                                    dst = acc[:, su * 512:(su + 1) * 512].rearrange("p (u i) -> p u i", u=4)
                                elif d == 4:
                                    dst = acc[:, su * 512:(su + 1) * 512].rearrange("p (i r) -> p r i", r=4)
                                else:
                                    dst = acc[:].rearrange("p (i r) -> p r i", r=16)[:, 4 * su:4 * su + 4, :]
                                if d == 1:
                                    S.op("vector" if eng == "vector" else "scalar",
                                         mk("tensor_copy", out=dst, in_=src) if eng == "vector" else
                                         mk("activation", out=dst, in_=src, func=AF.Copy),
                                         reads=[r_bkX], writes=[r_acc])
                                else:
                                    S.op("vector", mk("tensor_tensor", out=dst, in0=dst, in1=src, op=ALU.add),
                                         reads=[r_bkX], writes=[r_acc])
                    S.op("vector", mk("reciprocal", out=accD[:], in_=accD[:]), reads=[r_accD], writes=[r_accD])
                    S.op("vector", mk("tensor_tensor", out=mixT[:, fc, :], in0=accN[:], in1=accD[:], op=ALU.mult),
                         reads=[r_accN, r_accD], writes=r_bufA[fc])
                S.barrier()
            if debug:
                with ExitStack() as st_d:
                    dtmp = sbt(st_d, "dtmp2", [128, 8 * 512], F32); r_dtmp = Res()
                    for q4 in range(4):
                        S.op("vector", mk("tensor_copy", out=dtmp[:].rearrange("p (k t) -> p k t", k=8),
                                          in_=mixT[:, :, q4 * 512:(q4 + 1) * 512]), reads=[], writes=[r_dtmp])
                        S.dma(dbg["d_mixT"].rearrange("p (k t) -> p k t", k=8)[:, :, q4 * 512:(q4 + 1) * 512],
                              dtmp[:].rearrange("p (k t) -> p k t", k=8), reads=[r_dtmp])
                    S.barrier()
            with ExitStack() as st_o:
                wout = sbt(st_o, "wout", [128, 8, D], BF16); r_wout2 = RL(2)
                S.dma(wout[:, 0:4, :], w_out[0:512, :].rearrange("(kc p) n -> p kc n", p=128), writes=[r_wout2[0]], eng="gpsimd")
                S.dma(wout[:, 4:8, :], w_out[512:1024, :].rearrange("(kc p) n -> p kc n", p=128), writes=[r_wout2[1]], eng="gpsimd")
                for t in range(16):
                    xs, r_xs = xb[t % 2], r_xb[t % 2]
                    S.dma(xs[:], x_all[TOWN + t * 128:TOWN + (t + 1) * 128, :], writes=[r_xs])
                    for cc in range(2):
                        bk, r_bk = pb()
                        cs = slice(cc * 512, (cc + 1) * 512)
                        S.group("tensor", [mk("matmul", out=bk[:], lhsT=mixT[:, kc, t * 128:(t + 1) * 128],
                                              rhs=wout[:, kc, cs], start=(kc == 0), stop=(kc == 7)) for kc in range(8)],
                                reads=r_wout2 + [r_bufA[kc][t] for kc in range(8)], writes=[r_bk])
                        S.op("vector", mk("tensor_tensor", out=tmpf[:, cs], in0=bk[:], in1=mv2[:, cs], op=ALU.mult),
                             reads=[r_bk, r_mv2], writes=[r_tmpf])
                        S.op("gpsimd", mk("tensor_tensor", out=x1[:, t, cs], in0=tmpf[:, cs], in1=xs[:, cs], op=ALU.add),
                             reads=[r_tmpf, r_xs], writes=[r_x1[t]])
                S.barrier()
        if debug:
            for t in range(16):
                S.dma(dbg["d_x1"][t * 128:(t + 1) * 128, :], x1[:, t, :], reads=[r_x1[t]])
        I32 = mybir.dt.int32
        CAP = TOWN
        x_buf = nc.dram_tensor("x_buf", [NE * CAP, D], BF16, kind="Internal").ap()
        y_buf = nc.dram_tensor("y_buf", [NE * CAP, D], F32, kind="Internal").ap()
        with ExitStack() as st_f:
            wgus = [bufA, sbt(st_f, "wgu1", [128, 8, 2 * D], BF16)]; r_wgus = [RL(4), RL(4)]
            wgus[0] = bufA[:].rearrange("p k t -> p (k t)").rearrange("p (k t) -> p k t", k=8)
            wd = sbt(st_f, "wd", [128, 8, D], BF16); r_wd = RL(2)
            gk = sbt(st_f, "gk", [128, 16, 4], F32); r_gk = RL(16)
            desti = sbt(st_f, "desti", [128, 16, 4], I32); r_desti = RL(16)
            cnti = sbt(st_f, "cnti", [128, NE], I32); r_cnti = Res()

            bgrow = [sbt(st_f, "bgrow%d" % i, [2, 2 * D], BF16) for i in range(2)]; r_bgrow = RL(2)
            ones_r = sbt(st_f, "ones_r", [2, 128], BF16); r_ones_r = Res()
            S.op("gpsimd", mk("memset", ap=ones_r[:], constant=1.0), writes=[r_ones_r])
            for i in range(2):
                S.op("gpsimd", mk("memset", ap=bgrow[i][:, 0:D], constant=0.0), writes=[r_bgrow[i]])
                S.op("gpsimd", mk("memset", ap=bgrow[i][:, D:2 * D], constant=1.0), writes=[r_bgrow[i]])

            def load_wgu(e):
                w_, r_w = wgus[e % 2], r_wgus[e % 2]
                for q4 in range(4):
                    S.dma(w_[:, :, q4 * 512:(q4 + 1) * 512], wview(w_gu[e], q4 * 512, 512), writes=[r_w[q4]], eng="gpsimd")
                S.dma(bgrow[e % 2][0:1, :], b_gu_d[e:e + 1, :], writes=[r_bgrow[e % 2]], eng="gpsimd")

            def load_wd(e):
                S.dma(wd[:, 0:4, :], w_dn[e][0:512, :].rearrange("(kc p) n -> p kc n", p=128), writes=[r_wd[0]], eng="gpsimd")
                S.dma(wd[:, 4:8, :], w_dn[e][512:1024, :].rearrange("(kc p) n -> p kc n", p=128), writes=[r_wd[1]], eng="gpsimd")

            load_wgu(0)
            load_wd(0)
            with ExitStack() as st_r2:
                wr = sbt(st_r2, "wr", [128, 8, NE], BF16); r_wr = Res()
                btmp = sbt(st_r2, "btmp", [128, D], F32); r_btmp = RL(2)
                brt = sbt(st_r2, "brt", [128, NE], F32); r_brt = Res()
                lg_2 = [sbt(st_r2, "lg_%d" % i_, [128, NE], F32) for i_ in range(2)]; r_lg_2 = RL(2)
                ex_2 = [sbt(st_r2, "ex_%d" % i_, [128, NE], F32) for i_ in range(2)]; r_ex_2 = RL(2)
                gt_2 = [sbt(st_r2, "gt_%d" % i_, [128, NE], F32) for i_ in range(2)]; r_gt_2 = RL(2)
                posb_2 = [sbt(st_r2, "posb_%d" % i_, [128, NE], F32) for i_ in range(2)]; r_posb_2 = RL(2)
                scr4_2 = [sbt(st_r2, "scr4_%d" % i_, [128, 4, NE], F32) for i_ in range(2)]; r_scr_2 = RL(2)
                ebase = sbt(st_r2, "ebase", [128, NE], F32); r_ebase = Res()
                m8_2 = [sbt(st_r2, "m8_%d" % i_, [128, 8], F32) for i_ in range(2)]; r_m8_2 = RL(2)
                e4_2 = [sbt(st_r2, "e4_%d" % i_, [128, 4], F32) for i_ in range(2)]; r_e4_2 = RL(2)
                destf_2 = [sbt(st_r2, "destf_%d" % i_, [128, 4], F32) for i_ in range(2)]; r_destf_2 = RL(2)
                nmx_2 = [sbt(st_r2, "nmx_%d" % i_, [128, 1], F32) for i_ in range(2)]; r_nmx_2 = RL(2)
                sm_2 = [sbt(st_r2, "sm_%d" % i_, [128, 1], F32) for i_ in range(2)]; r_sm_2 = RL(2)
                gT_2 = [sbt(st_r2, "gT_%d" % i_, [32, 128], F32) for i_ in range(2)]; r_gT_2 = RL(2)
                bdn = sbt(st_r2, "bdn", [32, D], F32); r_bdn = Res()
                maskall = sbt(st_r2, "maskall", [128, 16, NE], BF16); r_mask = RL(16)
                ltri = sbt(st_r2, "ltri", [128, 128], BF16); r_ltri = Res()
                ones_f = sbt(st_r2, "ones_full", [128, 128], BF16); r_onesf = Res()
                h2Tt_2 = [sbt(st_r2, "h2Tt_%d" % i_, [128, 8, 128], BF16) for i_ in range(2)]; r_h2Tt_2 = RL(2)
                S.dma(wr[:], w_router.rearrange("(kc p) n -> p kc n", p=128), writes=[r_wr], eng="gpsimd")
                S.dma(brt[:], b_router.partition_broadcast(128), writes=[r_brt])
                S.dma(bdn[:], b_dn, writes=[r_bdn])
                S.op("gpsimd", mk("iota", out=ebase[:], pattern=[[CAP, NE]], base=0, channel_multiplier=0,
                                  allow_small_or_imprecise_dtypes=True), writes=[r_ebase])
                S.op("gpsimd", mk("memset", ap=ones_f[:], constant=1.0), writes=[r_onesf])
                S.op("gpsimd", mk("memset", ap=ltri[:], constant=1.0), writes=[r_ltri])
                S.op("gpsimd", mk("affine_select", out=ltri[:], in_=ltri[:], pattern=[[1, 128]], compare_op=ALU.is_gt,
                                  fill=0.0, base=0, channel_multiplier=-1), reads=[r_ltri], writes=[r_ltri])
                for t in range(16):
                    rms_stats(x1[:, t, :], r_x1[t], 32 + t)
                for t in range(16):
                    hbt, r_hbt = hb[t % 2], r_hb[t % 2]
                    lg, r_lg = lg_2[t % 2], r_lg_2[t % 2]
                    ex, r_ex = ex_2[t % 2], r_ex_2[t % 2]
                    gt, r_gt = gt_2[t % 2], r_gt_2[t % 2]
                    posb, r_posb = posb_2[t % 2], r_posb_2[t % 2]
                    scr4, r_scr = scr4_2[t % 2], r_scr_2[t % 2]
                    m8, r_m8 = m8_2[t % 2], r_m8_2[t % 2]
                    e4, r_e4 = e4_2[t % 2], r_e4_2[t % 2]
                    destf, r_destf = destf_2[t % 2], r_destf_2[t % 2]
                    nmx, r_nmx = nmx_2[t % 2], r_nmx_2[t % 2]
                    sm, r_sm = sm_2[t % 2], r_sm_2[t % 2]
                    gT, r_gT = gT_2[t % 2], r_gT_2[t % 2]
                    h2Tt, r_h2Tt = h2Tt_2[t % 2], r_h2Tt_2[t % 2]
                    rms_tile(x1[:, t, :], r_x1[t], 32 + t, mv4[:], r_mv4, mv3[:], r_mv3, hbt[:], r_hbt, stats=False)
                    transpose_tile(hbt, r_hbt, h2Tt[:], [r_h2Tt])
                    bk, r_bk = pb()
                    S.group("tensor", [mk("matmul", out=bk[:, 0:NE], lhsT=h2Tt[:, kc, :], rhs=wr[:, kc, :],
                                          start=(kc == 0), stop=(kc == 7)) for kc in range(8)],
                            reads=[r_wr, r_h2Tt], writes=[r_bk])
                    S.op("vector", mk("tensor_tensor", out=lg[:], in0=bk[:, 0:NE], in1=brt[:], op=ALU.add),
                         reads=[r_bk, r_brt], writes=[r_lg])
                    S.op("vector", mk("max", out=m8[:], in_=lg[:]), reads=[r_lg], writes=[r_m8])
                    S.op("vector", mk("tensor_scalar", out=nmx[:], in0=m8[:, 0:1], scalar1=-1.0, scalar2=None, op0=ALU.mult),
                         reads=[r_m8], writes=[r_nmx])
                    S.op("scalar", mk("activation", out=ex[:], in_=lg[:], func=AF.Exp, bias=nmx[:, 0:1], scale=1.0),
                         reads=[r_lg, r_nmx], writes=[r_ex])
                    S.op("scalar", mk("activation", out=e4[:], in_=m8[:, 0:4], func=AF.Exp, bias=nmx[:, 0:1], scale=1.0),
                         reads=[r_m8, r_nmx], writes=[r_e4])
                    S.op("vector", mk("tensor_scalar", out=maskall[:, t, :], in0=lg[:], scalar1=m8[:, 3:4], scalar2=None,
                                      op0=ALU.is_ge), reads=[r_lg, r_m8], writes=[r_mask[t]])
                    S.op("vector", mk("tensor_tensor", out=ex[:], in0=ex[:], in1=maskall[:, t, :], op=ALU.mult),
                         reads=[r_mask[t]], writes=[r_ex])
                    S.op("vector", mk("tensor_reduce", out=sm[:], in_=ex[:], axis=mybir.AxisListType.X, op=ALU.add),
                         reads=[r_ex], writes=[r_sm])
                    S.op("vector", mk("reciprocal", out=sm[:], in_=sm[:]), reads=[r_sm], writes=[r_sm])
                    S.op("vector", mk("tensor_scalar", out=gt[:], in0=ex[:], scalar1=sm[:, 0:1], scalar2=None,
                                      op0=ALU.mult), reads=[r_ex, r_sm], writes=[r_gt])
                    S.op("vector", mk("tensor_scalar", out=gk[:, t, :], in0=e4[:], scalar1=sm[:, 0:1], scalar2=None,
                                      op0=ALU.mult), reads=[r_e4, r_sm], writes=[r_gk[t]])
                    bkp, r_bkp = pb()
                    fns = [mk("matmul", out=bkp[:, 0:NE], lhsT=ones_f[:], rhs=maskall[:, tp, :], start=(tp == 0), stop=False)
                           for tp in range(t)]
                    fns.append(mk("matmul", out=bkp[:, 0:NE], lhsT=ltri[:], rhs=maskall[:, t, :], start=(t == 0), stop=True))
                    S.group("tensor", fns, reads=[r_onesf, r_ltri] + r_mask[:t + 1], writes=[r_bkp])
                    S.op("vector", mk("tensor_tensor", out=posb[:], in0=bkp[:, 0:NE], in1=ebase[:], op=ALU.add),
                         reads=[r_bkp, r_ebase], writes=[r_posb])
                    for k in range(4):
                        S.op("vector", mk("scalar_tensor_tensor", out=scr4[:, k, :], in0=lg[:], scalar=m8[:, k:k + 1], in1=posb[:],
                                          op0=ALU.is_equal, op1=ALU.mult),
                             reads=[r_lg, r_m8, r_posb], writes=[r_scr])
                    S.op("vector", mk("tensor_reduce", out=destf[:], in_=scr4[:], axis=mybir.AxisListType.X, op=ALU.add),
                         reads=[r_scr], writes=[r_destf])
                    S.op("vector", mk("tensor_scalar", out=destf[:], in0=destf[:], scalar1=0.0, scalar2=float(NE * CAP - 1),
                                      op0=ALU.max, op1=ALU.min), reads=[r_destf], writes=[r_destf])
                    S.op("vector", mk("tensor_copy", out=desti[:, t, :], in_=destf[:]), reads=[r_destf], writes=[r_desti[t]])
                    for k in range(4):
                        def sc(e, t=t, k=k, hbt=hbt):
                            return e.indirect_dma_start(out=x_buf[:, :],
                                                        out_offset=bass.IndirectOffsetOnAxis(ap=desti[:, t, k:k + 1], axis=0),
                                                        in_=hbt[:, :], in_offset=None)
                        S.dma_fn("gpsimd", sc, reads=[r_desti[t], r_hbt], writes=[])
                    bk2, r_bk2 = pb()
                    S.group("tensor", [mk("transpose", out=bk2[0:NE, 0:128], in_=gt[:], identity=identf[:])],
                            reads=[r_gt, r_identf], writes=[r_bk2])
                    S.op("vector", mk("tensor_copy", out=gT[:], in_=bk2[0:NE, 0:128]), reads=[r_bk2], writes=[r_gT])
                    for cc in range(2):
                        cs = slice(cc * 512, (cc + 1) * 512)
                        bk3, r_bk3 = pb()
                        S.group("tensor", [mk("matmul", out=bk3[:], lhsT=gT[:], rhs=bdn[:, cs], start=True, stop=True)],
                                reads=[r_gT, r_bdn], writes=[r_bk3])
                        S.op("vector", mk("tensor_tensor", out=btmp[:, cs], in0=bk3[:], in1=mv5[:, cs], op=ALU.mult),
                             reads=[r_bk3, r_mv5], writes=[r_btmp[cc]])
                        S.op("vector", mk("tensor_tensor", out=x1[:, t, cs], in0=btmp[:, cs], in1=x1[:, t, cs], op=ALU.add),
                             reads=[r_btmp[cc]], writes=[r_x1[t]])
                bkc, r_bkc = pb()
                S.group("tensor", [mk("matmul", out=bkc[:, 0:NE], lhsT=ones_f[:], rhs=maskall[:, tp, :], start=(tp == 0),
                                      stop=(tp == 15)) for tp in range(16)], reads=[r_onesf] + r_mask, writes=[r_bkc])
                S.op("vector", mk("tensor_copy", out=cnti[:], in_=bkc[:, 0:NE]), reads=[r_bkc], writes=[r_cnti])
                if debug:
                    S.dma(dbg["d_gates"][:, 0:64], gk[:].rearrange("p t e -> p (t e)"), reads=r_gk)
                S.barrier()
            S.dma(mv3[:], g_final.partition_broadcast(128), writes=[r_mv3])
            with ExitStack() as st_m:
                xblk = [hb[0], hb[1]]; r_xblk = RL(2)
                xT1 = sbt(st_m, "xT1", [128, 8, 128], BF16)
                xTs = [junk[:].rearrange("p (k t) -> p k t", k=8), xT1[:]]; r_xT = RL(2)
                actT = [sbt(st_m, "actT%d" % i, [128, 8, 128], BF16) for i in range(2)]; r_act = [RL(2), RL(2)]
                yblk = [xb[0], xb[1]]; r_yblk = RL(2)
                sgx = sbt(st_m, "sgx", [128, D], F32)
                ucx = sbt(st_m, "ucx", [128, D], F32)
                gc2 = [[tmpf[:, 0:512], tmpf[:, 512:1024]], [sgx[:, 0:512], sgx[:, 512:1024]]]; r_gc2 = [RL(2), RL(2)]
                actk = [sbt(st_m, "actk%d" % i, [128, D], BF16) for i in range(2)]; r_actk = [RL(2), RL(2)]
                ucs = [[mv4[:, 0:512], mv4[:, 512:1024]], [ucx[:, 0:512], ucx[:, 512:1024]]]; r_uc = [RL(2), RL(2)]
                evac_dve_only[0] = True
                blk_rr = 0
                S.load_counts(lambda k: cnti[0:1, k:k + 1], NE, [r_cnti])
                for e in range(n_experts if stage >= 2 else 0):
                    w_, r_w = wgus[e % 2], r_wgus[e % 2]
                    if e + 1 < n_experts:
                        load_wgu(e + 1)
                    for jp in range(0, 16, 2):
                        S.begin_guard((e, jp), 128 * jp + 1)
                        pair = (jp, jp + 1)

                        NESTED = True

                        def inner(j, ph):
                            if j != jp and NESTED:
                                S.begin_inner((e, j, ph), 128 * j + 1)

                        def inner_end(j):
                            if j != jp and NESTED:
                                S.end_inner()
                        for j in pair:
                            s_ = j % 2
                            row0 = e * CAP + 128 * j
                            inner(j, 1)
                            S.dma(xblk[s_][:], x_buf[row0:row0 + 128, :], writes=[r_xblk[s_]])
                            transpose_tile(xblk[s_], r_xblk[s_], xTs[s_], [r_xT[s_]])
                            inner_end(j)
                        for j in pair:
                            s_ = j % 2
                            xT = xTs[s_]
                            inner(j, 2)
                            for half in range(2):
                                gub = []
                                for c0 in (half * 512, D + half * 512):
                                    bkX, r_bkX = pb()
                                    fns = [mk("matmul", out=bkX[:], lhsT=ones_r[0:2, :], rhs=bgrow[e % 2][0:2, c0:c0 + 512],
                                              start=True, stop=False)]
                                    for kc in range(8):
                                        fns.append(mk("matmul", out=bkX[:], lhsT=xT[:, kc, :], rhs=w_[:, kc, c0:c0 + 512],
                                                      start=False, stop=(kc == 7)))
                                    S.group("tensor", fns, reads=[r_w[c0 // 512], r_xT[s_], r_ones_r, r_bgrow[e % 2]], writes=[r_bkX])
                                    gub.append((bkX, r_bkX))
                                (bkG, r_bkG), (bkU, r_bkU) = gub
                                uc, r_uc_ = ucs[s_][half], r_uc[s_][half]
                                gc, r_gc_ = gc2[s_][half], r_gc2[s_][half]
                                S.op("scalar", mk("activation", out=gc, in_=bkG[:], func=AF.Gelu_apprx_sigmoid),
                                     reads=[r_bkG], writes=[r_gc_])
                                S.op("vector", mk("tensor_scalar", out=uc, in0=bkU[:], scalar1=8.0, scalar2=-6.0,
                                                  op0=ALU.min, op1=ALU.max), reads=[r_bkU], writes=[r_uc_])
                                S.op("vector", mk("scalar_tensor_tensor", out=actk[s_][:, half * 512:(half + 1) * 512], in0=gc,
                                                  scalar=GLU7, in1=uc, op0=ALU.min, op1=ALU.mult),
                                     reads=[r_uc_, r_gc_], writes=[r_actk[s_][half]])
                            inner_end(j)
                        for j in pair:
                            s_ = j % 2
                            row0 = e * CAP + 128 * j
                            inner(j, 3)
                            for half in range(2):
                                transpose_tile(actk[s_], r_actk[s_][half], actT[s_][:, 4 * half:4 * half + 4, :],
                                               [r_act[s_][half]], k0=4 * half, k1=4 * half + 4)
                            for cc in range(2):
                                cs = slice(cc * 512, (cc + 1) * 512)
                                bk, r_bk = pb()
                                S.group("tensor", [mk("matmul", out=bk[:], lhsT=actT[s_][:, kc, :], rhs=wd[:, kc, cs],
                                                      start=(kc == 0), stop=(kc == 7)) for kc in range(8)],
                                        reads=r_wd + r_act[s_], writes=[r_bk])
                                S.op("vector", mk("tensor_tensor", out=yblk[s_][:, cs], in0=bk[:], in1=mv5[:, cs], op=ALU.mult),
                                     reads=[r_bk, r_mv5], writes=[r_yblk[s_]])
                            S.dma(y_buf[row0:row0 + 128, :], yblk[s_][:], reads=[r_yblk[s_]], eng="gpsimd")
                            inner_end(j)
                        S.end_guard()
                    if e + 1 < n_experts:
                        load_wd(e + 1)
                evac_dve_only[0] = False
                S.barrier()
            st_c = st_f.enter_context(ExitStack())
            ykb = [mv4, tmpf] + [sbt(st_c, "ykb%d" % i, [128, D], F32) for i in range(4)]; r_ykb = RL(6)
            gi = 0
            for t in range(16):
                for k in range(4 if stage >= 3 else 0):
                    yk, r_yk = ykb[gi % 6], r_ykb[gi % 6]
                    gi += 1

                    def ga(e, t=t, k=k, yk=yk):
                        return e.indirect_dma_start(out=yk[:, :], out_offset=None, in_=y_buf[:, :],
                                                    in_offset=bass.IndirectOffsetOnAxis(ap=desti[:, t, k:k + 1], axis=0))
                    S.dma_fn("gpsimd", ga, reads=[r_desti[t]], writes=[r_yk])
                    S.op("vector", mk("scalar_tensor_tensor", out=x1[:, t, :], in0=yk[:], scalar=gk[:, t, k:k + 1],
                                      in1=x1[:, t, :], op0=ALU.mult, op1=ALU.add),
                         reads=[r_yk, r_gk[t]], writes=[r_x1[t]])
            for t in range(16):
                ob = xb[t % 2]; r_ob = r_xb[t % 2]
                rms_tile(x1[:, t, :], r_x1[t], 48 + t, mv3[:], r_mv3, None, None, ob[:], r_ob)
                S.dma(out_d[t * 128:(t + 1) * 128, :], ob[:], reads=[r_ob])
            S.barrier()
        with nc.Block() as block:
            S.emit(block)
    return nc


_NC_CACHE = {}


def make_in_maps(inputs, cores, n_experts=NE):
    f = lambda a: np.ascontiguousarray(np.asarray(a, dtype=np.float32))
    x = f(inputs["x"]); c = f(inputs["c"])
    w_ada = f(inputs["w_ada"])[0]; b_ada = f(inputs["b_ada"])[0][None, :]
    g_mix = f(inputs["g_mix"])[0][None, :]; g_ffn = f(inputs["g_ffn"])[0][None, :]
    g_final = f(inputs["g_final"])[None, :]
    w_in = f(inputs["w_in"])[0]; w_out = f(inputs["w_out"])[0]
    conv_w = f(inputs["conv_w"])[0]; conv_b = f(inputs["conv_b"])[0]
    b_a = f(inputs["b_rg_a"])[0]; b_x = f(inputs["b_rg_x"])[0]; lam = f(inputs["lam"])[0]
    recp = np.zeros((128, 4, 8), np.float32)
    cols = [conv_w[0], conv_w[1], conv_w[2], conv_w[3], conv_b, b_a, b_x, lam]
    for j, v in enumerate(cols):
        recp[:, :, j] = v.reshape(4, 128).T
    def blockdiag(w):
        w = f(w)[0]
        o = np.zeros((128, 4, 128), np.float32)
        for blk in range(8):
            cch, hh = divmod(blk, 2)
            o[hh * 64:(hh + 1) * 64, cch, hh * 64:(hh + 1) * 64] = w[blk]
        return o
    wa_bd = blockdiag(inputs["w_rg_a"]); wx_bd = blockdiag(inputs["w_rg_x"])
    w_router = f(inputs["w_router"])[0]; b_router = f(inputs["b_router"])[0][None, :]
    w_gu = f(inputs["w_gate_up"])[0][:n_experts]; b_gu = f(inputs["b_gate_up"])[0]
    bgu_t = np.ascontiguousarray(b_gu.reshape(NE, 16, 128).transpose(2, 0, 1))
    w_dn = f(inputs["w_down"])[0][:n_experts]; b_dn = f(inputs["b_down"])[0]
    maps = []
    for core in cores:
        b, half = divmod(core, 2)
        x_all = np.zeros((TALL, D), np.float32)
        if half == 0:
            x_all[TOWN:] = x[b, :TOWN]
        else:
            x_all[:] = x[b]
        c_rep = np.ascontiguousarray(np.broadcast_to(c[b].reshape(8, 128).T[:, :, None], (128, 8, 128)))
        flag = np.full((128, 1), float(half), np.float32)
        maps.append({
            "x_all": x_all, "c_rep": c_rep, "flag": flag, "w_ada": w_ada, "b_ada": b_ada, "g_mix": g_mix,
            "g_ffn": g_ffn, "g_final": g_final, "w_in": w_in, "w_out": w_out, "recp": recp, "wa_bd": wa_bd,
            "wx_bd": wx_bd, "w_router": w_router, "b_router": b_router, "w_gate_up": w_gu, "bgu_t": bgu_t, "b_gu": b_gu,
            "w_down": w_dn, "b_down": b_dn,
        })
    return maps


def kernel(**inputs):
    if "nc" not in _NC_CACHE:
        _NC_CACHE["nc"] = build()
    nc = _NC_CACHE["nc"]
    cores = list(range(8))
    in_maps = make_in_maps(inputs, cores)
    res = run_bass_kernel_spmd(nc, in_maps, core_ids=cores)
    out = np.zeros((4, 4096, D), np.float32)
    for core in cores:
        b, half = divmod(core, 2)
        out[b, half * TOWN:(half + 1) * TOWN] = res.results[core]["out"]
    return out
```

```python
import numpy as np
from contextlib import ExitStack
import concourse.bass as bass
import concourse.mybir as mybir
from concourse.bass_utils import run_bass_kernel_spmd

F32 = mybir.dt.float32
BF16 = mybir.dt.bfloat16
AF = mybir.ActivationFunctionType
ALU = mybir.AluOpType

D = 1024
TOWN = 2048
TALL = 4096
NE = 32
EPS = 1e-6
GLU7 = float(np.float32(7.0) / (np.float32(1.0) + np.exp(np.float32(-1.702 * 7.0))))
SEM_LIMIT = 30000


class Res:
    __slots__ = ("w", "r")

    def __init__(self):
        self.w = None
        self.r = {}


def RL(n):
    return [Res() for _ in range(n)]


class Sched:
    ENGS = ("tensor", "vector", "scalar", "gpsimd", "sync")

    def __init__(self, nc, stack, n_dma_sems=8):
        self.nc = nc
        self.stack = stack
        self.ops = {e: [] for e in self.ENGS}
        self.known = {e: {} for e in self.ENGS}
        self.dom_sem = {}
        self.dom_max = {}
        self.ndom = 0
        self.cur_dom = {}
        self.cur_cnt = {}
        self.guard = None
        self.guard_snap = {}
        self.pe_skip_ap = None
        self.regs = {}
        for e in self.ENGS:
            self.cur_dom[e] = self._new_dom(e)
            self.cur_cnt[e] = 0
        self.dma_pool = {}
        for q in ("sync", "gpsimd"):
            self.dma_pool[q] = {"doms": [self._new_dom("dma_%s%d" % (q, i)) for i in range(n_dma_sems)],
                                "cnt": [0] * n_dma_sems, "rr": 0}

    def _new_dom(self, name):
        d = self.ndom
        self.ndom += 1
        self.dom_sem[d] = self.stack.enter_context(self.nc.semaphore("s_%s_%d" % (name, d)))
        self.dom_max[d] = 0
        return d

    def _collect(self, eng, reads, writes):
        need = {}
        for R in reads:
            if R.w is not None:
                d, v = R.w
                if need.get(d, 0) < v:
                    need[d] = v
        for R in writes:
            if R.w is not None:
                d, v = R.w
                if need.get(d, 0) < v:
                    need[d] = v
            for d, v in R.r.items():
                if need.get(d, 0) < v:
                    need[d] = v
        kn = self.known[eng]
        waits = []
        for d, v in need.items():
            if eng == "tensor" and d == self.cur_dom["tensor"]:
                continue
            if kn.get(d, 0) < v:
                kn[d] = v
                waits.append((d, v))
        return waits

    def _tick(self, eng):
        self.cur_cnt[eng] += 1
        d = self.cur_dom[eng]
        v = self.cur_cnt[eng]
        self.dom_max[d] = v
        return d, v

    def _mark(self, d, v, reads, writes):
        for R in reads:
            R.r[d] = v
        for R in writes:
            R.w = (d, v)
            R.r = {}

    def op(self, eng, fn, reads=(), writes=()):
        waits = self._collect(eng, reads, writes)
        d, v = self._tick(eng)
        self.ops[eng].append((waits, fn, self.dom_sem[d], 1, self.guard))
        self._mark(d, v, reads, writes)

    def group(self, eng, fns, reads=(), writes=()):
        waits = self._collect(eng, reads, writes)
        d, v = self._tick(eng)
        n = len(fns)
        for i, fn in enumerate(fns):
            self.ops[eng].append((waits if i == 0 else [], fn,
                                  self.dom_sem[d] if i == n - 1 else None, 1, self.guard))
        self._mark(d, v, reads, writes)

    def dma_fn(self, eng, fn, reads=(), writes=()):
        pool = self.dma_pool[eng]
        i = pool["rr"]
        pool["rr"] = (i + 1) % len(pool["doms"])
        d = pool["doms"][i]
        waits = self._collect(eng, reads, writes)
        prev = pool["cnt"][i]
        kn = self.known[eng]
        if prev > 0 and kn.get(d, 0) < prev:
            kn[d] = prev
            waits.append((d, prev))
        pool["cnt"][i] += 16
        v = pool["cnt"][i]
        self.dom_max[d] = v
        self.ops[eng].append((waits, fn, self.dom_sem[d], 16, self.guard))
        self._mark(d, v, reads, writes)

    def dma(self, out, in_, reads=(), writes=(), eng="sync"):
        def fn(e, out=out, in_=in_):
            return e.dma_start(out=out, in_=in_)
        self.dma_fn(eng, fn, reads, writes)

    def load_counts(self, ap_of, n, reads):
        self.n_count_regs = n
        for eng in self.ENGS:
            waits = self._collect(eng, reads, ())
            sched = self
            for k in range(n):
                def fn(e, eng=eng, k=k):
                    return e.reg_load(sched.regs[eng][k], ap_of(k))
                self.ops[eng].append((waits if k == 0 else [], fn, None, 0, None))

    def begin_guard(self, gid, thr):
        self.guard = (gid, thr, None, 0)
        self.guard_snap[gid] = dict(self.dom_max)

    def begin_inner(self, gid, thr):
        g = self.guard
        self.guard = (g[0], g[1], gid, thr)
        self.guard_snap[gid] = dict(self.dom_max)

    def end_inner(self):
        g = self.guard
        self.guard = (g[0], g[1], None, 0)

    def end_guard(self):
        self.guard = None

    def barrier(self):
        assert self.guard is None
        for eng in self.ENGS:
            kn = self.known[eng]
            waits = []
            for d, v in self.dom_max.items():
                if v > 0 and kn.get(d, 0) < v:
                    kn[d] = v
                    waits.append((d, v))
            if waits:
                self.ops[eng].append((waits, None, None, 0, None))

    def emit(self, block):
        sched = self

        def emit_one(e, item):
            waits, fn, sem, inc, _ = item
            for (d_, v) in waits:
                e.wait_ge(sched.dom_sem[d_], v)
            if fn is None:
                return
            ins = fn(e)
            if sem is not None:
                ins.then_inc(sem, inc)

        def skip_path(e, grp, snap):
            incs = []
            need = {}
            for waits, fn, sem, inc, _ in grp:
                for (d_, v) in waits:
                    v = min(v, snap.get(d_, 0))
                    if v > need.get(d_, 0):
                        need[d_] = v
            for d_, v in need.items():
                e.wait_ge(sched.dom_sem[d_], v)
            for waits, fn, sem, inc, _ in grp:
                if sem is not None:
                    for k_ in range(len(incs)):
                        if incs[k_][0] is sem:
                            incs[k_][1] += inc
                            break
                    else:
                        incs.append([sem, inc])
            if sched.pe_skip_ap is not None and len(incs) == 1 and e is sched.nc.tensor:
                e.ldweights(sched.pe_skip_ap).then_inc(incs[0][0], incs[0][1])
                return
            e.drain()
            for sem, tot in incs:
                e.sem_inc(sem, tot)

        def emit_region(e, reg, grp):
            a = 0
            m = len(grp)
            while a < m:
                gi = grp[a][4]
                if gi[2] is None:
                    emit_one(e, grp[a])
                    a += 1
                    continue
                b = a
                while b < m and grp[b][4][2] == gi[2]:
                    b += 1
                sub = grp[a:b]
                with e.If_lt(reg, gi[3]):
                    skip_path(e, sub, sched.guard_snap[gi[2]])
                with e.Else():
                    for it in sub:
                        emit_one(e, it)
                a = b

        def emit_chain(e, reg, regions, idx):
            if idx == len(regions):
                return
            grp = regions[idx]
            g = grp[0][4]
            rest = [it for r_ in regions[idx:] for it in r_]
            with e.If_lt(reg, g[1]):
                skip_path(e, rest, sched.guard_snap[g[0]])
            with e.Else():
                emit_region(e, reg, grp)
                emit_chain(e, reg, regions, idx + 1)

        def make(engname):
            def body(e):
                ops = sched.ops[engname]
                sched.regs[engname] = [e.alloc_register("cnt%d_%s" % (k_, engname))
                                       for k_ in range(getattr(sched, "n_count_regs", 1))]
                i = 0
                n = len(ops)
                while i < n:
                    g = ops[i][4]
                    if g is None:
                        emit_one(e, ops[i])
                        i += 1
                        continue
                    regions = []
                    j = i
                    while j < n and ops[j][4] is not None and ops[j][4][0][0] == g[0][0]:
                        k = j
                        while k < n and ops[k][4] is not None and ops[k][4][0] == ops[j][4][0]:
                            k += 1
                        regions.append(ops[j:k])
                        j = k
                    emit_chain(e, sched.regs[engname][g[0][0]], regions, 0)
                    i = j
            return body
        for engname in self.ENGS:
            if self.ops[engname]:
                getattr(block, engname)(make(engname))


def mk(method, **kw):
    return lambda e: getattr(e, method)(**kw)


def build(debug=False, n_experts=NE, stage=3):
    nc = bass.Bass("TRN2", target_bir_lowering=False)

    def din(name, shape):
        return nc.dram_tensor(name, shape, F32, kind="ExternalInput").ap()

    x_all = din("x_all", [TALL, D])
    c_rep = din("c_rep", [128, 8, 128])
    flag_d = din("flag", [128, 1])
    w_ada = din("w_ada", [D, 6 * D])
    b_ada = din("b_ada", [1, 6 * D])
    g_mix = din("g_mix", [1, D])
    g_ffn = din("g_ffn", [1, D])
    g_final = din("g_final", [1, D])
    w_in = din("w_in", [D, 2560])
    w_out = din("w_out", [D, D])
    recp = din("recp", [128, 4, 8])
    wa_bd = din("wa_bd", [128, 4, 128])
    wx_bd = din("wx_bd", [128, 4, 128])
    w_router = din("w_router", [D, NE])
    b_router = din("b_router", [1, NE])
    w_gu = din("w_gate_up", [n_experts, D, 2 * D])
    bgu_t = din("bgu_t", [128, NE, 16])
    b_gu_d = din("b_gu", [NE, 2 * D])
    w_dn = din("w_down", [n_experts, D, D])
    b_dn = din("b_down", [NE, D])
    out_d = nc.dram_tensor("out", [TOWN, D], F32, kind="ExternalOutput").ap()
    dbg = {}
    if debug:
        def dout(name, shape):
            dbg[name] = nc.dram_tensor(name, shape, F32, kind="ExternalOutput").ap()
        dout("d_mod", [128, 6 * D])
        dout("d_hT", [128, 8 * 512])
        dout("d_mixT", [128, 8 * TOWN])
        dout("d_x1", [TOWN, D])
        dout("d_gates", [128, 16 * NE])

    with ExitStack() as st:
        S = Sched(nc, st)

        def sbt(stack, name, shape, dt):
            return stack.enter_context(nc.sbuf_tensor(name, shape, dt))

        arena = sbt(st, "arena", [128, 16384], F32)
        hT = arena[:].bitcast(BF16).rearrange("p (k t) -> p k t", k=8)
        x1 = arena[:].rearrange("p (t f) -> p t f", t=16)
        r_hT = RL(32)
        r_x1 = RL(16)
        bufA = sbt(st, "bufA", [128, 8, TOWN], BF16)
        r_bufA = [RL(16) for _ in range(8)]
        mv3 = sbt(st, "mv3", [128, D], F32); r_mv3 = Res()
        mv4 = sbt(st, "mv4", [128, D], F32); r_mv4 = Res()
        mv5 = sbt(st, "mv5", [128, D], F32); r_mv5 = Res()
        xb = [sbt(st, "xb%d" % i, [128, D], F32) for i in range(2)]; r_xb = RL(2)
        tmpf = sbt(st, "tmpf", [128, D], F32); r_tmpf = Res()
        hb = [sbt(st, "hb%d" % i, [128, D], BF16) for i in range(2)]; r_hb = RL(2)
        junk = sbt(st, "junk", [128, D], BF16)
        ss = sbt(st, "ss", [128, 64], F32); r_ss = RL(64)
        ms = sbt(st, "ms", [128, 64], F32); r_ms = RL(64)
        rstd = sbt(st, "rstd", [128, 64], F32); r_rstd = RL(64)
        ident = sbt(st, "ident", [128, 128], BF16); r_ident = Res()
        identf = sbt(st, "identf", [128, 128], F32); r_identf = Res()
        neghalf = sbt(st, "neghalf", [128, 1], F32); r_nh = Res()
        flag = sbt(st, "flag_sb", [128, 1], F32); r_flag = Res()
        flagb = sbt(st, "flagb", [128, 1], F32); r_flagb = Res()
        negmask = sbt(st, "negmask", [128, 512], BF16); r_negmask = Res()
        maskB = sbt(st, "maskB", [128, 512], BF16); r_maskB = Res()
        ones_a = sbt(st, "ones_a", [128, 128], BF16); r_ones = Res()
        ones_b = sbt(st, "ones_b", [128, 128], BF16)

        banks = [st.enter_context(nc.psum_tensor("bank%d" % i, [128, 512], F32)) for i in range(8)]
        r_bank = RL(8)
        bank_rr = [0]

        def pb():
            i = bank_rr[0]
            bank_rr[0] = (i + 1) % 8
            return banks[i], r_bank[i]

        cp_rr = [0]
        evac_dve_only = [False]

        def evac(out, in_, reads, writes):
            cp_rr[0] ^= 1
            if cp_rr[0] and not evac_dve_only[0]:
                S.op("scalar", mk("activation", out=out, in_=in_, func=AF.Copy), reads=reads, writes=writes)
            else:
                S.op("vector", mk("tensor_copy", out=out, in_=in_), reads=reads, writes=writes)

        S.dma(flag[:], flag_d, writes=[r_flag])
        S.op("gpsimd", mk("memset", ap=neghalf[:], constant=-0.5), writes=[r_nh])
        S.op("gpsimd", mk("memset", ap=ident[:], constant=1.0), writes=[r_ident])
        S.op("gpsimd", mk("affine_select", out=ident[:], in_=ident[:], pattern=[[-1, 128]],
                          compare_op=ALU.is_equal, fill=0.0, base=0, channel_multiplier=1),
             reads=[r_ident], writes=[r_ident])
        S.op("gpsimd", mk("memset", ap=identf[:], constant=1.0), writes=[r_identf])
        S.op("gpsimd", mk("affine_select", out=identf[:], in_=identf[:], pattern=[[-1, 128]],
                          compare_op=ALU.is_equal, fill=0.0, base=0, channel_multiplier=1),
             reads=[r_identf], writes=[r_identf])
        S.op("gpsimd", mk("memset", ap=negmask[:], constant=0.0), writes=[r_negmask])
        for blk in range(4):
            sl = negmask[:, blk * 128:(blk + 1) * 128]
            if blk % 2 == 0:
                S.op("gpsimd", mk("affine_select", out=sl, in_=sl, pattern=[[-1, 128]], compare_op=ALU.is_ge,
                                  fill=-30000.0, base=0, channel_multiplier=1), reads=[r_negmask], writes=[r_negmask])
            else:
                S.op("gpsimd", mk("affine_select", out=sl, in_=sl, pattern=[[1, 128]], compare_op=ALU.is_ge,
                                  fill=-30000.0, base=0, channel_multiplier=-1), reads=[r_negmask], writes=[r_negmask])
        S.op("vector", mk("tensor_scalar", out=flagb[:], in0=flag[:], scalar1=-1.0, scalar2=30000.0,
                          op0=ALU.add, op1=ALU.mult), reads=[r_flag], writes=[r_flagb])
        S.op("vector", mk("tensor_copy", out=maskB[:], in_=negmask[:]), reads=[r_negmask], writes=[r_maskB])
        for blk in (0, 2):
            sl = maskB[:, blk * 128:(blk + 1) * 128]
            S.op("vector", mk("tensor_scalar", out=sl, in0=sl, scalar1=flagb[:, 0:1], scalar2=None, op0=ALU.add),
                 reads=[r_maskB, r_flagb], writes=[r_maskB])
        S.op("gpsimd", mk("memset", ap=ones_a[:], constant=0.0), writes=[r_ones])
        S.op("gpsimd", mk("memset", ap=ones_a[:, 0:64], constant=1.0), writes=[r_ones])
        S.op("gpsimd", mk("memset", ap=ones_b[:], constant=0.0), writes=[r_ones])
        S.op("gpsimd", mk("memset", ap=ones_b[:, 64:128], constant=1.0), writes=[r_ones])

        def wview(w2d, c0, n):
            return w2d[:, c0:c0 + n].rearrange("(kc p) n -> p kc n", p=128)

        r_junk = Res()

        def rms_stats(src, r_src, col):
            S.op("scalar", mk("activation", out=junk[:], in_=src, func=AF.Square, accum_out=ss[:, col:col + 1]),
                 reads=[r_src], writes=[r_ss[col], r_junk])
            S.op("vector", mk("tensor_scalar", out=ms[:, col:col + 1], in0=ss[:, col:col + 1], scalar1=1.0 / D,
                              scalar2=EPS, op0=ALU.mult, op1=ALU.add), reads=[r_ss[col]], writes=[r_ms[col]])
            S.op("gpsimd", mk("tensor_tensor", out=rstd[:, col:col + 1], in0=ms[:, col:col + 1], in1=neghalf[:],
                              op=ALU.pow), reads=[r_ms[col], r_nh], writes=[r_rstd[col]])

        def rms_tile(src, r_src, col, A_vec, r_A, B_vec, r_B, hbt, r_hbt, stats=True):
            if stats:
                rms_stats(src, r_src, col)
            if B_vec is None:
                S.op("vector", mk("scalar_tensor_tensor", out=hbt, in0=src, scalar=rstd[:, col:col + 1], in1=A_vec,
                                  op0=ALU.mult, op1=ALU.mult), reads=[r_src, r_rstd[col], r_A], writes=[r_hbt])
                return
            S.op("vector", mk("scalar_tensor_tensor", out=tmpf[:], in0=src, scalar=rstd[:, col:col + 1], in1=A_vec,
                              op0=ALU.mult, op1=ALU.mult), reads=[r_src, r_rstd[col], r_A], writes=[r_tmpf])
            S.op("vector", mk("tensor_tensor", out=hbt, in0=tmpf[:], in1=B_vec, op=ALU.add),
                 reads=[r_tmpf, r_B], writes=[r_hbt])

        def transpose_tile(hbt, r_hbt, dstT, r_dst, k0=0, k1=8):
            bk, r_bk = pb()
            pv = bk[:].bitcast(BF16).rearrange("p (k t) -> p k t", k=8)
            S.group("tensor", [mk("transpose", out=pv[:, kc, :], in_=hbt[:, kc * 128:(kc + 1) * 128], identity=ident[:])
                               for kc in range(k0, k1)], reads=[r_hbt, r_ident], writes=[r_bk])
            evac(dstT, pv[:, k0:k1, :], [r_bk], r_dst)

        with ExitStack() as st_mix:
            mv2 = sbt(st_mix, "mv2", [128, D], F32); r_mv2 = Res()
            with ExitStack() as st_a:
                mv0 = sbt(st_a, "mv0", [128, D], F32); r_mv0 = Res()
                mv1 = sbt(st_a, "mv1", [128, D], F32); r_mv1 = Res()
                mvs = [mv0, mv1, mv2, mv3, mv4, mv5]
                r_mvs = [r_mv0, r_mv1, r_mv2, r_mv3, r_mv4, r_mv5]
                with ExitStack() as st0:
                    bada = sbt(st0, "bada", [128, 6 * D], F32); r_bada = Res()
                    gmix_bc = sbt(st0, "gmix_bc", [128, D], F32); r_gmix = Res()
                    gffn_bc = sbt(st0, "gffn_bc", [128, D], F32); r_gffn = Res()
                    wab = [sbt(st0, "wab%d" % i, [128, 8, 512], BF16) for i in range(2)]; r_wab = RL(2)
                    c_bf = sbt(st0, "c_bf", [128, 8, 128], BF16); r_cbf = Res()
                    S.dma(c_bf[:], c_rep, writes=[r_cbf], eng="gpsimd")
                    S.dma(bada[:], b_ada.partition_broadcast(128), writes=[r_bada])
                    S.dma(gmix_bc[:], g_mix.partition_broadcast(128), writes=[r_gmix])
                    S.dma(gffn_bc[:], g_ffn.partition_broadcast(128), writes=[r_gffn])
                    for cc in range(12):
                        wb_, r_wb_ = wab[cc % 2], r_wab[cc % 2]
                        S.dma(wb_[:], wview(w_ada, cc * 512, 512), writes=[r_wb_], eng="gpsimd")
                        bk, r_bk = pb()
                        S.group("tensor", [mk("matmul", out=bk[:], lhsT=c_bf[:, kc, :], rhs=wb_[:, kc, :],
                                              start=(kc == 0), stop=(kc == 7)) for kc in range(8)],
                                reads=[r_cbf, r_wb_], writes=[r_bk])
                        dst = mvs[cc // 2][:, (cc % 2) * 512:(cc % 2 + 1) * 512]
                        S.op("vector", mk("tensor_tensor", out=dst, in0=bk[:], in1=bada[:, cc * 512:(cc + 1) * 512],
                                          op=ALU.add), reads=[r_bk, r_bada], writes=[r_mvs[cc // 2]])
                    if debug:
                        for i in range(6):
                            S.dma(dbg["d_mod"][:, i * D:(i + 1) * D], mvs[i][:], reads=[r_mvs[i]])
                    S.op("vector", mk("scalar_tensor_tensor", out=mv1[:], in0=mv1[:], scalar=1.0, in1=gmix_bc[:],
                                      op0=ALU.add, op1=ALU.mult), reads=[r_gmix], writes=[r_mv1])
                    S.op("vector", mk("scalar_tensor_tensor", out=mv4[:], in0=mv4[:], scalar=1.0, in1=gffn_bc[:],
                                      op0=ALU.add, op1=ALU.mult), reads=[r_gffn], writes=[r_mv4])
                    S.barrier()
                for t in range(32):
                    xs, r_xs = xb[t % 2], r_xb[t % 2]
                    S.dma(xs[:], x_all[t * 128:(t + 1) * 128, :], writes=[r_xs])
                    rms_tile(xs[:], r_xs, t, mv1[:], r_mv1, mv0[:], r_mv0, hb[t % 2][:], r_hb[t % 2])
                    transpose_tile(hb[t % 2], r_hb[t % 2], hT[:, :, t * 128:(t + 1) * 128], [r_hT[t]])
                if debug:
                    S.barrier()
                    with ExitStack() as st_d:
                        dtmp = sbt(st_d, "dtmp", [128, 8 * 512], F32); r_dtmp = Res()
                        S.op("vector", mk("tensor_copy", out=dtmp[:].rearrange("p (k t) -> p k t", k=8),
                                          in_=hT[:, :, 1792:2304]), reads=r_hT, writes=[r_dtmp])
                        S.dma(dbg["d_hT"], dtmp[:], reads=[r_dtmp])
                        S.barrier()
                S.barrier()
            mixT = bufA
            with ExitStack() as st_r:
                NB = 1024
                xr = sbt(st_r, "xr", [128, NB + 3], F32); r_xr = Res()
                xc = sbt(st_r, "xc", [128, NB], F32); r_xc = Res()
                xcb = sbt(st_r, "xcb", [128, NB], BF16); r_xcb = Res()
                rg = sbt(st_r, "rg", [128, NB], F32); r_rg = Res()
                ig = sbt(st_r, "ig", [128, NB], F32); r_ig = Res()
                ag = sbt(st_r, "ag", [128, NB], F32); r_ag = Res()
                t1 = sbt(st_r, "t1", [128, NB], F32); r_t1 = Res()
                hs = sbt(st_r, "hs", [128, NB], F32); r_hs = Res()
                gg = sbt(st_r, "gg", [128, NB], F32); r_gg = Res()
                wxr = [sbt(st_r, "wxr%d" % i, [128, 8, 128], BF16) for i in range(2)]; r_wxr = RL(2)
                wgr = [sbt(st_r, "wgr%d" % i, [128, 8, 128], BF16) for i in range(2)]; r_wgr = RL(2)
                wbd = [sbt(st_r, "wbd%d" % i, [128, 4, 128], BF16) for i in range(2)]; r_wbd = RL(2)
                rp = sbt(st_r, "rp", [128, 4, 8], F32); r_rp = Res()
                clam = sbt(st_r, "clam", [128, 4], F32); r_clam = Res()
                state = sbt(st_r, "state", [128, 4], F32); r_state = Res()
                S.dma(rp[:], recp, writes=[r_rp])
                S.dma(wbd[0][:], wa_bd, writes=[r_wbd[0]], eng="gpsimd")
                S.dma(wbd[1][:], wx_bd, writes=[r_wbd[1]], eng="gpsimd")
                S.op("scalar", mk("activation", out=clam[:], in_=rp[:, :, 7], func=AF.Exp, scale=-1.0),
                     reads=[r_rp], writes=[r_clam])
                S.op("scalar", mk("activation", out=clam[:], in_=clam[:], func=AF.Ln, bias=1.0, scale=1.0),
                     reads=[r_clam], writes=[r_clam])
                S.op("vector", mk("tensor_scalar", out=clam[:], in0=clam[:], scalar1=-8.0, scalar2=None, op0=ALU.mult),
                     reads=[r_clam], writes=[r_clam])
                S.op("vector", mk("memset", ap=state[:], constant=0.0), writes=[r_state])
                for cch in range(4):
                    sl = cch % 2
                    S.dma(wxr[sl][:], wview(w_in, 1536 + cch * 128, 128), writes=[r_wxr[sl]], eng="gpsimd")
                    S.dma(wgr[sl][:], wview(w_in, 2048 + cch * 128, 128), writes=[r_wgr[sl]], eng="gpsimd")
                    S.op("vector", mk("memset", ap=xr[:, 0:3], constant=0.0), writes=[r_xr])
                    for seg in range(4):
                        t0 = seg * NB
                        rh = r_hT[seg * 8:(seg + 1) * 8]
                        if seg == 2:
                            S.op("vector", mk("tensor_scalar", out=xr[:, 0:3], in0=xr[:, 0:3], scalar1=flag[:, 0:1],
                                              scalar2=None, op0=ALU.mult), reads=[r_flag], writes=[r_xr])
                            S.op("vector", mk("tensor_scalar", out=state[:, cch:cch + 1], in0=state[:, cch:cch + 1],
                                              scalar1=flag[:, 0:1], scalar2=None, op0=ALU.mult),
                                 reads=[r_flag], writes=[r_state])
                        for h2 in range(2):
                            bk, r_bk = pb()
                            S.group("tensor", [mk("matmul", out=bk[:], lhsT=wxr[sl][:, kc, :],
                                                  rhs=hT[:, kc, t0 + h2 * 512:t0 + (h2 + 1) * 512],
                                                  start=(kc == 0), stop=(kc == 7)) for kc in range(8)],
                                    reads=[r_wxr[sl]] + rh, writes=[r_bk])
                            S.op("scalar", mk("activation", out=xr[:, 3 + h2 * 512:3 + (h2 + 1) * 512], in_=bk[:],
                                              func=AF.Copy), reads=[r_bk], writes=[r_xr])
                        S.op("vector", mk("tensor_scalar", out=xc[:], in0=xr[:, 3:NB + 3], scalar1=rp[:, cch, 3:4],
                                          scalar2=rp[:, cch, 4:5], op0=ALU.mult, op1=ALU.add),
                             reads=[r_xr, r_rp], writes=[r_xc])
                        for j in range(3):
                            S.op("vector", mk("scalar_tensor_tensor", out=xc[:], in0=xr[:, j:j + NB],
                                              scalar=rp[:, cch, j:j + 1], in1=xc[:], op0=ALU.mult, op1=ALU.add),
                                 reads=[r_xr, r_rp], writes=[r_xc])
                        S.op("vector", mk("tensor_copy", out=xr[:, 0:3], in_=xr[:, NB:NB + 3]), writes=[r_xr])
                        S.op("scalar", mk("activation", out=xcb[:], in_=xc[:], func=AF.Copy), reads=[r_xc], writes=[r_xcb])
                        for which, (dst, r_dst, bcol) in enumerate(((rg, r_rg, 5), (ig, r_ig, 6))):
                            for h2 in range(2):
                                bk, r_bk = pb()
                                S.group("tensor", [mk("matmul", out=bk[:], lhsT=wbd[which][:, cch, :],
                                                      rhs=xcb[:, h2 * 512:(h2 + 1) * 512], start=True, stop=True)],
                                        reads=[r_wbd[which], r_xcb], writes=[r_bk])
                                S.op("scalar", mk("activation", out=dst[:, h2 * 512:(h2 + 1) * 512], in_=bk[:],
                                                  func=AF.Sigmoid, bias=rp[:, cch, bcol:bcol + 1], scale=1.0),
                                     reads=[r_bk, r_rp], writes=[r_dst])
                        S.op("scalar", mk("activation", out=ag[:], in_=rg[:], func=AF.Exp, scale=clam[:, cch:cch + 1]),
                             reads=[r_rg, r_clam], writes=[r_ag])
                        S.op("vector", mk("tensor_tensor", out=t1[:], in0=ag[:], in1=ag[:], op=ALU.mult),
                             reads=[r_ag], writes=[r_t1])
                        S.op("vector", mk("tensor_scalar", out=t1[:], in0=t1[:], scalar1=-1.0, scalar2=1.0,
                                          op0=ALU.mult, op1=ALU.add), reads=[r_t1], writes=[r_t1])
                        S.op("vector", mk("tensor_scalar", out=t1[:], in0=t1[:], scalar1=1e-30, scalar2=None,
                                          op0=ALU.max), reads=[r_t1], writes=[r_t1])
                        S.op("scalar", mk("activation", out=t1[:], in_=t1[:], func=AF.Sqrt), reads=[r_t1], writes=[r_t1])
                        S.op("gpsimd", mk("tensor_tensor", out=ig[:], in0=ig[:], in1=xc[:], op=ALU.mult),
                             reads=[r_xc], writes=[r_ig])
                        S.op("gpsimd", mk("tensor_tensor", out=ig[:], in0=ig[:], in1=t1[:], op=ALU.mult),
                             reads=[r_t1], writes=[r_ig])
                        S.op("vector", mk("tensor_tensor_scan", out=hs[:], data0=ag[:], data1=ig[:],
                                          initial=state[:, cch:cch + 1], op0=ALU.mult, op1=ALU.add),
                             reads=[r_ag, r_ig, r_state], writes=[r_hs])
                        S.op("vector", mk("tensor_copy", out=state[:, cch:cch + 1], in_=hs[:, NB - 1:NB]),
                             reads=[r_hs], writes=[r_state])
                        if seg >= 2:
                            o0 = (seg - 2) * NB
                            for h2 in range(2):
                                bk, r_bk = pb()
                                S.group("tensor", [mk("matmul", out=bk[:], lhsT=wgr[sl][:, kc, :],
                                                      rhs=hT[:, kc, t0 + h2 * 512:t0 + (h2 + 1) * 512],
                                                      start=(kc == 0), stop=(kc == 7)) for kc in range(8)],
                                        reads=[r_wgr[sl]] + rh, writes=[r_bk])
                                S.op("scalar", mk("activation", out=gg[:, h2 * 512:(h2 + 1) * 512], in_=bk[:],
                                                  func=AF.Gelu_apprx_tanh), reads=[r_bk], writes=[r_gg])
                            S.op("gpsimd", mk("tensor_tensor", out=mixT[:, 4 + cch, o0:o0 + NB], in0=hs[:], in1=gg[:],
                                              op=ALU.mult), reads=[r_hs, r_gg],
                                 writes=r_bufA[4 + cch][(seg - 2) * 8:(seg - 1) * 8])
                S.barrier()
            with ExitStack() as st_at:
                wqkv = sbt(st_at, "wqkv", [128, 3, 8, 128], BF16); r_wqkv3 = RL(3)
                qTa = sbt(st_at, "qTa", [128, TOWN], BF16); r_qTa = Res()
                qTb = sbt(st_at, "qTb", [128, TOWN], BF16); r_qTb = Res()
                kT = sbt(st_at, "kT", [128, TALL], BF16); r_kT = Res()
                vT = sbt(st_at, "vT", [128, TALL], BF16); r_vT = Res()
                Vta = sbt(st_at, "Vta", [128, 32, 128], BF16); r_Vt = RL(8)
                Vtb = sbt(st_at, "Vtb", [128, 32, 128], BF16)
                accN = sbt(st_at, "accN", [128, TOWN], F32); r_accN = Res()
                accD = sbt(st_at, "accD", [128, TOWN], F32); r_accD = Res()
                PT = [sbt(st_at, "PT%d" % i, [128, 512], BF16) for i in range(2)]; r_PT = RL(2)
                pt_rr = 0
                S.op("gpsimd", mk("memset", ap=Vta[:], constant=0.0), writes=r_Vt)
                S.op("gpsimd", mk("memset", ap=Vtb[:], constant=0.0), writes=r_Vt)
                S.op("gpsimd", mk("memset", ap=qTa[:], constant=0.0), writes=[r_qTa])
                S.op("gpsimd", mk("memset", ap=qTb[:], constant=0.0), writes=[r_qTb])
                for fc in range(4):
                    for i3 in range(3):
                        S.dma(wqkv[:, i3, :, :], wview(w_in, i3 * 512 + fc * 128, 128), writes=[r_wqkv3[i3]], eng="gpsimd")
                    for tb in range(4):
                        bk, r_bk = pb()
                        c0 = TOWN + tb * 512
                        S.group("tensor", [mk("matmul", out=bk[:], lhsT=wqkv[:, 0, kc, :], rhs=hT[:, kc, c0:c0 + 512],
                                              start=(kc == 0), stop=(kc == 7)) for kc in range(8)],
                                reads=[r_wqkv3[0]] + r_hT[16 + tb * 4:16 + (tb + 1) * 4], writes=[r_bk])
                        S.op("scalar", mk("activation", out=qTa[0:64, tb * 512:(tb + 1) * 512], in_=bk[0:64, :],
                                          func=AF.Copy), reads=[r_bk], writes=[r_qTa])
                        S.op("vector", mk("tensor_copy", out=qTb[64:128, tb * 512:(tb + 1) * 512], in_=bk[64:128, :]),
                             reads=[r_bk], writes=[r_qTb])
                    for tb in range(8):
                        bk, r_bk = pb()
                        c0 = tb * 512
                        S.group("tensor", [mk("matmul", out=bk[:], lhsT=wqkv[:, 1, kc, :], rhs=hT[:, kc, c0:c0 + 512],
                                              start=(kc == 0), stop=(kc == 7)) for kc in range(8)],
                                reads=[r_wqkv3[1]] + r_hT[tb * 4:(tb + 1) * 4], writes=[r_bk])
                        evac(kT[:, c0:c0 + 512], bk[:], [r_bk], [r_kT])
                    for tb in range(8):
                        bk, r_bk = pb()
                        c0 = tb * 512
                        S.group("tensor", [mk("matmul", out=bk[:], lhsT=wqkv[:, 2, kc, :], rhs=hT[:, kc, c0:c0 + 512],
                                              start=(kc == 0), stop=(kc == 7)) for kc in range(8)],
                                reads=[r_wqkv3[2]] + r_hT[tb * 4:(tb + 1) * 4], writes=[r_bk])
                        evac(vT[:, c0:c0 + 512], bk[:], [r_bk], [r_vT])
                    for pat, d in enumerate((1, 4, 16)):
                        L = TALL // d
                        nj = L // 128
                        for g8 in range(4):
                            bk, r_bk = pb()
                            pv = bk[:].bitcast(BF16).rearrange("p (u c) -> p u c", u=8)
                            fns = []
                            for u in range(8):
                                ti = g8 * 8 + u
                                r_, j_ = divmod(ti, nj)
                                s0 = r_ + d * 128 * j_
                                fns.append(mk("transpose", out=pv[:, u, :], in_=vT[:, s0:s0 + d * 127 + 1:d], identity=ident[:]))
                            S.group("tensor", fns, reads=[r_vT, r_ident], writes=[r_bk])
                            S.op("scalar", mk("activation", out=Vta[:, g8 * 8:(g8 + 1) * 8, 0:64], in_=pv[:, :, 0:64],
                                              func=AF.Copy), reads=[r_bk], writes=[r_Vt[2 * g8], r_Vt[2 * g8 + 1]])
                            S.op("vector", mk("tensor_copy", out=Vtb[:, g8 * 8:(g8 + 1) * 8, 64:128], in_=pv[:, :, 64:128]),
                                 reads=[r_bk], writes=[r_Vt[2 * g8], r_Vt[2 * g8 + 1]])
                        for su in range(4):
                            bkN, r_bkN = pb()
                            bkD, r_bkD = pb()
                            for u in range(4):
                                if d == 1:
                                    r_, jq = 0, 16 + 4 * su + u
                                elif d == 4:
                                    r_, jq = u, 4 + su
                                else:
                                    r_, jq = 4 * su + u, 1
                                q0 = r_ + d * 128 * jq - TOWN
                                kp0 = r_ + d * 128 * (jq - 1)
                                kc0 = r_ + d * 128 * jq
                                tip = r_ * nj + jq - 1
                                tic = r_ * nj + jq
                                boundary = (jq == nj // 2)
                                span = d * 127 + 1
                                bkS, r_bkS = pb()
                                msk = maskB if boundary else negmask
                                fns = [mk("matmul", out=bkS[:], lhsT=ident[:], rhs=msk[:], start=True, stop=False)]
                                for bi, (qq, k0) in enumerate(((qTa, kp0), (qTa, kc0), (qTb, kp0), (qTb, kc0))):
                                    fns.append(mk("matmul", out=bkS[:, bi * 128:(bi + 1) * 128],
                                                  lhsT=kT[:, k0:k0 + span:d], rhs=qq[:, q0:q0 + span:d],
                                                  start=False, stop=(bi == 3)))
                                S.group("tensor", fns, reads=[r_ident, r_negmask, r_maskB, r_kT, r_qTa, r_qTb],
                                        writes=[r_bkS])
                                pt, r_pt = PT[pt_rr], r_PT[pt_rr]
                                pt_rr ^= 1
                                S.op("scalar", mk("activation", out=pt[:], in_=bkS[:], func=AF.Exp, scale=0.125),
                                     reads=[r_bkS], writes=[r_pt])
                                oc = slice(u * 128, (u + 1) * 128)
                                fnsN = [
                                    mk("matmul", out=bkN[:, oc], lhsT=Vta[:, tip, :], rhs=pt[:, 0:128], start=True, stop=False),
                                    mk("matmul", out=bkN[:, oc], lhsT=Vta[:, tic, :], rhs=pt[:, 128:256], start=False, stop=False),
                                    mk("matmul", out=bkN[:, oc], lhsT=Vtb[:, tip, :], rhs=pt[:, 256:384], start=False, stop=False),
                                    mk("matmul", out=bkN[:, oc], lhsT=Vtb[:, tic, :], rhs=pt[:, 384:512], start=False, stop=True),
                                ]
                                S.group("tensor", fnsN, reads=[r_pt, r_Vt[tip // 4], r_Vt[tic // 4]], writes=[r_bkN])
                                fnsD = [
                                    mk("matmul", out=bkD[:, oc], lhsT=ones_a[:], rhs=pt[:, 0:128], start=True, stop=False),
                                    mk("matmul", out=bkD[:, oc], lhsT=ones_a[:], rhs=pt[:, 128:256], start=False, stop=False),
                                    mk("matmul", out=bkD[:, oc], lhsT=ones_b[:], rhs=pt[:, 256:384], start=False, stop=False),
                                    mk("matmul", out=bkD[:, oc], lhsT=ones_b[:], rhs=pt[:, 384:512], start=False, stop=True),
                                ]
                                S.group("tensor", fnsD, reads=[r_pt, r_ones], writes=[r_bkD])
                            for acc, r_acc, bkX, r_bkX, eng in ((accN, r_accN, bkN, r_bkN, "vector"),
                                                               (accD, r_accD, bkD, r_bkD, "gpsimd")):
                                src = bkX[:].rearrange("p (u i) -> p u i", u=4)
                                if d == 1:
                                    dst = acc[:, su * 512:(su + 1) * 512].rearrange("p (u i) -> p u i", u=4)
                                elif d == 4:
                                    dst = acc[:, su * 512:(su + 1) * 512].rearrange("p (i r) -> p r i", r=4)
                                else:
                                    dst = acc[:].rearrange("p (i r) -> p r i", r=16)[:, 4 * su:4 * su + 4, :]
                                if d == 1:
                                    S.op("vector" if eng == "vector" else "scalar",
                                         mk("tensor_copy", out=dst, in_=src) if eng == "vector" else
                                         mk("activation", out=dst, in_=src, func=AF.Copy),
                                         reads=[r_bkX], writes=[r_acc])
                                else:
                                    S.op("vector", mk("tensor_tensor", out=dst, in0=dst, in1=src, op=ALU.add),
                                         reads=[r_bkX], writes=[r_acc])
                    S.op("vector", mk("reciprocal", out=accD[:], in_=accD[:]), reads=[r_accD], writes=[r_accD])
                    S.op("vector", mk("tensor_tensor", out=mixT[:, fc, :], in0=accN[:], in1=accD[:], op=ALU.mult),
                         reads=[r_accN, r_accD], writes=r_bufA[fc])
                S.barrier()
            if debug:
                with ExitStack() as st_d:
                    dtmp = sbt(st_d, "dtmp2", [128, 8 * 512], F32); r_dtmp = Res()
                    for q4 in range(4):
                        S.op("vector", mk("tensor_copy", out=dtmp[:].rearrange("p (k t) -> p k t", k=8),
                                          in_=mixT[:, :, q4 * 512:(q4 + 1) * 512]), reads=[], writes=[r_dtmp])
                        S.dma(dbg["d_mixT"].rearrange("p (k t) -> p k t", k=8)[:, :, q4 * 512:(q4 + 1) * 512],
                              dtmp[:].rearrange("p (k t) -> p k t", k=8), reads=[r_dtmp])
                    S.barrier()
            with ExitStack() as st_o:
                wout = sbt(st_o, "wout", [128, 8, D], BF16); r_wout2 = RL(2)
                S.dma(wout[:, 0:4, :], w_out[0:512, :].rearrange("(kc p) n -> p kc n", p=128), writes=[r_wout2[0]], eng="gpsimd")
                S.dma(wout[:, 4:8, :], w_out[512:1024, :].rearrange("(kc p) n -> p kc n", p=128), writes=[r_wout2[1]], eng="gpsimd")
                for t in range(16):
                    xs, r_xs = xb[t % 2], r_xb[t % 2]
                    S.dma(xs[:], x_all[TOWN + t * 128:TOWN + (t + 1) * 128, :], writes=[r_xs])
                    for cc in range(2):
                        bk, r_bk = pb()
                        cs = slice(cc * 512, (cc + 1) * 512)
                        S.group("tensor", [mk("matmul", out=bk[:], lhsT=mixT[:, kc, t * 128:(t + 1) * 128],
                                              rhs=wout[:, kc, cs], start=(kc == 0), stop=(kc == 7)) for kc in range(8)],
                                reads=r_wout2 + [r_bufA[kc][t] for kc in range(8)], writes=[r_bk])
                        S.op("vector", mk("tensor_tensor", out=tmpf[:, cs], in0=bk[:], in1=mv2[:, cs], op=ALU.mult),
                             reads=[r_bk, r_mv2], writes=[r_tmpf])
                        S.op("gpsimd", mk("tensor_tensor", out=x1[:, t, cs], in0=tmpf[:, cs], in1=xs[:, cs], op=ALU.add),
                             reads=[r_tmpf, r_xs], writes=[r_x1[t]])
                S.barrier()
        if debug:
            for t in range(16):
                S.dma(dbg["d_x1"][t * 128:(t + 1) * 128, :], x1[:, t, :], reads=[r_x1[t]])
        I32 = mybir.dt.int32
        CAP = TOWN
        x_buf = nc.dram_tensor("x_buf", [NE * CAP, D], BF16, kind="Internal").ap()
        y_buf = nc.dram_tensor("y_buf", [NE * CAP, D], F32, kind="Internal").ap()
        with ExitStack() as st_f:
            wgus = [bufA, sbt(st_f, "wgu1", [128, 8, 2 * D], BF16)]; r_wgus = [RL(4), RL(4)]
            wgus[0] = bufA[:].rearrange("p k t -> p (k t)").rearrange("p (k t) -> p k t", k=8)
            wd = sbt(st_f, "wd", [128, 8, D], BF16); r_wd = RL(2)
            gk = sbt(st_f, "gk", [128, 16, 4], F32); r_gk = RL(16)
            desti = sbt(st_f, "desti", [128, 16, 4], I32); r_desti = RL(16)
            cnti = sbt(st_f, "cnti", [128, NE], I32); r_cnti = Res()

            bgrow = [sbt(st_f, "bgrow%d" % i, [2, 2 * D], BF16) for i in range(2)]; r_bgrow = RL(2)
            ones_r = sbt(st_f, "ones_r", [2, 128], BF16); r_ones_r = Res()
            S.op("gpsimd", mk("memset", ap=ones_r[:], constant=1.0), writes=[r_ones_r])
            for i in range(2):
                S.op("gpsimd", mk("memset", ap=bgrow[i][:, 0:D], constant=0.0), writes=[r_bgrow[i]])
                S.op("gpsimd", mk("memset", ap=bgrow[i][:, D:2 * D], constant=1.0), writes=[r_bgrow[i]])

            def load_wgu(e):
                w_, r_w = wgus[e % 2], r_wgus[e % 2]
                for q4 in range(4):
                    S.dma(w_[:, :, q4 * 512:(q4 + 1) * 512], wview(w_gu[e], q4 * 512, 512), writes=[r_w[q4]], eng="gpsimd")
                S.dma(bgrow[e % 2][0:1, :], b_gu_d[e:e + 1, :], writes=[r_bgrow[e % 2]], eng="gpsimd")

            def load_wd(e):
                S.dma(wd[:, 0:4, :], w_dn[e][0:512, :].rearrange("(kc p) n -> p kc n", p=128), writes=[r_wd[0]], eng="gpsimd")
                S.dma(wd[:, 4:8, :], w_dn[e][512:1024, :].rearrange("(kc p) n -> p kc n", p=128), writes=[r_wd[1]], eng="gpsimd")

            load_wgu(0)
            load_wd(0)
            with ExitStack() as st_r2:
                wr = sbt(st_r2, "wr", [128, 8, NE], BF16); r_wr = Res()
                btmp = sbt(st_r2, "btmp", [128, D], F32); r_btmp = RL(2)
                brt = sbt(st_r2, "brt", [128, NE], F32); r_brt = Res()
                lg_2 = [sbt(st_r2, "lg_%d" % i_, [128, NE], F32) for i_ in range(2)]; r_lg_2 = RL(2)
                ex_2 = [sbt(st_r2, "ex_%d" % i_, [128, NE], F32) for i_ in range(2)]; r_ex_2 = RL(2)
                gt_2 = [sbt(st_r2, "gt_%d" % i_, [128, NE], F32) for i_ in range(2)]; r_gt_2 = RL(2)
                posb_2 = [sbt(st_r2, "posb_%d" % i_, [128, NE], F32) for i_ in range(2)]; r_posb_2 = RL(2)
                scr4_2 = [sbt(st_r2, "scr4_%d" % i_, [128, 4, NE], F32) for i_ in range(2)]; r_scr_2 = RL(2)
                ebase = sbt(st_r2, "ebase", [128, NE], F32); r_ebase = Res()
                m8_2 = [sbt(st_r2, "m8_%d" % i_, [128, 8], F32) for i_ in range(2)]; r_m8_2 = RL(2)
                e4_2 = [sbt(st_r2, "e4_%d" % i_, [128, 4], F32) for i_ in range(2)]; r_e4_2 = RL(2)
                destf_2 = [sbt(st_r2, "destf_%d" % i_, [128, 4], F32) for i_ in range(2)]; r_destf_2 = RL(2)
                nmx_2 = [sbt(st_r2, "nmx_%d" % i_, [128, 1], F32) for i_ in range(2)]; r_nmx_2 = RL(2)
                sm_2 = [sbt(st_r2, "sm_%d" % i_, [128, 1], F32) for i_ in range(2)]; r_sm_2 = RL(2)
                gT_2 = [sbt(st_r2, "gT_%d" % i_, [32, 128], F32) for i_ in range(2)]; r_gT_2 = RL(2)
                bdn = sbt(st_r2, "bdn", [32, D], F32); r_bdn = Res()
                maskall = sbt(st_r2, "maskall", [128, 16, NE], BF16); r_mask = RL(16)
                ltri = sbt(st_r2, "ltri", [128, 128], BF16); r_ltri = Res()
                ones_f = sbt(st_r2, "ones_full", [128, 128], BF16); r_onesf = Res()
                h2Tt_2 = [sbt(st_r2, "h2Tt_%d" % i_, [128, 8, 128], BF16) for i_ in range(2)]; r_h2Tt_2 = RL(2)
                S.dma(wr[:], w_router.rearrange("(kc p) n -> p kc n", p=128), writes=[r_wr], eng="gpsimd")
                S.dma(brt[:], b_router.partition_broadcast(128), writes=[r_brt])
                S.dma(bdn[:], b_dn, writes=[r_bdn])
                S.op("gpsimd", mk("iota", out=ebase[:], pattern=[[CAP, NE]], base=0, channel_multiplier=0,
                                  allow_small_or_imprecise_dtypes=True), writes=[r_ebase])
                S.op("gpsimd", mk("memset", ap=ones_f[:], constant=1.0), writes=[r_onesf])
                S.op("gpsimd", mk("memset", ap=ltri[:], constant=1.0), writes=[r_ltri])
                S.op("gpsimd", mk("affine_select", out=ltri[:], in_=ltri[:], pattern=[[1, 128]], compare_op=ALU.is_gt,
                                  fill=0.0, base=0, channel_multiplier=-1), reads=[r_ltri], writes=[r_ltri])
                for t in range(16):
                    rms_stats(x1[:, t, :], r_x1[t], 32 + t)
                for t in range(16):
                    hbt, r_hbt = hb[t % 2], r_hb[t % 2]
                    lg, r_lg = lg_2[t % 2], r_lg_2[t % 2]
                    ex, r_ex = ex_2[t % 2], r_ex_2[t % 2]
                    gt, r_gt = gt_2[t % 2], r_gt_2[t % 2]
                    posb, r_posb = posb_2[t % 2], r_posb_2[t % 2]
                    scr4, r_scr = scr4_2[t % 2], r_scr_2[t % 2]
                    m8, r_m8 = m8_2[t % 2], r_m8_2[t % 2]
                    e4, r_e4 = e4_2[t % 2], r_e4_2[t % 2]
                    destf, r_destf = destf_2[t % 2], r_destf_2[t % 2]
                    nmx, r_nmx = nmx_2[t % 2], r_nmx_2[t % 2]
                    sm, r_sm = sm_2[t % 2], r_sm_2[t % 2]
                    gT, r_gT = gT_2[t % 2], r_gT_2[t % 2]
                    h2Tt, r_h2Tt = h2Tt_2[t % 2], r_h2Tt_2[t % 2]
                    rms_tile(x1[:, t, :], r_x1[t], 32 + t, mv4[:], r_mv4, mv3[:], r_mv3, hbt[:], r_hbt, stats=False)
                    transpose_tile(hbt, r_hbt, h2Tt[:], [r_h2Tt])
                    bk, r_bk = pb()
                    S.group("tensor", [mk("matmul", out=bk[:, 0:NE], lhsT=h2Tt[:, kc, :], rhs=wr[:, kc, :],
                                          start=(kc == 0), stop=(kc == 7)) for kc in range(8)],
                            reads=[r_wr, r_h2Tt], writes=[r_bk])
                    S.op("vector", mk("tensor_tensor", out=lg[:], in0=bk[:, 0:NE], in1=brt[:], op=ALU.add),
                         reads=[r_bk, r_brt], writes=[r_lg])
                    S.op("vector", mk("max", out=m8[:], in_=lg[:]), reads=[r_lg], writes=[r_m8])
                    S.op("vector", mk("tensor_scalar", out=nmx[:], in0=m8[:, 0:1], scalar1=-1.0, scalar2=None, op0=ALU.mult),
                         reads=[r_m8], writes=[r_nmx])
                    S.op("scalar", mk("activation", out=ex[:], in_=lg[:], func=AF.Exp, bias=nmx[:, 0:1], scale=1.0),
                         reads=[r_lg, r_nmx], writes=[r_ex])
                    S.op("scalar", mk("activation", out=e4[:], in_=m8[:, 0:4], func=AF.Exp, bias=nmx[:, 0:1], scale=1.0),
                         reads=[r_m8, r_nmx], writes=[r_e4])
                    S.op("vector", mk("tensor_scalar", out=maskall[:, t, :], in0=lg[:], scalar1=m8[:, 3:4], scalar2=None,
                                      op0=ALU.is_ge), reads=[r_lg, r_m8], writes=[r_mask[t]])
                    S.op("vector", mk("tensor_tensor", out=ex[:], in0=ex[:], in1=maskall[:, t, :], op=ALU.mult),
                         reads=[r_mask[t]], writes=[r_ex])
                    S.op("vector", mk("tensor_reduce", out=sm[:], in_=ex[:], axis=mybir.AxisListType.X, op=ALU.add),
                         reads=[r_ex], writes=[r_sm])
                    S.op("vector", mk("reciprocal", out=sm[:], in_=sm[:]), reads=[r_sm], writes=[r_sm])
                    S.op("vector", mk("tensor_scalar", out=gt[:], in0=ex[:], scalar1=sm[:, 0:1], scalar2=None,
                                      op0=ALU.mult), reads=[r_ex, r_sm], writes=[r_gt])
                    S.op("vector", mk("tensor_scalar", out=gk[:, t, :], in0=e4[:], scalar1=sm[:, 0:1], scalar2=None,
                                      op0=ALU.mult), reads=[r_e4, r_sm], writes=[r_gk[t]])
                    bkp, r_bkp = pb()
                    fns = [mk("matmul", out=bkp[:, 0:NE], lhsT=ones_f[:], rhs=maskall[:, tp, :], start=(tp == 0), stop=False)
                           for tp in range(t)]
                    fns.append(mk("matmul", out=bkp[:, 0:NE], lhsT=ltri[:], rhs=maskall[:, t, :], start=(t == 0), stop=True))
                    S.group("tensor", fns, reads=[r_onesf, r_ltri] + r_mask[:t + 1], writes=[r_bkp])
                    S.op("vector", mk("tensor_tensor", out=posb[:], in0=bkp[:, 0:NE], in1=ebase[:], op=ALU.add),
                         reads=[r_bkp, r_ebase], writes=[r_posb])
                    for k in range(4):
                        S.op("vector", mk("scalar_tensor_tensor", out=scr4[:, k, :], in0=lg[:], scalar=m8[:, k:k + 1], in1=posb[:],
                                          op0=ALU.is_equal, op1=ALU.mult),
                             reads=[r_lg, r_m8, r_posb], writes=[r_scr])
                    S.op("vector", mk("tensor_reduce", out=destf[:], in_=scr4[:], axis=mybir.AxisListType.X, op=ALU.add),
                         reads=[r_scr], writes=[r_destf])
                    S.op("vector", mk("tensor_scalar", out=destf[:], in0=destf[:], scalar1=0.0, scalar2=float(NE * CAP - 1),
                                      op0=ALU.max, op1=ALU.min), reads=[r_destf], writes=[r_destf])
                    S.op("vector", mk("tensor_copy", out=desti[:, t, :], in_=destf[:]), reads=[r_destf], writes=[r_desti[t]])
                    for k in range(4):
                        def sc(e, t=t, k=k, hbt=hbt):
                            return e.indirect_dma_start(out=x_buf[:, :],
                                                        out_offset=bass.IndirectOffsetOnAxis(ap=desti[:, t, k:k + 1], axis=0),
                                                        in_=hbt[:, :], in_offset=None)
                        S.dma_fn("gpsimd", sc, reads=[r_desti[t], r_hbt], writes=[])
                    bk2, r_bk2 = pb()
                    S.group("tensor", [mk("transpose", out=bk2[0:NE, 0:128], in_=gt[:], identity=identf[:])],
                            reads=[r_gt, r_identf], writes=[r_bk2])
                    S.op("vector", mk("tensor_copy", out=gT[:], in_=bk2[0:NE, 0:128]), reads=[r_bk2], writes=[r_gT])
                    for cc in range(2):
                        cs = slice(cc * 512, (cc + 1) * 512)
                        bk3, r_bk3 = pb()
                        S.group("tensor", [mk("matmul", out=bk3[:], lhsT=gT[:], rhs=bdn[:, cs], start=True, stop=True)],
                                reads=[r_gT, r_bdn], writes=[r_bk3])
                        S.op("vector", mk("tensor_tensor", out=btmp[:, cs], in0=bk3[:], in1=mv5[:, cs], op=ALU.mult),
                             reads=[r_bk3, r_mv5], writes=[r_btmp[cc]])
                        S.op("vector", mk("tensor_tensor", out=x1[:, t, cs], in0=btmp[:, cs], in1=x1[:, t, cs], op=ALU.add),
                             reads=[r_btmp[cc]], writes=[r_x1[t]])
                bkc, r_bkc = pb()
                S.group("tensor", [mk("matmul", out=bkc[:, 0:NE], lhsT=ones_f[:], rhs=maskall[:, tp, :], start=(tp == 0),
                                      stop=(tp == 15)) for tp in range(16)], reads=[r_onesf] + r_mask, writes=[r_bkc])
                S.op("vector", mk("tensor_copy", out=cnti[:], in_=bkc[:, 0:NE]), reads=[r_bkc], writes=[r_cnti])
                if debug:
                    S.dma(dbg["d_gates"][:, 0:64], gk[:].rearrange("p t e -> p (t e)"), reads=r_gk)
                S.barrier()
            S.dma(mv3[:], g_final.partition_broadcast(128), writes=[r_mv3])
            with ExitStack() as st_m:
                xblk = [hb[0], hb[1]]; r_xblk = RL(2)
                xT1 = sbt(st_m, "xT1", [128, 8, 128], BF16)
                xTs = [junk[:].rearrange("p (k t) -> p k t", k=8), xT1[:]]; r_xT = RL(2)
                actT = [sbt(st_m, "actT%d" % i, [128, 8, 128], BF16) for i in range(2)]; r_act = [RL(2), RL(2)]
                yblk = [xb[0], xb[1]]; r_yblk = RL(2)
                sgx = sbt(st_m, "sgx", [128, D], F32)
                ucx = sbt(st_m, "ucx", [128, D], F32)
                gc2 = [[tmpf[:, 0:512], tmpf[:, 512:1024]], [sgx[:, 0:512], sgx[:, 512:1024]]]; r_gc2 = [RL(2), RL(2)]
                actk = [sbt(st_m, "actk%d" % i, [128, D], BF16) for i in range(2)]; r_actk = [RL(2), RL(2)]
                ucs = [[mv4[:, 0:512], mv4[:, 512:1024]], [ucx[:, 0:512], ucx[:, 512:1024]]]; r_uc = [RL(2), RL(2)]
                evac_dve_only[0] = True
                S.pe_skip_ap = ident[:]
                blk_rr = 0
                S.load_counts(lambda k: cnti[0:1, k:k + 1], NE, [r_cnti])
                for e in range(n_experts if stage >= 2 else 0):
                    w_, r_w = wgus[e % 2], r_wgus[e % 2]
                    if e + 1 < n_experts:
                        load_wgu(e + 1)
                    for jp in range(0, 16, 2):
                        S.begin_guard((e, jp), 128 * jp + 1)
                        pair = (jp, jp + 1)

                        NESTED = True

                        def inner(j, ph):
                            if j != jp and NESTED:
                                S.begin_inner((e, j, ph), 128 * j + 1)

                        def inner_end(j):
                            if j != jp and NESTED:
                                S.end_inner()
                        for j in pair:
                            s_ = j % 2
                            row0 = e * CAP + 128 * j
                            inner(j, 1)
                            S.dma(xblk[s_][:], x_buf[row0:row0 + 128, :], writes=[r_xblk[s_]])
                            transpose_tile(xblk[s_], r_xblk[s_], xTs[s_], [r_xT[s_]])
                            inner_end(j)
                        for j in pair:
                            s_ = j % 2
                            xT = xTs[s_]
                            inner(j, 2)
                            for half in range(2):
                                gub = []
                                for c0 in (half * 512, D + half * 512):
                                    bkX, r_bkX = pb()
                                    fns = [mk("matmul", out=bkX[:], lhsT=ones_r[0:2, :], rhs=bgrow[e % 2][0:2, c0:c0 + 512],
                                              start=True, stop=False)]
                                    for kc in range(8):
                                        fns.append(mk("matmul", out=bkX[:], lhsT=xT[:, kc, :], rhs=w_[:, kc, c0:c0 + 512],
                                                      start=False, stop=(kc == 7)))
                                    S.group("tensor", fns, reads=[r_w[c0 // 512], r_xT[s_], r_ones_r, r_bgrow[e % 2]], writes=[r_bkX])
                                    gub.append((bkX, r_bkX))
                                (bkG, r_bkG), (bkU, r_bkU) = gub
                                uc, r_uc_ = ucs[s_][half], r_uc[s_][half]
                                gc, r_gc_ = gc2[s_][half], r_gc2[s_][half]
                                S.op("scalar", mk("activation", out=gc, in_=bkG[:], func=AF.Gelu_apprx_sigmoid),
                                     reads=[r_bkG], writes=[r_gc_])
                                S.op("vector", mk("tensor_scalar", out=uc, in0=bkU[:], scalar1=8.0, scalar2=-6.0,
                                                  op0=ALU.min, op1=ALU.max), reads=[r_bkU], writes=[r_uc_])
                                S.op("vector", mk("scalar_tensor_tensor", out=actk[s_][:, half * 512:(half + 1) * 512], in0=gc,
                                                  scalar=GLU7, in1=uc, op0=ALU.min, op1=ALU.mult),
                                     reads=[r_uc_, r_gc_], writes=[r_actk[s_][half]])
                            inner_end(j)
                        for j in pair:
                            s_ = j % 2
                            row0 = e * CAP + 128 * j
                            inner(j, 3)
                            for half in range(2):
                                transpose_tile(actk[s_], r_actk[s_][half], actT[s_][:, 4 * half:4 * half + 4, :],
                                               [r_act[s_][half]], k0=4 * half, k1=4 * half + 4)
                            for cc in range(2):
                                cs = slice(cc * 512, (cc + 1) * 512)
                                bk, r_bk = pb()
                                S.group("tensor", [mk("matmul", out=bk[:], lhsT=actT[s_][:, kc, :], rhs=wd[:, kc, cs],
                                                      start=(kc == 0), stop=(kc == 7)) for kc in range(8)],
                                        reads=r_wd + r_act[s_], writes=[r_bk])
                                S.op("vector", mk("tensor_tensor", out=yblk[s_][:, cs], in0=bk[:], in1=mv5[:, cs], op=ALU.mult),
                                     reads=[r_bk, r_mv5], writes=[r_yblk[s_]])
                            S.dma(y_buf[row0:row0 + 128, :], yblk[s_][:], reads=[r_yblk[s_]], eng="gpsimd")
                            inner_end(j)
                        S.end_guard()
                    if e + 1 < n_experts:
                        load_wd(e + 1)
                evac_dve_only[0] = False
                S.barrier()
            st_c = st_f.enter_context(ExitStack())
            ykb = [mv4, tmpf] + [sbt(st_c, "ykb%d" % i, [128, D], F32) for i in range(4)]; r_ykb = RL(6)
            gi = 0
            for t in range(16):
                for k in range(4 if stage >= 3 else 0):
                    yk, r_yk = ykb[gi % 6], r_ykb[gi % 6]
                    gi += 1

                    def ga(e, t=t, k=k, yk=yk):
                        return e.indirect_dma_start(out=yk[:, :], out_offset=None, in_=y_buf[:, :],
                                                    in_offset=bass.IndirectOffsetOnAxis(ap=desti[:, t, k:k + 1], axis=0))
                    S.dma_fn("gpsimd", ga, reads=[r_desti[t]], writes=[r_yk])
                    S.op("vector", mk("scalar_tensor_tensor", out=x1[:, t, :], in0=yk[:], scalar=gk[:, t, k:k + 1],
                                      in1=x1[:, t, :], op0=ALU.mult, op1=ALU.add),
                         reads=[r_yk, r_gk[t]], writes=[r_x1[t]])
            for t in range(16):
                ob = xb[t % 2]; r_ob = r_xb[t % 2]
                rms_tile(x1[:, t, :], r_x1[t], 48 + t, mv3[:], r_mv3, None, None, ob[:], r_ob)
                S.dma(out_d[t * 128:(t + 1) * 128, :], ob[:], reads=[r_ob])
            S.barrier()
        with nc.Block() as block:
            S.emit(block)
    return nc


_NC_CACHE = {}


def make_in_maps(inputs, cores, n_experts=NE):
    f = lambda a: np.ascontiguousarray(np.asarray(a, dtype=np.float32))
    x = f(inputs["x"]); c = f(inputs["c"])
    w_ada = f(inputs["w_ada"])[0]; b_ada = f(inputs["b_ada"])[0][None, :]
    g_mix = f(inputs["g_mix"])[0][None, :]; g_ffn = f(inputs["g_ffn"])[0][None, :]
    g_final = f(inputs["g_final"])[None, :]
    w_in = f(inputs["w_in"])[0]; w_out = f(inputs["w_out"])[0]
    conv_w = f(inputs["conv_w"])[0]; conv_b = f(inputs["conv_b"])[0]
    b_a = f(inputs["b_rg_a"])[0]; b_x = f(inputs["b_rg_x"])[0]; lam = f(inputs["lam"])[0]
    recp = np.zeros((128, 4, 8), np.float32)
    cols = [conv_w[0], conv_w[1], conv_w[2], conv_w[3], conv_b, b_a, b_x, lam]
    for j, v in enumerate(cols):
        recp[:, :, j] = v.reshape(4, 128).T
    def blockdiag(w):
        w = f(w)[0]
        o = np.zeros((128, 4, 128), np.float32)
        for blk in range(8):
            cch, hh = divmod(blk, 2)
            o[hh * 64:(hh + 1) * 64, cch, hh * 64:(hh + 1) * 64] = w[blk]
        return o
    wa_bd = blockdiag(inputs["w_rg_a"]); wx_bd = blockdiag(inputs["w_rg_x"])
    w_router = f(inputs["w_router"])[0]; b_router = f(inputs["b_router"])[0][None, :]
    w_gu = f(inputs["w_gate_up"])[0][:n_experts]; b_gu = f(inputs["b_gate_up"])[0]
    bgu_t = np.ascontiguousarray(b_gu.reshape(NE, 16, 128).transpose(2, 0, 1))
    w_dn = f(inputs["w_down"])[0][:n_experts]; b_dn = f(inputs["b_down"])[0]
    maps = []
    for core in cores:
        b, half = divmod(core, 2)
        x_all = np.zeros((TALL, D), np.float32)
        if half == 0:
            x_all[TOWN:] = x[b, :TOWN]
        else:
            x_all[:] = x[b]
        c_rep = np.ascontiguousarray(np.broadcast_to(c[b].reshape(8, 128).T[:, :, None], (128, 8, 128)))
        flag = np.full((128, 1), float(half), np.float32)
        maps.append({
            "x_all": x_all, "c_rep": c_rep, "flag": flag, "w_ada": w_ada, "b_ada": b_ada, "g_mix": g_mix,
            "g_ffn": g_ffn, "g_final": g_final, "w_in": w_in, "w_out": w_out, "recp": recp, "wa_bd": wa_bd,
            "wx_bd": wx_bd, "w_router": w_router, "b_router": b_router, "w_gate_up": w_gu, "bgu_t": bgu_t, "b_gu": b_gu,
            "w_down": w_dn, "b_down": b_dn,
        })
    return maps


def kernel(**inputs):
    if "nc" not in _NC_CACHE:
        _NC_CACHE["nc"] = build()
    nc = _NC_CACHE["nc"]
    cores = list(range(8))
    in_maps = make_in_maps(inputs, cores)
    res = run_bass_kernel_spmd(nc, in_maps, core_ids=cores)
    out = np.zeros((4, 4096, D), np.float32)
    for core in cores:
        b, half = divmod(core, 2)
        out[b, half * TOWN:(half + 1) * TOWN] = res.results[core]["out"]
    return out
```

```python
import numpy as np
from contextlib import ExitStack
import concourse.bass as bass
import concourse.mybir as mybir
from concourse.bass_utils import run_bass_kernel_spmd

F32 = mybir.dt.float32
BF16 = mybir.dt.bfloat16
AF = mybir.ActivationFunctionType
ALU = mybir.AluOpType

D = 1024
TOWN = 2048
TALL = 4096
NE = 32
EPS = 1e-6
GLU7 = float(np.float32(7.0) / (np.float32(1.0) + np.exp(np.float32(-1.702 * 7.0))))
SEM_LIMIT = 30000


class Res:
    __slots__ = ("w", "r")

    def __init__(self):
        self.w = None
        self.r = {}


def RL(n):
    return [Res() for _ in range(n)]


class Sched:
    ENGS = ("tensor", "vector", "scalar", "gpsimd", "sync")

    def __init__(self, nc, stack, n_dma_sems=8):
        self.nc = nc
        self.stack = stack
        self.ops = {e: [] for e in self.ENGS}
        self.known = {e: {} for e in self.ENGS}
        self.dom_sem = {}
        self.dom_max = {}
        self.ndom = 0
        self.cur_dom = {}
        self.cur_cnt = {}
        self.guard = None
        self.guard_snap = {}
        self.regs = {}
        for e in self.ENGS:
            self.cur_dom[e] = self._new_dom(e)
            self.cur_cnt[e] = 0
        self.dma_pool = {}
        for q in ("sync", "gpsimd"):
            self.dma_pool[q] = {"doms": [self._new_dom("dma_%s%d" % (q, i)) for i in range(n_dma_sems)],
                                "cnt": [0] * n_dma_sems, "rr": 0}

    def _new_dom(self, name):
        d = self.ndom
        self.ndom += 1
        self.dom_sem[d] = self.stack.enter_context(self.nc.semaphore("s_%s_%d" % (name, d)))
        self.dom_max[d] = 0
        return d

    def _collect(self, eng, reads, writes):
        need = {}
        for R in reads:
            if R.w is not None:
                d, v = R.w
                if need.get(d, 0) < v:
                    need[d] = v
        for R in writes:
            if R.w is not None:
                d, v = R.w
                if need.get(d, 0) < v:
                    need[d] = v
            for d, v in R.r.items():
                if need.get(d, 0) < v:
                    need[d] = v
        kn = self.known[eng]
        waits = []
        for d, v in need.items():
            if eng == "tensor" and d == self.cur_dom["tensor"]:
                continue
            if kn.get(d, 0) < v:
                kn[d] = v
                waits.append((d, v))
        return waits

    def _tick(self, eng):
        self.cur_cnt[eng] += 1
        d = self.cur_dom[eng]
        v = self.cur_cnt[eng]
        self.dom_max[d] = v
        return d, v

    def _mark(self, d, v, reads, writes):
        for R in reads:
            R.r[d] = v
        for R in writes:
            R.w = (d, v)
            R.r = {}

    def op(self, eng, fn, reads=(), writes=()):
        waits = self._collect(eng, reads, writes)
        d, v = self._tick(eng)
        self.ops[eng].append((waits, fn, self.dom_sem[d], 1, self.guard))
        self._mark(d, v, reads, writes)

    def group(self, eng, fns, reads=(), writes=()):
        waits = self._collect(eng, reads, writes)
        d, v = self._tick(eng)
        n = len(fns)
        for i, fn in enumerate(fns):
            self.ops[eng].append((waits if i == 0 else [], fn,
                                  self.dom_sem[d] if i == n - 1 else None, 1, self.guard))
        self._mark(d, v, reads, writes)

    def dma_fn(self, eng, fn, reads=(), writes=()):
        pool = self.dma_pool[eng]
        i = pool["rr"]
        pool["rr"] = (i + 1) % len(pool["doms"])
        d = pool["doms"][i]
        waits = self._collect(eng, reads, writes)
        prev = pool["cnt"][i]
        kn = self.known[eng]
        if prev > 0 and kn.get(d, 0) < prev:
            kn[d] = prev
            waits.append((d, prev))
        pool["cnt"][i] += 16
        v = pool["cnt"][i]
        self.dom_max[d] = v
        self.ops[eng].append((waits, fn, self.dom_sem[d], 16, self.guard))
        self._mark(d, v, reads, writes)

    def dma(self, out, in_, reads=(), writes=(), eng="sync"):
        def fn(e, out=out, in_=in_):
            return e.dma_start(out=out, in_=in_)
        self.dma_fn(eng, fn, reads, writes)

    def load_count(self, ap, reads):
        for eng in self.ENGS:
            waits = self._collect(eng, reads, ())
            sched = self

            def fn(e, eng=eng, ap=ap):
                return e.reg_load(sched.regs[eng], ap)
            self.ops[eng].append((waits, fn, None, 0, None))
        for R in reads:
            for eng in self.ENGS:
                pass

    def begin_guard(self, gid, thr):
        self.guard = (gid, thr, None, 0)
        self.guard_snap[gid] = dict(self.dom_max)

    def begin_inner(self, gid, thr):
        g = self.guard
        self.guard = (g[0], g[1], gid, thr)
        self.guard_snap[gid] = dict(self.dom_max)

    def end_inner(self):
        g = self.guard
        self.guard = (g[0], g[1], None, 0)

    def end_guard(self):
        self.guard = None

    def barrier(self):
        assert self.guard is None
        for eng in self.ENGS:
            kn = self.known[eng]
            waits = []
            for d, v in self.dom_max.items():
                if v > 0 and kn.get(d, 0) < v:
                    kn[d] = v
                    waits.append((d, v))
            if waits:
                self.ops[eng].append((waits, None, None, 0, None))

    def emit(self, block):
        sched = self

        def emit_one(e, item):
            waits, fn, sem, inc, _ = item
            for (d_, v) in waits:
                e.wait_ge(sched.dom_sem[d_], v)
            if fn is None:
                return
            ins = fn(e)
            if sem is not None:
                ins.then_inc(sem, inc)

        def skip_path(e, grp, snap):
            incs = []
            need = {}
            for waits, fn, sem, inc, _ in grp:
                for (d_, v) in waits:
                    v = min(v, snap.get(d_, 0))
                    if v > need.get(d_, 0):
                        need[d_] = v
            for d_, v in need.items():
                e.wait_ge(sched.dom_sem[d_], v)
            for waits, fn, sem, inc, _ in grp:
                if sem is not None:
                    for k_ in range(len(incs)):
                        if incs[k_][0] is sem:
                            incs[k_][1] += inc
                            break
                    else:
                        incs.append([sem, inc])
            e.drain()
            for sem, tot in incs:
                e.sem_inc(sem, tot)

        def emit_region(e, reg, grp):
            a = 0
            m = len(grp)
            while a < m:
                gi = grp[a][4]
                if gi[2] is None:
                    emit_one(e, grp[a])
                    a += 1
                    continue
                b = a
                while b < m and grp[b][4][2] == gi[2]:
                    b += 1
                sub = grp[a:b]
                with e.If_lt(reg, gi[3]):
                    skip_path(e, sub, sched.guard_snap[gi[2]])
                with e.Else():
                    for it in sub:
                        emit_one(e, it)
                a = b

        def emit_chain(e, reg, regions, idx):
            if idx == len(regions):
                return
            grp = regions[idx]
            g = grp[0][4]
            rest = [it for r_ in regions[idx:] for it in r_]
            with e.If_lt(reg, g[1]):
                skip_path(e, rest, sched.guard_snap[g[0]])
            with e.Else():
                emit_region(e, reg, grp)
                emit_chain(e, reg, regions, idx + 1)

        def make(engname):
            def body(e):
                ops = sched.ops[engname]
                sched.regs[engname] = e.alloc_register("cnt_" + engname)
                reg = sched.regs[engname]
                i = 0
                n = len(ops)
                while i < n:
                    g = ops[i][4]
                    if g is None:
                        emit_one(e, ops[i])
                        i += 1
                        continue
                    regions = []
                    j = i
                    while j < n and ops[j][4] is not None and ops[j][4][0][0] == g[0][0]:
                        k = j
                        while k < n and ops[k][4] is not None and ops[k][4][0] == ops[j][4][0]:
                            k += 1
                        regions.append(ops[j:k])
                        j = k
                    emit_chain(e, reg, regions, 0)
                    i = j
            return body
        for engname in self.ENGS:
            if self.ops[engname]:
                getattr(block, engname)(make(engname))


def mk(method, **kw):
    return lambda e: getattr(e, method)(**kw)


def build(debug=False, n_experts=NE, stage=3):
    nc = bass.Bass("TRN2", target_bir_lowering=False)

    def din(name, shape):
        return nc.dram_tensor(name, shape, F32, kind="ExternalInput").ap()

    x_all = din("x_all", [TALL, D])
    c_rep = din("c_rep", [128, 8, 128])
    flag_d = din("flag", [128, 1])
    w_ada = din("w_ada", [D, 6 * D])
    b_ada = din("b_ada", [1, 6 * D])
    g_mix = din("g_mix", [1, D])
    g_ffn = din("g_ffn", [1, D])
    g_final = din("g_final", [1, D])
    w_in = din("w_in", [D, 2560])
    w_out = din("w_out", [D, D])
    recp = din("recp", [128, 4, 8])
    wa_bd = din("wa_bd", [128, 4, 128])
    wx_bd = din("wx_bd", [128, 4, 128])
    w_router = din("w_router", [D, NE])
    b_router = din("b_router", [1, NE])
    w_gu = din("w_gate_up", [n_experts, D, 2 * D])
    bgu_t = din("bgu_t", [128, NE, 16])
    b_gu_d = din("b_gu", [NE, 2 * D])
    w_dn = din("w_down", [n_experts, D, D])
    b_dn = din("b_down", [NE, D])
    out_d = nc.dram_tensor("out", [TOWN, D], F32, kind="ExternalOutput").ap()
    dbg = {}
    if debug:
        def dout(name, shape):
            dbg[name] = nc.dram_tensor(name, shape, F32, kind="ExternalOutput").ap()
        dout("d_mod", [128, 6 * D])
        dout("d_hT", [128, 8 * 512])
        dout("d_mixT", [128, 8 * TOWN])
        dout("d_x1", [TOWN, D])
        dout("d_gates", [128, 16 * NE])

    with ExitStack() as st:
        S = Sched(nc, st)

        def sbt(stack, name, shape, dt):
            return stack.enter_context(nc.sbuf_tensor(name, shape, dt))

        arena = sbt(st, "arena", [128, 16384], F32)
        hT = arena[:].bitcast(BF16).rearrange("p (k t) -> p k t", k=8)
        x1 = arena[:].rearrange("p (t f) -> p t f", t=16)
        r_hT = RL(32)
        r_x1 = RL(16)
        bufA = sbt(st, "bufA", [128, 8, TOWN], BF16)
        r_bufA = [RL(16) for _ in range(8)]
        mv3 = sbt(st, "mv3", [128, D], F32); r_mv3 = Res()
        mv4 = sbt(st, "mv4", [128, D], F32); r_mv4 = Res()
        mv5 = sbt(st, "mv5", [128, D], F32); r_mv5 = Res()
        xb = [sbt(st, "xb%d" % i, [128, D], F32) for i in range(2)]; r_xb = RL(2)
        tmpf = sbt(st, "tmpf", [128, D], F32); r_tmpf = Res()
        hb = [sbt(st, "hb%d" % i, [128, D], BF16) for i in range(2)]; r_hb = RL(2)
        junk = sbt(st, "junk", [128, D], BF16)
        ss = sbt(st, "ss", [128, 64], F32); r_ss = RL(64)
        ms = sbt(st, "ms", [128, 64], F32); r_ms = RL(64)
        rstd = sbt(st, "rstd", [128, 64], F32); r_rstd = RL(64)
        ident = sbt(st, "ident", [128, 128], BF16); r_ident = Res()
        identf = sbt(st, "identf", [128, 128], F32); r_identf = Res()
        neghalf = sbt(st, "neghalf", [128, 1], F32); r_nh = Res()
        flag = sbt(st, "flag_sb", [128, 1], F32); r_flag = Res()
        flagb = sbt(st, "flagb", [128, 1], F32); r_flagb = Res()
        negmask = sbt(st, "negmask", [128, 512], BF16); r_negmask = Res()
        maskB = sbt(st, "maskB", [128, 512], BF16); r_maskB = Res()
        ones_a = sbt(st, "ones_a", [128, 128], BF16); r_ones = Res()
        ones_b = sbt(st, "ones_b", [128, 128], BF16)

        banks = [st.enter_context(nc.psum_tensor("bank%d" % i, [128, 512], F32)) for i in range(8)]
        r_bank = RL(8)
        bank_rr = [0]

        def pb():
            i = bank_rr[0]
            bank_rr[0] = (i + 1) % 8
            return banks[i], r_bank[i]

        cp_rr = [0]
        evac_dve_only = [False]

        def evac(out, in_, reads, writes):
            cp_rr[0] ^= 1
            if cp_rr[0] and not evac_dve_only[0]:
                S.op("scalar", mk("activation", out=out, in_=in_, func=AF.Copy), reads=reads, writes=writes)
            else:
                S.op("vector", mk("tensor_copy", out=out, in_=in_), reads=reads, writes=writes)

        S.dma(flag[:], flag_d, writes=[r_flag])
        S.op("gpsimd", mk("memset", ap=neghalf[:], constant=-0.5), writes=[r_nh])
        S.op("gpsimd", mk("memset", ap=ident[:], constant=1.0), writes=[r_ident])
        S.op("gpsimd", mk("affine_select", out=ident[:], in_=ident[:], pattern=[[-1, 128]],
                          compare_op=ALU.is_equal, fill=0.0, base=0, channel_multiplier=1),
             reads=[r_ident], writes=[r_ident])
        S.op("gpsimd", mk("memset", ap=identf[:], constant=1.0), writes=[r_identf])
        S.op("gpsimd", mk("affine_select", out=identf[:], in_=identf[:], pattern=[[-1, 128]],
                          compare_op=ALU.is_equal, fill=0.0, base=0, channel_multiplier=1),
             reads=[r_identf], writes=[r_identf])
        S.op("gpsimd", mk("memset", ap=negmask[:], constant=0.0), writes=[r_negmask])
        for blk in range(4):
            sl = negmask[:, blk * 128:(blk + 1) * 128]
            if blk % 2 == 0:
                S.op("gpsimd", mk("affine_select", out=sl, in_=sl, pattern=[[-1, 128]], compare_op=ALU.is_ge,
                                  fill=-30000.0, base=0, channel_multiplier=1), reads=[r_negmask], writes=[r_negmask])
            else:
                S.op("gpsimd", mk("affine_select", out=sl, in_=sl, pattern=[[1, 128]], compare_op=ALU.is_ge,
                                  fill=-30000.0, base=0, channel_multiplier=-1), reads=[r_negmask], writes=[r_negmask])
        S.op("vector", mk("tensor_scalar", out=flagb[:], in0=flag[:], scalar1=-1.0, scalar2=30000.0,
                          op0=ALU.add, op1=ALU.mult), reads=[r_flag], writes=[r_flagb])
        S.op("vector", mk("tensor_copy", out=maskB[:], in_=negmask[:]), reads=[r_negmask], writes=[r_maskB])
        for blk in (0, 2):
            sl = maskB[:, blk * 128:(blk + 1) * 128]
            S.op("vector", mk("tensor_scalar", out=sl, in0=sl, scalar1=flagb[:, 0:1], scalar2=None, op0=ALU.add),
                 reads=[r_maskB, r_flagb], writes=[r_maskB])
        S.op("gpsimd", mk("memset", ap=ones_a[:], constant=0.0), writes=[r_ones])
        S.op("gpsimd", mk("memset", ap=ones_a[:, 0:64], constant=1.0), writes=[r_ones])
        S.op("gpsimd", mk("memset", ap=ones_b[:], constant=0.0), writes=[r_ones])
        S.op("gpsimd", mk("memset", ap=ones_b[:, 64:128], constant=1.0), writes=[r_ones])

        def wview(w2d, c0, n):
            return w2d[:, c0:c0 + n].rearrange("(kc p) n -> p kc n", p=128)

        r_junk = Res()
        add_rr = [0]

        def rms_stats(src, r_src, col):
            S.op("scalar", mk("activation", out=junk[:], in_=src, func=AF.Square, accum_out=ss[:, col:col + 1]),
                 reads=[r_src], writes=[r_ss[col], r_junk])
            S.op("vector", mk("tensor_scalar", out=ms[:, col:col + 1], in0=ss[:, col:col + 1], scalar1=1.0 / D,
                              scalar2=EPS, op0=ALU.mult, op1=ALU.add), reads=[r_ss[col]], writes=[r_ms[col]])
            S.op("gpsimd", mk("tensor_tensor", out=rstd[:, col:col + 1], in0=ms[:, col:col + 1], in1=neghalf[:],
                              op=ALU.pow), reads=[r_ms[col], r_nh], writes=[r_rstd[col]])

        def rms_tile(src, r_src, col, A_vec, r_A, B_vec, r_B, hbt, r_hbt, stats=True):
            if stats:
                rms_stats(src, r_src, col)
            if B_vec is None:
                S.op("vector", mk("scalar_tensor_tensor", out=hbt, in0=src, scalar=rstd[:, col:col + 1], in1=A_vec,
                                  op0=ALU.mult, op1=ALU.mult), reads=[r_src, r_rstd[col], r_A], writes=[r_hbt])
                return
            S.op("vector", mk("scalar_tensor_tensor", out=tmpf[:], in0=src, scalar=rstd[:, col:col + 1], in1=A_vec,
                              op0=ALU.mult, op1=ALU.mult), reads=[r_src, r_rstd[col], r_A], writes=[r_tmpf])
            add_rr[0] ^= 1
            S.op("vector" if add_rr[0] else "gpsimd", mk("tensor_tensor", out=hbt, in0=tmpf[:], in1=B_vec, op=ALU.add),
                 reads=[r_tmpf, r_B], writes=[r_hbt])

        def transpose_tile(hbt, r_hbt, dstT, r_dst, k0=0, k1=8):
            bk, r_bk = pb()
            pv = bk[:].bitcast(BF16).rearrange("p (k t) -> p k t", k=8)
            S.group("tensor", [mk("transpose", out=pv[:, kc, :], in_=hbt[:, kc * 128:(kc + 1) * 128], identity=ident[:])
                               for kc in range(k0, k1)], reads=[r_hbt, r_ident], writes=[r_bk])
            evac(dstT, pv[:, k0:k1, :], [r_bk], r_dst)

        with ExitStack() as st_mix:
            mv2 = sbt(st_mix, "mv2", [128, D], F32); r_mv2 = Res()
            with ExitStack() as st_a:
                mv0 = sbt(st_a, "mv0", [128, D], F32); r_mv0 = Res()
                mv1 = sbt(st_a, "mv1", [128, D], F32); r_mv1 = Res()
                mvs = [mv0, mv1, mv2, mv3, mv4, mv5]
                r_mvs = [r_mv0, r_mv1, r_mv2, r_mv3, r_mv4, r_mv5]
                with ExitStack() as st0:
                    bada = sbt(st0, "bada", [128, 6 * D], F32); r_bada = Res()
                    gmix_bc = sbt(st0, "gmix_bc", [128, D], F32); r_gmix = Res()
                    gffn_bc = sbt(st0, "gffn_bc", [128, D], F32); r_gffn = Res()
                    wab = [sbt(st0, "wab%d" % i, [128, 8, 512], BF16) for i in range(2)]; r_wab = RL(2)
                    c_bf = sbt(st0, "c_bf", [128, 8, 128], BF16); r_cbf = Res()
                    S.dma(c_bf[:], c_rep, writes=[r_cbf], eng="gpsimd")
                    S.dma(bada[:], b_ada.partition_broadcast(128), writes=[r_bada])
                    S.dma(gmix_bc[:], g_mix.partition_broadcast(128), writes=[r_gmix])
                    S.dma(gffn_bc[:], g_ffn.partition_broadcast(128), writes=[r_gffn])
                    for cc in range(12):
                        wb_, r_wb_ = wab[cc % 2], r_wab[cc % 2]
                        S.dma(wb_[:], wview(w_ada, cc * 512, 512), writes=[r_wb_], eng="gpsimd")
                        bk, r_bk = pb()
                        S.group("tensor", [mk("matmul", out=bk[:], lhsT=c_bf[:, kc, :], rhs=wb_[:, kc, :],
                                              start=(kc == 0), stop=(kc == 7)) for kc in range(8)],
                                reads=[r_cbf, r_wb_], writes=[r_bk])
                        dst = mvs[cc // 2][:, (cc % 2) * 512:(cc % 2 + 1) * 512]
                        S.op("vector", mk("tensor_tensor", out=dst, in0=bk[:], in1=bada[:, cc * 512:(cc + 1) * 512],
                                          op=ALU.add), reads=[r_bk, r_bada], writes=[r_mvs[cc // 2]])
                    if debug:
                        for i in range(6):
                            S.dma(dbg["d_mod"][:, i * D:(i + 1) * D], mvs[i][:], reads=[r_mvs[i]])
                    S.op("vector", mk("scalar_tensor_tensor", out=mv1[:], in0=mv1[:], scalar=1.0, in1=gmix_bc[:],
                                      op0=ALU.add, op1=ALU.mult), reads=[r_gmix], writes=[r_mv1])
                    S.op("vector", mk("scalar_tensor_tensor", out=mv4[:], in0=mv4[:], scalar=1.0, in1=gffn_bc[:],
                                      op0=ALU.add, op1=ALU.mult), reads=[r_gffn], writes=[r_mv4])
                    S.barrier()
                for t in range(32):
                    xs, r_xs = xb[t % 2], r_xb[t % 2]
                    S.dma(xs[:], x_all[t * 128:(t + 1) * 128, :], writes=[r_xs])
                    rms_tile(xs[:], r_xs, t, mv1[:], r_mv1, mv0[:], r_mv0, hb[t % 2][:], r_hb[t % 2])
                    transpose_tile(hb[t % 2], r_hb[t % 2], hT[:, :, t * 128:(t + 1) * 128], [r_hT[t]])
                if debug:
                    S.barrier()
                    with ExitStack() as st_d:
                        dtmp = sbt(st_d, "dtmp", [128, 8 * 512], F32); r_dtmp = Res()
                        S.op("vector", mk("tensor_copy", out=dtmp[:].rearrange("p (k t) -> p k t", k=8),
                                          in_=hT[:, :, 1792:2304]), reads=r_hT, writes=[r_dtmp])
                        S.dma(dbg["d_hT"], dtmp[:], reads=[r_dtmp])
                        S.barrier()
                S.barrier()
            mixT = bufA
            with ExitStack() as st_r:
                NB = 1024
                xr = sbt(st_r, "xr", [128, NB + 3], F32); r_xr = Res()
                xc = sbt(st_r, "xc", [128, NB], F32); r_xc = Res()
                xcb = sbt(st_r, "xcb", [128, NB], BF16); r_xcb = Res()
                rg = sbt(st_r, "rg", [128, NB], F32); r_rg = Res()
                ig = sbt(st_r, "ig", [128, NB], F32); r_ig = Res()
                ag = sbt(st_r, "ag", [128, NB], F32); r_ag = Res()
                t1 = sbt(st_r, "t1", [128, NB], F32); r_t1 = Res()
                hs = sbt(st_r, "hs", [128, NB], F32); r_hs = Res()
                gg = sbt(st_r, "gg", [128, NB], F32); r_gg = Res()
                wxr = [sbt(st_r, "wxr%d" % i, [128, 8, 128], BF16) for i in range(2)]; r_wxr = RL(2)
                wgr = [sbt(st_r, "wgr%d" % i, [128, 8, 128], BF16) for i in range(2)]; r_wgr = RL(2)
                wbd = [sbt(st_r, "wbd%d" % i, [128, 4, 128], BF16) for i in range(2)]; r_wbd = RL(2)
                rp = sbt(st_r, "rp", [128, 4, 8], F32); r_rp = Res()
                clam = sbt(st_r, "clam", [128, 4], F32); r_clam = Res()
                state = sbt(st_r, "state", [128, 4], F32); r_state = Res()
                S.dma(rp[:], recp, writes=[r_rp])
                S.dma(wbd[0][:], wa_bd, writes=[r_wbd[0]], eng="gpsimd")
                S.dma(wbd[1][:], wx_bd, writes=[r_wbd[1]], eng="gpsimd")
                S.op("scalar", mk("activation", out=clam[:], in_=rp[:, :, 7], func=AF.Exp, scale=-1.0),
                     reads=[r_rp], writes=[r_clam])
                S.op("scalar", mk("activation", out=clam[:], in_=clam[:], func=AF.Ln, bias=1.0, scale=1.0),
                     reads=[r_clam], writes=[r_clam])
                S.op("vector", mk("tensor_scalar", out=clam[:], in0=clam[:], scalar1=-8.0, scalar2=None, op0=ALU.mult),
                     reads=[r_clam], writes=[r_clam])
                S.op("vector", mk("memset", ap=state[:], constant=0.0), writes=[r_state])
                for cch in range(4):
                    sl = cch % 2
                    S.dma(wxr[sl][:], wview(w_in, 1536 + cch * 128, 128), writes=[r_wxr[sl]], eng="gpsimd")
                    S.dma(wgr[sl][:], wview(w_in, 2048 + cch * 128, 128), writes=[r_wgr[sl]], eng="gpsimd")
                    S.op("vector", mk("memset", ap=xr[:, 0:3], constant=0.0), writes=[r_xr])
                    for seg in range(4):
                        t0 = seg * NB
                        rh = r_hT[seg * 8:(seg + 1) * 8]
                        if seg == 2:
                            S.op("vector", mk("tensor_scalar", out=xr[:, 0:3], in0=xr[:, 0:3], scalar1=flag[:, 0:1],
                                              scalar2=None, op0=ALU.mult), reads=[r_flag], writes=[r_xr])
                            S.op("vector", mk("tensor_scalar", out=state[:, cch:cch + 1], in0=state[:, cch:cch + 1],
                                              scalar1=flag[:, 0:1], scalar2=None, op0=ALU.mult),
                                 reads=[r_flag], writes=[r_state])
                        for h2 in range(2):
                            bk, r_bk = pb()
                            S.group("tensor", [mk("matmul", out=bk[:], lhsT=wxr[sl][:, kc, :],
                                                  rhs=hT[:, kc, t0 + h2 * 512:t0 + (h2 + 1) * 512],
                                                  start=(kc == 0), stop=(kc == 7)) for kc in range(8)],
                                    reads=[r_wxr[sl]] + rh, writes=[r_bk])
                            S.op("scalar", mk("activation", out=xr[:, 3 + h2 * 512:3 + (h2 + 1) * 512], in_=bk[:],
                                              func=AF.Copy), reads=[r_bk], writes=[r_xr])
                        S.op("vector", mk("tensor_scalar", out=xc[:], in0=xr[:, 3:NB + 3], scalar1=rp[:, cch, 3:4],
                                          scalar2=rp[:, cch, 4:5], op0=ALU.mult, op1=ALU.add),
                             reads=[r_xr, r_rp], writes=[r_xc])
                        for j in range(3):
                            S.op("vector", mk("scalar_tensor_tensor", out=xc[:], in0=xr[:, j:j + NB],
                                              scalar=rp[:, cch, j:j + 1], in1=xc[:], op0=ALU.mult, op1=ALU.add),
                                 reads=[r_xr, r_rp], writes=[r_xc])
                        S.op("vector", mk("tensor_copy", out=xr[:, 0:3], in_=xr[:, NB:NB + 3]), writes=[r_xr])
                        S.op("scalar", mk("activation", out=xcb[:], in_=xc[:], func=AF.Copy), reads=[r_xc], writes=[r_xcb])
                        for which, (dst, r_dst, bcol) in enumerate(((rg, r_rg, 5), (ig, r_ig, 6))):
                            for h2 in range(2):
                                bk, r_bk = pb()
                                S.group("tensor", [mk("matmul", out=bk[:], lhsT=wbd[which][:, cch, :],
                                                      rhs=xcb[:, h2 * 512:(h2 + 1) * 512], start=True, stop=True)],
                                        reads=[r_wbd[which], r_xcb], writes=[r_bk])
                                S.op("scalar", mk("activation", out=dst[:, h2 * 512:(h2 + 1) * 512], in_=bk[:],
                                                  func=AF.Sigmoid, bias=rp[:, cch, bcol:bcol + 1], scale=1.0),
                                     reads=[r_bk, r_rp], writes=[r_dst])
                        S.op("scalar", mk("activation", out=ag[:], in_=rg[:], func=AF.Exp, scale=clam[:, cch:cch + 1]),
                             reads=[r_rg, r_clam], writes=[r_ag])
                        S.op("vector", mk("tensor_tensor", out=t1[:], in0=ag[:], in1=ag[:], op=ALU.mult),
                             reads=[r_ag], writes=[r_t1])
                        S.op("vector", mk("tensor_scalar", out=t1[:], in0=t1[:], scalar1=-1.0, scalar2=1.0,
                                          op0=ALU.mult, op1=ALU.add), reads=[r_t1], writes=[r_t1])
                        S.op("vector", mk("tensor_scalar", out=t1[:], in0=t1[:], scalar1=1e-30, scalar2=None,
                                          op0=ALU.max), reads=[r_t1], writes=[r_t1])
                        S.op("scalar", mk("activation", out=t1[:], in_=t1[:], func=AF.Sqrt), reads=[r_t1], writes=[r_t1])
                        S.op("gpsimd", mk("tensor_tensor", out=ig[:], in0=ig[:], in1=xc[:], op=ALU.mult),
                             reads=[r_xc], writes=[r_ig])
                        S.op("gpsimd", mk("tensor_tensor", out=ig[:], in0=ig[:], in1=t1[:], op=ALU.mult),
                             reads=[r_t1], writes=[r_ig])
                        S.op("vector", mk("tensor_tensor_scan", out=hs[:], data0=ag[:], data1=ig[:],
                                          initial=state[:, cch:cch + 1], op0=ALU.mult, op1=ALU.add),
                             reads=[r_ag, r_ig, r_state], writes=[r_hs])
                        S.op("vector", mk("tensor_copy", out=state[:, cch:cch + 1], in_=hs[:, NB - 1:NB]),
                             reads=[r_hs], writes=[r_state])
                        if seg >= 2:
                            o0 = (seg - 2) * NB
                            for h2 in range(2):
                                bk, r_bk = pb()
                                S.group("tensor", [mk("matmul", out=bk[:], lhsT=wgr[sl][:, kc, :],
                                                      rhs=hT[:, kc, t0 + h2 * 512:t0 + (h2 + 1) * 512],
                                                      start=(kc == 0), stop=(kc == 7)) for kc in range(8)],
                                        reads=[r_wgr[sl]] + rh, writes=[r_bk])
                                S.op("scalar", mk("activation", out=gg[:, h2 * 512:(h2 + 1) * 512], in_=bk[:],
                                                  func=AF.Gelu_apprx_tanh), reads=[r_bk], writes=[r_gg])
                            S.op("gpsimd", mk("tensor_tensor", out=mixT[:, 4 + cch, o0:o0 + NB], in0=hs[:], in1=gg[:],
                                              op=ALU.mult), reads=[r_hs, r_gg],
                                 writes=r_bufA[4 + cch][(seg - 2) * 8:(seg - 1) * 8])
                S.barrier()
            with ExitStack() as st_at:
                wqkv = sbt(st_at, "wqkv", [128, 3, 8, 128], BF16); r_wqkv3 = RL(3)
                qTa = sbt(st_at, "qTa", [128, TOWN], BF16); r_qTa = Res()
                qTb = sbt(st_at, "qTb", [128, TOWN], BF16); r_qTb = Res()
                kT = sbt(st_at, "kT", [128, TALL], BF16); r_kT = Res()
                vT = sbt(st_at, "vT", [128, TALL], BF16); r_vT = Res()
                Vta = sbt(st_at, "Vta", [128, 32, 128], BF16); r_Vt = RL(8)
                Vtb = sbt(st_at, "Vtb", [128, 32, 128], BF16)
                accN = sbt(st_at, "accN", [128, TOWN], F32); r_accN = Res()
                accD = sbt(st_at, "accD", [128, TOWN], F32); r_accD = Res()
                PT = [sbt(st_at, "PT%d" % i, [128, 512], BF16) for i in range(2)]; r_PT = RL(2)
                pt_rr = 0
                S.op("gpsimd", mk("memset", ap=Vta[:], constant=0.0), writes=r_Vt)
                S.op("gpsimd", mk("memset", ap=Vtb[:], constant=0.0), writes=r_Vt)
                S.op("gpsimd", mk("memset", ap=qTa[:], constant=0.0), writes=[r_qTa])
                S.op("gpsimd", mk("memset", ap=qTb[:], constant=0.0), writes=[r_qTb])
                for fc in range(4):
                    for i3 in range(3):
                        S.dma(wqkv[:, i3, :, :], wview(w_in, i3 * 512 + fc * 128, 128), writes=[r_wqkv3[i3]], eng="gpsimd")
                    for tb in range(4):
                        bk, r_bk = pb()
                        c0 = TOWN + tb * 512
                        S.group("tensor", [mk("matmul", out=bk[:], lhsT=wqkv[:, 0, kc, :], rhs=hT[:, kc, c0:c0 + 512],
                                              start=(kc == 0), stop=(kc == 7)) for kc in range(8)],
                                reads=[r_wqkv3[0]] + r_hT[16 + tb * 4:16 + (tb + 1) * 4], writes=[r_bk])
                        S.op("scalar", mk("activation", out=qTa[0:64, tb * 512:(tb + 1) * 512], in_=bk[0:64, :],
                                          func=AF.Copy), reads=[r_bk], writes=[r_qTa])
                        S.op("vector", mk("tensor_copy", out=qTb[64:128, tb * 512:(tb + 1) * 512], in_=bk[64:128, :]),
                             reads=[r_bk], writes=[r_qTb])
                    for tb in range(8):
                        bk, r_bk = pb()
                        c0 = tb * 512
                        S.group("tensor", [mk("matmul", out=bk[:], lhsT=wqkv[:, 1, kc, :], rhs=hT[:, kc, c0:c0 + 512],
                                              start=(kc == 0), stop=(kc == 7)) for kc in range(8)],
                                reads=[r_wqkv3[1]] + r_hT[tb * 4:(tb + 1) * 4], writes=[r_bk])
                        evac(kT[:, c0:c0 + 512], bk[:], [r_bk], [r_kT])
                    for tb in range(8):
                        bk, r_bk = pb()
                        c0 = tb * 512
                        S.group("tensor", [mk("matmul", out=bk[:], lhsT=wqkv[:, 2, kc, :], rhs=hT[:, kc, c0:c0 + 512],
                                              start=(kc == 0), stop=(kc == 7)) for kc in range(8)],
                                reads=[r_wqkv3[2]] + r_hT[tb * 4:(tb + 1) * 4], writes=[r_bk])
                        evac(vT[:, c0:c0 + 512], bk[:], [r_bk], [r_vT])
                    for pat, d in enumerate((1, 4, 16)):
                        L = TALL // d
                        nj = L // 128
                        for g8 in range(4):
                            bk, r_bk = pb()
                            pv = bk[:].bitcast(BF16).rearrange("p (u c) -> p u c", u=8)
                            fns = []
                            for u in range(8):
                                ti = g8 * 8 + u
                                r_, j_ = divmod(ti, nj)
                                s0 = r_ + d * 128 * j_
                                fns.append(mk("transpose", out=pv[:, u, :], in_=vT[:, s0:s0 + d * 127 + 1:d], identity=ident[:]))
                            S.group("tensor", fns, reads=[r_vT, r_ident], writes=[r_bk])
                            S.op("scalar", mk("activation", out=Vta[:, g8 * 8:(g8 + 1) * 8, 0:64], in_=pv[:, :, 0:64],
                                              func=AF.Copy), reads=[r_bk], writes=[r_Vt[2 * g8], r_Vt[2 * g8 + 1]])
                            S.op("vector", mk("tensor_copy", out=Vtb[:, g8 * 8:(g8 + 1) * 8, 64:128], in_=pv[:, :, 64:128]),
                                 reads=[r_bk], writes=[r_Vt[2 * g8], r_Vt[2 * g8 + 1]])
                        for su in range(4):
                            bkN, r_bkN = pb()
                            bkD, r_bkD = pb()
                            for u in range(4):
                                if d == 1:
                                    r_, jq = 0, 16 + 4 * su + u
                                elif d == 4:
                                    r_, jq = u, 4 + su
                                else:
                                    r_, jq = 4 * su + u, 1
                                q0 = r_ + d * 128 * jq - TOWN
                                kp0 = r_ + d * 128 * (jq - 1)
                                kc0 = r_ + d * 128 * jq
                                tip = r_ * nj + jq - 1
                                tic = r_ * nj + jq
                                boundary = (jq == nj // 2)
                                span = d * 127 + 1
                                bkS, r_bkS = pb()
                                msk = maskB if boundary else negmask
                                fns = [mk("matmul", out=bkS[:], lhsT=ident[:], rhs=msk[:], start=True, stop=False)]
                                for bi, (qq, k0) in enumerate(((qTa, kp0), (qTa, kc0), (qTb, kp0), (qTb, kc0))):
                                    fns.append(mk("matmul", out=bkS[:, bi * 128:(bi + 1) * 128],
                                                  lhsT=kT[:, k0:k0 + span:d], rhs=qq[:, q0:q0 + span:d],
                                                  start=False, stop=(bi == 3)))
                                S.group("tensor", fns, reads=[r_ident, r_negmask, r_maskB, r_kT, r_qTa, r_qTb],
                                        writes=[r_bkS])
                                pt, r_pt = PT[pt_rr], r_PT[pt_rr]
                                pt_rr ^= 1
                                S.op("scalar", mk("activation", out=pt[:], in_=bkS[:], func=AF.Exp, scale=0.125),
                                     reads=[r_bkS], writes=[r_pt])
                                oc = slice(u * 128, (u + 1) * 128)
                                fnsN = [
                                    mk("matmul", out=bkN[:, oc], lhsT=Vta[:, tip, :], rhs=pt[:, 0:128], start=True, stop=False),
                                    mk("matmul", out=bkN[:, oc], lhsT=Vta[:, tic, :], rhs=pt[:, 128:256], start=False, stop=False),
                                    mk("matmul", out=bkN[:, oc], lhsT=Vtb[:, tip, :], rhs=pt[:, 256:384], start=False, stop=False),
                                    mk("matmul", out=bkN[:, oc], lhsT=Vtb[:, tic, :], rhs=pt[:, 384:512], start=False, stop=True),
                                ]
                                S.group("tensor", fnsN, reads=[r_pt, r_Vt[tip // 4], r_Vt[tic // 4]], writes=[r_bkN])
                                fnsD = [
                                    mk("matmul", out=bkD[:, oc], lhsT=ones_a[:], rhs=pt[:, 0:128], start=True, stop=False),
                                    mk("matmul", out=bkD[:, oc], lhsT=ones_a[:], rhs=pt[:, 128:256], start=False, stop=False),
                                    mk("matmul", out=bkD[:, oc], lhsT=ones_b[:], rhs=pt[:, 256:384], start=False, stop=False),
                                    mk("matmul", out=bkD[:, oc], lhsT=ones_b[:], rhs=pt[:, 384:512], start=False, stop=True),
                                ]
                                S.group("tensor", fnsD, reads=[r_pt, r_ones], writes=[r_bkD])
                            for acc, r_acc, bkX, r_bkX, eng in ((accN, r_accN, bkN, r_bkN, "vector"),
                                                               (accD, r_accD, bkD, r_bkD, "gpsimd")):
                                src = bkX[:].rearrange("p (u i) -> p u i", u=4)
                                if d == 1:
                                    dst = acc[:, su * 512:(su + 1) * 512].rearrange("p (u i) -> p u i", u=4)
                                elif d == 4:
                                    dst = acc[:, su * 512:(su + 1) * 512].rearrange("p (i r) -> p r i", r=4)
                                else:
                                    dst = acc[:].rearrange("p (i r) -> p r i", r=16)[:, 4 * su:4 * su + 4, :]
                                if d == 1:
                                    S.op("vector" if eng == "vector" else "scalar",
                                         mk("tensor_copy", out=dst, in_=src) if eng == "vector" else
                                         mk("activation", out=dst, in_=src, func=AF.Copy),
                                         reads=[r_bkX], writes=[r_acc])
                                else:
                                    S.op("vector", mk("tensor_tensor", out=dst, in0=dst, in1=src, op=ALU.add),
                                         reads=[r_bkX], writes=[r_acc])
                    S.op("vector", mk("reciprocal", out=accD[:], in_=accD[:]), reads=[r_accD], writes=[r_accD])
                    S.op("vector", mk("tensor_tensor", out=mixT[:, fc, :], in0=accN[:], in1=accD[:], op=ALU.mult),
                         reads=[r_accN, r_accD], writes=r_bufA[fc])
                S.barrier()
            if debug:
                with ExitStack() as st_d:
                    dtmp = sbt(st_d, "dtmp2", [128, 8 * 512], F32); r_dtmp = Res()
                    for q4 in range(4):
                        S.op("vector", mk("tensor_copy", out=dtmp[:].rearrange("p (k t) -> p k t", k=8),
                                          in_=mixT[:, :, q4 * 512:(q4 + 1) * 512]), reads=[], writes=[r_dtmp])
                        S.dma(dbg["d_mixT"].rearrange("p (k t) -> p k t", k=8)[:, :, q4 * 512:(q4 + 1) * 512],
                              dtmp[:].rearrange("p (k t) -> p k t", k=8), reads=[r_dtmp])
                    S.barrier()
            with ExitStack() as st_o:
                wout = sbt(st_o, "wout", [128, 8, D], BF16); r_wout2 = RL(2)
                S.dma(wout[:, 0:4, :], w_out[0:512, :].rearrange("(kc p) n -> p kc n", p=128), writes=[r_wout2[0]], eng="gpsimd")
                S.dma(wout[:, 4:8, :], w_out[512:1024, :].rearrange("(kc p) n -> p kc n", p=128), writes=[r_wout2[1]], eng="gpsimd")
                for t in range(16):
                    xs, r_xs = xb[t % 2], r_xb[t % 2]
                    S.dma(xs[:], x_all[TOWN + t * 128:TOWN + (t + 1) * 128, :], writes=[r_xs])
                    for cc in range(2):
                        bk, r_bk = pb()
                        cs = slice(cc * 512, (cc + 1) * 512)
                        S.group("tensor", [mk("matmul", out=bk[:], lhsT=mixT[:, kc, t * 128:(t + 1) * 128],
                                              rhs=wout[:, kc, cs], start=(kc == 0), stop=(kc == 7)) for kc in range(8)],
                                reads=r_wout2 + [r_bufA[kc][t] for kc in range(8)], writes=[r_bk])
                        S.op("vector", mk("tensor_tensor", out=tmpf[:, cs], in0=bk[:], in1=mv2[:, cs], op=ALU.mult),
                             reads=[r_bk, r_mv2], writes=[r_tmpf])
                        S.op("gpsimd", mk("tensor_tensor", out=x1[:, t, cs], in0=tmpf[:, cs], in1=xs[:, cs], op=ALU.add),
                             reads=[r_tmpf, r_xs], writes=[r_x1[t]])
                S.barrier()
        if debug:
            for t in range(16):
                S.dma(dbg["d_x1"][t * 128:(t + 1) * 128, :], x1[:, t, :], reads=[r_x1[t]])
        I32 = mybir.dt.int32
        CAP = TOWN
        x_buf = nc.dram_tensor("x_buf", [NE * CAP, D], BF16, kind="Internal").ap()
        y_buf = nc.dram_tensor("y_buf", [NE * CAP, D], F32, kind="Internal").ap()
        with ExitStack() as st_f:
            wgus = [bufA, sbt(st_f, "wgu1", [128, 8, 2 * D], BF16)]; r_wgus = [RL(4), RL(4)]
            wgus[0] = bufA[:].rearrange("p k t -> p (k t)").rearrange("p (k t) -> p k t", k=8)
            wd = sbt(st_f, "wd", [128, 8, D], BF16); r_wd = RL(2)
            gk = sbt(st_f, "gk", [128, 16, 4], F32); r_gk = RL(16)
            desti = sbt(st_f, "desti", [128, 16, 4], I32); r_desti = RL(16)
            cnti = sbt(st_f, "cnti", [128, NE], I32); r_cnti = Res()

            bgrow = [sbt(st_f, "bgrow%d" % i, [2, 2 * D], BF16) for i in range(2)]; r_bgrow = RL(2)
            ones_r = sbt(st_f, "ones_r", [2, 128], BF16); r_ones_r = Res()
            S.op("gpsimd", mk("memset", ap=ones_r[:], constant=1.0), writes=[r_ones_r])
            for i in range(2):
                S.op("gpsimd", mk("memset", ap=bgrow[i][:, 0:D], constant=0.0), writes=[r_bgrow[i]])
                S.op("gpsimd", mk("memset", ap=bgrow[i][:, D:2 * D], constant=1.0), writes=[r_bgrow[i]])

            def load_wgu(e):
                w_, r_w = wgus[e % 2], r_wgus[e % 2]
                for q4 in range(4):
                    S.dma(w_[:, :, q4 * 512:(q4 + 1) * 512], wview(w_gu[e], q4 * 512, 512), writes=[r_w[q4]], eng="gpsimd")
                S.dma(bgrow[e % 2][0:1, :], b_gu_d[e:e + 1, :], writes=[r_bgrow[e % 2]], eng="gpsimd")

            def load_wd(e):
                S.dma(wd[:, 0:4, :], w_dn[e][0:512, :].rearrange("(kc p) n -> p kc n", p=128), writes=[r_wd[0]], eng="gpsimd")
                S.dma(wd[:, 4:8, :], w_dn[e][512:1024, :].rearrange("(kc p) n -> p kc n", p=128), writes=[r_wd[1]], eng="gpsimd")

            load_wgu(0)
            load_wd(0)
            with ExitStack() as st_r2:
                wr = sbt(st_r2, "wr", [128, 8, NE], BF16); r_wr = Res()
                btmp = sbt(st_r2, "btmp", [128, D], F32); r_btmp = RL(2)
                brt = sbt(st_r2, "brt", [128, NE], F32); r_brt = Res()
                lg_2 = [sbt(st_r2, "lg_%d" % i_, [128, NE], F32) for i_ in range(2)]; r_lg_2 = RL(2)
                ex_2 = [sbt(st_r2, "ex_%d" % i_, [128, NE], F32) for i_ in range(2)]; r_ex_2 = RL(2)
                gt_2 = [sbt(st_r2, "gt_%d" % i_, [128, NE], F32) for i_ in range(2)]; r_gt_2 = RL(2)
                posb_2 = [sbt(st_r2, "posb_%d" % i_, [128, NE], F32) for i_ in range(2)]; r_posb_2 = RL(2)
                scr4_2 = [sbt(st_r2, "scr4_%d" % i_, [128, 4, NE], F32) for i_ in range(2)]; r_scr_2 = RL(2)
                ebase = sbt(st_r2, "ebase", [128, NE], F32); r_ebase = Res()
                m8_2 = [sbt(st_r2, "m8_%d" % i_, [128, 8], F32) for i_ in range(2)]; r_m8_2 = RL(2)
                e4_2 = [sbt(st_r2, "e4_%d" % i_, [128, 4], F32) for i_ in range(2)]; r_e4_2 = RL(2)
                destf_2 = [sbt(st_r2, "destf_%d" % i_, [128, 4], F32) for i_ in range(2)]; r_destf_2 = RL(2)
                nmx_2 = [sbt(st_r2, "nmx_%d" % i_, [128, 1], F32) for i_ in range(2)]; r_nmx_2 = RL(2)
                sm_2 = [sbt(st_r2, "sm_%d" % i_, [128, 1], F32) for i_ in range(2)]; r_sm_2 = RL(2)
                gT_2 = [sbt(st_r2, "gT_%d" % i_, [32, 128], F32) for i_ in range(2)]; r_gT_2 = RL(2)
                bdn = sbt(st_r2, "bdn", [32, D], F32); r_bdn = Res()
                maskall = sbt(st_r2, "maskall", [128, 16, NE], BF16); r_mask = RL(16)
                ltri = sbt(st_r2, "ltri", [128, 128], BF16); r_ltri = Res()
                ones_f = sbt(st_r2, "ones_full", [128, 128], BF16); r_onesf = Res()
                h2Tt_2 = [sbt(st_r2, "h2Tt_%d" % i_, [128, 8, 128], BF16) for i_ in range(2)]; r_h2Tt_2 = RL(2)
                S.dma(wr[:], w_router.rearrange("(kc p) n -> p kc n", p=128), writes=[r_wr], eng="gpsimd")
                S.dma(brt[:], b_router.partition_broadcast(128), writes=[r_brt])
                S.dma(bdn[:], b_dn, writes=[r_bdn])
                S.op("gpsimd", mk("iota", out=ebase[:], pattern=[[CAP, NE]], base=0, channel_multiplier=0,
                                  allow_small_or_imprecise_dtypes=True), writes=[r_ebase])
                S.op("gpsimd", mk("memset", ap=ones_f[:], constant=1.0), writes=[r_onesf])
                S.op("gpsimd", mk("memset", ap=ltri[:], constant=1.0), writes=[r_ltri])
                S.op("gpsimd", mk("affine_select", out=ltri[:], in_=ltri[:], pattern=[[1, 128]], compare_op=ALU.is_gt,
                                  fill=0.0, base=0, channel_multiplier=-1), reads=[r_ltri], writes=[r_ltri])
                for t in range(16):
                    rms_stats(x1[:, t, :], r_x1[t], 32 + t)
                for t in range(16):
                    hbt, r_hbt = hb[t % 2], r_hb[t % 2]
                    lg, r_lg = lg_2[t % 2], r_lg_2[t % 2]
                    ex, r_ex = ex_2[t % 2], r_ex_2[t % 2]
                    gt, r_gt = gt_2[t % 2], r_gt_2[t % 2]
                    posb, r_posb = posb_2[t % 2], r_posb_2[t % 2]
                    scr4, r_scr = scr4_2[t % 2], r_scr_2[t % 2]
                    m8, r_m8 = m8_2[t % 2], r_m8_2[t % 2]
                    e4, r_e4 = e4_2[t % 2], r_e4_2[t % 2]
                    destf, r_destf = destf_2[t % 2], r_destf_2[t % 2]
                    nmx, r_nmx = nmx_2[t % 2], r_nmx_2[t % 2]
                    sm, r_sm = sm_2[t % 2], r_sm_2[t % 2]
                    gT, r_gT = gT_2[t % 2], r_gT_2[t % 2]
                    h2Tt, r_h2Tt = h2Tt_2[t % 2], r_h2Tt_2[t % 2]
                    rms_tile(x1[:, t, :], r_x1[t], 32 + t, mv4[:], r_mv4, mv3[:], r_mv3, hbt[:], r_hbt, stats=False)
                    transpose_tile(hbt, r_hbt, h2Tt[:], [r_h2Tt])
                    bk, r_bk = pb()
                    S.group("tensor", [mk("matmul", out=bk[:, 0:NE], lhsT=h2Tt[:, kc, :], rhs=wr[:, kc, :],
                                          start=(kc == 0), stop=(kc == 7)) for kc in range(8)],
                            reads=[r_wr, r_h2Tt], writes=[r_bk])
                    S.op("vector", mk("tensor_tensor", out=lg[:], in0=bk[:, 0:NE], in1=brt[:], op=ALU.add),
                         reads=[r_bk, r_brt], writes=[r_lg])
                    S.op("vector", mk("max", out=m8[:], in_=lg[:]), reads=[r_lg], writes=[r_m8])
                    S.op("vector", mk("tensor_scalar", out=nmx[:], in0=m8[:, 0:1], scalar1=-1.0, scalar2=None, op0=ALU.mult),
                         reads=[r_m8], writes=[r_nmx])
                    S.op("scalar", mk("activation", out=ex[:], in_=lg[:], func=AF.Exp, bias=nmx[:, 0:1], scale=1.0),
                         reads=[r_lg, r_nmx], writes=[r_ex])
                    S.op("scalar", mk("activation", out=e4[:], in_=m8[:, 0:4], func=AF.Exp, bias=nmx[:, 0:1], scale=1.0),
                         reads=[r_m8, r_nmx], writes=[r_e4])
                    S.op("vector", mk("tensor_scalar", out=maskall[:, t, :], in0=lg[:], scalar1=m8[:, 3:4], scalar2=None,
                                      op0=ALU.is_ge), reads=[r_lg, r_m8], writes=[r_mask[t]])
                    S.op("vector", mk("tensor_tensor", out=ex[:], in0=ex[:], in1=maskall[:, t, :], op=ALU.mult),
                         reads=[r_mask[t]], writes=[r_ex])
                    S.op("vector", mk("tensor_reduce", out=sm[:], in_=ex[:], axis=mybir.AxisListType.X, op=ALU.add),
                         reads=[r_ex], writes=[r_sm])
                    S.op("vector", mk("reciprocal", out=sm[:], in_=sm[:]), reads=[r_sm], writes=[r_sm])
                    S.op("vector", mk("tensor_scalar", out=gt[:], in0=ex[:], scalar1=sm[:, 0:1], scalar2=None,
                                      op0=ALU.mult), reads=[r_ex, r_sm], writes=[r_gt])
                    S.op("vector", mk("tensor_scalar", out=gk[:, t, :], in0=e4[:], scalar1=sm[:, 0:1], scalar2=None,
                                      op0=ALU.mult), reads=[r_e4, r_sm], writes=[r_gk[t]])
                    bkp, r_bkp = pb()
                    fns = [mk("matmul", out=bkp[:, 0:NE], lhsT=ones_f[:], rhs=maskall[:, tp, :], start=(tp == 0), stop=False)
                           for tp in range(t)]
                    fns.append(mk("matmul", out=bkp[:, 0:NE], lhsT=ltri[:], rhs=maskall[:, t, :], start=(t == 0), stop=True))
                    S.group("tensor", fns, reads=[r_onesf, r_ltri] + r_mask[:t + 1], writes=[r_bkp])
                    S.op("vector", mk("tensor_tensor", out=posb[:], in0=bkp[:, 0:NE], in1=ebase[:], op=ALU.add),
                         reads=[r_bkp, r_ebase], writes=[r_posb])
                    for k in range(4):
                        S.op("vector", mk("scalar_tensor_tensor", out=scr4[:, k, :], in0=lg[:], scalar=m8[:, k:k + 1], in1=posb[:],
                                          op0=ALU.is_equal, op1=ALU.mult),
                             reads=[r_lg, r_m8, r_posb], writes=[r_scr])
                    S.op("vector", mk("tensor_reduce", out=destf[:], in_=scr4[:], axis=mybir.AxisListType.X, op=ALU.add),
                         reads=[r_scr], writes=[r_destf])
                    S.op("vector", mk("tensor_scalar", out=destf[:], in0=destf[:], scalar1=0.0, scalar2=float(NE * CAP - 1),
                                      op0=ALU.max, op1=ALU.min), reads=[r_destf], writes=[r_destf])
                    S.op("vector", mk("tensor_copy", out=desti[:, t, :], in_=destf[:]), reads=[r_destf], writes=[r_desti[t]])
                    for k in range(4):
                        def sc(e, t=t, k=k, hbt=hbt):
                            return e.indirect_dma_start(out=x_buf[:, :],
                                                        out_offset=bass.IndirectOffsetOnAxis(ap=desti[:, t, k:k + 1], axis=0),
                                                        in_=hbt[:, :], in_offset=None)
                        S.dma_fn("gpsimd", sc, reads=[r_desti[t], r_hbt], writes=[])
                    bk2, r_bk2 = pb()
                    S.group("tensor", [mk("transpose", out=bk2[0:NE, 0:128], in_=gt[:], identity=identf[:])],
                            reads=[r_gt, r_identf], writes=[r_bk2])
                    S.op("vector", mk("tensor_copy", out=gT[:], in_=bk2[0:NE, 0:128]), reads=[r_bk2], writes=[r_gT])
                    for cc in range(2):
                        cs = slice(cc * 512, (cc + 1) * 512)
                        bk3, r_bk3 = pb()
                        S.group("tensor", [mk("matmul", out=bk3[:], lhsT=gT[:], rhs=bdn[:, cs], start=True, stop=True)],
                                reads=[r_gT, r_bdn], writes=[r_bk3])
                        S.op("vector", mk("tensor_tensor", out=btmp[:, cs], in0=bk3[:], in1=mv5[:, cs], op=ALU.mult),
                             reads=[r_bk3, r_mv5], writes=[r_btmp[cc]])
                        S.op("vector", mk("tensor_tensor", out=x1[:, t, cs], in0=btmp[:, cs], in1=x1[:, t, cs], op=ALU.add),
                             reads=[r_btmp[cc]], writes=[r_x1[t]])
                bkc, r_bkc = pb()
                S.group("tensor", [mk("matmul", out=bkc[:, 0:NE], lhsT=ones_f[:], rhs=maskall[:, tp, :], start=(tp == 0),
                                      stop=(tp == 15)) for tp in range(16)], reads=[r_onesf] + r_mask, writes=[r_bkc])
                S.op("vector", mk("tensor_copy", out=cnti[:], in_=bkc[:, 0:NE]), reads=[r_bkc], writes=[r_cnti])
                if debug:
                    S.dma(dbg["d_gates"][:, 0:64], gk[:].rearrange("p t e -> p (t e)"), reads=r_gk)
                S.barrier()
            S.dma(mv3[:], g_final.partition_broadcast(128), writes=[r_mv3])
            with ExitStack() as st_m:
                xblk = [hb[0], hb[1]]; r_xblk = RL(2)
                xT1 = sbt(st_m, "xT1", [128, 8, 128], BF16)
                xTs = [junk[:].rearrange("p (k t) -> p k t", k=8), xT1[:]]; r_xT = RL(2)
                actT = [sbt(st_m, "actT%d" % i, [128, 8, 128], BF16) for i in range(2)]; r_act = [RL(2), RL(2)]
                yblk = [xb[0], xb[1]]; r_yblk = RL(2)
                sgx = sbt(st_m, "sgx", [128, D], F32)
                ucx = sbt(st_m, "ucx", [128, D], F32)
                gc2 = [[tmpf[:, 0:512], tmpf[:, 512:1024]], [sgx[:, 0:512], sgx[:, 512:1024]]]; r_gc2 = [RL(2), RL(2)]
                actk = [sbt(st_m, "actk%d" % i, [128, D], BF16) for i in range(2)]; r_actk = [RL(2), RL(2)]
                ucs = [[mv4[:, 0:512], mv4[:, 512:1024]], [ucx[:, 0:512], ucx[:, 512:1024]]]; r_uc = [RL(2), RL(2)]
                evac_dve_only[0] = True
                blk_rr = 0
                for e in range(n_experts if stage >= 2 else 0):
                    w_, r_w = wgus[e % 2], r_wgus[e % 2]
                    if e + 1 < n_experts:
                        load_wgu(e + 1)
                    S.load_count(cnti[0:1, e:e + 1], [r_cnti])
                    for jp in range(0, 16, 2):
                        S.begin_guard((e, jp), 128 * jp + 1)
                        pair = (jp, jp + 1)

                        NESTED = True

                        def inner(j, ph):
                            if j != jp and NESTED:
                                S.begin_inner((e, j, ph), 128 * j + 1)

                        def inner_end(j):
                            if j != jp and NESTED:
                                S.end_inner()
                        for j in pair:
                            s_ = j % 2
                            row0 = e * CAP + 128 * j
                            inner(j, 1)
                            S.dma(xblk[s_][:], x_buf[row0:row0 + 128, :], writes=[r_xblk[s_]])
                            transpose_tile(xblk[s_], r_xblk[s_], xTs[s_], [r_xT[s_]])
                            inner_end(j)
                        for j in pair:
                            s_ = j % 2
                            xT = xTs[s_]
                            inner(j, 2)
                            for half in range(2):
                                gub = []
                                for c0 in (half * 512, D + half * 512):
                                    bkX, r_bkX = pb()
                                    fns = [mk("matmul", out=bkX[:], lhsT=ones_r[0:2, :], rhs=bgrow[e % 2][0:2, c0:c0 + 512],
                                              start=True, stop=False)]
                                    for kc in range(8):
                                        fns.append(mk("matmul", out=bkX[:], lhsT=xT[:, kc, :], rhs=w_[:, kc, c0:c0 + 512],
                                                      start=False, stop=(kc == 7)))
                                    S.group("tensor", fns, reads=[r_w[c0 // 512], r_xT[s_], r_ones_r, r_bgrow[e % 2]], writes=[r_bkX])
                                    gub.append((bkX, r_bkX))
                                (bkG, r_bkG), (bkU, r_bkU) = gub
                                uc, r_uc_ = ucs[s_][half], r_uc[s_][half]
                                gc, r_gc_ = gc2[s_][half], r_gc2[s_][half]
                                S.op("scalar", mk("activation", out=gc, in_=bkG[:], func=AF.Gelu_apprx_sigmoid),
                                     reads=[r_bkG], writes=[r_gc_])
                                S.op("vector", mk("tensor_scalar", out=uc, in0=bkU[:], scalar1=8.0, scalar2=-6.0,
                                                  op0=ALU.min, op1=ALU.max), reads=[r_bkU], writes=[r_uc_])
                                S.op("vector", mk("scalar_tensor_tensor", out=actk[s_][:, half * 512:(half + 1) * 512], in0=gc,
                                                  scalar=GLU7, in1=uc, op0=ALU.min, op1=ALU.mult),
                                     reads=[r_uc_, r_gc_], writes=[r_actk[s_][half]])
                            inner_end(j)
                        for j in pair:
                            s_ = j % 2
                            row0 = e * CAP + 128 * j
                            inner(j, 3)
                            for half in range(2):
                                transpose_tile(actk[s_], r_actk[s_][half], actT[s_][:, 4 * half:4 * half + 4, :],
                                               [r_act[s_][half]], k0=4 * half, k1=4 * half + 4)
                            for cc in range(2):
                                cs = slice(cc * 512, (cc + 1) * 512)
                                bk, r_bk = pb()
                                S.group("tensor", [mk("matmul", out=bk[:], lhsT=actT[s_][:, kc, :], rhs=wd[:, kc, cs],
                                                      start=(kc == 0), stop=(kc == 7)) for kc in range(8)],
                                        reads=r_wd + r_act[s_], writes=[r_bk])
                                S.op("vector", mk("tensor_tensor", out=yblk[s_][:, cs], in0=bk[:], in1=mv5[:, cs], op=ALU.mult),
                                     reads=[r_bk, r_mv5], writes=[r_yblk[s_]])
                            S.dma(y_buf[row0:row0 + 128, :], yblk[s_][:], reads=[r_yblk[s_]], eng="gpsimd")
                            inner_end(j)
                        S.end_guard()
                    if e + 1 < n_experts:
                        load_wd(e + 1)
                evac_dve_only[0] = False
                S.barrier()
            st_c = st_f.enter_context(ExitStack())
            ykb = [mv4, tmpf] + [sbt(st_c, "ykb%d" % i, [128, D], F32) for i in range(4)]; r_ykb = RL(6)
            gi = 0
            for t in range(16):
                for k in range(4 if stage >= 3 else 0):
                    yk, r_yk = ykb[gi % 6], r_ykb[gi % 6]
                    gi += 1

                    def ga(e, t=t, k=k, yk=yk):
                        return e.indirect_dma_start(out=yk[:, :], out_offset=None, in_=y_buf[:, :],
                                                    in_offset=bass.IndirectOffsetOnAxis(ap=desti[:, t, k:k + 1], axis=0))
                    S.dma_fn("gpsimd", ga, reads=[r_desti[t]], writes=[r_yk])
                    S.op("vector", mk("scalar_tensor_tensor", out=x1[:, t, :], in0=yk[:], scalar=gk[:, t, k:k + 1],
                                      in1=x1[:, t, :], op0=ALU.mult, op1=ALU.add),
                         reads=[r_yk, r_gk[t]], writes=[r_x1[t]])
            for t in range(16):
                ob = xb[t % 2]; r_ob = r_xb[t % 2]
                rms_tile(x1[:, t, :], r_x1[t], 48 + t, mv3[:], r_mv3, None, None, ob[:], r_ob)
                S.dma(out_d[t * 128:(t + 1) * 128, :], ob[:], reads=[r_ob])
            S.barrier()
        with nc.Block() as block:
            S.emit(block)
    return nc


_NC_CACHE = {}


def make_in_maps(inputs, cores, n_experts=NE):
    f = lambda a: np.ascontiguousarray(np.asarray(a, dtype=np.float32))
    x = f(inputs["x"]); c = f(inputs["c"])
    w_ada = f(inputs["w_ada"])[0]; b_ada = f(inputs["b_ada"])[0][None, :]
    g_mix = f(inputs["g_mix"])[0][None, :]; g_ffn = f(inputs["g_ffn"])[0][None, :]
    g_final = f(inputs["g_final"])[None, :]
    w_in = f(inputs["w_in"])[0]; w_out = f(inputs["w_out"])[0]
    conv_w = f(inputs["conv_w"])[0]; conv_b = f(inputs["conv_b"])[0]
    b_a = f(inputs["b_rg_a"])[0]; b_x = f(inputs["b_rg_x"])[0]; lam = f(inputs["lam"])[0]
    recp = np.zeros((128, 4, 8), np.float32)
    cols = [conv_w[0], conv_w[1], conv_w[2], conv_w[3], conv_b, b_a, b_x, lam]
    for j, v in enumerate(cols):
        recp[:, :, j] = v.reshape(4, 128).T
    def blockdiag(w):
        w = f(w)[0]
        o = np.zeros((128, 4, 128), np.float32)
        for blk in range(8):
            cch, hh = divmod(blk, 2)
            o[hh * 64:(hh + 1) * 64, cch, hh * 64:(hh + 1) * 64] = w[blk]
        return o
    wa_bd = blockdiag(inputs["w_rg_a"]); wx_bd = blockdiag(inputs["w_rg_x"])
    w_router = f(inputs["w_router"])[0]; b_router = f(inputs["b_router"])[0][None, :]
    w_gu = f(inputs["w_gate_up"])[0][:n_experts]; b_gu = f(inputs["b_gate_up"])[0]
    bgu_t = np.ascontiguousarray(b_gu.reshape(NE, 16, 128).transpose(2, 0, 1))
    w_dn = f(inputs["w_down"])[0][:n_experts]; b_dn = f(inputs["b_down"])[0]
    maps = []
    for core in cores:
        b, half = divmod(core, 2)
        x_all = np.zeros((TALL, D), np.float32)
        if half == 0:
            x_all[TOWN:] = x[b, :TOWN]
        else:
            x_all[:] = x[b]
        c_rep = np.ascontiguousarray(np.broadcast_to(c[b].reshape(8, 128).T[:, :, None], (128, 8, 128)))
        flag = np.full((128, 1), float(half), np.float32)
        maps.append({
            "x_all": x_all, "c_rep": c_rep, "flag": flag, "w_ada": w_ada, "b_ada": b_ada, "g_mix": g_mix,
            "g_ffn": g_ffn, "g_final": g_final, "w_in": w_in, "w_out": w_out, "recp": recp, "wa_bd": wa_bd,
            "wx_bd": wx_bd, "w_router": w_router, "b_router": b_router, "w_gate_up": w_gu, "bgu_t": bgu_t, "b_gu": b_gu,
            "w_down": w_dn, "b_down": b_dn,
        })
    return maps


def kernel(**inputs):
    if "nc" not in _NC_CACHE:
        _NC_CACHE["nc"] = build()
    nc = _NC_CACHE["nc"]
    cores = list(range(8))
    in_maps = make_in_maps(inputs, cores)
    res = run_bass_kernel_spmd(nc, in_maps, core_ids=cores)
    out = np.zeros((4, 4096, D), np.float32)
    for core in cores:
        b, half = divmod(core, 2)
        out[b, half * TOWN:(half + 1) * TOWN] = res.results[core]["out"]
    return out
```
